# Optimizing a Trainium2 kernel written in Bass

```python
import math
import jax, jax.numpy as jnp
from jax import lax
import numpy as np


D_MODEL = 1024
BATCH = 8
SEQ = 4096
DEPTH = 1

D_MIX = D_MODEL
D_MLSTM = D_MIX // 2
D_DIFF = D_MIX - D_MLSTM
ML_HEADS = 4
ML_HD = D_MLSTM // ML_HEADS
CONV_W = 4
CHUNK = 128
DA_HEADS = 4
DA_VD = D_DIFF // DA_HEADS
DA_QD = DA_VD // 2
Q_BLOCK = 128
REL_BUCKETS = 32
REL_MAX_DIST = 128
N_GROUPS = 4
EXP_PER_GROUP = 8
N_EXPERTS = N_GROUPS * EXP_PER_GROUP
TOP_K_FINE = 2
D_FF_EXP = 512
EPS = 1e-6
SUBLN_EPS = 1e-5
IN_COLS = 3 * D_MLSTM + 3 * D_DIFF

kernel_name = 'hybrid_mlstm_diffattn_hmoe'


def rmsnorm(x, g, eps=EPS):
    xf = x.astype(jnp.float32)
    y = xf * lax.rsqrt(jnp.mean(xf * xf, axis=-1, keepdims=True) + eps)
    return (y * g.astype(jnp.float32)).astype(x.dtype)


def head_layernorm(h, g):
    mu = jnp.mean(h, axis=-1, keepdims=True)
    var = jnp.mean(jnp.square(h - mu), axis=-1, keepdims=True)
    return (h - mu) * lax.rsqrt(var + EPS) * g.astype(jnp.float32)


def causal_conv(x, w, b):
    c = x.shape[-1]
    y = lax.conv_general_dilated(x, w[:, None, :].astype(x.dtype), window_strides=(1,),
                                 padding=[(CONV_W - 1, 0)],
                                 dimension_numbers=('NWC', 'WIO', 'NWC'),
                                 feature_group_count=c)
    return y + b.astype(x.dtype)


def mlstm_chunkwise(q, k, v, i_pre, logf):
    bsz, s, nh, d = q.shape
    nc = s // CHUNK
    def chunks(t):
        return t.reshape(bsz, nc, CHUNK, nh, d).transpose(1, 0, 3, 2, 4)
    def gchunks(t):
        return t.reshape(bsz, nc, CHUNK, nh).transpose(1, 0, 3, 2)
    tril = jnp.tril(jnp.ones((CHUNK, CHUNK), dtype=bool))

    def step(carry, xs):
        cmat, nvec, m = carry
        qc, kc, vc, ic, fc = xs
        b = jnp.cumsum(fc, axis=-1)
        inter = b + m[..., None]
        dmat = jnp.where(tril, b[..., :, None] - b[..., None, :] + ic[..., None, :], -jnp.inf)
        m_t = jnp.maximum(inter, jnp.max(dmat, axis=-1))
        w = jnp.exp(dmat - m_t[..., None]) * jnp.einsum('bhtd,bhsd->bhts', qc, kc)
        sp = jnp.exp(inter - m_t)
        num = sp[..., None] * jnp.einsum('bhtk,bhvk->bhtv', qc, cmat) + jnp.einsum('bhts,bhsv->bhtv', w, vc)
        den = sp * jnp.einsum('bhtk,bhk->bht', qc, nvec) + jnp.sum(w, axis=-1)
        h = num / jnp.maximum(jnp.abs(den), jnp.exp(-m_t))[..., None]
        b_end = b[..., -1]
        g = b_end[..., None] - b + ic
        m_new = jnp.maximum(b_end + m, jnp.max(g, axis=-1))
        wk = jnp.exp(g - m_new[..., None])
        decay = jnp.exp(b_end + m - m_new)
        c_new = decay[..., None, None] * cmat + jnp.einsum('bhs,bhsv,bhsk->bhvk', wk, vc, kc)
        n_new = decay[..., None] * nvec + jnp.einsum('bhs,bhsk->bhk', wk, kc)
        return (c_new, n_new, m_new), h

    init = (jnp.zeros((bsz, nh, d, d), jnp.float32), jnp.zeros((bsz, nh, d), jnp.float32),
            jnp.zeros((bsz, nh), jnp.float32))
    _, hs = lax.scan(step, init, (chunks(q), chunks(k), chunks(v), gchunks(i_pre), gchunks(logf)))
    return hs.transpose(1, 0, 3, 2, 4).reshape(bsz, s, nh, d)


def mlstm_group(c, v, z, conv_w, conv_b, w_mq, w_mk, w_mgate, b_mgate, m_norm_g, m_skip):
    bsz, s, _ = c.shape
    c_act = jax.nn.silu(causal_conv(c, conv_w, conv_b))
    ch = c_act.reshape(bsz, s, ML_HEADS, ML_HD)
    q = jnp.einsum('bshd,hde->bshe', ch, w_mq)
    k = jnp.einsum('bshd,hde->bshe', ch, w_mk)
    vh = v.reshape(bsz, s, ML_HEADS, ML_HD)
    gin = jnp.concatenate([q.reshape(bsz, s, -1), k.reshape(bsz, s, -1), v], axis=-1)
    gates = (gin @ w_mgate).astype(jnp.float32) + b_mgate.astype(jnp.float32)
    i_pre = gates[..., :ML_HEADS]
    logf = jax.nn.log_sigmoid(gates[..., ML_HEADS:])
    h = mlstm_chunkwise(q.astype(jnp.float32), (k * (ML_HD ** -0.5)).astype(jnp.float32),
                        vh.astype(jnp.float32), i_pre, logf)
    h = head_layernorm(h, m_norm_g.reshape(ML_HEADS, ML_HD)).reshape(bsz, s, D_MLSTM)
    h = h + m_skip.astype(jnp.float32) * c_act.astype(jnp.float32)
    o = jax.nn.sigmoid(z.astype(jnp.float32))
    return (o * h).astype(c.dtype)


def rel_bucket(qpos, kpos):
    n = jnp.maximum(qpos[:, None] - kpos[None, :], 0)
    max_exact = REL_BUCKETS // 2
    large = max_exact + (jnp.log(jnp.maximum(n, 1).astype(jnp.float32) / max_exact)
                         / math.log(REL_MAX_DIST / max_exact) * (REL_BUCKETS - max_exact)).astype(jnp.int32)
    large = jnp.minimum(large, REL_BUCKETS - 1)
    return jnp.where(n < max_exact, n, large)


def diff_attention(q, k, v, lam, rel_bias):
    bsz, s = q.shape[0], q.shape[1]
    nb = s // Q_BLOCK
    scale = DA_QD ** -0.5
    qb = jnp.moveaxis(q.reshape(bsz, nb, Q_BLOCK, DA_HEADS, 2, DA_QD), 1, 0)
    kpos = jnp.arange(s)

    def block(args):
        q_blk, bi = args
        qpos = bi * Q_BLOCK + jnp.arange(Q_BLOCK)
        bias = rel_bias[rel_bucket(qpos, kpos)].transpose(2, 0, 1).astype(jnp.float32)
        mask = kpos[None, :] <= qpos[:, None]
        logits = jnp.einsum('bqhcd,bkhcd->bchqk', q_blk, k).astype(jnp.float32) * scale + bias[None, None]
        p = jax.nn.softmax(jnp.where(mask, logits, -jnp.inf), axis=-1)
        a = p[:, 0] - lam * p[:, 1]
        return jnp.einsum('bhqk,bkhd->bqhd', a.astype(v.dtype), v)

    out = lax.map(block, (qb, jnp.arange(nb)))
    return jnp.moveaxis(out, 0, 1).reshape(bsz, s, DA_HEADS, DA_VD)


def hier_moe(h, w_rg, b_rg, w_re, b_re, w_eg, w_eu, w_ed):
    bsz, s, d = h.shape
    t = h.reshape(-1, d)
    lg = (t @ w_rg).astype(jnp.float32) + b_rg.astype(jnp.float32)
    pg = jax.nn.softmax(lg, axis=-1)
    _, g_idx = lax.top_k(lg, 1)
    pg_sel = jnp.take_along_axis(pg, g_idx, axis=-1)
    le = jnp.einsum('nd,dge->nge', t, w_re).astype(jnp.float32) + b_re.astype(jnp.float32)
    le_sel = jnp.take_along_axis(le, g_idx[:, :, None], axis=1)[:, 0]
    top_v, top_i = lax.top_k(le_sel, TOP_K_FINE)
    pw = jax.nn.softmax(top_v, axis=-1)
    eid = g_idx * EXP_PER_GROUP + top_i
    gates = jnp.sum(jax.nn.one_hot(eid, N_EXPERTS, dtype=jnp.float32) * (pg_sel * pw)[..., None], axis=1)
    gates = gates.astype(t.dtype)
    out = jnp.zeros_like(t)
    for e in range(N_EXPERTS):
        hid = jax.nn.silu(t @ w_eg[e]) * (t @ w_eu[e])
        out = out + gates[:, e:e + 1] * (hid @ w_ed[e])
    return out.reshape(bsz, s, d)


def setup_inputs(seed: int = 0) -> dict:
    key = jax.random.key(seed)
    ks = jax.random.split(key, 32)
    f32 = jnp.float32
    def nrm(k, shape, sc):
        return jax.random.normal(k, shape, f32) * sc
    L = DEPTH
    b_i = nrm(ks[7], (L, ML_HEADS), 0.1)
    b_f = jnp.linspace(3.0, 6.0, ML_HEADS, dtype=f32)[None, :] + nrm(ks[8], (L, ML_HEADS), 0.01)
    return {
        'x': nrm(ks[0], (BATCH, SEQ, D_MODEL), 1.0),
        'w_in': nrm(ks[1], (L, D_MODEL, IN_COLS), D_MODEL ** -0.5),
        'conv_w': nrm(ks[2], (L, CONV_W, D_MLSTM), CONV_W ** -0.5),
        'conv_b': nrm(ks[3], (L, D_MLSTM), 0.01),
        'w_mq': nrm(ks[4], (L, ML_HEADS, ML_HD, ML_HD), ML_HD ** -0.5),
        'w_mk': nrm(ks[5], (L, ML_HEADS, ML_HD, ML_HD), ML_HD ** -0.5),
        'w_mgate': nrm(ks[6], (L, 3 * D_MLSTM, 2 * ML_HEADS), 0.1 * (3 * D_MLSTM) ** -0.5),
        'b_mgate': jnp.concatenate([b_i, b_f], axis=-1),
        'm_norm_g': 1.0 + nrm(ks[9], (L, D_MLSTM), 0.02),
        'm_skip': 1.0 + nrm(ks[10], (L, D_MLSTM), 0.02),
        'lambda_qk': nrm(ks[11], (L, 4, DA_QD), 0.1),
        'da_norm_g': 1.0 + nrm(ks[12], (L, DA_VD), 0.02),
        'rel_bias': nrm(ks[13], (REL_BUCKETS, DA_HEADS), 0.2),
        'w_out': nrm(ks[14], (L, D_MIX, D_MODEL), D_MIX ** -0.5),
        'norm1_g': 1.0 + nrm(ks[15], (L, D_MODEL), 0.02),
        'norm2_g': 1.0 + nrm(ks[16], (L, D_MODEL), 0.02),
        'w_rg': nrm(ks[17], (L, D_MODEL, N_GROUPS), D_MODEL ** -0.5),
        'b_rg': nrm(ks[18], (L, N_GROUPS), 0.01),
        'w_re': nrm(ks[19], (L, D_MODEL, N_GROUPS, EXP_PER_GROUP), D_MODEL ** -0.5),
        'b_re': nrm(ks[20], (L, N_GROUPS, EXP_PER_GROUP), 0.01),
        'w_eg': nrm(ks[21], (L, N_EXPERTS, D_MODEL, D_FF_EXP), D_MODEL ** -0.5),
        'w_eu': nrm(ks[22], (L, N_EXPERTS, D_MODEL, D_FF_EXP), D_MODEL ** -0.5),
        'w_ed': nrm(ks[23], (L, N_EXPERTS, D_FF_EXP, D_MODEL), D_FF_EXP ** -0.5),
        'normf_g': 1.0 + nrm(ks[24], (D_MODEL,), 0.02),
    }


def reference(x, w_in, conv_w, conv_b, w_mq, w_mk, w_mgate, b_mgate, m_norm_g, m_skip,
              lambda_qk, da_norm_g, rel_bias, w_out, norm1_g, norm2_g, w_rg, b_rg, w_re, b_re,
              w_eg, w_eu, w_ed, normf_g):
    bsz, s, _ = x.shape
    splits = [D_MLSTM, 2 * D_MLSTM, 3 * D_MLSTM, 3 * D_MLSTM + D_DIFF, 3 * D_MLSTM + 2 * D_DIFF]
    for l in range(DEPTH):
        lam_init = 0.8 - 0.6 * math.exp(-0.3 * l)
        h = rmsnorm(x, norm1_g[l])
        proj = h @ w_in[l]
        c, vm, z, qd, kd, vd = jnp.split(proj, splits, axis=-1)
        y_m = mlstm_group(c, vm, z, conv_w[l], conv_b[l], w_mq[l], w_mk[l], w_mgate[l], b_mgate[l],
                          m_norm_g[l], m_skip[l])
        lq = lambda_qk[l].astype(jnp.float32)
        lam = jnp.exp(jnp.dot(lq[0], lq[1])) - jnp.exp(jnp.dot(lq[2], lq[3])) + lam_init
        y_d = diff_attention(qd.reshape(bsz, s, DA_HEADS, 2, DA_QD), kd.reshape(bsz, s, DA_HEADS, 2, DA_QD),
                             vd.reshape(bsz, s, DA_HEADS, DA_VD), lam, rel_bias)
        y_d = (rmsnorm(y_d, da_norm_g[l], SUBLN_EPS) * (1.0 - lam_init)).reshape(bsz, s, D_DIFF)
        x = x + jnp.concatenate([y_m, y_d.astype(x.dtype)], axis=-1) @ w_out[l]
        h2 = rmsnorm(x, norm2_g[l])
        x = x + hier_moe(h2, w_rg[l], b_rg[l], w_re[l], b_re[l], w_eg[l], w_eu[l], w_ed[l])
    return rmsnorm(x, normf_g)
```

```python
import math
import contextlib
import numpy as np
import concourse.bass as bass
import concourse.mybir as mybir
from concourse.bass_utils import run_bass_kernel_spmd

F32 = mybir.dt.float32
BF16 = mybir.dt.bfloat16
AF = mybir.ActivationFunctionType
ALU = mybir.AluOpType
AX = mybir.AxisListType

S = 4096
D = 1024
NT = 32
EPS = 1e-6
SUBLN_EPS = 1e-5
N_EXP = 32
DFF = 512
LAM_INIT = 0.8 - 0.6 * math.exp(-0.3 * 0)
ML_SCALE = 128.0 ** -0.5
DA_SCALE = 64.0 ** -0.5
NEG = -30000.0

COMPUTE = ("pe", "act", "dve", "pool")
QUEUES = ("sp", "act", "pool")
N_DMA_SEMS = 8
DEBUG = False
STAGES = (0, 1, 2, 3, 4, 5)


class Sems:
    def __init__(self, nc, st):
        self.esem = {e: st.enter_context(nc.semaphore("s_" + e)) for e in COMPUTE}
        self.dsem = {(q, s): st.enter_context(nc.semaphore("d_%s_%d" % (q, s))) for q in QUEUES for s in range(N_DMA_SEMS)}
        self.cnt = {e: 0 for e in COMPUTE}
        self.dcnt = {k: 0 for k in self.dsem}
        self.rr = {q: 0 for q in QUEUES}


class Op:
    __slots__ = ("eng", "fn", "deps", "is_dma", "signal", "val", "sem", "slot", "prev")

    def __init__(self, eng, fn, is_dma):
        self.eng, self.fn, self.is_dma = eng, fn, is_dma
        self.deps = []
        self.signal = False
        self.val = None
        self.sem = None
        self.slot = None
        self.prev = None


class Prog:
    def __init__(self, nc, sems):
        self.nc = nc
        self.sems = sems
        self.ops = []
        self.last_writer = {}
        self.readers = {}
        self.slot_last = {}

    def _add(self, op, reads, writes):
        pr = [r for r in reads if r in PSUM_KEYS]
        if pr:
            reads = [r for r in reads if r not in PSUM_KEYS]
            writes = list(writes) + [r for r in pr if r not in writes]
        deps = []
        for r in reads:
            w = self.last_writer.get(r)
            if w is not None:
                deps.append(w)
        for w in writes:
            lw = self.last_writer.get(w)
            if lw is not None:
                deps.append(lw)
            deps.extend(self.readers.get(w, ()))
        seen = set()
        for d in deps:
            if id(d) not in seen and d is not op:
                seen.add(id(d))
                op.deps.append(d)
        for r in reads:
            self.readers.setdefault(r, []).append(op)
        for w in writes:
            self.last_writer[w] = op
            self.readers[w] = []
        self.ops.append(op)
        return op

    def op(self, eng, fn, reads=(), writes=()):
        return self._add(Op(eng, fn, False), reads, writes)

    def dma(self, queue, fn, reads=(), writes=()):
        op = Op(queue, fn, True)
        s = self.sems
        op.slot = (queue, s.rr[queue] % N_DMA_SEMS)
        s.rr[queue] += 1
        op.prev = self.slot_last.get(op.slot)
        self.slot_last[op.slot] = op
        return self._add(op, reads, writes)

    def mm(self, out, lhsT, rhs, r, w, start=True, stop=True, skip=False):
        if skip:
            return self.op("pe", lambda e: e.matmul(out, lhsT=lhsT, rhs=rhs, start=start, stop=stop, skip_group_check=True), r, w)
        return self.op("pe", lambda e: e.matmul(out, lhsT=lhsT, rhs=rhs, start=start, stop=stop), r, w)

    def tr(self, out, in_, ident, r, w):
        return self.op("pe", lambda e: e.transpose(out=out, in_=in_, identity=ident), r, w)

    def act(self, out, in_, func, r, w, bias=None, scale=None, accum_out=None):
        kw = {}
        if bias is not None:
            kw["bias"] = bias
        if scale is not None:
            kw["scale"] = scale
        if accum_out is not None:
            kw["accum_out"] = accum_out
        return self.op("act", lambda e: e.activation(out=out, in_=in_, func=func, **kw), r, w)

    def copy(self, eng, out, in_, r, w):
        if eng == "act":
            return self.op("act", lambda e: e.copy(out=out, in_=in_), r, w)
        return self.op(eng, lambda e: e.tensor_copy(out=out, in_=in_), r, w)

    def tt(self, eng, out, in0, in1, op, r, w):
        return self.op(eng, lambda e: e.tensor_tensor(out=out, in0=in0, in1=in1, op=op), r, w)

    def ts(self, eng, out, in0, s1, s2, op0, op1, r, w):
        if s2 is None:
            return self.op(eng, lambda e: e.tensor_scalar(out=out, in0=in0, scalar1=s1, scalar2=None, op0=op0), r, w)
        return self.op(eng, lambda e: e.tensor_scalar(out=out, in0=in0, scalar1=s1, scalar2=s2, op0=op0, op1=op1), r, w)

    def stt(self, eng, out, in0, scalar, in1, op0, op1, r, w):
        eng = "dve"
        return self.op(eng, lambda e: e.scalar_tensor_tensor(out=out, in0=in0, scalar=scalar, in1=in1, op0=op0, op1=op1), r, w)

    def memset(self, eng, ap, val, w):
        return self.op(eng, lambda e: e.memset(ap, val), (), w)

    def load(self, q, out, in_, r, w):
        return self.dma(q, lambda e: e.dma_start(out=out, in_=in_), r, w)

    def emit(self):
        nc, s, ops = self.nc, self.sems, self.ops

        def same_skip(d, o):
            return (not d.is_dma) and (not o.is_dma) and d.eng == o.eng and d.eng == "pe"

        for o in ops:
            for d in o.deps:
                if d.is_dma or same_skip(d, o):
                    continue
                d.signal = True
        for o in ops:
            if o.is_dma:
                s.dcnt[o.slot] += 16
                o.val = s.dcnt[o.slot]
                o.sem = s.dsem[o.slot]
            else:
                o.sem = s.esem[o.eng]
                if o.signal:
                    s.cnt[o.eng] += 1
                    o.val = s.cnt[o.eng]
        by_eng = {e: [] for e in ("pe", "act", "dve", "pool", "sp")}
        for o in ops:
            by_eng[o.eng].append(o)
        final = dict(s.dcnt)

        def run(engname, e):
            waited = {}

            def wait(sem, val):
                if waited.get(id(sem), 0) >= val:
                    return
                waited[id(sem)] = val
                e.wait_ge(sem, val)

            for o in by_eng[engname]:
                for d in o.deps:
                    if same_skip(d, o):
                        continue
                    wait(d.sem, d.val)
                if o.is_dma and o.prev is not None:
                    wait(o.prev.sem, o.prev.val)
                ins = o.fn(e)
                if o.is_dma:
                    ins.then_inc(o.sem, 16)
                elif o.signal:
                    ins.then_inc(o.sem, 1)
            if engname == "sp":
                for k, v in final.items():
                    if v > 0:
                        wait(s.dsem[k], v)

        with nc.Block() as block:
            block.sync(lambda e: run("sp", e))
            if by_eng["pe"]:
                block.tensor(lambda e: run("pe", e))
            if by_eng["act"]:
                block.scalar(lambda e: run("act", e))
            if by_eng["dve"]:
                block.vector(lambda e: run("dve", e))
            if by_eng["pool"]:
                block.gpsimd(lambda e: run("pool", e))
        return {k: len(v) for k, v in by_eng.items()}


class Rot:
    def __init__(self, n):
        self.n, self.i = n, 0

    def next(self):
        v = self.i % self.n
        self.i += 1
        return v


def build_program():
    nc = bass.Bass("TRN2", target_bir_lowering=False)
    I = lambda name, shape, dt=F32: nc.dram_tensor(name, list(shape), dt, kind="ExternalInput").ap()
    skind = "ExternalOutput" if DEBUG else "Internal"
    SC = lambda name, shape, dt: nc.dram_tensor(name, list(shape), dt, kind=skind).ap()
    T = {}
    T["x"] = I("x", [S, D])
    T["w_in"] = I("w_in", [D, 3072])
    T["conv_w"] = I("conv_w", [4, 512])
    T["conv_b"] = I("conv_b", [512])
    T["w_mq"] = I("w_mq", [4, 128, 128])
    T["w_mk"] = I("w_mk", [4, 128, 128])
    T["w_mgate"] = I("w_mgate", [1536, 8])
    T["b_mgate"] = I("b_mgate", [8])
    T["m_norm_g"] = I("m_norm_g", [512])
    T["m_skip"] = I("m_skip", [512])
    T["lambda_qk"] = I("lambda_qk", [256])
    T["da_norm_g"] = I("da_norm_g", [128])
    T["rel_bias"] = I("rel_bias", [128])
    T["w_out"] = I("w_out", [D, D])
    T["norm1_g"] = I("norm1_g", [D])
    T["norm2_g"] = I("norm2_g", [D])
    T["w_r"] = I("w_r", [D, 36])
    T["b_r"] = I("b_r", [36])
    T["w_eg"] = I("w_eg", [N_EXP, D, DFF])
    T["w_eu"] = I("w_eu", [N_EXP, D, DFF])
    T["w_ed"] = I("w_ed", [N_EXP, DFF, D])
    T["normf_g"] = I("normf_g", [D])
    T["ident"] = I("ident", [128, 128])
    T["tri"] = I("tri", [128, 128])
    T["sel"] = I("sel", [4, 512])
    T["oh"] = I("oh", [128, 2 * 33 * 128])
    T["out"] = nc.dram_tensor("out", [S, D], F32, kind="ExternalOutput").ap()
    T["featT"] = SC("featT", [5, 512, S], BF16)
    T["vm_tok"] = SC("vm_tok", [S, 512], BF16)
    T["vd_tok"] = SC("vd_tok", [S, 512], BF16)
    T["ymT"] = SC("ymT", [512, S], BF16)
    T["ydT"] = SC("ydT", [512, S], BF16)
    T["x2"] = SC("x2", [S, D], F32)
    T["h2T"] = SC("h2T", [D, S], BF16)
    T["gates"] = SC("gates", [128, NT * 32], F32)
    T["wg_bf"] = nc.dram_tensor("wg_bf", [N_EXP, D, DFF], BF16, kind="Internal").ap()
    T["wu_bf"] = nc.dram_tensor("wu_bf", [N_EXP, D, DFF], BF16, kind="Internal").ap()
    T["wd_bf"] = nc.dram_tensor("wd_bf", [N_EXP, DFF, D], BF16, kind="Internal").ap()

    stats = {}
    with contextlib.ExitStack() as gst:
        gst.enter_context(nc.allow_non_contiguous_dma(reason="small strided parameter loads"))
        sems = Sems(nc, gst)
        if 0 in STAGES:
            stats["s0"] = stage0(nc, sems, T)
        if 1 in STAGES:
            stats["s1"] = stage1(nc, sems, T)
        if 2 in STAGES:
            stats["s2"] = stage2(nc, sems, T)
        if 3 in STAGES:
            stats["s3"] = stage3(nc, sems, T)
        if 4 in STAGES:
            stats["s4"] = stage4(nc, sems, T)
        if 5 in STAGES:
            stats["s5"] = stage5(nc, sems, T)
    return nc, stats


_TN = [0]
PSUM_KEYS = set()


def tens(nc, st):
    _TN[0] += 1
    pre = "t%d_" % _TN[0]
    sb = lambda n, s, d=F32: st.enter_context(nc.sbuf_tensor(pre + n, list(s), d))
    def ps(n, s, d=F32):
        PSUM_KEYS.add(n)
        return st.enter_context(nc.psum_tensor(pre + n, list(s), d))
    return sb, ps


def stage0(nc, sems, T):
    with contextlib.ExitStack() as st:
        sb, ps = tens(nc, st)
        P = Prog(nc, sems)
        NB = 3
        stg = [sb("w0s%d" % i, [128, 8, 512], F32) for i in range(NB)]
        cvt = [sb("w0c%d" % i, [128, 8, 512], BF16) for i in range(NB)]
        rot = Rot(NB)
        engs = ["dve", "pool", "act"]
        k = 0
        for name, dst in (("w_eg", "wg_bf"), ("w_eu", "wu_bf"), ("w_ed", "wd_bf")):
            for e in range(N_EXP):
                b = rot.next()
                src = T[name][e].rearrange("(c p) f -> p c f", p=128)
                dstap = T[dst][e].rearrange("(c p) f -> p c f", p=128)
                if name == "w_ed":
                    sv = stg[b][:].rearrange("p (c h) f -> p c (h f)", c=4)
                    cv = cvt[b][:].rearrange("p (c h) f -> p c (h f)", c=4)
                else:
                    sv, cv = stg[b][:], cvt[b][:]
                P.load("sp" if k % 2 == 0 else "act", sv, src, [], ["stg%d" % b])
                eng = engs[k % 3]
                P.copy(eng, cvt[b][:], stg[b][:], ["stg%d" % b], ["cvt%d" % b])
                P.load("sp" if k % 2 == 1 else "act", dstap, cv, ["cvt%d" % b], [dst])
                k += 1
        return P.emit()


def stage1(nc, sems, T):
    with contextlib.ExitStack() as st:
        sb, ps = tens(nc, st)
        P = Prog(nc, sems)
        ident = sb("ident", [128, 128])
        identb = sb("identb", [128, 128], BF16)
        g1 = sb("g1", [128, 8])
        w_bf = sb("w_in_bf", [128, 8, 3072], BF16)
        wst = [sb("wst%d" % i, [128, 3072]) for i in range(2)]
        xt = [sb("xt%d" % i, [128, D]) for i in range(2)]
        junk = sb("junk", [128, D], BF16)
        stat = sb("stat", [128, 4])
        xn = [sb("xn%d" % i, [128, D], BF16) for i in range(2)]
        hT = [sb("hT%d" % i, [128, 8, 512], BF16) for i in range(2)]
        fstg = [sb("fstg%d" % i, [128, 4, 512], BF16) for i in range(2)]
        tstg = [sb("tstg%d" % i, [128, 4, 512], BF16) for i in range(2)]
        pt = [ps("pt%d" % i, [128, D], BF16) for i in range(2)]
        pp = [ps("pp%d" % i, [128, 512]) for i in range(4)]

        P.load("sp", ident[:], T["ident"], [], ["ident"])
        P.copy("dve", identb[:], ident[:], ["ident"], ["identb"])
        P.load("sp", g1[:], T["norm1_g"].rearrange("(c p) -> p c", p=128), [], ["g1"])
        for kc in range(8):
            b = kc % 2
            P.load("sp" if kc % 2 == 0 else "act", wst[b][:], T["w_in"][kc * 128:(kc + 1) * 128, :], [], ["wst%d" % b])
            P.ts("dve" if kc % 2 == 0 else "pool", w_bf[:, kc, :], wst[b][:], g1[:, kc:kc + 1], None, ALU.mult, None,
                 ["wst%d" % b, "g1"], ["w_bf"])
        rpp = Rot(4)
        for g in range(8):
            hb = g % 2
            for ti in range(4):
                t = g * 4 + ti
                b = t % 2
                P.load("sp", xt[b][:], T["x"][t * 128:(t + 1) * 128, :], [], ["xt%d" % b])
                P.act(junk[:], xt[b][:], AF.Square, ["xt%d" % b], ["junk", "stat"], accum_out=stat[:, 0:1])
                P.ts("dve", stat[:, 1:2], stat[:, 0:1], 1.0 / D, EPS, ALU.mult, ALU.add, ["stat"], ["stat"])
                P.act(stat[:, 2:3], stat[:, 1:2], AF.Ln, ["stat"], ["stat"])
                P.act(stat[:, 3:4], stat[:, 2:3], AF.Exp, ["stat"], ["stat"], scale=-0.5)
                P.ts("dve", xn[b][:], xt[b][:], stat[:, 3:4], None, ALU.mult, None, ["xt%d" % b, "stat"], ["xn%d" % b])
                for kc in range(8):
                    P.tr(pt[b][:, kc * 128:(kc + 1) * 128], xn[b][:, kc * 128:(kc + 1) * 128], identb[:], ["xn%d" % b, "identb"], ["pt%d" % b])
                P.copy("act" if ti % 2 == 0 else "dve", hT[hb][:, :, ti * 128:(ti + 1) * 128], pt[b][:, :].rearrange("p (k t) -> p k t", k=8),
                       ["pt%d" % b], ["hT%d" % hb])
            for blk in range(5):
                fb = (g * 5 + blk) % 2
                for ch in range(4):
                    col0 = blk * 512 + ch * 128
                    pb = rpp.next()
                    for kc in range(8):
                        P.mm(pp[pb][:, :], w_bf[:, kc, col0:col0 + 128], hT[hb][:, kc, :], ["w_bf", "hT%d" % hb], ["pp%d" % pb],
                             start=(kc == 0), stop=(kc == 7))
                    P.copy("act" if ch % 2 == 0 else "dve", fstg[fb][:, ch, :], pp[pb][:, :], ["pp%d" % pb], ["fstg%d" % fb])
                P.load("act", T["featT"][blk].rearrange("(c p) t -> p c t", p=128)[:, :, g * 512:(g + 1) * 512], fstg[fb][:],
                       ["fstg%d" % fb], ["featT"])
            for bi, (blk, dst) in enumerate(((1, "vm_tok"), (5, "vd_tok"))):
                tb = (g * 2 + bi) % 2
                for ti in range(4):
                    pb = rpp.next()
                    for kc in range(8):
                        P.mm(pp[pb][:, :], hT[hb][:, kc, ti * 128:(ti + 1) * 128], w_bf[:, kc, blk * 512:(blk + 1) * 512],
                             ["w_bf", "hT%d" % hb], ["pp%d" % pb], start=(kc == 0), stop=(kc == 7))
                    P.copy("dve" if ti % 2 == 0 else "act", tstg[tb][:, ti, :], pp[pb][:, :], ["pp%d" % pb], ["tstg%d" % tb])
                P.load("sp", T[dst][g * 512:(g + 1) * 512, :].rearrange("(t p) f -> p t f", p=128), tstg[tb][:], ["tstg%d" % tb], [dst])
        return P.emit()


def stage2(nc, sems, T):
    with contextlib.ExitStack() as st:
        sb, ps = tens(nc, st)
        P = Prog(nc, sems)
        ident = sb("ident", [128, 128])
        identb = sb("identb", [128, 128], BF16)
        tri = sb("tri", [128, 128])
        sel = sb("sel", [4, 512])
        cw = sb("cw", [128, 4, 4])
        cb = sb("cb", [128, 4])
        mg = sb("mg", [128, 4])
        msk = sb("msk", [128, 4])
        wq = sb("wq", [128, 4, 128], BF16)
        wk = sb("wk", [128, 4, 128], BF16)
        wgt = sb("wgt", [128, 12, 8], BF16)
        bi = sb("bi", [4, 1])
        bfn = sb("bfn", [4, 1])
        zeros = sb("zeros", [4, 512])
        carryB = sb("carryB", [4, 1])
        carryM = sb("carryM", [4, 1])
        Cf = sb("Cf", [128, 4, 129])
        Cb = sb("Cb", [128, 4, 129], BF16)
        c_sb = [sb("c_sb%d" % i, [128, 4, 515], BF16) for i in range(2)]
        z_sb = [sb("z_sb%d" % i, [128, 4, 512], BF16) for i in range(2)]
        vmT = [sb("vmT%d" % i, [128, 4, 512], BF16) for i in range(2)]
        vaug = [sb("vaug%d" % i, [128, 4, 4, 129], BF16) for i in range(2)]
        cacc = [sb("cacc%d" % i, [128, 512]) for i in range(2)]
        cact = sb("cact", [128, 4, 512], BF16)
        sigz = sb("sigz", [128, 4, 512], BF16)
        scs = sb("scs", [128, 4, 512], BF16)
        qT = sb("qT", [128, 4, 512], BF16)
        kT = sb("kT", [128, 4, 512], BF16)
        ktok = sb("ktok", [128, 4, 4, 128], BF16)
        i_row = sb("i_row", [4, 512])
        e_row = sb("e_row", [4, 512])
        sp_row = sb("sp_row", [4, 512])
        Bn = sb("Bn", [4, 513])
        A_row = sb("A_row", [4, 512])
        Mx = sb("Mx", [4, 513])
        N_row = sb("N_row", [4, 512])
        cols = sb("cols", [128, 4, 3, 4])
        eN = sb("eN", [128, 4, 4])
        Mb = sb("Mb", [128, 4, 5])
        nMb = sb("nMb", [128, 4, 5])
        dec = sb("dec", [128, 4, 4])
        spa = sb("spa", [128, 4, 4])
        Mrow = sb("Mrow", [128, 4, 512])
        tmpD = [sb("tmpD%d" % i, [128, 128]) for i in range(2)]
        Dt = [sb("Dt%d" % i, [128, 128]) for i in range(2)]
        Dm = [sb("Dm%d" % i, [128, 128]) for i in range(2)]
        wT = [sb("wT%d" % i, [128, 128], BF16) for i in range(2)]
        intra = [sb("intra%d" % i, [128, 129]) for i in range(2)]
        comb = [sb("comb%d" % i, [128, 129]) for i in range(2)]
        sm = [sb("sm%d" % i, [128, 16]) for i in range(2)]
        hh = [sb("hh%d" % i, [128, 128]) for i in range(2)]
        hn = [sb("hn%d" % i, [128, 128], BF16) for i in range(2)]
        y1 = [sb("y1%d" % i, [128, 128], BF16) for i in range(2)]
        ymg = [sb("ymg%d" % i, [128, 4, 512], BF16) for i in range(2)]
        wkc = [sb("wkc%d" % i, [128, 1]) for i in range(2)]
        vw = [sb("vw%d" % i, [128, 129], BF16) for i in range(2)]
        pA = ps("pA", [128, 512])
        pB = ps("pB", [128, 512])
        pG = ps("pG", [128, 512])
        ptb = ps("ptb", [128, 1024], BF16)
        pS = [ps("pS%d" % i, [128, 512]) for i in range(2)]
        pO = [ps("pO%d" % i, [128, 512]) for i in range(2)]
        P.load("sp", ident[:], T["ident"], [], ["ident"])
        P.copy("dve", identb[:], ident[:], ["ident"], ["identb"])
        P.load("sp", tri[:], T["tri"], [], ["tri"])
        P.load("sp", sel[:], T["sel"], [], ["sel"])
        P.load("sp", cw[:], T["conv_w"].rearrange("j (c p) -> p j c", p=128), [], ["cw"])
        P.load("sp", cb[:], T["conv_b"].rearrange("(c p) -> p c", p=128), [], ["cb"])
        P.load("sp", mg[:], T["m_norm_g"].rearrange("(c p) -> p c", p=128), [], ["mg"])
        P.load("sp", msk[:], T["m_skip"].rearrange("(c p) -> p c", p=128), [], ["msk"])
        P.load("pool", wq[:], T["w_mq"].rearrange("h d e -> d h e"), [], ["wq"])
        P.load("pool", wk[:], T["w_mk"].rearrange("h d e -> d h e"), [], ["wk"])
        P.load("pool", wgt[:], T["w_mgate"].rearrange("(c p) g -> p c g", p=128), [], ["wgt"])
        P.load("sp", bi[:], T["b_mgate"][0:4].rearrange("(p o) -> p o", o=1), [], ["bi"])
        P.load("sp", bfn[:], T["b_mgate"][4:8].rearrange("(p o) -> p o", o=1), [], ["bfn"])
        P.ts("dve", bfn[:], bfn[:], -1.0, None, ALU.mult, None, ["bfn"], ["bfn"])
        P.memset("pool", zeros[:], 0.0, ["zeros"])
        P.memset("pool", carryB[:], 0.0, ["carryB"])
        P.memset("pool", carryM[:], 0.0, ["carryM"])
        P.memset("pool", Cf[:], 0.0, ["Cf"])
        P.memset("pool", Cb[:], 0.0, ["Cb"])
        for i in range(2):
            P.memset("pool", vaug[i][:], 1.0, ["vaug%d" % i])
            P.memset("pool", c_sb[i][:], 0.0, ["c_sb%d" % i])

        featT = T["featT"]
        rS, rO, r2 = Rot(2), Rot(2), Rot(2)
        for g in range(8):
            b = g % 2
            t0 = g * 512
            kc_, kz, kv, kva = "c_sb%d" % b, "z_sb%d" % b, "vmT%d" % b, "vaug%d" % b
            cview = featT[0].rearrange("(c p) t -> p c t", p=128)
            if g == 0:
                P.load("sp", c_sb[b][:, :, 3:515], cview[:, :, 0:512], [], [kc_])
            else:
                P.load("sp", c_sb[b][:, :, 0:515], cview[:, :, t0 - 3:t0 + 512], [], [kc_])
            P.load("act", z_sb[b][:], featT[2].rearrange("(c p) t -> p c t", p=128)[:, :, t0:t0 + 512], [], [kz])
            P.load("act", vmT[b][:], featT[1].rearrange("(c p) t -> p c t", p=128)[:, :, t0:t0 + 512], [], [kv])
            for ti in range(4):
                P.load("sp" if ti % 2 == 0 else "act", vaug[b][:, ti, :, 0:128],
                       T["vm_tok"][t0 + ti * 128:t0 + (ti + 1) * 128, :].rearrange("p (h e) -> p h e", e=128), [kva], [kva])
            for ch in range(4):
                ab = ch % 2
                ka = "cacc%d" % ab
                e1 = "dve" if ch % 2 == 0 else "pool"
                P.ts("dve", cacc[ab][:], c_sb[b][:, ch, 0:512], cw[:, 0, ch:ch + 1], cb[:, ch:ch + 1], ALU.mult, ALU.add, [kc_, "cw", "cb"], [ka])
                for j in range(1, 4):
                    P.stt("dve" if j % 2 == 0 else "pool", cacc[ab][:], c_sb[b][:, ch, j:j + 512], cw[:, j, ch:ch + 1], cacc[ab][:], ALU.mult, ALU.add,
                          [kc_, "cw", ka], [ka])
                P.act(cact[:, ch, :], cacc[ab][:], AF.Silu, [ka], ["cact"])
                P.ts("pool", scs[:, ch, :], cact[:, ch, :], msk[:, ch:ch + 1], None, ALU.mult, None, ["cact", "msk"], ["scs"])
            P.act(sigz[:].rearrange("p c t -> p (c t)"), z_sb[b][:].rearrange("p c t -> p (c t)"), AF.Sigmoid, [kz], ["sigz"])
            for h in range(4):
                P.mm(pA[:, :], wq[:, h, :], cact[:, h, :], ["wq", "cact"], ["pA"])
                P.copy("act", qT[:, h, :], pA[:, :], ["pA"], ["qT"])
                P.mm(pB[:, :], wk[:, h, :], cact[:, h, :], ["wk", "cact"], ["pB"])
                P.copy("dve", kT[:, h, :], pB[:, :], ["pB"], ["kT"])
            for ti in range(4):
                pz = pA if ti % 2 == 0 else pB
                kz_ = "pA" if ti % 2 == 0 else "pB"
                for h in range(4):
                    P.mm(pz[:, h * 128:(h + 1) * 128], cact[:, h, ti * 128:(ti + 1) * 128], wk[:, h, :], ["cact", "wk"], [kz_])
                P.copy("act" if ti % 2 == 0 else "dve", ktok[:, ti, :, :].rearrange("p h e -> p (h e)"), pz[:, :], [kz_], ["ktok"])
            srcs = [(qT, "qT")] * 4 + [(kT, "kT")] * 4 + [(vmT[b], kv)] * 4
            for c in range(12):
                sap, skey = srcs[c]
                P.mm(pG[0:4, :], wgt[:, c, 0:4], sap[:, c % 4, :], ["wgt", skey], ["pG"], start=(c == 0), stop=(c == 11))
            for c in range(12):
                sap, skey = srcs[c]
                P.mm(pB[0:4, :], wgt[:, c, 4:8], sap[:, c % 4, :], ["wgt", skey], ["pB"], start=(c == 0), stop=(c == 11))
            P.ts("dve", i_row[:], pG[0:4, :], bi[:, 0:1], None, ALU.add, None, ["pG", "bi"], ["i_row"])
            P.act(e_row[:], pB[0:4, :], AF.Exp, ["pB", "bfn"], ["e_row"], bias=bfn[:, 0:1], scale=-1.0)
            P.act(sp_row[:], e_row[:], AF.Ln, ["e_row"], ["sp_row"], bias=1.0)
            P.op("dve", (lambda o, d0, d1, ini: (lambda e: e.tensor_tensor_scan(out=o, data0=d0, data1=d1, initial=ini, op0=ALU.add, op1=ALU.add)))(
                Bn[:, 1:513], sp_row[:], zeros[:], carryB[:, 0:1]), ["sp_row", "zeros", "carryB"], ["Bn"])
            P.tt("dve", A_row[:], i_row[:], Bn[:, 1:513], ALU.add, ["i_row", "Bn"], ["A_row"])
            P.copy("dve", Mx[:, 0:1], carryM[:, 0:1], ["carryM"], ["Mx"])
            P.op("dve", (lambda o, d0, d1, ini: (lambda e: e.tensor_tensor_scan(out=o, data0=d0, data1=d1, initial=ini, op0=ALU.max, op1=ALU.max)))(
                Mx[:, 1:513], A_row[:], A_row[:], carryM[:, 0:1]), ["A_row", "carryM", "Mx"], ["Mx"])
            P.copy("dve", carryB[:, 0:1], Bn[:, 512:513], ["Bn"], ["carryB"])
            P.copy("dve", carryM[:, 0:1], Mx[:, 512:513], ["Mx"], ["carryM"])
            P.tt("dve", N_row[:], Bn[:, 1:513], Mx[:, 1:513], ALU.subtract, ["Bn", "Mx"], ["N_row"])
            for c in range(4):
                for k3, (rap, rkey, off) in enumerate(((A_row, "A_row", 0), (Mx, "Mx", 1), (N_row, "N_row", 0))):
                    o0 = c * 12 + k3 * 4
                    P.tr(pA[:, o0:o0 + 4], rap[:, off + c * 128: off + (c + 1) * 128], ident[0:4, 0:4], [rkey, "ident"], ["pA"])
            P.copy("dve", cols[:].rearrange("p c k h -> p (c k h)"), pA[:, 0:48], ["pA"], ["cols"])
            P.act(eN[:], cols[:, :, 2, :], AF.Exp, ["cols"], ["eN"])
            for h in range(4):
                P.mm(pB[:, h * 5:(h + 1) * 5], sel[:, h * 128:(h + 1) * 128], Mx[:, 0:513:128], ["sel", "Mx"], ["pB"])
            P.copy("dve", Mb[:].rearrange("p h c -> p (h c)"), pB[:, 0:20], ["pB"], ["Mb"])
            P.ts("dve", nMb[:], Mb[:], -1.0, None, ALU.mult, None, ["Mb"], ["nMb"])
            P.tt("dve", dec[:], Mb[:, :, 0:4], Mb[:, :, 1:5], ALU.subtract, ["Mb"], ["dec"])
            P.act(dec[:], dec[:], AF.Exp, ["dec"], ["dec"])
            P.tt("dve", spa[:], Mb[:, :, 0:4].rearrange("p h c -> p c h"), cols[:, :, 1, :], ALU.subtract, ["Mb", "cols"], ["spa"])
            P.act(spa[:], spa[:], AF.Exp, ["spa"], ["spa"])
            for h in range(4):
                pz, kz_ = (pA, "pA") if h % 2 == 0 else (pB, "pB")
                P.mm(pz[:, :], sel[:, h * 128:(h + 1) * 128], Mx[:, 1:513], ["sel", "Mx"], [kz_])
                P.copy("act" if h % 2 == 0 else "dve", Mrow[:, h, :], pz[:, :], [kz_], ["Mrow"])
            for c in range(4):
                cs = slice(c * 128, (c + 1) * 128)
                for h in range(4):
                    i2 = r2.next()
                    sB, oB = rS.next(), rO.next()
                    kS, kO = "pS%d" % sB, "pO%d" % oB
                    Acol = cols[:, c, 0, h:h + 1]
                    P.mm(pS[sB][:, 0:128], kT[:, h, cs], qT[:, h, cs], ["kT", "qT"], [kS])
                    P.ts("dve", tmpD[i2][:], Mrow[:, h, cs], Acol, 0.0, ALU.subtract, ALU.max, ["Mrow", "cols"], ["tmpD%d" % i2])
                    P.act(Dt[i2][:], tmpD[i2][:], AF.Exp, ["tmpD%d" % i2], ["Dt%d" % i2], scale=-1.0)
                    P.tt("pool", Dm[i2][:], Dt[i2][:], tri[:], ALU.mult, ["Dt%d" % i2, "tri"], ["Dm%d" % i2])
                    P.tt("dve", wT[i2][:], pS[sB][:, 0:128], Dm[i2][:], ALU.mult, [kS, "Dm%d" % i2], ["wT%d" % i2])
                    P.mm(pO[oB][:, 0:129], wT[i2][:], vaug[b][:, c, h, :], ["wT%d" % i2, kva], [kO])
                    P.mm(pO[oB][:, 256:385], qT[:, h, cs], Cb[:, h, :], ["qT", "Cb"], [kO])
                    P.copy("act", intra[i2][:], pO[oB][:, 0:129], [kO], ["intra%d" % i2])
                    P.stt("dve", comb[i2][:], pO[oB][:, 256:385], spa[:, c, h:h + 1], intra[i2][:], ALU.mult, ALU.add,
                          [kO, "spa", "intra%d" % i2], ["comb%d" % i2])
                    ks = "sm%d" % i2
                    smt = sm[i2]
                    P.stt("dve", smt[:, 0:1], comb[i2][:, 128:129], -1.0, comb[i2][:, 128:129], ALU.mult, ALU.max, ["comb%d" % i2], [ks])
                    P.stt("dve", smt[:, 1:2], smt[:, 0:1], ML_SCALE, eN[:, c, h:h + 1], ALU.mult, ALU.max, [ks, "eN"], [ks])
                    P.op("dve", (lambda o, i_: (lambda e: e.reciprocal(out=o, in_=i_)))(smt[:, 2:3], smt[:, 1:2]), [ks], [ks])
                    P.ts("dve", hh[i2][:], comb[i2][:, 0:128], smt[:, 2:3], ML_SCALE, ALU.mult, ALU.mult, ["comb%d" % i2, ks], ["hh%d" % i2])
                    P.op("dve", (lambda o, i_: (lambda e: e.bn_stats(out=o, in_=i_)))(smt[:, 4:10], hh[i2][:]), ["hh%d" % i2], [ks])
                    P.op("dve", (lambda o, i_: (lambda e: e.bn_aggr(out=o, in_=i_)))(smt[:, 10:12], smt[:, 4:10]), [ks], [ks])
                    P.ts("dve", smt[:, 12:13], smt[:, 11:12], EPS, None, ALU.add, None, [ks], [ks])
                    P.act(smt[:, 13:14], smt[:, 12:13], AF.Ln, [ks], [ks])
                    P.act(smt[:, 14:15], smt[:, 13:14], AF.Exp, [ks], [ks], scale=-0.5)
                    P.ts("dve", hn[i2][:], hh[i2][:], smt[:, 10:11], smt[:, 14:15], ALU.subtract, ALU.mult, ["hh%d" % i2, ks], ["hn%d" % i2])
                    P.tr(ptb[:, i2 * 512:i2 * 512 + 128], hn[i2][:], identb[:], ["hn%d" % i2, "identb"], ["ptb"])
                    P.stt("dve", y1[i2][:], ptb[:, i2 * 512:i2 * 512 + 128], mg[:, h:h + 1], scs[:, h, cs], ALU.mult, ALU.add,
                          ["ptb", "mg", "scs"], ["y1%d" % i2])
                    P.tt("pool", ymg[b][:, h, cs], y1[i2][:], sigz[:, h, cs], ALU.mult, ["y1%d" % i2, "sigz"], ["ymg%d" % b])
                    P.act(wkc[i2][:], Acol, AF.Exp, ["cols", "nMb"], ["wkc%d" % i2], bias=nMb[:, h, c + 1:c + 2])
                    P.ts("pool", vw[i2][:], vaug[b][:, c, h, :], wkc[i2][:, 0:1], None, ALU.mult, None, [kva, "wkc%d" % i2], ["vw%d" % i2])
                    P.mm(pS[sB][:, 256:385], ktok[:, c, h, :], vw[i2][:], ["ktok", "vw%d" % i2], [kS])
                    P.stt("dve", Cf[:, h, :], Cf[:, h, :], dec[:, h, c:c + 1], pS[sB][:, 256:385], ALU.mult, ALU.add, ["Cf", "dec", kS], ["Cf"])
                    P.copy("pool", Cb[:, h, :], Cf[:, h, :], ["Cf"], ["Cb"])
            P.load("sp", T["ymT"].rearrange("(c p) t -> p c t", p=128)[:, :, t0:t0 + 512], ymg[b][:], ["ymg%d" % b], ["ymT"])
        return P.emit()


def stage3(nc, sems, T):
    with contextlib.ExitStack() as st:
        sb, ps = tens(nc, st)
        P = Prog(nc, sems)
        ident = sb("ident", [128, 128])
        identb = sb("identb", [128, 128], BF16)
        qT = sb("qT", [128, 4, S], BF16)
        kT = sb("kT", [128, 4, S], BF16)
        vaug = sb("vaug", [128, NT, 4, 129], BF16)
        oh = sb("oh", [128, 2, 33, 128])
        rbb = sb("rbb", [128, 128])
        lqb = sb("lqb", [128, 256])
        lt = sb("lt", [128, 64])
        lam = sb("lam", [128, 8])
        dag = sb("dag", [128, 1])
        biasT = sb("biasT", [128, 2, 4, 128])
        PT = [sb("PT%d" % i, [128, 512], BF16) for i in range(3)]
        tmpn = [sb("tmpn%d" % i, [128, 128]) for i in range(2)]
        t0s = [sb("t0s%d" % i, [128, 128]) for i in range(2)]
        av = [sb("av%d" % i, [128, 128]) for i in range(2)]
        junk = sb("junk3", [128, 128])
        sm = [sb("sm3%d" % i, [128, 8]) for i in range(2)]
        an = [sb("an%d" % i, [128, 128], BF16) for i in range(2)]
        ydg = [sb("ydg%d" % i, [128, 512], BF16) for i in range(2)]
        pS = [ps("pS%d" % i, [128, 512]) for i in range(2)]
        acc = [ps("acc%d" % i, [128, 512]) for i in range(4)]
        ptb = ps("ptb", [128, 1024], BF16)

        P.load("sp", ident[:], T["ident"], [], ["ident"])
        P.copy("dve", identb[:], ident[:], ["ident"], ["identb"])
        P.memset("pool", vaug[:], 1.0, ["vaug"])
        P.load("sp", qT[:], T["featT"][3].rearrange("(h p) t -> p h t", p=128), [], ["qT"])
        P.load("act", kT[:], T["featT"][4].rearrange("(h p) t -> p h t", p=128), [], ["kT"])
        for t in range(NT):
            P.load("sp" if t % 2 == 0 else "act", vaug[:, t, :, 0:128],
                   T["vd_tok"][t * 128:(t + 1) * 128, :].rearrange("p (h e) -> p h e", e=128), ["vaug"], ["vaug"])
        P.load("sp", oh[:].rearrange("p a b c -> p (a b c)"), T["oh"], [], ["oh"])
        P.load("sp", rbb[:], T["rel_bias"].partition_broadcast(128), [], ["rbb"])
        P.load("sp", lqb[:], T["lambda_qk"].partition_broadcast(128), [], ["lqb"])
        P.load("sp", dag[:], T["da_norm_g"].rearrange("(p o) -> p o", o=1), [], ["dag"])
        P.ts("dve", dag[:], dag[:], 1.0 - LAM_INIT, None, ALU.mult, None, ["dag"], ["dag"])
        for i in range(2):
            P.tt("dve", lt[:], lqb[:, (2 * i) * 64:(2 * i + 1) * 64], lqb[:, (2 * i + 1) * 64:(2 * i + 2) * 64], ALU.mult, ["lqb", "lt"], ["lt"])
            P.op("dve", (lambda o, i_: (lambda e: e.reduce_sum(out=o, in_=i_, axis=AX.X)))(lam[:, 4 + i:5 + i], lt[:]), ["lt"], ["lam"])
        P.act(lam[:, 0:2], lam[:, 4:6], AF.Exp, ["lam"], ["lam"])
        P.tt("dve", lam[:, 2:3], lam[:, 0:1], lam[:, 1:2], ALU.subtract, ["lam"], ["lam"])
        P.ts("dve", lam[:, 3:4], lam[:, 2:3], LAM_INIT, -1.0, ALU.add, ALU.mult, ["lam"], ["lam"])
        for kind in range(2):
            for h in range(4):
                eng = "dve" if (kind * 4 + h) % 2 == 0 else "pool"
                dst = biasT[:, kind, h, :]
                P.ts(eng, dst, oh[:, kind, 32, :], NEG, None, ALU.mult, None, ["oh"], ["biasT"])
                for b_ in range(32):
                    P.stt(eng, dst, oh[:, kind, b_, :], rbb[:, b_ * 4 + h:b_ * 4 + h + 1], dst, ALU.mult, ALU.add, ["oh", "rbb", "biasT"], ["biasT"])
        rS, rP, r2 = Rot(2), Rot(3), Rot(2)
        for h in range(4):
            for g in range(8):
                for a in range(4):
                    P.memset("dve", acc[a][:, :], 0.0, ["acc%d" % a])
                for c in range(2):
                    prow = slice(c * 64, (c + 1) * 64)
                    for j in range(4 * g + 4):
                        i_lo = max(j, 4 * g) - 4 * g
                        sB = rS.next()
                        pb = rP.next()
                        kS, kP = "pS%d" % sB, "PT%d" % pb
                        P.mm(pS[sB][:, i_lo * 128:512], kT[prow, h, j * 128:(j + 1) * 128], qT[prow, h, g * 512 + i_lo * 128:(g + 1) * 512],
                             ["kT", "qT"], [kS])
                        far_lo = None
                        for i in range(i_lo, 4):
                            dist = 4 * g + i - j
                            if dist >= 2:
                                far_lo = i
                                break
                            n2 = r2.next()
                            P.stt("dve", tmpn[n2][:], pS[sB][:, i * 128:(i + 1) * 128], DA_SCALE, biasT[:, dist, h, :], ALU.mult, ALU.add,
                                  [kS, "biasT"], ["tmpn%d" % n2])
                            P.act(PT[pb][:, i * 128:(i + 1) * 128], tmpn[n2][:], AF.Exp, ["tmpn%d" % n2], [kP])
                        if far_lo is not None:
                            P.act(PT[pb][:, far_lo * 128:512], pS[sB][:, far_lo * 128:512], AF.Exp, [kS, "rbb"], [kP],
                                  bias=rbb[:, 31 * 4 + h:31 * 4 + h + 1], scale=DA_SCALE)
                        for i in range(i_lo, 4):
                            a = c * 2 + i // 2
                            off = (i % 2) * 256
                            P.mm(acc[a][:, off:off + 129], PT[pb][:, i * 128:(i + 1) * 128], vaug[:, j, h, :], [kP, "vaug"], ["acc%d" % a],
                                 start=False, stop=False, skip=True)
                yb = (h * 8 + g) % 2
                for i in range(4):
                    n2 = r2.next()
                    ks = "sm3%d" % n2
                    smt = sm[n2]
                    a0, a1 = acc[i // 2], acc[2 + i // 2]
                    k0, k1 = "acc%d" % (i // 2), "acc%d" % (2 + i // 2)
                    off = (i % 2) * 256
                    P.op("dve", (lambda o, i_: (lambda e: e.reciprocal(out=o, in_=i_)))(smt[:, 0:1], a0[:, off + 128:off + 129]), [k0], [ks])
                    P.op("dve", (lambda o, i_: (lambda e: e.reciprocal(out=o, in_=i_)))(smt[:, 1:2], a1[:, off + 128:off + 129]), [k1], [ks])
                    P.tt("dve", smt[:, 2:3], smt[:, 1:2], lam[:, 3:4], ALU.mult, [ks, "lam"], [ks])
                    P.op("act", (lambda o, i_, sc: (lambda e: e.activation(out=o, in_=i_, func=AF.Copy, scale=sc)))(t0s[n2][:], a0[:, off:off + 128], smt[:, 0:1]),
                         [k0, ks], ["t0s%d" % n2])
                    P.stt("dve", av[n2][:], a1[:, off:off + 128], smt[:, 2:3], t0s[n2][:], ALU.mult, ALU.add, [k1, ks, "t0s%d" % n2], ["av%d" % n2])
                    P.act(junk[:], av[n2][:], AF.Square, ["av%d" % n2], ["junk3", ks], accum_out=smt[:, 3:4])
                    P.ts("dve", smt[:, 4:5], smt[:, 3:4], 1.0 / 128, SUBLN_EPS, ALU.mult, ALU.add, [ks], [ks])
                    P.act(smt[:, 5:6], smt[:, 4:5], AF.Ln, [ks], [ks])
                    P.act(smt[:, 6:7], smt[:, 5:6], AF.Exp, [ks], [ks], scale=-0.5)
                    P.ts("dve", an[n2][:], av[n2][:], smt[:, 6:7], None, ALU.mult, None, ["av%d" % n2, ks], ["an%d" % n2])
                    P.tr(ptb[:, n2 * 512:n2 * 512 + 128], an[n2][:], identb[:], ["an%d" % n2, "identb"], ["ptb"])
                    P.ts("dve", ydg[yb][:, i * 128:(i + 1) * 128], ptb[:, n2 * 512:n2 * 512 + 128], dag[:, 0:1], None, ALU.mult, None,
                         ["ptb", "dag"], ["ydg%d" % yb])
                P.load("sp", T["ydT"][h * 128:(h + 1) * 128, g * 512:(g + 1) * 512], ydg[yb][:], ["ydg%d" % yb], ["ydT"])
        return P.emit()


def stage4(nc, sems, T):
    with contextlib.ExitStack() as st:
        sb, ps = tens(nc, st)
        P = Prog(nc, sems)
        ident = sb("ident", [128, 128])
        wo = sb("wo", [128, 8, D], BF16)
        yT = sb("yT", [128, 8, S], BF16)
        g2b = sb("g2b", [128, D])
        wr = sb("wr", [128, 8, 36])
        brb = sb("brb", [128, 36])
        xt = [sb("xt%d" % i, [128, D]) for i in range(2)]
        x2 = [sb("x2%d" % i, [128, D]) for i in range(2)]
        junk = sb("junk4", [128, D], BF16)
        stat = [sb("stat4%d" % i, [128, 4]) for i in range(2)]
        h2 = [sb("h2%d" % i, [128, D]) for i in range(2)]
        h2T = [sb("h2T%d" % i, [128, 8, 128]) for i in range(2)]
        h2Tb = [sb("h2Tb%d" % i, [128, 8, 128], BF16) for i in range(2)]
        lgt = sb("lgt", [128, NT, 36])
        mxg = sb("mxg", [128, NT])
        ohg = sb("ohg", [128, NT, 4])
        eg = sb("eg", [128, NT, 4])
        sg = sb("sg", [128, NT])
        tmp4 = sb("tmp4", [128, NT, 4, 8])
        les = sb("les", [128, NT, 8])
        le2 = sb("le2", [128, NT, 8])
        m1 = sb("m1", [128, NT])
        m2 = sb("m2", [128, NT])
        oh1 = sb("oh1", [128, NT, 8])
        oh2 = sb("oh2", [128, NT, 8])
        w1 = sb("w1", [128, NT])
        w2 = sb("w2", [128, NT])
        gf = sb("gf", [128, NT, 8])
        gts = sb("gts", [128, NT, 4, 8])
        pO = [ps("pO%d" % i, [128, 512]) for i in range(4)]
        pT = [ps("pT%d" % i, [128, 512]) for i in range(2)]
        pR = ps("pR", [128, 512])

        P.load("sp", ident[:], T["ident"], [], ["ident"])
        P.load("pool", wo[:], T["w_out"].rearrange("(c p) n -> p c n", p=128), [], ["wo"])
        P.load("sp", yT[:, 0:4, :], T["ymT"].rearrange("(c p) t -> p c t", p=128), [], ["yT"])
        P.load("act", yT[:, 4:8, :], T["ydT"].rearrange("(c p) t -> p c t", p=128), [], ["yT"])
        P.load("sp", g2b[:], T["norm2_g"].partition_broadcast(128), [], ["g2b"])
        P.load("sp", wr[:], T["w_r"].rearrange("(c p) n -> p c n", p=128), [], ["wr"])
        P.load("sp", brb[:], T["b_r"].partition_broadcast(128), [], ["brb"])
        rO = Rot(2)
        for t in range(NT):
            b = t % 2
            ts_ = slice(t * 128, (t + 1) * 128)
            P.load("sp", xt[b][:], T["x"][ts_, :], [], ["xt%d" % b])
            for half in range(2):
                pb = rO.next() * 2 + half
                for kc in range(8):
                    P.mm(pO[pb][:, :], yT[:, kc, ts_], wo[:, kc, half * 512:(half + 1) * 512], ["yT", "wo"], ["pO%d" % pb], start=(kc == 0), stop=(kc == 7))
                P.tt("dve", x2[b][:, half * 512:(half + 1) * 512], pO[pb][:, :], xt[b][:, half * 512:(half + 1) * 512], ALU.add,
                     ["pO%d" % pb, "xt%d" % b], ["x2%d" % b])
            P.load("act", T["x2"][ts_, :], x2[b][:], ["x2%d" % b], ["x2d"])
            sk = "stat4%d" % b
            P.act(junk[:], x2[b][:], AF.Square, ["x2%d" % b], ["junk4", sk], accum_out=stat[b][:, 0:1])
            P.ts("dve", stat[b][:, 1:2], stat[b][:, 0:1], 1.0 / D, EPS, ALU.mult, ALU.add, [sk], [sk])
            P.act(stat[b][:, 2:3], stat[b][:, 1:2], AF.Ln, [sk], [sk])
            P.act(stat[b][:, 3:4], stat[b][:, 2:3], AF.Exp, [sk], [sk], scale=-0.5)
            P.stt("dve", h2[b][:], x2[b][:], stat[b][:, 3:4], g2b[:], ALU.mult, ALU.mult, ["x2%d" % b, sk, "g2b"], ["h2%d" % b])
            for kc in range(8):
                pz = pT[kc // 4]
                P.tr(pz[:, (kc % 4) * 128:(kc % 4 + 1) * 128], h2[b][:, kc * 128:(kc + 1) * 128], ident[:], ["h2%d" % b, "ident"], ["pT%d" % (kc // 4)])
            for hf in range(2):
                P.copy("act", h2T[b][:, hf * 4:(hf + 1) * 4, :].rearrange("p k t -> p (k t)"), pT[hf][:, :], ["pT%d" % hf], ["h2T%d" % b])
                P.copy("dve", h2Tb[b][:, hf * 4:(hf + 1) * 4, :].rearrange("p k t -> p (k t)"), pT[hf][:, :], ["pT%d" % hf], ["h2Tb%d" % b])
            P.load("sp", T["h2T"].rearrange("(c p) t -> p c t", p=128)[:, :, ts_], h2Tb[b][:], ["h2Tb%d" % b], ["h2Td"])
            for kc in range(8):
                P.mm(pR[:, 0:36], h2T[b][:, kc, :], wr[:, kc, :], ["h2T%d" % b, "wr"], ["pR"], start=(kc == 0), stop=(kc == 7))
            P.tt("dve", lgt[:, t, :], pR[:, 0:36], brb[:], ALU.add, ["pR", "brb"], ["lgt"])
        lg = lgt[:, :, 0:4]
        le = lgt[:, :, 4:36].rearrange("p t (g e) -> p t g e", e=8)
        red = lambda o, i_, op: (lambda e: e.tensor_reduce(out=o, in_=i_, axis=AX.X, op=op))
        P.op("dve", red(mxg[:], lg, ALU.max), ["lgt"], ["mxg"])
        P.tt("dve", ohg[:], lg, mxg[:].unsqueeze(2).to_broadcast([128, NT, 4]), ALU.is_ge, ["lgt", "mxg"], ["ohg"])
        P.tt("dve", eg[:], lg, mxg[:].unsqueeze(2).to_broadcast([128, NT, 4]), ALU.subtract, ["lgt", "mxg"], ["eg"])
        P.act(eg[:], eg[:], AF.Exp, ["eg"], ["eg"])
        P.op("dve", red(sg[:], eg[:], ALU.add), ["eg"], ["sg"])
        P.op("dve", (lambda o, i_: (lambda e: e.reciprocal(out=o, in_=i_)))(sg[:], sg[:]), ["sg"], ["sg"])
        P.tt("dve", tmp4[:], le, ohg[:].unsqueeze(3).to_broadcast([128, NT, 4, 8]), ALU.mult, ["lgt", "ohg"], ["tmp4"])
        P.op("dve", red(les[:], tmp4[:].rearrange("p t g e -> p t e g"), ALU.add), ["tmp4"], ["les"])
        P.op("dve", red(m1[:], les[:], ALU.max), ["les"], ["m1"])
        P.tt("dve", oh1[:], les[:], m1[:].unsqueeze(2).to_broadcast([128, NT, 8]), ALU.is_ge, ["les", "m1"], ["oh1"])
        P.stt("dve", le2[:], oh1[:], -1e30, les[:], ALU.mult, ALU.add, ["oh1", "les"], ["le2"])
        P.op("dve", red(m2[:], le2[:], ALU.max), ["le2"], ["m2"])
        P.tt("dve", oh2[:], le2[:], m2[:].unsqueeze(2).to_broadcast([128, NT, 8]), ALU.is_ge, ["le2", "m2"], ["oh2"])
        P.tt("dve", w2[:], m2[:], m1[:], ALU.subtract, ["m1", "m2"], ["w2"])
        P.act(w2[:], w2[:], AF.Exp, ["w2"], ["w2"])
        P.ts("dve", w1[:], w2[:], 1.0, None, ALU.add, None, ["w2"], ["w1"])
        P.op("dve", (lambda o, i_: (lambda e: e.reciprocal(out=o, in_=i_)))(w1[:], w1[:]), ["w1"], ["w1"])
        P.tt("dve", w2[:], w2[:], w1[:], ALU.mult, ["w1", "w2"], ["w2"])
        P.tt("dve", w1[:], w1[:], sg[:], ALU.mult, ["w1", "sg"], ["w1"])
        P.tt("dve", w2[:], w2[:], sg[:], ALU.mult, ["w2", "sg"], ["w2"])
        P.tt("dve", oh1[:], oh1[:], w1[:].unsqueeze(2).to_broadcast([128, NT, 8]), ALU.mult, ["oh1", "w1"], ["oh1"])
        P.tt("dve", oh2[:], oh2[:], w2[:].unsqueeze(2).to_broadcast([128, NT, 8]), ALU.mult, ["oh2", "w2"], ["oh2"])
        P.tt("dve", gf[:], oh1[:], oh2[:], ALU.add, ["oh1", "oh2"], ["gf"])
        P.tt("dve", gts[:], ohg[:].unsqueeze(3).to_broadcast([128, NT, 4, 8]), gf[:].unsqueeze(2).to_broadcast([128, NT, 4, 8]), ALU.mult,
             ["ohg", "gf"], ["gts"])
        P.load("sp", T["gates"], gts[:].rearrange("p t g e -> p (t g e)"), ["gts"], ["gatesd"])
        return P.emit()


def stage5(nc, sems, T):
    TG = 1024
    with contextlib.ExitStack() as st:
        sb, ps = tens(nc, st)
        P = Prog(nc, sems)
        gts = sb("gts", [128, NT, 32])
        gfb = sb("gfb", [128, D])
        h2T = [sb("h2T%d" % i, [128, 8, TG], BF16) for i in range(2)]
        acc = sb("acc", [128, 8, D])
        wg = [sb("wg%d" % i, [128, 8, DFF], BF16) for i in range(2)]
        wu = [sb("wu%d" % i, [128, 8, DFF], BF16) for i in range(2)]
        wd = [sb("wd%d" % i, [128, 4, D], BF16) for i in range(2)]
        sgl = [sb("sgl%d" % i, [128, 512], BF16) for i in range(2)]
        hidT = sb("hidT", [128, 4, TG], BF16)
        x2 = [sb("x2%d" % i, [128, D]) for i in range(2)]
        junk = sb("junk5", [128, D], BF16)
        stat = [sb("stat5%d" % i, [128, 4]) for i in range(2)]
        ot = [sb("ot%d" % i, [128, D]) for i in range(2)]
        pg = [ps("pg%d" % i, [128, 512]) for i in range(2)]
        pu = [ps("pu%d" % i, [128, 512]) for i in range(2)]
        po = [ps("po%d" % i, [128, 512]) for i in range(4)]

        P.load("sp", gts[:].rearrange("p t e -> p (t e)"), T["gates"], [], ["gts"])
        P.load("sp", gfb[:], T["normf_g"].partition_broadcast(128), [], ["gfb"])
        rg, ro = Rot(2), Rot(4)
        k = 0
        for G in range(S // TG):
            hb = G % 2
            P.load("act", h2T[hb][:], T["h2T"].rearrange("(c p) t -> p c t", p=128)[:, :, G * TG:(G + 1) * TG], [], ["h2T%d" % hb])
            P.memset("pool", acc[:], 0.0, ["acc"])
            for e in range(N_EXP):
                wb = k % 2
                k += 1
                P.load("sp", wg[wb][:], T["wg_bf"][e].rearrange("(c p) f -> p c f", p=128), [], ["wg%d" % wb])
                P.load("act", wu[wb][:], T["wu_bf"][e].rearrange("(c p) f -> p c f", p=128), [], ["wu%d" % wb])
                P.load("sp", wd[wb][:], T["wd_bf"][e].rearrange("(c p) d -> p c d", p=128), [], ["wd%d" % wb])
                for fc in range(4):
                    for half in range(TG // 512):
                        gb = rg.next()
                        hs = slice(half * 512, (half + 1) * 512)
                        for kc in range(8):
                            P.mm(pg[gb][:, :], wg[wb][:, kc, fc * 128:(fc + 1) * 128], h2T[hb][:, kc, hs], ["wg%d" % wb, "h2T%d" % hb], ["pg%d" % gb],
                                 start=(kc == 0), stop=(kc == 7))
                        for kc in range(8):
                            P.mm(pu[gb][:, :], wu[wb][:, kc, fc * 128:(fc + 1) * 128], h2T[hb][:, kc, hs], ["wu%d" % wb, "h2T%d" % hb], ["pu%d" % gb],
                                 start=(kc == 0), stop=(kc == 7))
                        P.act(sgl[gb][:], pg[gb][:, :], AF.Silu, ["pg%d" % gb], ["sgl%d" % gb])
                        P.tt("dve", hidT[:, fc, hs], sgl[gb][:], pu[gb][:, :], ALU.mult, ["sgl%d" % gb, "pu%d" % gb], ["hidT"])
                for tl in range(TG // 128):
                    t = G * (TG // 128) + tl
                    for ch in range(2):
                        ob = ro.next()
                        for fc in range(4):
                            P.mm(po[ob][:, :], hidT[:, fc, tl * 128:(tl + 1) * 128], wd[wb][:, fc, ch * 512:(ch + 1) * 512], ["hidT", "wd%d" % wb],
                                 ["po%d" % ob], start=(fc == 0), stop=(fc == 3))
                        P.stt("dve", acc[:, tl, ch * 512:(ch + 1) * 512], po[ob][:, :], gts[:, t, e:e + 1], acc[:, tl, ch * 512:(ch + 1) * 512],
                              ALU.mult, ALU.add, ["po%d" % ob, "gts", "acc"], ["acc"])
            for tl in range(TG // 128):
                t = G * (TG // 128) + tl
                b = t % 2
                ts_ = slice(t * 128, (t + 1) * 128)
                sk = "stat5%d" % b
                P.load("sp", x2[b][:], T["x2"][ts_, :], [], ["x2%d" % b])
                P.tt("pool", x2[b][:], x2[b][:], acc[:, tl, :], ALU.add, ["x2%d" % b, "acc"], ["x2%d" % b])
                P.act(junk[:], x2[b][:], AF.Square, ["x2%d" % b], ["junk5", sk], accum_out=stat[b][:, 0:1])
                P.ts("dve", stat[b][:, 1:2], stat[b][:, 0:1], 1.0 / D, EPS, ALU.mult, ALU.add, [sk], [sk])
                P.act(stat[b][:, 2:3], stat[b][:, 1:2], AF.Ln, [sk], [sk])
                P.act(stat[b][:, 3:4], stat[b][:, 2:3], AF.Exp, [sk], [sk], scale=-0.5)
                P.stt("dve", ot[b][:], x2[b][:], stat[b][:, 3:4], gfb[:], ALU.mult, ALU.mult, ["x2%d" % b, sk, "gfb"], ["ot%d" % b])
                P.load("act", T["out"][ts_, :], ot[b][:], ["ot%d" % b], ["outd"])
        return P.emit()


def _rel_bucket_np(n):
    n = np.maximum(n, 0)
    max_exact = 16
    nf = np.maximum(n, 1).astype(np.float32)
    large = max_exact + (np.log(nf / np.float32(max_exact)) / np.float32(math.log(128 / max_exact)) * np.float32(16)).astype(np.int32)
    large = np.minimum(large, 31)
    return np.where(n < max_exact, n, large)


def _constants():
    ident = np.eye(128, dtype=np.float32)
    s_ = np.arange(128)[:, None]
    t_ = np.arange(128)[None, :]
    tri = (s_ <= t_).astype(np.float32)
    sel = np.zeros((4, 4, 128), np.float32)
    for h in range(4):
        sel[h, h, :] = 1.0
    oh = np.zeros((128, 2, 33, 128), np.float32)
    for kind in range(2):
        n = (t_ - s_) + 128 * kind
        bk = _rel_bucket_np(n)
        valid = n >= 0
        for b in range(32):
            oh[:, kind, b, :] = ((bk == b) & valid).astype(np.float32)
        oh[:, kind, 32, :] = (~valid).astype(np.float32)
    return dict(ident=ident, tri=tri, sel=sel.reshape(4, 512), oh=oh.reshape(128, -1))


_CACHE = {}


def kernel(x, w_in, conv_w, conv_b, w_mq, w_mk, w_mgate, b_mgate, m_norm_g, m_skip, lambda_qk, da_norm_g, rel_bias, w_out,
           norm1_g, norm2_g, w_rg, b_rg, w_re, b_re, w_eg, w_eu, w_ed, normf_g):
    f = lambda a: np.ascontiguousarray(np.asarray(a, dtype=np.float32))
    if "nc" not in _CACHE:
        _CACHE["nc"], _CACHE["stats"] = build_program()
    nc = _CACHE["nc"]
    shared = dict(
        w_in=f(w_in)[0], conv_w=f(conv_w)[0], conv_b=f(conv_b)[0], w_mq=f(w_mq)[0], w_mk=f(w_mk)[0], w_mgate=f(w_mgate)[0],
        b_mgate=f(b_mgate)[0], m_norm_g=f(m_norm_g)[0], m_skip=f(m_skip)[0], lambda_qk=f(lambda_qk)[0].reshape(256),
        da_norm_g=f(da_norm_g)[0], rel_bias=f(rel_bias).reshape(128), w_out=f(w_out)[0], norm1_g=f(norm1_g)[0], norm2_g=f(norm2_g)[0],
        w_r=np.ascontiguousarray(np.concatenate([f(w_rg)[0], f(w_re)[0].reshape(D, 32)], axis=1)),
        b_r=np.ascontiguousarray(np.concatenate([f(b_rg)[0], f(b_re)[0].reshape(32)])),
        w_eg=f(w_eg)[0], w_eu=f(w_eu)[0], w_ed=f(w_ed)[0], normf_g=f(normf_g),
    )
    shared.update(_constants())
    xs = f(x)
    in_maps = []
    for b in range(8):
        m = dict(shared)
        m["x"] = xs[b]
        in_maps.append(m)
    res = run_bass_kernel_spmd(nc, in_maps, core_ids=list(range(8)))
    _CACHE["res"] = res
    return np.stack([np.asarray(r["out"], dtype=np.float32) for r in res.results], axis=0)
```

```python
import math
import contextlib
import numpy as np
import concourse.bass as bass
import concourse.mybir as mybir
from concourse.bass_utils import run_bass_kernel_spmd

F32 = mybir.dt.float32
BF16 = mybir.dt.bfloat16
AF = mybir.ActivationFunctionType
ALU = mybir.AluOpType
AX = mybir.AxisListType

S = 4096
D = 1024
NT = 32
EPS = 1e-6
SUBLN_EPS = 1e-5
N_EXP = 32
DFF = 512
LAM_INIT = 0.8 - 0.6 * math.exp(-0.3 * 0)
ML_SCALE = 128.0 ** -0.5
DA_SCALE = 64.0 ** -0.5
NEG = -30000.0

COMPUTE = ("pe", "act", "dve", "pool")
QUEUES = ("sp", "act", "pool")
N_DMA_SEMS = 8
DEBUG = False
STAGES = (1, 2, 3, 4, 5)


class Sems:
    def __init__(self, nc, st):
        self.esem = {e: st.enter_context(nc.semaphore("s_" + e)) for e in COMPUTE}
        self.dsem = {(q, s): st.enter_context(nc.semaphore("d_%s_%d" % (q, s))) for q in QUEUES for s in range(N_DMA_SEMS)}
        self.cnt = {e: 0 for e in COMPUTE}
        self.dcnt = {k: 0 for k in self.dsem}
        self.rr = {q: 0 for q in QUEUES}


class Op:
    __slots__ = ("eng", "fn", "deps", "is_dma", "signal", "val", "sem", "slot", "prev")

    def __init__(self, eng, fn, is_dma):
        self.eng, self.fn, self.is_dma = eng, fn, is_dma
        self.deps = []
        self.signal = False
        self.val = None
        self.sem = None
        self.slot = None
        self.prev = None


class Prog:
    def __init__(self, nc, sems):
        self.nc = nc
        self.sems = sems
        self.ops = []
        self.last_writer = {}
        self.readers = {}
        self.slot_last = {}

    def _add(self, op, reads, writes):
        pr = [r for r in reads if r in PSUM_KEYS]
        if pr:
            reads = [r for r in reads if r not in PSUM_KEYS]
            writes = list(writes) + [r for r in pr if r not in writes]
        deps = []
        for r in reads:
            w = self.last_writer.get(r)
            if w is not None:
                deps.append(w)
        for w in writes:
            lw = self.last_writer.get(w)
            if lw is not None:
                deps.append(lw)
            deps.extend(self.readers.get(w, ()))
        seen = set()
        for d in deps:
            if id(d) not in seen and d is not op:
                seen.add(id(d))
                op.deps.append(d)
        for r in reads:
            self.readers.setdefault(r, []).append(op)
        for w in writes:
            self.last_writer[w] = op
            self.readers[w] = []
        self.ops.append(op)
        return op

    def op(self, eng, fn, reads=(), writes=()):
        return self._add(Op(eng, fn, False), reads, writes)

    def dma(self, queue, fn, reads=(), writes=()):
        op = Op(queue, fn, True)
        s = self.sems
        op.slot = (queue, s.rr[queue] % N_DMA_SEMS)
        s.rr[queue] += 1
        op.prev = self.slot_last.get(op.slot)
        self.slot_last[op.slot] = op
        return self._add(op, reads, writes)

    def mm(self, out, lhsT, rhs, r, w, start=True, stop=True, skip=False):
        if skip:
            return self.op("pe", lambda e: e.matmul(out, lhsT=lhsT, rhs=rhs, start=start, stop=stop, skip_group_check=True), r, w)
        return self.op("pe", lambda e: e.matmul(out, lhsT=lhsT, rhs=rhs, start=start, stop=stop), r, w)

    def tr(self, out, in_, ident, r, w):
        return self.op("pe", lambda e: e.transpose(out=out, in_=in_, identity=ident), r, w)

    def act(self, out, in_, func, r, w, bias=None, scale=None, accum_out=None):
        kw = {}
        if bias is not None:
            kw["bias"] = bias
        if scale is not None:
            kw["scale"] = scale
        if accum_out is not None:
            kw["accum_out"] = accum_out
        return self.op("act", lambda e: e.activation(out=out, in_=in_, func=func, **kw), r, w)

    def copy(self, eng, out, in_, r, w):
        if eng == "act":
            return self.op("act", lambda e: e.copy(out=out, in_=in_), r, w)
        return self.op(eng, lambda e: e.tensor_copy(out=out, in_=in_), r, w)

    def tt(self, eng, out, in0, in1, op, r, w):
        return self.op(eng, lambda e: e.tensor_tensor(out=out, in0=in0, in1=in1, op=op), r, w)

    def ts(self, eng, out, in0, s1, s2, op0, op1, r, w):
        if s2 is None:
            return self.op(eng, lambda e: e.tensor_scalar(out=out, in0=in0, scalar1=s1, scalar2=None, op0=op0), r, w)
        return self.op(eng, lambda e: e.tensor_scalar(out=out, in0=in0, scalar1=s1, scalar2=s2, op0=op0, op1=op1), r, w)

    def stt(self, eng, out, in0, scalar, in1, op0, op1, r, w):
        eng = "dve"
        return self.op(eng, lambda e: e.scalar_tensor_tensor(out=out, in0=in0, scalar=scalar, in1=in1, op0=op0, op1=op1), r, w)

    def memset(self, eng, ap, val, w):
        return self.op(eng, lambda e: e.memset(ap, val), (), w)

    def load(self, q, out, in_, r, w):
        return self.dma(q, lambda e: e.dma_start(out=out, in_=in_), r, w)

    def emit(self):
        nc, s, ops = self.nc, self.sems, self.ops

        def same_skip(d, o):
            return (not d.is_dma) and (not o.is_dma) and d.eng == o.eng and d.eng == "pe"

        for o in ops:
            for d in o.deps:
                if d.is_dma or same_skip(d, o):
                    continue
                d.signal = True
        for o in ops:
            if o.is_dma:
                s.dcnt[o.slot] += 16
                o.val = s.dcnt[o.slot]
                o.sem = s.dsem[o.slot]
            else:
                o.sem = s.esem[o.eng]
                if o.signal:
                    s.cnt[o.eng] += 1
                    o.val = s.cnt[o.eng]
        by_eng = {e: [] for e in ("pe", "act", "dve", "pool", "sp")}
        for o in ops:
            by_eng[o.eng].append(o)
        final = dict(s.dcnt)

        def run(engname, e):
            waited = {}

            def wait(sem, val):
                if waited.get(id(sem), 0) >= val:
                    return
                waited[id(sem)] = val
                e.wait_ge(sem, val)

            for o in by_eng[engname]:
                for d in o.deps:
                    if same_skip(d, o):
                        continue
                    wait(d.sem, d.val)
                if o.is_dma and o.prev is not None:
                    wait(o.prev.sem, o.prev.val)
                ins = o.fn(e)
                if o.is_dma:
                    ins.then_inc(o.sem, 16)
                elif o.signal:
                    ins.then_inc(o.sem, 1)
            if engname == "sp":
                for k, v in final.items():
                    if v > 0:
                        wait(s.dsem[k], v)

        with nc.Block() as block:
            block.sync(lambda e: run("sp", e))
            if by_eng["pe"]:
                block.tensor(lambda e: run("pe", e))
            if by_eng["act"]:
                block.scalar(lambda e: run("act", e))
            if by_eng["dve"]:
                block.vector(lambda e: run("dve", e))
            if by_eng["pool"]:
                block.gpsimd(lambda e: run("pool", e))
        return {k: len(v) for k, v in by_eng.items()}


class Rot:
    def __init__(self, n):
        self.n, self.i = n, 0

    def next(self):
        v = self.i % self.n
        self.i += 1
        return v


def build_program():
    nc = bass.Bass("TRN2", target_bir_lowering=False)
    I = lambda name, shape, dt=F32: nc.dram_tensor(name, list(shape), dt, kind="ExternalInput").ap()
    skind = "ExternalOutput" if DEBUG else "Internal"
    SC = lambda name, shape, dt: nc.dram_tensor(name, list(shape), dt, kind=skind).ap()
    T = {}
    T["x"] = I("x", [S, D])
    T["w_in"] = I("w_in", [D, 3072])
    T["conv_w"] = I("conv_w", [4, 512])
    T["conv_b"] = I("conv_b", [512])
    T["w_mq"] = I("w_mq", [4, 128, 128])
    T["w_mk"] = I("w_mk", [4, 128, 128])
    T["w_mgate"] = I("w_mgate", [1536, 8])
    T["b_mgate"] = I("b_mgate", [8])
    T["m_norm_g"] = I("m_norm_g", [512])
    T["m_skip"] = I("m_skip", [512])
    T["lambda_qk"] = I("lambda_qk", [256])
    T["da_norm_g"] = I("da_norm_g", [128])
    T["rel_bias"] = I("rel_bias", [128])
    T["w_out"] = I("w_out", [D, D])
    T["norm1_g"] = I("norm1_g", [D])
    T["norm2_g"] = I("norm2_g", [D])
    T["w_r"] = I("w_r", [D, 36])
    T["b_r"] = I("b_r", [36])
    T["w_eg"] = I("w_eg", [N_EXP, D, DFF])
    T["w_eu"] = I("w_eu", [N_EXP, D, DFF])
    T["w_ed"] = I("w_ed", [N_EXP, DFF, D])
    T["normf_g"] = I("normf_g", [D])
    T["ident"] = I("ident", [128, 128])
    T["tri"] = I("tri", [128, 128])
    T["sel"] = I("sel", [4, 512])
    T["oh"] = I("oh", [128, 2 * 33 * 128])
    T["out"] = nc.dram_tensor("out", [S, D], F32, kind="ExternalOutput").ap()
    T["featT"] = SC("featT", [5, 512, S], BF16)
    T["vm_tok"] = SC("vm_tok", [S, 512], BF16)
    T["vd_tok"] = SC("vd_tok", [S, 512], BF16)
    T["ymT"] = SC("ymT", [512, S], BF16)
    T["ydT"] = SC("ydT", [512, S], BF16)
    T["x2"] = SC("x2", [S, D], F32)
    T["h2T"] = SC("h2T", [D, S], BF16)
    T["gates"] = SC("gates", [128, NT * 32], F32)
    T["wg_bf"] = nc.dram_tensor("wg_bf", [N_EXP, D, DFF], BF16, kind="Internal").ap()
    T["wu_bf"] = nc.dram_tensor("wu_bf", [N_EXP, D, DFF], BF16, kind="Internal").ap()
    T["wd_bf"] = nc.dram_tensor("wd_bf", [N_EXP, DFF, D], BF16, kind="Internal").ap()

    stats = {}
    with contextlib.ExitStack() as gst:
        gst.enter_context(nc.allow_non_contiguous_dma(reason="small strided parameter loads"))
        sems = Sems(nc, gst)
        T["biasT_sb"] = gst.enter_context(nc.sbuf_tensor("g_biasT", [128, 2, 4, 128], F32))
        T["rbb_sb"] = gst.enter_context(nc.sbuf_tensor("g_rbb", [128, 128], F32))
        if 0 in STAGES:
            stats["s0"] = stage0(nc, sems, T)
        if 1 in STAGES:
            stats["s1"] = stage1(nc, sems, T)
        if 2 in STAGES:
            stats["s2"] = stage2(nc, sems, T)
        if 3 in STAGES:
            stats["s3"] = stage3(nc, sems, T)
        if 4 in STAGES:
            stats["s4"] = stage4(nc, sems, T)
        if 5 in STAGES:
            stats["s5"] = stage5(nc, sems, T)
    return nc, stats


_TN = [0]
PSUM_KEYS = set()


def tens(nc, st):
    _TN[0] += 1
    pre = "t%d_" % _TN[0]
    sb = lambda n, s, d=F32: st.enter_context(nc.sbuf_tensor(pre + n, list(s), d))
    def ps(n, s, d=F32):
        PSUM_KEYS.add(n)
        return st.enter_context(nc.psum_tensor(pre + n, list(s), d))
    return sb, ps


def conv_jobs():
    return [(name, dst, e) for name, dst in (("w_eg", "wg_bf"), ("w_eu", "wu_bf"), ("w_ed", "wd_bf")) for e in range(N_EXP)]


class Conv:
    def __init__(self, P, sb, T, engs=("pool",), queues=("sp", "sp"), nb=3):
        self.P, self.T = P, T
        self.stg = [sb("w0s%d" % i, [128, 8, 512], F32) for i in range(nb)]
        self.cvt = [sb("w0c%d" % i, [128, 8, 512], BF16) for i in range(nb)]
        self.rot = Rot(nb)
        self.engs, self.queues = engs, queues
        self.jobs = conv_jobs()
        self.k = 0

    def emit(self, n):
        P, T = self.P, self.T
        for _ in range(n):
            if self.k >= len(self.jobs):
                return
            name, dst, e = self.jobs[self.k]
            b = self.rot.next()
            src = T[name][e].rearrange("(c p) f -> p c f", p=128)
            dstap = T[dst][e].rearrange("(c p) f -> p c f", p=128)
            if name == "w_ed":
                sv = self.stg[b][:].rearrange("p (c h) f -> p c (h f)", c=4)
                cv = self.cvt[b][:].rearrange("p (c h) f -> p c (h f)", c=4)
            else:
                sv, cv = self.stg[b][:], self.cvt[b][:]
            P.load(self.queues[0], sv, src, [], ["stg%d" % b])
            P.copy(self.engs[self.k % len(self.engs)], self.cvt[b][:], self.stg[b][:], ["stg%d" % b], ["cvt%d" % b])
            P.load(self.queues[1], dstap, cv, ["cvt%d" % b], [dst])
            self.k += 1


def stage0(nc, sems, T):
    with contextlib.ExitStack() as st:
        sb, ps = tens(nc, st)
        P = Prog(nc, sems)
        cv = Conv(P, sb, T, engs=("dve", "pool", "act"), queues=("sp", "act"))
        cv.emit(96)
        return P.emit()


def stage1(nc, sems, T):
    with contextlib.ExitStack() as st:
        sb, ps = tens(nc, st)
        P = Prog(nc, sems)
        ident = sb("ident", [128, 128])
        identb = sb("identb", [128, 128], BF16)
        g1 = sb("g1", [128, 8])
        w_bf = sb("w_in_bf", [128, 8, 3072], BF16)
        wst = [sb("wst%d" % i, [128, 3072]) for i in range(2)]
        xt = [sb("xt%d" % i, [128, D]) for i in range(2)]
        junk = sb("junk", [128, D], BF16)
        stat = sb("stat", [128, 4])
        xn = [sb("xn%d" % i, [128, D], BF16) for i in range(2)]
        hT = [sb("hT%d" % i, [128, 8, 512], BF16) for i in range(2)]
        fstg = [sb("fstg%d" % i, [128, 4, 512], BF16) for i in range(2)]
        tstg = [sb("tstg%d" % i, [128, 4, 512], BF16) for i in range(2)]
        pt = [ps("pt%d" % i, [128, D], BF16) for i in range(2)]
        pp = [ps("pp%d" % i, [128, 512]) for i in range(4)]

        P.load("sp", ident[:], T["ident"], [], ["ident"])
        P.copy("dve", identb[:], ident[:], ["ident"], ["identb"])
        P.load("sp", g1[:], T["norm1_g"].rearrange("(c p) -> p c", p=128), [], ["g1"])
        for kc in range(8):
            b = kc % 2
            P.load("sp" if kc % 2 == 0 else "act", wst[b][:], T["w_in"][kc * 128:(kc + 1) * 128, :], [], ["wst%d" % b])
            P.ts("dve" if kc % 2 == 0 else "pool", w_bf[:, kc, :], wst[b][:], g1[:, kc:kc + 1], None, ALU.mult, None,
                 ["wst%d" % b, "g1"], ["w_bf"])
        oh = sb("oh", [128, 2, 33, 128])
        rbb, biasT = T["rbb_sb"], T["biasT_sb"]
        P.load("act", oh[:].rearrange("p a b c -> p (a b c)"), T["oh"], [], ["oh"])
        P.load("act", rbb[:], T["rel_bias"].partition_broadcast(128), [], ["rbb"])

        def emit_bias(idx):
            kind, h = idx // 4, idx % 4
            dst_ = biasT[:, kind, h, :]
            kb = "biasT%d%d" % (kind, h)
            P.ts("pool", dst_, oh[:, kind, 32, :], NEG, None, ALU.mult, None, ["oh"], [kb])
            for b_ in range(32):
                P.stt("dve", dst_, oh[:, kind, b_, :], rbb[:, b_ * 4 + h:b_ * 4 + h + 1], dst_, ALU.mult, ALU.add, ["oh", "rbb", kb], [kb])

        rpp = Rot(4)
        for g in range(8):
            hb = g % 2
            emit_bias(g)
            for ti in range(4):
                t = g * 4 + ti
                b = t % 2
                P.load("sp", xt[b][:], T["x"][t * 128:(t + 1) * 128, :], [], ["xt%d" % b])
                P.act(junk[:], xt[b][:], AF.Square, ["xt%d" % b], ["junk", "stat"], accum_out=stat[:, 0:1])
                P.ts("dve", stat[:, 1:2], stat[:, 0:1], 1.0 / D, EPS, ALU.mult, ALU.add, ["stat"], ["stat"])
                P.act(stat[:, 2:3], stat[:, 1:2], AF.Ln, ["stat"], ["stat"])
                P.act(stat[:, 3:4], stat[:, 2:3], AF.Exp, ["stat"], ["stat"], scale=-0.5)
                P.ts("dve", xn[b][:], xt[b][:], stat[:, 3:4], None, ALU.mult, None, ["xt%d" % b, "stat"], ["xn%d" % b])
                for kc in range(8):
                    P.tr(pt[b][:, kc * 128:(kc + 1) * 128], xn[b][:, kc * 128:(kc + 1) * 128], identb[:], ["xn%d" % b, "identb"], ["pt%d" % b])
                P.copy("act" if ti % 2 == 0 else "dve", hT[hb][:, :, ti * 128:(ti + 1) * 128], pt[b][:, :].rearrange("p (k t) -> p k t", k=8),
                       ["pt%d" % b], ["hT%d" % hb])
            for blk in range(5):
                fb = (g * 5 + blk) % 2
                for ch in range(4):
                    col0 = blk * 512 + ch * 128
                    pb = rpp.next()
                    for kc in range(8):
                        P.mm(pp[pb][:, :], w_bf[:, kc, col0:col0 + 128], hT[hb][:, kc, :], ["w_bf", "hT%d" % hb], ["pp%d" % pb],
                             start=(kc == 0), stop=(kc == 7))
                    P.copy("act" if ch % 2 == 0 else "dve", fstg[fb][:, ch, :], pp[pb][:, :], ["pp%d" % pb], ["fstg%d" % fb])
                P.load("act", T["featT"][blk].rearrange("(c p) t -> p c t", p=128)[:, :, g * 512:(g + 1) * 512], fstg[fb][:],
                       ["fstg%d" % fb], ["featT"])
            for bi, (blk, dst) in enumerate(((1, "vm_tok"), (5, "vd_tok"))):
                tb = (g * 2 + bi) % 2
                for ti in range(4):
                    pb = rpp.next()
                    for kc in range(8):
                        P.mm(pp[pb][:, :], hT[hb][:, kc, ti * 128:(ti + 1) * 128], w_bf[:, kc, blk * 512:(blk + 1) * 512],
                             ["w_bf", "hT%d" % hb], ["pp%d" % pb], start=(kc == 0), stop=(kc == 7))
                    P.copy("dve" if ti % 2 == 0 else "act", tstg[tb][:, ti, :], pp[pb][:, :], ["pp%d" % pb], ["tstg%d" % tb])
                P.load("sp", T[dst][g * 512:(g + 1) * 512, :].rearrange("(t p) f -> p t f", p=128), tstg[tb][:], ["tstg%d" % tb], [dst])
        return P.emit()


def stage2(nc, sems, T):
    with contextlib.ExitStack() as st:
        sb, ps = tens(nc, st)
        P = Prog(nc, sems)
        ident = sb("ident", [128, 128])
        identb = sb("identb", [128, 128], BF16)
        tri = sb("tri", [128, 128])
        sel = sb("sel", [4, 512])
        cw = sb("cw", [128, 4, 4])
        cb = sb("cb", [128, 4])
        mg = sb("mg", [128, 4])
        msk = sb("msk", [128, 4])
        wq = sb("wq", [128, 4, 128], BF16)
        wk = sb("wk", [128, 4, 128], BF16)
        wgt = sb("wgt", [128, 12, 8], BF16)
        bi = sb("bi", [4, 1])
        bfn = sb("bfn", [4, 1])
        zeros = sb("zeros", [4, 512])
        carryB = sb("carryB", [4, 1])
        carryM = sb("carryM", [4, 1])
        Cf = sb("Cf", [128, 4, 129])
        Cb = sb("Cb", [128, 4, 129], BF16)
        c_sb = [sb("c_sb%d" % i, [128, 4, 515], BF16) for i in range(2)]
        z_sb = [sb("z_sb%d" % i, [128, 4, 512], BF16) for i in range(2)]
        vmT = [sb("vmT%d" % i, [128, 4, 512], BF16) for i in range(2)]
        vaug = [sb("vaug%d" % i, [128, 4, 4, 129], BF16) for i in range(2)]
        cacc = [sb("cacc%d" % i, [128, 512]) for i in range(2)]
        cact = sb("cact", [128, 4, 512], BF16)
        sigz = sb("sigz", [128, 4, 512], BF16)
        scs = sb("scs", [128, 4, 512], BF16)
        qT = sb("qT", [128, 4, 512], BF16)
        kT = sb("kT", [128, 4, 512], BF16)
        ktok = sb("ktok", [128, 4, 4, 128], BF16)
        i_row = sb("i_row", [4, 512])
        e_row = sb("e_row", [4, 512])
        sp_row = sb("sp_row", [4, 512])
        Bn = sb("Bn", [4, 513])
        A_row = sb("A_row", [4, 512])
        Mx = sb("Mx", [4, 513])
        N_row = sb("N_row", [4, 512])
        cols = sb("cols", [128, 4, 3, 4])
        eN = sb("eN", [128, 4, 4])
        Mb = sb("Mb", [128, 4, 5])
        nMb = sb("nMb", [128, 4, 5])
        dec = sb("dec", [128, 4, 4])
        spa = sb("spa", [128, 4, 4])
        Mrow = sb("Mrow", [128, 4, 512])
        tmpD = [sb("tmpD%d" % i, [128, 128]) for i in range(2)]
        Dt = [sb("Dt%d" % i, [128, 128]) for i in range(2)]
        Dm = [sb("Dm%d" % i, [128, 128]) for i in range(2)]
        wT = [sb("wT%d" % i, [128, 128], BF16) for i in range(2)]
        intra = [sb("intra%d" % i, [128, 129]) for i in range(2)]
        comb = [sb("comb%d" % i, [128, 129]) for i in range(2)]
        sm = [sb("sm%d" % i, [128, 16]) for i in range(2)]
        hh = [sb("hh%d" % i, [128, 128]) for i in range(2)]
        hn = [sb("hn%d" % i, [128, 128], BF16) for i in range(2)]
        y1 = [sb("y1%d" % i, [128, 128], BF16) for i in range(2)]
        ymg = [sb("ymg%d" % i, [128, 4, 512], BF16) for i in range(2)]
        wkc = [sb("wkc%d" % i, [128, 1]) for i in range(2)]
        vw = [sb("vw%d" % i, [128, 129], BF16) for i in range(2)]
        pA = ps("pA", [128, 512])
        pB = ps("pB", [128, 512])
        pG = ps("pG", [128, 512])
        ptb = ps("ptb", [128, 1024], BF16)
        pS = [ps("pS%d" % i, [128, 512]) for i in range(2)]
        pO = [ps("pO%d" % i, [128, 512]) for i in range(2)]
        P.load("sp", ident[:], T["ident"], [], ["ident"])
        P.copy("dve", identb[:], ident[:], ["ident"], ["identb"])
        P.load("sp", tri[:], T["tri"], [], ["tri"])
        P.load("sp", sel[:], T["sel"], [], ["sel"])
        P.load("sp", cw[:], T["conv_w"].rearrange("j (c p) -> p j c", p=128), [], ["cw"])
        P.load("sp", cb[:], T["conv_b"].rearrange("(c p) -> p c", p=128), [], ["cb"])
        P.load("sp", mg[:], T["m_norm_g"].rearrange("(c p) -> p c", p=128), [], ["mg"])
        P.load("sp", msk[:], T["m_skip"].rearrange("(c p) -> p c", p=128), [], ["msk"])
        P.load("pool", wq[:], T["w_mq"].rearrange("h d e -> d h e"), [], ["wq"])
        P.load("pool", wk[:], T["w_mk"].rearrange("h d e -> d h e"), [], ["wk"])
        P.load("pool", wgt[:], T["w_mgate"].rearrange("(c p) g -> p c g", p=128), [], ["wgt"])
        P.load("sp", bi[:], T["b_mgate"][0:4].rearrange("(p o) -> p o", o=1), [], ["bi"])
        P.load("sp", bfn[:], T["b_mgate"][4:8].rearrange("(p o) -> p o", o=1), [], ["bfn"])
        P.ts("dve", bfn[:], bfn[:], -1.0, None, ALU.mult, None, ["bfn"], ["bfn"])
        P.memset("pool", zeros[:], 0.0, ["zeros"])
        P.memset("pool", carryB[:], 0.0, ["carryB"])
        P.memset("pool", carryM[:], 0.0, ["carryM"])
        P.memset("pool", Cf[:], 0.0, ["Cf"])
        P.memset("pool", Cb[:], 0.0, ["Cb"])
        for i in range(2):
            P.memset("pool", vaug[i][:], 1.0, ["vaug%d" % i])
            P.memset("pool", c_sb[i][:], 0.0, ["c_sb%d" % i])

        featT = T["featT"]
        rS, rO, r2 = Rot(2), Rot(2), Rot(2)
        for g in range(8):
            b = g % 2
            t0 = g * 512
            kc_, kz, kv, kva = "c_sb%d" % b, "z_sb%d" % b, "vmT%d" % b, "vaug%d" % b
            cview = featT[0].rearrange("(c p) t -> p c t", p=128)
            if g == 0:
                P.load("sp", c_sb[b][:, :, 3:515], cview[:, :, 0:512], [], [kc_])
            else:
                P.load("sp", c_sb[b][:, :, 0:515], cview[:, :, t0 - 3:t0 + 512], [], [kc_])
            P.load("act", z_sb[b][:], featT[2].rearrange("(c p) t -> p c t", p=128)[:, :, t0:t0 + 512], [], [kz])
            P.load("act", vmT[b][:], featT[1].rearrange("(c p) t -> p c t", p=128)[:, :, t0:t0 + 512], [], [kv])
            for ti in range(4):
                P.load("sp" if ti % 2 == 0 else "act", vaug[b][:, ti, :, 0:128],
                       T["vm_tok"][t0 + ti * 128:t0 + (ti + 1) * 128, :].rearrange("p (h e) -> p h e", e=128), [kva], [kva])
            for ch in range(4):
                ab = ch % 2
                ka = "cacc%d" % ab
                e1 = "dve" if ch % 2 == 0 else "pool"
                P.ts("dve", cacc[ab][:], c_sb[b][:, ch, 0:512], cw[:, 0, ch:ch + 1], cb[:, ch:ch + 1], ALU.mult, ALU.add, [kc_, "cw", "cb"], [ka])
                for j in range(1, 4):
                    P.stt("dve" if j % 2 == 0 else "pool", cacc[ab][:], c_sb[b][:, ch, j:j + 512], cw[:, j, ch:ch + 1], cacc[ab][:], ALU.mult, ALU.add,
                          [kc_, "cw", ka], [ka])
                P.act(cact[:, ch, :], cacc[ab][:], AF.Silu, [ka], ["cact"])
                P.ts("pool", scs[:, ch, :], cact[:, ch, :], msk[:, ch:ch + 1], None, ALU.mult, None, ["cact", "msk"], ["scs"])
            P.act(sigz[:].rearrange("p c t -> p (c t)"), z_sb[b][:].rearrange("p c t -> p (c t)"), AF.Sigmoid, [kz], ["sigz"])
            for h in range(4):
                P.mm(pA[:, :], wq[:, h, :], cact[:, h, :], ["wq", "cact"], ["pA"])
                P.copy("act", qT[:, h, :], pA[:, :], ["pA"], ["qT"])
                P.mm(pB[:, :], wk[:, h, :], cact[:, h, :], ["wk", "cact"], ["pB"])
                P.copy("dve", kT[:, h, :], pB[:, :], ["pB"], ["kT"])
            for ti in range(4):
                pz = pA if ti % 2 == 0 else pB
                kz_ = "pA" if ti % 2 == 0 else "pB"
                for h in range(4):
                    P.mm(pz[:, h * 128:(h + 1) * 128], cact[:, h, ti * 128:(ti + 1) * 128], wk[:, h, :], ["cact", "wk"], [kz_])
                P.copy("act" if ti % 2 == 0 else "dve", ktok[:, ti, :, :].rearrange("p h e -> p (h e)"), pz[:, :], [kz_], ["ktok"])
            srcs = [(qT, "qT")] * 4 + [(kT, "kT")] * 4 + [(vmT[b], kv)] * 4
            for c in range(12):
                sap, skey = srcs[c]
                P.mm(pG[0:4, :], wgt[:, c, 0:4], sap[:, c % 4, :], ["wgt", skey], ["pG"], start=(c == 0), stop=(c == 11))
            for c in range(12):
                sap, skey = srcs[c]
                P.mm(pB[0:4, :], wgt[:, c, 4:8], sap[:, c % 4, :], ["wgt", skey], ["pB"], start=(c == 0), stop=(c == 11))
            P.ts("dve", i_row[:], pG[0:4, :], bi[:, 0:1], None, ALU.add, None, ["pG", "bi"], ["i_row"])
            P.act(e_row[:], pB[0:4, :], AF.Exp, ["pB", "bfn"], ["e_row"], bias=bfn[:, 0:1], scale=-1.0)
            P.act(sp_row[:], e_row[:], AF.Ln, ["e_row"], ["sp_row"], bias=1.0)
            P.op("dve", (lambda o, d0, d1, ini: (lambda e: e.tensor_tensor_scan(out=o, data0=d0, data1=d1, initial=ini, op0=ALU.add, op1=ALU.add)))(
                Bn[:, 1:513], sp_row[:], zeros[:], carryB[:, 0:1]), ["sp_row", "zeros", "carryB"], ["Bn"])
            P.tt("dve", A_row[:], i_row[:], Bn[:, 1:513], ALU.add, ["i_row", "Bn"], ["A_row"])
            P.copy("dve", Mx[:, 0:1], carryM[:, 0:1], ["carryM"], ["Mx"])
            P.op("dve", (lambda o, d0, d1, ini: (lambda e: e.tensor_tensor_scan(out=o, data0=d0, data1=d1, initial=ini, op0=ALU.max, op1=ALU.max)))(
                Mx[:, 1:513], A_row[:], A_row[:], carryM[:, 0:1]), ["A_row", "carryM", "Mx"], ["Mx"])
            P.copy("dve", carryB[:, 0:1], Bn[:, 512:513], ["Bn"], ["carryB"])
            P.copy("dve", carryM[:, 0:1], Mx[:, 512:513], ["Mx"], ["carryM"])
            P.tt("dve", N_row[:], Bn[:, 1:513], Mx[:, 1:513], ALU.subtract, ["Bn", "Mx"], ["N_row"])
            for c in range(4):
                for k3, (rap, rkey, off) in enumerate(((A_row, "A_row", 0), (Mx, "Mx", 1), (N_row, "N_row", 0))):
                    o0 = c * 12 + k3 * 4
                    P.tr(pA[:, o0:o0 + 4], rap[:, off + c * 128: off + (c + 1) * 128], ident[0:4, 0:4], [rkey, "ident"], ["pA"])
            P.copy("dve", cols[:].rearrange("p c k h -> p (c k h)"), pA[:, 0:48], ["pA"], ["cols"])
            P.act(eN[:], cols[:, :, 2, :], AF.Exp, ["cols"], ["eN"])
            for h in range(4):
                P.mm(pB[:, h * 5:(h + 1) * 5], sel[:, h * 128:(h + 1) * 128], Mx[:, 0:513:128], ["sel", "Mx"], ["pB"])
            P.copy("dve", Mb[:].rearrange("p h c -> p (h c)"), pB[:, 0:20], ["pB"], ["Mb"])
            P.ts("dve", nMb[:], Mb[:], -1.0, None, ALU.mult, None, ["Mb"], ["nMb"])
            P.tt("dve", dec[:], Mb[:, :, 0:4], Mb[:, :, 1:5], ALU.subtract, ["Mb"], ["dec"])
            P.act(dec[:], dec[:], AF.Exp, ["dec"], ["dec"])
            P.tt("dve", spa[:], Mb[:, :, 0:4].rearrange("p h c -> p c h"), cols[:, :, 1, :], ALU.subtract, ["Mb", "cols"], ["spa"])
            P.act(spa[:], spa[:], AF.Exp, ["spa"], ["spa"])
            for h in range(4):
                pz, kz_ = (pA, "pA") if h % 2 == 0 else (pB, "pB")
                P.mm(pz[:, :], sel[:, h * 128:(h + 1) * 128], Mx[:, 1:513], ["sel", "Mx"], [kz_])
                P.copy("act" if h % 2 == 0 else "dve", Mrow[:, h, :], pz[:, :], [kz_], ["Mrow"])
            for c in range(4):
                cs = slice(c * 128, (c + 1) * 128)
                for h in range(4):
                    i2 = r2.next()
                    sB, oB = rS.next(), rO.next()
                    kS, kO = "pS%d" % sB, "pO%d" % oB
                    Acol = cols[:, c, 0, h:h + 1]
                    P.mm(pS[sB][:, 0:128], kT[:, h, cs], qT[:, h, cs], ["kT", "qT"], [kS])
                    P.ts("dve", tmpD[i2][:], Mrow[:, h, cs], Acol, 0.0, ALU.subtract, ALU.max, ["Mrow", "cols"], ["tmpD%d" % i2])
                    P.act(Dt[i2][:], tmpD[i2][:], AF.Exp, ["tmpD%d" % i2], ["Dt%d" % i2], scale=-1.0)
                    P.tt("pool", Dm[i2][:], Dt[i2][:], tri[:], ALU.mult, ["Dt%d" % i2, "tri"], ["Dm%d" % i2])
                    P.tt("dve", wT[i2][:], pS[sB][:, 0:128], Dm[i2][:], ALU.mult, [kS, "Dm%d" % i2], ["wT%d" % i2])
                    P.mm(pO[oB][:, 0:129], wT[i2][:], vaug[b][:, c, h, :], ["wT%d" % i2, kva], [kO])
                    P.mm(pO[oB][:, 256:385], qT[:, h, cs], Cb[:, h, :], ["qT", "Cb"], [kO])
                    P.copy("act", intra[i2][:], pO[oB][:, 0:129], [kO], ["intra%d" % i2])
                    P.stt("dve", comb[i2][:], pO[oB][:, 256:385], spa[:, c, h:h + 1], intra[i2][:], ALU.mult, ALU.add,
                          [kO, "spa", "intra%d" % i2], ["comb%d" % i2])
                    ks = "sm%d" % i2
                    smt = sm[i2]
                    P.stt("dve", smt[:, 0:1], comb[i2][:, 128:129], -1.0, comb[i2][:, 128:129], ALU.mult, ALU.max, ["comb%d" % i2], [ks])
                    P.stt("dve", smt[:, 1:2], smt[:, 0:1], ML_SCALE, eN[:, c, h:h + 1], ALU.mult, ALU.max, [ks, "eN"], [ks])
                    P.op("dve", (lambda o, i_: (lambda e: e.reciprocal(out=o, in_=i_)))(smt[:, 2:3], smt[:, 1:2]), [ks], [ks])
                    P.ts("dve", hh[i2][:], comb[i2][:, 0:128], smt[:, 2:3], ML_SCALE, ALU.mult, ALU.mult, ["comb%d" % i2, ks], ["hh%d" % i2])
                    P.op("dve", (lambda o, i_: (lambda e: e.bn_stats(out=o, in_=i_)))(smt[:, 4:10], hh[i2][:]), ["hh%d" % i2], [ks])
                    P.op("dve", (lambda o, i_: (lambda e: e.bn_aggr(out=o, in_=i_)))(smt[:, 10:12], smt[:, 4:10]), [ks], [ks])
                    P.ts("dve", smt[:, 12:13], smt[:, 11:12], EPS, None, ALU.add, None, [ks], [ks])
                    P.act(smt[:, 13:14], smt[:, 12:13], AF.Ln, [ks], [ks])
                    P.act(smt[:, 14:15], smt[:, 13:14], AF.Exp, [ks], [ks], scale=-0.5)
                    P.ts("dve", hn[i2][:], hh[i2][:], smt[:, 10:11], smt[:, 14:15], ALU.subtract, ALU.mult, ["hh%d" % i2, ks], ["hn%d" % i2])
                    P.tr(ptb[:, i2 * 512:i2 * 512 + 128], hn[i2][:], identb[:], ["hn%d" % i2, "identb"], ["ptb"])
                    P.stt("dve", y1[i2][:], ptb[:, i2 * 512:i2 * 512 + 128], mg[:, h:h + 1], scs[:, h, cs], ALU.mult, ALU.add,
                          ["ptb", "mg", "scs"], ["y1%d" % i2])
                    P.tt("pool", ymg[b][:, h, cs], y1[i2][:], sigz[:, h, cs], ALU.mult, ["y1%d" % i2, "sigz"], ["ymg%d" % b])
                    P.act(wkc[i2][:], Acol, AF.Exp, ["cols", "nMb"], ["wkc%d" % i2], bias=nMb[:, h, c + 1:c + 2])
                    P.ts("pool", vw[i2][:], vaug[b][:, c, h, :], wkc[i2][:, 0:1], None, ALU.mult, None, [kva, "wkc%d" % i2], ["vw%d" % i2])
                    P.mm(pS[sB][:, 256:385], ktok[:, c, h, :], vw[i2][:], ["ktok", "vw%d" % i2], [kS])
                    P.stt("dve", Cf[:, h, :], Cf[:, h, :], dec[:, h, c:c + 1], pS[sB][:, 256:385], ALU.mult, ALU.add, ["Cf", "dec", kS], ["Cf"])
                    P.copy("pool", Cb[:, h, :], Cf[:, h, :], ["Cf"], ["Cb"])
            P.load("sp", T["ymT"].rearrange("(c p) t -> p c t", p=128)[:, :, t0:t0 + 512], ymg[b][:], ["ymg%d" % b], ["ymT"])
        return P.emit()


def stage3(nc, sems, T):
    with contextlib.ExitStack() as st:
        sb, ps = tens(nc, st)
        P = Prog(nc, sems)
        ident = sb("ident", [128, 128])
        identb = sb("identb", [128, 128], BF16)
        qT = sb("qT", [128, 4, S], BF16)
        kT = sb("kT", [128, 4, S], BF16)
        vaug = sb("vaug", [128, NT, 4, 129], BF16)
        rbb = T["rbb_sb"]
        biasT = T["biasT_sb"]
        lqb = sb("lqb", [128, 256])
        lt = sb("lt", [128, 64])
        lam = sb("lam", [128, 8])
        dag = sb("dag", [128, 1])
        PT = [sb("PT%d" % i, [128, 512], BF16) for i in range(3)]
        tmpn = [sb("tmpn%d" % i, [128, 128]) for i in range(2)]
        t0s = [sb("t0s%d" % i, [128, 128]) for i in range(2)]
        av = [sb("av%d" % i, [128, 128]) for i in range(2)]
        junk = sb("junk3", [128, 128])
        sm = [sb("sm3%d" % i, [128, 8]) for i in range(2)]
        an = [sb("an%d" % i, [128, 128], BF16) for i in range(2)]
        ydg = [sb("ydg%d" % i, [128, 512], BF16) for i in range(2)]
        pS = [ps("pS%d" % i, [128, 512]) for i in range(2)]
        acc = [ps("acc%d" % i, [128, 512]) for i in range(4)]
        ptb = ps("ptb", [128, 1024], BF16)

        P.load("sp", ident[:], T["ident"], [], ["ident"])
        P.copy("dve", identb[:], ident[:], ["ident"], ["identb"])
        P.memset("pool", vaug[:], 1.0, ["vaug"])
        P.load("sp", qT[:], T["featT"][3].rearrange("(h p) t -> p h t", p=128), [], ["qT"])
        P.load("act", kT[:], T["featT"][4].rearrange("(h p) t -> p h t", p=128), [], ["kT"])
        for t in range(NT):
            P.load("sp" if t % 2 == 0 else "act", vaug[:, t, :, 0:128],
                   T["vd_tok"][t * 128:(t + 1) * 128, :].rearrange("p (h e) -> p h e", e=128), ["vaug"], ["vaug"])
        P.load("sp", lqb[:], T["lambda_qk"].partition_broadcast(128), [], ["lqb"])
        P.load("sp", dag[:], T["da_norm_g"].rearrange("(p o) -> p o", o=1), [], ["dag"])
        P.ts("dve", dag[:], dag[:], 1.0 - LAM_INIT, None, ALU.mult, None, ["dag"], ["dag"])
        for i in range(2):
            P.tt("dve", lt[:], lqb[:, (2 * i) * 64:(2 * i + 1) * 64], lqb[:, (2 * i + 1) * 64:(2 * i + 2) * 64], ALU.mult, ["lqb", "lt"], ["lt"])
            P.op("dve", (lambda o, i_: (lambda e: e.reduce_sum(out=o, in_=i_, axis=AX.X)))(lam[:, 4 + i:5 + i], lt[:]), ["lt"], ["lam"])
        P.act(lam[:, 0:2], lam[:, 4:6], AF.Exp, ["lam"], ["lam"])
        P.tt("dve", lam[:, 2:3], lam[:, 0:1], lam[:, 1:2], ALU.subtract, ["lam"], ["lam"])
        P.ts("dve", lam[:, 3:4], lam[:, 2:3], LAM_INIT, -1.0, ALU.add, ALU.mult, ["lam"], ["lam"])
        rS, rP, r2 = Rot(2), Rot(3), Rot(2)
        cvj = Conv(P, sb, T, engs=("pool",), queues=("sp", "sp")) if 0 not in STAGES else None
        for h in range(4):
            for g in range(8):
                if cvj is not None:
                    cvj.emit(3)
                for a in range(4):
                    P.memset("dve", acc[a][:, :], 0.0, ["acc%d" % a])
                for c in range(2):
                    prow = slice(c * 64, (c + 1) * 64)
                    for j in range(4 * g + 4):
                        i_lo = max(j, 4 * g) - 4 * g
                        sB = rS.next()
                        pb = rP.next()
                        kS, kP = "pS%d" % sB, "PT%d" % pb
                        P.mm(pS[sB][:, i_lo * 128:512], kT[prow, h, j * 128:(j + 1) * 128], qT[prow, h, g * 512 + i_lo * 128:(g + 1) * 512],
                             ["kT", "qT"], [kS])
                        far_lo = None
                        for i in range(i_lo, 4):
                            dist = 4 * g + i - j
                            if dist >= 2:
                                far_lo = i
                                break
                            n2 = r2.next()
                            P.stt("dve", tmpn[n2][:], pS[sB][:, i * 128:(i + 1) * 128], DA_SCALE, biasT[:, dist, h, :], ALU.mult, ALU.add,
                                  [kS, "biasT"], ["tmpn%d" % n2])
                            P.act(PT[pb][:, i * 128:(i + 1) * 128], tmpn[n2][:], AF.Exp, ["tmpn%d" % n2], [kP])
                        if far_lo is not None:
                            P.act(PT[pb][:, far_lo * 128:512], pS[sB][:, far_lo * 128:512], AF.Exp, [kS, "rbb"], [kP],
                                  bias=rbb[:, 31 * 4 + h:31 * 4 + h + 1], scale=DA_SCALE)
                        for i in range(i_lo, 4):
                            a = c * 2 + i // 2
                            off = (i % 2) * 256
                            P.mm(acc[a][:, off:off + 129], PT[pb][:, i * 128:(i + 1) * 128], vaug[:, j, h, :], [kP, "vaug"], ["acc%d" % a],
                                 start=False, stop=False, skip=True)
                yb = (h * 8 + g) % 2
                for i in range(4):
                    n2 = r2.next()
                    ks = "sm3%d" % n2
                    smt = sm[n2]
                    a0, a1 = acc[i // 2], acc[2 + i // 2]
                    k0, k1 = "acc%d" % (i // 2), "acc%d" % (2 + i // 2)
                    off = (i % 2) * 256
                    P.op("dve", (lambda o, i_: (lambda e: e.reciprocal(out=o, in_=i_)))(smt[:, 0:1], a0[:, off + 128:off + 129]), [k0], [ks])
                    P.op("dve", (lambda o, i_: (lambda e: e.reciprocal(out=o, in_=i_)))(smt[:, 1:2], a1[:, off + 128:off + 129]), [k1], [ks])
                    P.tt("dve", smt[:, 2:3], smt[:, 1:2], lam[:, 3:4], ALU.mult, [ks, "lam"], [ks])
                    P.op("act", (lambda o, i_, sc: (lambda e: e.activation(out=o, in_=i_, func=AF.Copy, scale=sc)))(t0s[n2][:], a0[:, off:off + 128], smt[:, 0:1]),
                         [k0, ks], ["t0s%d" % n2])
                    P.stt("dve", av[n2][:], a1[:, off:off + 128], smt[:, 2:3], t0s[n2][:], ALU.mult, ALU.add, [k1, ks, "t0s%d" % n2], ["av%d" % n2])
                    P.act(junk[:], av[n2][:], AF.Square, ["av%d" % n2], ["junk3", ks], accum_out=smt[:, 3:4])
                    P.ts("dve", smt[:, 4:5], smt[:, 3:4], 1.0 / 128, SUBLN_EPS, ALU.mult, ALU.add, [ks], [ks])
                    P.act(smt[:, 5:6], smt[:, 4:5], AF.Ln, [ks], [ks])
                    P.act(smt[:, 6:7], smt[:, 5:6], AF.Exp, [ks], [ks], scale=-0.5)
                    P.ts("dve", an[n2][:], av[n2][:], smt[:, 6:7], None, ALU.mult, None, ["av%d" % n2, ks], ["an%d" % n2])
                    P.tr(ptb[:, n2 * 512:n2 * 512 + 128], an[n2][:], identb[:], ["an%d" % n2, "identb"], ["ptb"])
                    P.ts("dve", ydg[yb][:, i * 128:(i + 1) * 128], ptb[:, n2 * 512:n2 * 512 + 128], dag[:, 0:1], None, ALU.mult, None,
                         ["ptb", "dag"], ["ydg%d" % yb])
                P.load("act", T["ydT"][h * 128:(h + 1) * 128, g * 512:(g + 1) * 512], ydg[yb][:], ["ydg%d" % yb], ["ydT"])
        return P.emit()


def stage4(nc, sems, T):
    with contextlib.ExitStack() as st:
        sb, ps = tens(nc, st)
        P = Prog(nc, sems)
        ident = sb("ident", [128, 128])
        wo = sb("wo", [128, 8, D], BF16)
        yT = sb("yT", [128, 8, S], BF16)
        g2b = sb("g2b", [128, D])
        wr = sb("wr", [128, 8, 36])
        brb = sb("brb", [128, 36])
        xt = [sb("xt%d" % i, [128, D]) for i in range(2)]
        x2 = [sb("x2%d" % i, [128, D]) for i in range(2)]
        junk = sb("junk4", [128, D], BF16)
        stat = [sb("stat4%d" % i, [128, 4]) for i in range(2)]
        h2 = [sb("h2%d" % i, [128, D]) for i in range(2)]
        h2T = [sb("h2T%d" % i, [128, 8, 128]) for i in range(2)]
        h2Tb = [sb("h2Tb%d" % i, [128, 8, 128], BF16) for i in range(2)]
        lgt = sb("lgt", [128, NT, 36])
        mxg = sb("mxg", [128, NT])
        ohg = sb("ohg", [128, NT, 4])
        eg = sb("eg", [128, NT, 4])
        sg = sb("sg", [128, NT])
        tmp4 = sb("tmp4", [128, NT, 4, 8])
        les = sb("les", [128, NT, 8])
        le2 = sb("le2", [128, NT, 8])
        m1 = sb("m1", [128, NT])
        m2 = sb("m2", [128, NT])
        oh1 = sb("oh1", [128, NT, 8])
        oh2 = sb("oh2", [128, NT, 8])
        w1 = sb("w1", [128, NT])
        w2 = sb("w2", [128, NT])
        gf = sb("gf", [128, NT, 8])
        gts = sb("gts", [128, NT, 4, 8])
        pO = [ps("pO%d" % i, [128, 512]) for i in range(4)]
        pT = [ps("pT%d" % i, [128, 512]) for i in range(2)]
        pR = ps("pR", [128, 512])

        P.load("sp", ident[:], T["ident"], [], ["ident"])
        P.load("pool", wo[:], T["w_out"].rearrange("(c p) n -> p c n", p=128), [], ["wo"])
        P.load("sp", yT[:, 0:4, :], T["ymT"].rearrange("(c p) t -> p c t", p=128), [], ["yT"])
        P.load("act", yT[:, 4:8, :], T["ydT"].rearrange("(c p) t -> p c t", p=128), [], ["yT"])
        P.load("sp", g2b[:], T["norm2_g"].partition_broadcast(128), [], ["g2b"])
        P.load("sp", wr[:], T["w_r"].rearrange("(c p) n -> p c n", p=128), [], ["wr"])
        P.load("sp", brb[:], T["b_r"].partition_broadcast(128), [], ["brb"])
        rO = Rot(2)
        for t in range(NT):
            b = t % 2
            ts_ = slice(t * 128, (t + 1) * 128)
            P.load("sp", xt[b][:], T["x"][ts_, :], [], ["xt%d" % b])
            for half in range(2):
                pb = rO.next() * 2 + half
                for kc in range(8):
                    P.mm(pO[pb][:, :], yT[:, kc, ts_], wo[:, kc, half * 512:(half + 1) * 512], ["yT", "wo"], ["pO%d" % pb], start=(kc == 0), stop=(kc == 7))
                P.tt("dve", x2[b][:, half * 512:(half + 1) * 512], pO[pb][:, :], xt[b][:, half * 512:(half + 1) * 512], ALU.add,
                     ["pO%d" % pb, "xt%d" % b], ["x2%d" % b])
            P.load("act", T["x2"][ts_, :], x2[b][:], ["x2%d" % b], ["x2d"])
            sk = "stat4%d" % b
            P.act(junk[:], x2[b][:], AF.Square, ["x2%d" % b], ["junk4", sk], accum_out=stat[b][:, 0:1])
            P.ts("dve", stat[b][:, 1:2], stat[b][:, 0:1], 1.0 / D, EPS, ALU.mult, ALU.add, [sk], [sk])
            P.act(stat[b][:, 2:3], stat[b][:, 1:2], AF.Ln, [sk], [sk])
            P.act(stat[b][:, 3:4], stat[b][:, 2:3], AF.Exp, [sk], [sk], scale=-0.5)
            P.stt("dve", h2[b][:], x2[b][:], stat[b][:, 3:4], g2b[:], ALU.mult, ALU.mult, ["x2%d" % b, sk, "g2b"], ["h2%d" % b])
            for kc in range(8):
                pz = pT[kc // 4]
                P.tr(pz[:, (kc % 4) * 128:(kc % 4 + 1) * 128], h2[b][:, kc * 128:(kc + 1) * 128], ident[:], ["h2%d" % b, "ident"], ["pT%d" % (kc // 4)])
            for hf in range(2):
                P.copy("act", h2T[b][:, hf * 4:(hf + 1) * 4, :].rearrange("p k t -> p (k t)"), pT[hf][:, :], ["pT%d" % hf], ["h2T%d" % b])
                P.copy("dve", h2Tb[b][:, hf * 4:(hf + 1) * 4, :].rearrange("p k t -> p (k t)"), pT[hf][:, :], ["pT%d" % hf], ["h2Tb%d" % b])
            P.load("sp", T["h2T"].rearrange("(c p) t -> p c t", p=128)[:, :, ts_], h2Tb[b][:], ["h2Tb%d" % b], ["h2Td"])
            for kc in range(8):
                P.mm(pR[:, 0:36], h2T[b][:, kc, :], wr[:, kc, :], ["h2T%d" % b, "wr"], ["pR"], start=(kc == 0), stop=(kc == 7))
            P.tt("dve", lgt[:, t, :], pR[:, 0:36], brb[:], ALU.add, ["pR", "brb"], ["lgt"])
        lg = lgt[:, :, 0:4]
        le = lgt[:, :, 4:36].rearrange("p t (g e) -> p t g e", e=8)
        red = lambda o, i_, op: (lambda e: e.tensor_reduce(out=o, in_=i_, axis=AX.X, op=op))
        P.op("dve", red(mxg[:], lg, ALU.max), ["lgt"], ["mxg"])
        P.tt("dve", ohg[:], lg, mxg[:].unsqueeze(2).to_broadcast([128, NT, 4]), ALU.is_ge, ["lgt", "mxg"], ["ohg"])
        P.tt("dve", eg[:], lg, mxg[:].unsqueeze(2).to_broadcast([128, NT, 4]), ALU.subtract, ["lgt", "mxg"], ["eg"])
        P.act(eg[:], eg[:], AF.Exp, ["eg"], ["eg"])
        P.op("dve", red(sg[:], eg[:], ALU.add), ["eg"], ["sg"])
        P.op("dve", (lambda o, i_: (lambda e: e.reciprocal(out=o, in_=i_)))(sg[:], sg[:]), ["sg"], ["sg"])
        P.tt("dve", tmp4[:], le, ohg[:].unsqueeze(3).to_broadcast([128, NT, 4, 8]), ALU.mult, ["lgt", "ohg"], ["tmp4"])
        P.op("dve", red(les[:], tmp4[:].rearrange("p t g e -> p t e g"), ALU.add), ["tmp4"], ["les"])
        P.op("dve", red(m1[:], les[:], ALU.max), ["les"], ["m1"])
        P.tt("dve", oh1[:], les[:], m1[:].unsqueeze(2).to_broadcast([128, NT, 8]), ALU.is_ge, ["les", "m1"], ["oh1"])
        P.stt("dve", le2[:], oh1[:], -1e30, les[:], ALU.mult, ALU.add, ["oh1", "les"], ["le2"])
        P.op("dve", red(m2[:], le2[:], ALU.max), ["le2"], ["m2"])
        P.tt("dve", oh2[:], le2[:], m2[:].unsqueeze(2).to_broadcast([128, NT, 8]), ALU.is_ge, ["le2", "m2"], ["oh2"])
        P.tt("dve", w2[:], m2[:], m1[:], ALU.subtract, ["m1", "m2"], ["w2"])
        P.act(w2[:], w2[:], AF.Exp, ["w2"], ["w2"])
        P.ts("dve", w1[:], w2[:], 1.0, None, ALU.add, None, ["w2"], ["w1"])
        P.op("dve", (lambda o, i_: (lambda e: e.reciprocal(out=o, in_=i_)))(w1[:], w1[:]), ["w1"], ["w1"])
        P.tt("dve", w2[:], w2[:], w1[:], ALU.mult, ["w1", "w2"], ["w2"])
        P.tt("dve", w1[:], w1[:], sg[:], ALU.mult, ["w1", "sg"], ["w1"])
        P.tt("dve", w2[:], w2[:], sg[:], ALU.mult, ["w2", "sg"], ["w2"])
        P.tt("dve", oh1[:], oh1[:], w1[:].unsqueeze(2).to_broadcast([128, NT, 8]), ALU.mult, ["oh1", "w1"], ["oh1"])
        P.tt("dve", oh2[:], oh2[:], w2[:].unsqueeze(2).to_broadcast([128, NT, 8]), ALU.mult, ["oh2", "w2"], ["oh2"])
        P.tt("dve", gf[:], oh1[:], oh2[:], ALU.add, ["oh1", "oh2"], ["gf"])
        P.tt("dve", gts[:], ohg[:].unsqueeze(3).to_broadcast([128, NT, 4, 8]), gf[:].unsqueeze(2).to_broadcast([128, NT, 4, 8]), ALU.mult,
             ["ohg", "gf"], ["gts"])
        P.load("sp", T["gates"], gts[:].rearrange("p t g e -> p (t g e)"), ["gts"], ["gatesd"])
        return P.emit()


def stage5(nc, sems, T):
    TG = 1024
    with contextlib.ExitStack() as st:
        sb, ps = tens(nc, st)
        P = Prog(nc, sems)
        gts = sb("gts", [128, NT, 32])
        gfb = sb("gfb", [128, D])
        h2T = [sb("h2T%d" % i, [128, 8, TG], BF16) for i in range(2)]
        acc = sb("acc", [128, 8, D])
        wg = [sb("wg%d" % i, [128, 8, DFF], BF16) for i in range(2)]
        wu = [sb("wu%d" % i, [128, 8, DFF], BF16) for i in range(2)]
        wd = [sb("wd%d" % i, [128, 4, D], BF16) for i in range(2)]
        sgl = [sb("sgl%d" % i, [128, 512], BF16) for i in range(2)]
        hidT = sb("hidT", [128, 4, TG], BF16)
        x2 = [sb("x2%d" % i, [128, D]) for i in range(2)]
        junk = sb("junk5", [128, D], BF16)
        stat = [sb("stat5%d" % i, [128, 4]) for i in range(2)]
        ot = [sb("ot%d" % i, [128, D]) for i in range(2)]
        pg = [ps("pg%d" % i, [128, 512]) for i in range(2)]
        pu = [ps("pu%d" % i, [128, 512]) for i in range(2)]
        po = [ps("po%d" % i, [128, 512]) for i in range(4)]

        P.load("sp", gts[:].rearrange("p t e -> p (t e)"), T["gates"], [], ["gts"])
        P.load("sp", gfb[:], T["normf_g"].partition_broadcast(128), [], ["gfb"])
        rg, ro = Rot(2), Rot(4)
        k = 0
        for G in range(S // TG):
            hb = G % 2
            P.load("act", h2T[hb][:], T["h2T"].rearrange("(c p) t -> p c t", p=128)[:, :, G * TG:(G + 1) * TG], [], ["h2T%d" % hb])
            P.memset("pool", acc[:], 0.0, ["acc"])
            for e in range(N_EXP):
                wb = k % 2
                k += 1
                P.load("sp", wg[wb][:], T["wg_bf"][e].rearrange("(c p) f -> p c f", p=128), [], ["wg%d" % wb])
                P.load("act", wu[wb][:], T["wu_bf"][e].rearrange("(c p) f -> p c f", p=128), [], ["wu%d" % wb])
                P.load("sp", wd[wb][:], T["wd_bf"][e].rearrange("(c p) d -> p c d", p=128), [], ["wd%d" % wb])
                for fc in range(4):
                    for half in range(TG // 512):
                        gb = rg.next()
                        hs = slice(half * 512, (half + 1) * 512)
                        for kc in range(8):
                            P.mm(pg[gb][:, :], wg[wb][:, kc, fc * 128:(fc + 1) * 128], h2T[hb][:, kc, hs], ["wg%d" % wb, "h2T%d" % hb], ["pg%d" % gb],
                                 start=(kc == 0), stop=(kc == 7))
                        for kc in range(8):
                            P.mm(pu[gb][:, :], wu[wb][:, kc, fc * 128:(fc + 1) * 128], h2T[hb][:, kc, hs], ["wu%d" % wb, "h2T%d" % hb], ["pu%d" % gb],
                                 start=(kc == 0), stop=(kc == 7))
                        P.act(sgl[gb][:], pg[gb][:, :], AF.Silu, ["pg%d" % gb], ["sgl%d" % gb])
                        P.tt("dve", hidT[:, fc, hs], sgl[gb][:], pu[gb][:, :], ALU.mult, ["sgl%d" % gb, "pu%d" % gb], ["hidT"])
                for tl in range(TG // 128):
                    t = G * (TG // 128) + tl
                    for ch in range(2):
                        ob = ro.next()
                        for fc in range(4):
                            P.mm(po[ob][:, :], hidT[:, fc, tl * 128:(tl + 1) * 128], wd[wb][:, fc, ch * 512:(ch + 1) * 512], ["hidT", "wd%d" % wb],
                                 ["po%d" % ob], start=(fc == 0), stop=(fc == 3))
                        P.stt("dve", acc[:, tl, ch * 512:(ch + 1) * 512], po[ob][:, :], gts[:, t, e:e + 1], acc[:, tl, ch * 512:(ch + 1) * 512],
                              ALU.mult, ALU.add, ["po%d" % ob, "gts", "acc"], ["acc"])
            for tl in range(TG // 128):
                t = G * (TG // 128) + tl
                b = t % 2
                ts_ = slice(t * 128, (t + 1) * 128)
                sk = "stat5%d" % b
                P.load("sp", x2[b][:], T["x2"][ts_, :], [], ["x2%d" % b])
                P.tt("pool", x2[b][:], x2[b][:], acc[:, tl, :], ALU.add, ["x2%d" % b, "acc"], ["x2%d" % b])
                P.act(junk[:], x2[b][:], AF.Square, ["x2%d" % b], ["junk5", sk], accum_out=stat[b][:, 0:1])
                P.ts("dve", stat[b][:, 1:2], stat[b][:, 0:1], 1.0 / D, EPS, ALU.mult, ALU.add, [sk], [sk])
                P.act(stat[b][:, 2:3], stat[b][:, 1:2], AF.Ln, [sk], [sk])
                P.act(stat[b][:, 3:4], stat[b][:, 2:3], AF.Exp, [sk], [sk], scale=-0.5)
                P.stt("dve", ot[b][:], x2[b][:], stat[b][:, 3:4], gfb[:], ALU.mult, ALU.mult, ["x2%d" % b, sk, "gfb"], ["ot%d" % b])
                P.load("act", T["out"][ts_, :], ot[b][:], ["ot%d" % b], ["outd"])
        return P.emit()


def _rel_bucket_np(n):
    n = np.maximum(n, 0)
    max_exact = 16
    nf = np.maximum(n, 1).astype(np.float32)
    large = max_exact + (np.log(nf / np.float32(max_exact)) / np.float32(math.log(128 / max_exact)) * np.float32(16)).astype(np.int32)
    large = np.minimum(large, 31)
    return np.where(n < max_exact, n, large)


def _constants():
    ident = np.eye(128, dtype=np.float32)
    s_ = np.arange(128)[:, None]
    t_ = np.arange(128)[None, :]
    tri = (s_ <= t_).astype(np.float32)
    sel = np.zeros((4, 4, 128), np.float32)
    for h in range(4):
        sel[h, h, :] = 1.0
    oh = np.zeros((128, 2, 33, 128), np.float32)
    for kind in range(2):
        n = (t_ - s_) + 128 * kind
        bk = _rel_bucket_np(n)
        valid = n >= 0
        for b in range(32):
            oh[:, kind, b, :] = ((bk == b) & valid).astype(np.float32)
        oh[:, kind, 32, :] = (~valid).astype(np.float32)
    return dict(ident=ident, tri=tri, sel=sel.reshape(4, 512), oh=oh.reshape(128, -1))


_CACHE = {}


def kernel(x, w_in, conv_w, conv_b, w_mq, w_mk, w_mgate, b_mgate, m_norm_g, m_skip, lambda_qk, da_norm_g, rel_bias, w_out,
           norm1_g, norm2_g, w_rg, b_rg, w_re, b_re, w_eg, w_eu, w_ed, normf_g):
    f = lambda a: np.ascontiguousarray(np.asarray(a, dtype=np.float32))
    if "nc" not in _CACHE:
        _CACHE["nc"], _CACHE["stats"] = build_program()
    nc = _CACHE["nc"]
    shared = dict(
        w_in=f(w_in)[0], conv_w=f(conv_w)[0], conv_b=f(conv_b)[0], w_mq=f(w_mq)[0], w_mk=f(w_mk)[0], w_mgate=f(w_mgate)[0],
        b_mgate=f(b_mgate)[0], m_norm_g=f(m_norm_g)[0], m_skip=f(m_skip)[0], lambda_qk=f(lambda_qk)[0].reshape(256),
        da_norm_g=f(da_norm_g)[0], rel_bias=f(rel_bias).reshape(128), w_out=f(w_out)[0], norm1_g=f(norm1_g)[0], norm2_g=f(norm2_g)[0],
        w_r=np.ascontiguousarray(np.concatenate([f(w_rg)[0], f(w_re)[0].reshape(D, 32)], axis=1)),
        b_r=np.ascontiguousarray(np.concatenate([f(b_rg)[0], f(b_re)[0].reshape(32)])),
        w_eg=f(w_eg)[0], w_eu=f(w_eu)[0], w_ed=f(w_ed)[0], normf_g=f(normf_g),
    )
    shared.update(_constants())
    xs = f(x)
    in_maps = []
    for b in range(8):
        m = dict(shared)
        m["x"] = xs[b]
        in_maps.append(m)
    res = run_bass_kernel_spmd(nc, in_maps, core_ids=list(range(8)))
    _CACHE["res"] = res
    return np.stack([np.asarray(r["out"], dtype=np.float32) for r in res.results], axis=0)
```

```python
import math
import contextlib
import numpy as np
import concourse.bass as bass
import concourse.mybir as mybir
from concourse.bass_utils import run_bass_kernel_spmd

F32 = mybir.dt.float32
BF16 = mybir.dt.bfloat16
AF = mybir.ActivationFunctionType
ALU = mybir.AluOpType
AX = mybir.AxisListType

S = 4096
D = 1024
NT = 32
EPS = 1e-6
SUBLN_EPS = 1e-5
N_EXP = 32
DFF = 512
LAM_INIT = 0.8 - 0.6 * math.exp(-0.3 * 0)
ML_SCALE = 128.0 ** -0.5
DA_SCALE = 64.0 ** -0.5
NEG = -30000.0
SUP = 256
NSUP = 63
NSLOT = NSUP * SUP
I32 = mybir.dt.int32

COMPUTE = ("pe", "act", "dve", "pool")
QUEUES = ("sp", "act", "pool")
N_DMA_SEMS = 8
DEBUG = False
CONV_PER_GROUP = {1: 3, 2: 4, 3: 2}
STAGES = (1, 2, 3, 4, 5)


class Sems:
    def __init__(self, nc, st):
        self.esem = {e: st.enter_context(nc.semaphore("s_" + e)) for e in COMPUTE}
        self.dsem = {(q, s): st.enter_context(nc.semaphore("d_%s_%d" % (q, s))) for q in QUEUES for s in range(N_DMA_SEMS)}
        self.cnt = {e: 0 for e in COMPUTE}
        self.dcnt = {k: 0 for k in self.dsem}
        self.rr = {q: 0 for q in QUEUES}


class Op:
    __slots__ = ("eng", "fn", "deps", "is_dma", "signal", "val", "sem", "slot", "prev")

    def __init__(self, eng, fn, is_dma):
        self.eng, self.fn, self.is_dma = eng, fn, is_dma
        self.deps = []
        self.signal = False
        self.val = None
        self.sem = None
        self.slot = None
        self.prev = None


class Prog:
    def __init__(self, nc, sems):
        self.nc = nc
        self.sems = sems
        self.ops = []
        self.last_writer = {}
        self.readers = {}
        self.slot_last = {}

    def _add(self, op, reads, writes):
        pr = [r for r in reads if r in PSUM_KEYS]
        if pr:
            reads = [r for r in reads if r not in PSUM_KEYS]
            writes = list(writes) + [r for r in pr if r not in writes]
        deps = []
        for r in reads:
            w = self.last_writer.get(r)
            if w is not None:
                deps.append(w)
        for w in writes:
            lw = self.last_writer.get(w)
            if lw is not None:
                deps.append(lw)
            deps.extend(self.readers.get(w, ()))
        seen = set()
        for d in deps:
            if id(d) not in seen and d is not op:
                seen.add(id(d))
                op.deps.append(d)
        for r in reads:
            self.readers.setdefault(r, []).append(op)
        for w in writes:
            self.last_writer[w] = op
            self.readers[w] = []
        self.ops.append(op)
        return op

    def op(self, eng, fn, reads=(), writes=()):
        return self._add(Op(eng, fn, False), reads, writes)

    def dma(self, queue, fn, reads=(), writes=()):
        op = Op(queue, fn, True)
        s = self.sems
        op.slot = (queue, s.rr[queue] % N_DMA_SEMS)
        s.rr[queue] += 1
        op.prev = self.slot_last.get(op.slot)
        self.slot_last[op.slot] = op
        return self._add(op, reads, writes)

    def mm(self, out, lhsT, rhs, r, w, start=True, stop=True, skip=False):
        if skip:
            return self.op("pe", lambda e: e.matmul(out, lhsT=lhsT, rhs=rhs, start=start, stop=stop, skip_group_check=True), r, w)
        return self.op("pe", lambda e: e.matmul(out, lhsT=lhsT, rhs=rhs, start=start, stop=stop), r, w)

    def tr(self, out, in_, ident, r, w):
        return self.op("pe", lambda e: e.transpose(out=out, in_=in_, identity=ident), r, w)

    def act(self, out, in_, func, r, w, bias=None, scale=None, accum_out=None):
        kw = {}
        if bias is not None:
            kw["bias"] = bias
        if scale is not None:
            kw["scale"] = scale
        if accum_out is not None:
            kw["accum_out"] = accum_out
        return self.op("act", lambda e: e.activation(out=out, in_=in_, func=func, **kw), r, w)

    def copy(self, eng, out, in_, r, w):
        if eng == "act":
            return self.op("act", lambda e: e.copy(out=out, in_=in_), r, w)
        return self.op(eng, lambda e: e.tensor_copy(out=out, in_=in_), r, w)

    def tt(self, eng, out, in0, in1, op, r, w):
        return self.op(eng, lambda e: e.tensor_tensor(out=out, in0=in0, in1=in1, op=op), r, w)

    def ts(self, eng, out, in0, s1, s2, op0, op1, r, w):
        if s2 is None:
            return self.op(eng, lambda e: e.tensor_scalar(out=out, in0=in0, scalar1=s1, scalar2=None, op0=op0), r, w)
        return self.op(eng, lambda e: e.tensor_scalar(out=out, in0=in0, scalar1=s1, scalar2=s2, op0=op0, op1=op1), r, w)

    def stt(self, eng, out, in0, scalar, in1, op0, op1, r, w):
        eng = "dve"
        return self.op(eng, lambda e: e.scalar_tensor_tensor(out=out, in0=in0, scalar=scalar, in1=in1, op0=op0, op1=op1), r, w)

    def memset(self, eng, ap, val, w):
        return self.op(eng, lambda e: e.memset(ap, val), (), w)

    def load(self, q, out, in_, r, w):
        return self.dma(q, lambda e: e.dma_start(out=out, in_=in_), r, w)

    def emit(self):
        nc, s, ops = self.nc, self.sems, self.ops

        def same_skip(d, o):
            return (not d.is_dma) and (not o.is_dma) and d.eng == o.eng and d.eng == "pe"

        for o in ops:
            for d in o.deps:
                if d.is_dma or same_skip(d, o):
                    continue
                d.signal = True
        for o in ops:
            if o.is_dma:
                s.dcnt[o.slot] += 16
                o.val = s.dcnt[o.slot]
                o.sem = s.dsem[o.slot]
            else:
                o.sem = s.esem[o.eng]
                if o.signal:
                    s.cnt[o.eng] += 1
                    o.val = s.cnt[o.eng]
        by_eng = {e: [] for e in ("pe", "act", "dve", "pool", "sp")}
        for o in ops:
            by_eng[o.eng].append(o)
        final = dict(s.dcnt)

        def run(engname, e):
            waited = {}

            def wait(sem, val):
                if waited.get(id(sem), 0) >= val:
                    return
                waited[id(sem)] = val
                e.wait_ge(sem, val)

            for o in by_eng[engname]:
                for d in o.deps:
                    if same_skip(d, o):
                        continue
                    wait(d.sem, d.val)
                if o.is_dma and o.prev is not None:
                    wait(o.prev.sem, o.prev.val)
                ins = o.fn(e)
                if o.is_dma:
                    ins.then_inc(o.sem, 16)
                elif o.signal:
                    ins.then_inc(o.sem, 1)
            if engname == "sp":
                for k, v in final.items():
                    if v > 0:
                        wait(s.dsem[k], v)

        with nc.Block() as block:
            block.sync(lambda e: run("sp", e))
            if by_eng["pe"]:
                block.tensor(lambda e: run("pe", e))
            if by_eng["act"]:
                block.scalar(lambda e: run("act", e))
            if by_eng["dve"]:
                block.vector(lambda e: run("dve", e))
            if by_eng["pool"]:
                block.gpsimd(lambda e: run("pool", e))
        return {k: len(v) for k, v in by_eng.items()}


class Rot:
    def __init__(self, n):
        self.n, self.i = n, 0

    def next(self):
        v = self.i % self.n
        self.i += 1
        return v


def build_program():
    nc = bass.Bass("TRN2", target_bir_lowering=False)
    I = lambda name, shape, dt=F32: nc.dram_tensor(name, list(shape), dt, kind="ExternalInput").ap()
    skind = "ExternalOutput" if DEBUG else "Internal"
    SC = lambda name, shape, dt: nc.dram_tensor(name, list(shape), dt, kind=skind).ap()
    T = {}
    T["x"] = I("x", [S, D])
    T["w_in"] = I("w_in", [D, 3072])
    T["conv_w"] = I("conv_w", [4, 512])
    T["conv_b"] = I("conv_b", [512])
    T["w_mq"] = I("w_mq", [4, 128, 128])
    T["w_mk"] = I("w_mk", [4, 128, 128])
    T["w_mgate"] = I("w_mgate", [1536, 8])
    T["b_mgate"] = I("b_mgate", [8])
    T["m_norm_g"] = I("m_norm_g", [512])
    T["m_skip"] = I("m_skip", [512])
    T["lambda_qk"] = I("lambda_qk", [256])
    T["da_norm_g"] = I("da_norm_g", [128])
    T["rel_bias"] = I("rel_bias", [128])
    T["w_out"] = I("w_out", [D, D])
    T["norm1_g"] = I("norm1_g", [D])
    T["norm2_g"] = I("norm2_g", [D])
    T["w_r"] = I("w_r", [D, 36])
    T["b_r"] = I("b_r", [36])
    T["w_eg"] = I("w_eg", [N_EXP, D, DFF])
    T["w_eu"] = I("w_eu", [N_EXP, D, DFF])
    T["w_ed"] = I("w_ed", [N_EXP, DFF, D])
    T["normf_g"] = I("normf_g", [D])
    T["ident"] = I("ident", [128, 128])
    T["tri"] = I("tri", [128, 128])
    T["sel"] = I("sel", [4, 512])
    T["oh"] = I("oh", [128, 2 * 33 * 128])
    T["lstrict"] = I("lstrict", [128, 128])
    T["thr"] = I("thr", [16 + NSUP])
    T["pidx"] = I("pidx", [128, 1])
    T["out"] = nc.dram_tensor("out", [S, D], F32, kind="ExternalOutput").ap()
    T["featT"] = SC("featT", [5, 512, S], BF16)
    T["vm_tok"] = SC("vm_tok", [S, 512], BF16)
    T["vd_tok"] = SC("vd_tok", [S, 512], BF16)
    T["ymT"] = SC("ymT", [512, S], BF16)
    T["ydT"] = SC("ydT", [512, S], BF16)
    T["x2"] = SC("x2", [S, D], F32)
    T["h2T"] = SC("h2T", [D, S], BF16)
    T["gates"] = SC("gates", [128, NT * 32], F32)
    T["h2b"] = SC("h2b", [S, D], BF16)
    T["xs"] = nc.dram_tensor("xs", [NSLOT, D], BF16, kind="Internal").ap()
    T["ys"] = nc.dram_tensor("ys", [NSLOT, D], BF16, kind="Internal").ap()
    T["wall"] = nc.dram_tensor("wall", [N_EXP * 128, 3 * 4096], BF16, kind="Internal").ap()

    stats = {}
    with contextlib.ExitStack() as gst:
        gst.enter_context(nc.allow_non_contiguous_dma(reason="small strided parameter loads"))
        sems = Sems(nc, gst)
        T["biasT_sb"] = gst.enter_context(nc.sbuf_tensor("g_biasT", [128, 2, 4, 128], F32))
        T["rbb_sb"] = gst.enter_context(nc.sbuf_tensor("g_rbb", [128, 128], F32))
        T["pos_i"] = gst.enter_context(nc.sbuf_tensor("g_pos_i", [128, NT, 2], I32))
        T["te_i"] = gst.enter_context(nc.sbuf_tensor("g_te_i", [128, 64], I32))
        T["wk_g"] = gst.enter_context(nc.sbuf_tensor("g_wk", [128, NT, 2], F32))
        if 0 in STAGES:
            stats["s0"] = stage0(nc, sems, T)
        if 1 in STAGES:
            stats["s1"] = stage1(nc, sems, T)
        if 2 in STAGES:
            stats["s2"] = stage2(nc, sems, T)
        if 3 in STAGES:
            stats["s3"] = stage3(nc, sems, T)
        if 4 in STAGES:
            stats["s4"] = stage4(nc, sems, T)
        if 5 in STAGES:
            stats["s5"] = stage5(nc, sems, T)
        if 6 in STAGES:
            stats["s6"] = stage6(nc, sems, T)
    return nc, stats


_TN = [0]
PSUM_KEYS = set()


def tens(nc, st):
    _TN[0] += 1
    pre = "t%d_" % _TN[0]
    sb = lambda n, s, d=F32: st.enter_context(nc.sbuf_tensor(pre + n, list(s), d))
    def ps(n, s, d=F32):
        PSUM_KEYS.add(n)
        return st.enter_context(nc.psum_tensor(pre + n, list(s), d))
    return sb, ps


def conv_jobs():
    return [(name, m, e) for m, name in enumerate(("w_eg", "w_eu", "w_ed")) for e in range(N_EXP)]


class Conv:
    def __init__(self, P, sb, T, engs=("pool",), queues=("sp", "sp"), nb=3):
        self.P, self.T = P, T
        self.stg = [sb("w0s%d" % i, [128, 8, 512], F32) for i in range(nb)]
        self.cvt = [sb("w0c%d" % i, [128, 8, 512], BF16) for i in range(nb)]
        self.rot = Rot(nb)
        self.engs, self.queues = engs, queues
        self.jobs = conv_jobs()
        self.k = 0

    def emit(self, n):
        P, T = self.P, self.T
        for _ in range(n):
            if self.k >= len(self.jobs):
                return
            name, m, e = self.jobs[self.k]
            b = self.rot.next()
            src = T[name][e].rearrange("(c p) f -> p c f", p=128)
            dstap = T["wall"][e * 128:(e + 1) * 128, m * 4096:(m + 1) * 4096].rearrange("p (c f) -> p c f", f=512)
            sv = self.stg[b][:].rearrange("p (c h) f -> p c (h f)", c=4) if name == "w_ed" else self.stg[b][:]
            P.load(self.queues[0], sv, src, [], ["stg%d" % b])
            P.copy(self.engs[self.k % len(self.engs)], self.cvt[b][:], self.stg[b][:], ["stg%d" % b], ["cvt%d" % b])
            P.load(self.queues[1], dstap, self.cvt[b][:], ["cvt%d" % b], ["wall"])
            self.k += 1


class ConvD:
    def __init__(self, P, T):
        self.P, self.T = P, T
        self.jobs = conv_jobs()
        self.k = 0

    def emit(self, n):
        P, T = self.P, self.T
        for _ in range(n):
            if self.k >= len(self.jobs):
                return
            name, m, e = self.jobs[self.k]
            cols = T["wall"][e * 128:(e + 1) * 128, m * 4096:(m + 1) * 4096]
            if name == "w_ed":
                src = T[name][e].rearrange("(c p) d -> p c d", p=128)
                dst = cols.rearrange("p (c d) -> p c d", d=1024)
            else:
                src = T[name][e].rearrange("(c p) f -> p c f", p=128)
                dst = cols.rearrange("p (c f) -> p c f", f=512)
            P.load("pool", dst, src, [], ["wall%d" % self.k])
            self.k += 1


def stage0(nc, sems, T):
    with contextlib.ExitStack() as st:
        sb, ps = tens(nc, st)
        P = Prog(nc, sems)
        cv = Conv(P, sb, T, engs=("dve", "pool", "act"), queues=("sp", "act"))
        cv.emit(96)
        return P.emit()


def stage1(nc, sems, T):
    with contextlib.ExitStack() as st:
        sb, ps = tens(nc, st)
        P = Prog(nc, sems)
        ident = sb("ident", [128, 128])
        identb = sb("identb", [128, 128], BF16)
        g1 = sb("g1", [128, 8])
        w_bf = sb("w_in_bf", [128, 8, 3072], BF16)
        wst = [sb("wst%d" % i, [128, 3072]) for i in range(2)]
        xt = [sb("xt%d" % i, [128, D]) for i in range(2)]
        junk = sb("junk", [128, D], BF16)
        stat = sb("stat", [128, 4])
        xn = [sb("xn%d" % i, [128, D], BF16) for i in range(2)]
        hT = [sb("hT%d" % i, [128, 8, 512], BF16) for i in range(2)]
        fstg = [sb("fstg%d" % i, [128, 4, 512], BF16) for i in range(2)]
        tstg = [sb("tstg%d" % i, [128, 4, 512], BF16) for i in range(2)]
        pt = [ps("pt%d" % i, [128, D], BF16) for i in range(2)]
        pp = [ps("pp%d" % i, [128, 512]) for i in range(4)]

        P.load("sp", ident[:], T["ident"], [], ["ident"])
        P.copy("dve", identb[:], ident[:], ["ident"], ["identb"])
        P.load("sp", g1[:], T["norm1_g"].rearrange("(c p) -> p c", p=128), [], ["g1"])
        for kc in range(8):
            b = kc % 2
            P.load("sp" if kc % 2 == 0 else "act", wst[b][:], T["w_in"][kc * 128:(kc + 1) * 128, :], [], ["wst%d" % b])
            P.ts("dve" if kc % 2 == 0 else "pool", w_bf[:, kc, :], wst[b][:], g1[:, kc:kc + 1], None, ALU.mult, None,
                 ["wst%d" % b, "g1"], ["w_bf"])
        oh = sb("oh", [128, 2, 33, 128])
        rbb, biasT = T["rbb_sb"], T["biasT_sb"]
        P.load("act", oh[:].rearrange("p a b c -> p (a b c)"), T["oh"], [], ["oh"])
        P.load("act", rbb[:], T["rel_bias"].partition_broadcast(128), [], ["rbb"])

        def emit_bias(idx):
            kind, h = idx // 4, idx % 4
            dst_ = biasT[:, kind, h, :]
            kb = "biasT%d%d" % (kind, h)
            P.ts("pool", dst_, oh[:, kind, 32, :], NEG, None, ALU.mult, None, ["oh"], [kb])
            for b_ in range(32):
                P.stt("dve", dst_, oh[:, kind, b_, :], rbb[:, b_ * 4 + h:b_ * 4 + h + 1], dst_, ALU.mult, ALU.add, ["oh", "rbb", kb], [kb])

        rpp = Rot(4)
        cvj = ConvD(P, T)
        for g in range(8):
            hb = g % 2
            emit_bias(g)
            cvj.emit(CONV_PER_GROUP[1])
            for ti in range(4):
                t = g * 4 + ti
                b = t % 2
                P.load("sp", xt[b][:], T["x"][t * 128:(t + 1) * 128, :], [], ["xt%d" % b])
                P.act(junk[:], xt[b][:], AF.Square, ["xt%d" % b], ["junk", "stat"], accum_out=stat[:, 0:1])
                P.ts("dve", stat[:, 1:2], stat[:, 0:1], 1.0 / D, EPS, ALU.mult, ALU.add, ["stat"], ["stat"])
                P.act(stat[:, 2:3], stat[:, 1:2], AF.Ln, ["stat"], ["stat"])
                P.act(stat[:, 3:4], stat[:, 2:3], AF.Exp, ["stat"], ["stat"], scale=-0.5)
                P.ts("dve", xn[b][:], xt[b][:], stat[:, 3:4], None, ALU.mult, None, ["xt%d" % b, "stat"], ["xn%d" % b])
                for kc in range(8):
                    P.tr(pt[b][:, kc * 128:(kc + 1) * 128], xn[b][:, kc * 128:(kc + 1) * 128], identb[:], ["xn%d" % b, "identb"], ["pt%d" % b])
                P.copy("act" if ti % 2 == 0 else "dve", hT[hb][:, :, ti * 128:(ti + 1) * 128], pt[b][:, :].rearrange("p (k t) -> p k t", k=8),
                       ["pt%d" % b], ["hT%d" % hb])
            for blk in range(5):
                fb = (g * 5 + blk) % 2
                for ch in range(4):
                    col0 = blk * 512 + ch * 128
                    pb = rpp.next()
                    for kc in range(8):
                        P.mm(pp[pb][:, :], w_bf[:, kc, col0:col0 + 128], hT[hb][:, kc, :], ["w_bf", "hT%d" % hb], ["pp%d" % pb],
                             start=(kc == 0), stop=(kc == 7))
                    P.copy("act" if ch % 2 == 0 else "dve", fstg[fb][:, ch, :], pp[pb][:, :], ["pp%d" % pb], ["fstg%d" % fb])
                P.load("act", T["featT"][blk].rearrange("(c p) t -> p c t", p=128)[:, :, g * 512:(g + 1) * 512], fstg[fb][:],
                       ["fstg%d" % fb], ["featT"])
            for bi, (blk, dst) in enumerate(((1, "vm_tok"), (5, "vd_tok"))):
                tb = (g * 2 + bi) % 2
                for ti in range(4):
                    pb = rpp.next()
                    for kc in range(8):
                        P.mm(pp[pb][:, :], hT[hb][:, kc, ti * 128:(ti + 1) * 128], w_bf[:, kc, blk * 512:(blk + 1) * 512],
                             ["w_bf", "hT%d" % hb], ["pp%d" % pb], start=(kc == 0), stop=(kc == 7))
                    P.copy("dve" if ti % 2 == 0 else "act", tstg[tb][:, ti, :], pp[pb][:, :], ["pp%d" % pb], ["tstg%d" % tb])
                P.load("sp", T[dst][g * 512:(g + 1) * 512, :].rearrange("(t p) f -> p t f", p=128), tstg[tb][:], ["tstg%d" % tb], [dst])
        return P.emit()


def stage2(nc, sems, T):
    with contextlib.ExitStack() as st:
        sb, ps = tens(nc, st)
        P = Prog(nc, sems)
        ident = sb("ident", [128, 128])
        identb = sb("identb", [128, 128], BF16)
        tri = sb("tri", [128, 128])
        bigtri = sb("bigtri", [128, 128])
        sel = sb("sel", [4, 512])
        cw = sb("cw", [128, 4, 4])
        cb = sb("cb", [128, 4])
        mg = sb("mg", [128, 4])
        msk = sb("msk", [128, 4])
        wq = sb("wq", [128, 4, 128], BF16)
        wk = sb("wk", [128, 4, 128], BF16)
        wgt = sb("wgt", [128, 12, 8], BF16)
        bi = sb("bi", [4, 1])
        bfn = sb("bfn", [4, 1])
        zeros = sb("zeros", [4, 512])
        carryB = sb("carryB", [4, 1])
        carryM = sb("carryM", [4, 1])
        Cf = sb("Cf", [128, 4, 129])
        Cb = sb("Cb", [128, 4, 129], BF16)
        c_sb = [sb("c_sb%d" % i, [128, 4, 515], BF16) for i in range(2)]
        z_sb = [sb("z_sb%d" % i, [128, 4, 512], BF16) for i in range(2)]
        vmT = [sb("vmT%d" % i, [128, 4, 512], BF16) for i in range(2)]
        vaug = [sb("vaug%d" % i, [128, 4, 4, 129], BF16) for i in range(2)]
        cacc = [sb("cacc%d" % i, [128, 512]) for i in range(2)]
        cact = sb("cact", [128, 4, 512], BF16)
        sigz = sb("sigz", [128, 4, 512], BF16)
        scs = sb("scs", [128, 4, 512], BF16)
        qT = sb("qT", [128, 4, 512], BF16)
        kT = sb("kT", [128, 4, 512], BF16)
        ktok = sb("ktok", [128, 4, 4, 128], BF16)
        i_row = sb("i_row", [4, 512])
        e_row = sb("e_row", [4, 512])
        sp_row = sb("sp_row", [4, 512])
        Bn = sb("Bn", [4, 513])
        A_row = sb("A_row", [4, 512])
        Mx = sb("Mx", [4, 513])
        N_row = sb("N_row", [4, 512])
        cols = sb("cols", [128, 4, 3, 4])
        eN = sb("eN", [128, 4, 4])
        Mb = sb("Mb", [128, 4, 5])
        nMb = sb("nMb", [128, 4, 5])
        dec = sb("dec", [128, 4, 4])
        spa = sb("spa", [128, 4, 4])
        Mrow = sb("Mrow", [128, 4, 512])
        tmpD = [sb("tmpD%d" % i, [128, 128]) for i in range(4)]
        Dt = [sb("Dt%d" % i, [128, 128]) for i in range(4)]
        Dm = [sb("Dm%d" % i, [128, 128]) for i in range(2)]
        wT = [sb("wT%d" % i, [128, 128], BF16) for i in range(4)]
        intra = [sb("intra%d" % i, [128, 129]) for i in range(4)]
        comb = [sb("comb%d" % i, [128, 129]) for i in range(4)]
        sm = [sb("sm%d" % i, [128, 16]) for i in range(4)]
        hh = [sb("hh%d" % i, [128, 128]) for i in range(4)]
        hn = [sb("hn%d" % i, [128, 128], BF16) for i in range(4)]
        y1 = [sb("y1%d" % i, [128, 128], BF16) for i in range(4)]
        ymg = [sb("ymg%d" % i, [128, 4, 512], BF16) for i in range(2)]
        wkc = [sb("wkc%d" % i, [128, 1]) for i in range(4)]
        vw = [sb("vw%d" % i, [128, 129], BF16) for i in range(4)]
        pA = ps("pA", [128, 512])
        pB = ps("pB", [128, 512])
        pG = ps("pG", [128, 512])
        ptb = ps("ptb", [128, 1024], BF16)
        pS = [ps("pS%d" % i, [128, 512]) for i in range(2)]
        pO = [ps("pO%d" % i, [128, 512]) for i in range(2)]
        P.load("sp", ident[:], T["ident"], [], ["ident"])
        P.copy("dve", identb[:], ident[:], ["ident"], ["identb"])
        P.load("sp", tri[:], T["tri"], [], ["tri"])
        P.ts("dve", bigtri[:], tri[:], -1.0, -1.0e4, ALU.add, ALU.mult, ["tri"], ["bigtri"])
        P.load("sp", sel[:], T["sel"], [], ["sel"])
        P.load("sp", cw[:], T["conv_w"].rearrange("j (c p) -> p j c", p=128), [], ["cw"])
        P.load("sp", cb[:], T["conv_b"].rearrange("(c p) -> p c", p=128), [], ["cb"])
        P.load("sp", mg[:], T["m_norm_g"].rearrange("(c p) -> p c", p=128), [], ["mg"])
        P.load("sp", msk[:], T["m_skip"].rearrange("(c p) -> p c", p=128), [], ["msk"])
        P.load("pool", wq[:], T["w_mq"].rearrange("h d e -> d h e"), [], ["wq"])
        P.load("pool", wk[:], T["w_mk"].rearrange("h d e -> d h e"), [], ["wk"])
        P.load("pool", wgt[:], T["w_mgate"].rearrange("(c p) g -> p c g", p=128), [], ["wgt"])
        P.load("sp", bi[:], T["b_mgate"][0:4].rearrange("(p o) -> p o", o=1), [], ["bi"])
        P.load("sp", bfn[:], T["b_mgate"][4:8].rearrange("(p o) -> p o", o=1), [], ["bfn"])
        P.ts("dve", bfn[:], bfn[:], -1.0, None, ALU.mult, None, ["bfn"], ["bfn"])
        P.memset("pool", zeros[:], 0.0, ["zeros"])
        P.memset("pool", carryB[:], 0.0, ["carryB"])
        P.memset("pool", carryM[:], 0.0, ["carryM"])
        P.memset("pool", Cf[:], 0.0, ["Cf%d" % h_ for h_ in range(4)])
        P.memset("pool", Cb[:], 0.0, ["Cb%d" % h_ for h_ in range(4)])
        for i in range(2):
            P.memset("pool", vaug[i][:], 1.0, ["vaug%d" % i])
            P.memset("pool", c_sb[i][:], 0.0, ["c_sb%d" % i])

        featT = T["featT"]
        rS, rO, r2 = Rot(2), Rot(2), Rot(2)
        cvj = ConvD(P, T)
        cvj.k = 8 * CONV_PER_GROUP[1]
        for g in range(8):
            b = g % 2
            t0 = g * 512
            cvj.emit(CONV_PER_GROUP[2])
            kc_, kz, kv, kva = "c_sb%d" % b, "z_sb%d" % b, "vmT%d" % b, "vaug%d" % b
            cview = featT[0].rearrange("(c p) t -> p c t", p=128)
            if g == 0:
                P.load("sp", c_sb[b][:, :, 3:515], cview[:, :, 0:512], [], [kc_])
            else:
                P.load("sp", c_sb[b][:, :, 0:515], cview[:, :, t0 - 3:t0 + 512], [], [kc_])
            P.load("act", z_sb[b][:], featT[2].rearrange("(c p) t -> p c t", p=128)[:, :, t0:t0 + 512], [], [kz])
            P.load("act", vmT[b][:], featT[1].rearrange("(c p) t -> p c t", p=128)[:, :, t0:t0 + 512], [], [kv])
            for ti in range(4):
                P.load("sp" if ti % 2 == 0 else "act", vaug[b][:, ti, :, 0:128],
                       T["vm_tok"][t0 + ti * 128:t0 + (ti + 1) * 128, :].rearrange("p (h e) -> p h e", e=128), [kva], [kva])
            for ch in range(4):
                ab = ch % 2
                ka = "cacc%d" % ab
                e1 = "dve" if ch % 2 == 0 else "pool"
                P.ts("dve", cacc[ab][:], c_sb[b][:, ch, 0:512], cw[:, 0, ch:ch + 1], cb[:, ch:ch + 1], ALU.mult, ALU.add, [kc_, "cw", "cb"], [ka])
                for j in range(1, 4):
                    P.stt("dve" if j % 2 == 0 else "pool", cacc[ab][:], c_sb[b][:, ch, j:j + 512], cw[:, j, ch:ch + 1], cacc[ab][:], ALU.mult, ALU.add,
                          [kc_, "cw", ka], [ka])
                P.act(cact[:, ch, :], cacc[ab][:], AF.Silu, [ka], ["cact"])
                P.ts("pool", scs[:, ch, :], cact[:, ch, :], msk[:, ch:ch + 1], None, ALU.mult, None, ["cact", "msk"], ["scs"])
            P.act(sigz[:].rearrange("p c t -> p (c t)"), z_sb[b][:].rearrange("p c t -> p (c t)"), AF.Sigmoid, [kz], ["sigz"])
            for h in range(4):
                P.mm(pA[:, :], wq[:, h, :], cact[:, h, :], ["wq", "cact"], ["pA"])
                P.copy("act", qT[:, h, :], pA[:, :], ["pA"], ["qT"])
                P.mm(pB[:, :], wk[:, h, :], cact[:, h, :], ["wk", "cact"], ["pB"])
                P.copy("dve", kT[:, h, :], pB[:, :], ["pB"], ["kT"])
            for ti in range(4):
                pz = pA if ti % 2 == 0 else pB
                kz_ = "pA" if ti % 2 == 0 else "pB"
                for h in range(4):
                    P.mm(pz[:, h * 128:(h + 1) * 128], cact[:, h, ti * 128:(ti + 1) * 128], wk[:, h, :], ["cact", "wk"], [kz_])
                P.copy("act" if ti % 2 == 0 else "dve", ktok[:, ti, :, :].rearrange("p h e -> p (h e)"), pz[:, :], [kz_], ["ktok"])
            srcs = [(qT, "qT")] * 4 + [(kT, "kT")] * 4 + [(vmT[b], kv)] * 4
            for c in range(12):
                sap, skey = srcs[c]
                P.mm(pG[0:4, :], wgt[:, c, 0:4], sap[:, c % 4, :], ["wgt", skey], ["pG"], start=(c == 0), stop=(c == 11))
            for c in range(12):
                sap, skey = srcs[c]
                P.mm(pB[0:4, :], wgt[:, c, 4:8], sap[:, c % 4, :], ["wgt", skey], ["pB"], start=(c == 0), stop=(c == 11))
            P.ts("dve", i_row[:], pG[0:4, :], bi[:, 0:1], None, ALU.add, None, ["pG", "bi"], ["i_row"])
            P.act(e_row[:], pB[0:4, :], AF.Exp, ["pB", "bfn"], ["e_row"], bias=bfn[:, 0:1], scale=-1.0)
            P.act(sp_row[:], e_row[:], AF.Ln, ["e_row"], ["sp_row"], bias=1.0)
            P.op("dve", (lambda o, d0, d1, ini: (lambda e: e.tensor_tensor_scan(out=o, data0=d0, data1=d1, initial=ini, op0=ALU.add, op1=ALU.add)))(
                Bn[:, 1:513], sp_row[:], zeros[:], carryB[:, 0:1]), ["sp_row", "zeros", "carryB"], ["Bn"])
            P.tt("dve", A_row[:], i_row[:], Bn[:, 1:513], ALU.add, ["i_row", "Bn"], ["A_row"])
            P.copy("dve", Mx[:, 0:1], carryM[:, 0:1], ["carryM"], ["Mx"])
            P.op("dve", (lambda o, d0, d1, ini: (lambda e: e.tensor_tensor_scan(out=o, data0=d0, data1=d1, initial=ini, op0=ALU.max, op1=ALU.max)))(
                Mx[:, 1:513], A_row[:], A_row[:], carryM[:, 0:1]), ["A_row", "carryM", "Mx"], ["Mx"])
            P.copy("dve", carryB[:, 0:1], Bn[:, 512:513], ["Bn"], ["carryB"])
            P.copy("dve", carryM[:, 0:1], Mx[:, 512:513], ["Mx"], ["carryM"])
            P.tt("dve", N_row[:], Bn[:, 1:513], Mx[:, 1:513], ALU.subtract, ["Bn", "Mx"], ["N_row"])
            for c in range(4):
                for k3, (rap, rkey, off) in enumerate(((A_row, "A_row", 0), (Mx, "Mx", 1), (N_row, "N_row", 0))):
                    o0 = c * 12 + k3 * 4
                    P.tr(pA[:, o0:o0 + 4], rap[:, off + c * 128: off + (c + 1) * 128], ident[0:4, 0:4], [rkey, "ident"], ["pA"])
            P.copy("dve", cols[:].rearrange("p c k h -> p (c k h)"), pA[:, 0:48], ["pA"], ["cols"])
            P.act(eN[:], cols[:, :, 2, :], AF.Exp, ["cols"], ["eN"])
            for h in range(4):
                P.mm(pB[:, h * 5:(h + 1) * 5], sel[:, h * 128:(h + 1) * 128], Mx[:, 0:513:128], ["sel", "Mx"], ["pB"])
            P.copy("dve", Mb[:].rearrange("p h c -> p (h c)"), pB[:, 0:20], ["pB"], ["Mb"])
            P.ts("dve", nMb[:], Mb[:], -1.0, None, ALU.mult, None, ["Mb"], ["nMb"])
            P.tt("dve", dec[:], Mb[:, :, 0:4], Mb[:, :, 1:5], ALU.subtract, ["Mb"], ["dec"])
            P.act(dec[:], dec[:], AF.Exp, ["dec"], ["dec"])
            P.tt("dve", spa[:], Mb[:, :, 0:4].rearrange("p h c -> p c h"), cols[:, :, 1, :], ALU.subtract, ["Mb", "cols"], ["spa"])
            P.act(spa[:], spa[:], AF.Exp, ["spa"], ["spa"])
            for h in range(4):
                pz, kz_ = (pA, "pA") if h % 2 == 0 else (pB, "pB")
                P.mm(pz[:, :], sel[:, h * 128:(h + 1) * 128], Mx[:, 1:513], ["sel", "Mx"], [kz_])
                P.tt("dve", Mrow[:, h, :].rearrange("p (c t) -> p c t", t=128), pz[:, :].rearrange("p (c t) -> p c t", t=128),
                     bigtri[:].unsqueeze(1).to_broadcast([128, 4, 128]), ALU.add, [kz_, "bigtri"], ["Mrow"])
            HB = [(pS[0], "pS0"), (pS[1], "pS1"), (pO[0], "pO0"), (pO[1], "pO1")]
            UB = [(pA, "pA"), (pB, "pB")]
            for c in range(4):
                cs = slice(c * 128, (c + 1) * 128)

                def phases(h, c=c, cs=cs):
                    hb_, kH = HB[h]
                    ub_, kU = UB[h // 2]
                    uo = (h % 2) * 256
                    Acol = cols[:, c, 0, h:h + 1]
                    ks = "sm%d" % h
                    smt = sm[h]
                    kCf, kCb = "Cf%d" % h, "Cb%d" % h

                    def p0():
                        P.mm(hb_[:, 0:128], kT[:, h, cs], qT[:, h, cs], ["kT", "qT"], [kH])
                        P.ts("dve", tmpD[h][:], Mrow[:, h, cs], Acol, 0.0, ALU.subtract, ALU.max, ["Mrow", "cols"], ["tmpD%d" % h])
                        P.act(wkc[h][:], Acol, AF.Exp, ["cols", "nMb"], ["wkc%d" % h], bias=nMb[:, h, c + 1:c + 2])

                    def p1():
                        P.act(Dt[h][:], tmpD[h][:], AF.Exp, ["tmpD%d" % h], ["Dt%d" % h], scale=-1.0)
                        P.op("act", (lambda o, i_, sc: (lambda e: e.activation(out=o, in_=i_, func=AF.Copy, scale=sc)))(
                            vw[h][:], vaug[b][:, c, h, :], wkc[h][:, 0:1]), [kva, "wkc%d" % h], ["vw%d" % h])

                    def p2():
                        P.tt("dve", wT[h][:], hb_[:, 0:128], Dt[h][:], ALU.mult, [kH, "Dt%d" % h], ["wT%d" % h])

                    def p3():
                        P.mm(hb_[:, 128:257], wT[h][:], vaug[b][:, c, h, :], ["wT%d" % h, kva], [kH])
                        P.mm(hb_[:, 257:386], qT[:, h, cs], Cb[:, h, :], ["qT", kCb], [kH])
                        P.mm(ub_[:, uo:uo + 129], ktok[:, c, h, :], vw[h][:], ["ktok", "vw%d" % h], [kU])

                    def p4():
                        P.copy("act", intra[h][:], hb_[:, 128:257], [kH], ["intra%d" % h])
                        P.stt("dve", Cf[:, h, :], Cf[:, h, :], dec[:, h, c:c + 1], ub_[:, uo:uo + 129], ALU.mult, ALU.add, [kCf, "dec", kU], [kCf])

                    def p5():
                        P.stt("dve", comb[h][:], hb_[:, 257:386], spa[:, c, h:h + 1], intra[h][:], ALU.mult, ALU.add,
                              [kH, "spa", "intra%d" % h], ["comb%d" % h])
                        P.copy("act", Cb[:, h, :], Cf[:, h, :], [kCf], [kCb])

                    def p6():
                        P.stt("dve", smt[:, 0:1], comb[h][:, 128:129], -1.0, comb[h][:, 128:129], ALU.mult, ALU.max, ["comb%d" % h], [ks])
                        P.stt("dve", smt[:, 1:2], smt[:, 0:1], ML_SCALE, eN[:, c, h:h + 1], ALU.mult, ALU.max, [ks, "eN"], [ks])
                        P.op("dve", (lambda o, i_: (lambda e: e.reciprocal(out=o, in_=i_)))(smt[:, 2:3], smt[:, 1:2]), [ks], [ks])
                        P.ts("dve", hh[h][:], comb[h][:, 0:128], smt[:, 2:3], ML_SCALE, ALU.mult, ALU.mult, ["comb%d" % h, ks], ["hh%d" % h])

                    def p7():
                        P.op("dve", (lambda o, i_: (lambda e: e.bn_stats(out=o, in_=i_)))(smt[:, 4:10], hh[h][:]), ["hh%d" % h], [ks])
                        P.op("dve", (lambda o, i_: (lambda e: e.bn_aggr(out=o, in_=i_)))(smt[:, 10:12], smt[:, 4:10]), [ks], [ks])
                        P.ts("dve", smt[:, 12:13], smt[:, 11:12], EPS, None, ALU.add, None, [ks], [ks])

                    def p8():
                        P.act(smt[:, 13:14], smt[:, 12:13], AF.Ln, [ks], [ks])
                        P.act(smt[:, 14:15], smt[:, 13:14], AF.Exp, [ks], [ks], scale=-0.5)

                    def p9():
                        P.ts("dve", hn[h][:], hh[h][:], smt[:, 10:11], smt[:, 14:15], ALU.subtract, ALU.mult, ["hh%d" % h, ks], ["hn%d" % h])

                    def p10():
                        P.tr(ptb[:, h * 128:(h + 1) * 128], hn[h][:], identb[:], ["hn%d" % h, "identb"], ["ptb"])

                    def p11():
                        P.stt("dve", y1[h][:], ptb[:, h * 128:(h + 1) * 128], mg[:, h:h + 1], scs[:, h, cs], ALU.mult, ALU.add,
                              ["ptb", "mg", "scs"], ["y1%d" % h])
                        P.tt("dve", ymg[b][:, h, cs], y1[h][:], sigz[:, h, cs], ALU.mult, ["y1%d" % h, "sigz"], ["ymg%d_%d" % (b, h)])

                    return [p0, p1, p2, p3, p4, p5, p6, p7, p8, p9, p10, p11]

                plist = [phases(h) for h in range(4)]
                for k_ in range(12):
                    for h in range(4):
                        plist[h][k_]()
            P.load("sp", T["ymT"].rearrange("(c p) t -> p c t", p=128)[:, :, t0:t0 + 512], ymg[b][:], ["ymg%d_%d" % (b, h_) for h_ in range(4)], ["ymT"])
        return P.emit()


def stage3(nc, sems, T):
    with contextlib.ExitStack() as st:
        sb, ps = tens(nc, st)
        P = Prog(nc, sems)
        ident = sb("ident", [128, 128])
        identb = sb("identb", [128, 128], BF16)
        qT = sb("qT", [128, 4, S], BF16)
        kT = sb("kT", [128, 4, S], BF16)
        vaug = sb("vaug", [128, NT, 4, 129], BF16)
        rbb = T["rbb_sb"]
        biasT = T["biasT_sb"]
        lqb = sb("lqb", [128, 256])
        lt = sb("lt", [128, 64])
        lam = sb("lam", [128, 8])
        dag = sb("dag", [128, 1])
        PT = [sb("PT%d" % i, [128, 512], BF16) for i in range(4)]
        tmpn = [sb("tmpn%d" % i, [128, 128]) for i in range(2)]
        t0s = [sb("t0s%d" % i, [128, 128]) for i in range(2)]
        av = [sb("av%d" % i, [128, 128]) for i in range(2)]
        junk = sb("junk3", [128, 128])
        sm = [sb("sm3%d" % i, [128, 8]) for i in range(4)]
        an = [sb("an%d" % i, [128, 128], BF16) for i in range(2)]
        ydg = [sb("ydg%d" % i, [128, 512], BF16) for i in range(2)]
        pS = [ps("pS%d" % i, [128, 512]) for i in range(3)]
        acc = [ps("acc%d" % i, [128, 512]) for i in range(4)]
        ptb = ps("ptb", [128, 1024], BF16)

        P.load("sp", ident[:], T["ident"], [], ["ident"])
        P.copy("dve", identb[:], ident[:], ["ident"], ["identb"])
        P.memset("pool", vaug[:], 1.0, ["vaug"])
        P.load("sp", qT[:], T["featT"][3].rearrange("(h p) t -> p h t", p=128), [], ["qT"])
        P.load("act", kT[:], T["featT"][4].rearrange("(h p) t -> p h t", p=128), [], ["kT"])
        for t in range(NT):
            P.load("sp" if t % 2 == 0 else "act", vaug[:, t, :, 0:128],
                   T["vd_tok"][t * 128:(t + 1) * 128, :].rearrange("p (h e) -> p h e", e=128), ["vaug"], ["vaug"])
        P.load("sp", lqb[:], T["lambda_qk"].partition_broadcast(128), [], ["lqb"])
        P.load("sp", dag[:], T["da_norm_g"].rearrange("(p o) -> p o", o=1), [], ["dag"])
        P.ts("dve", dag[:], dag[:], 1.0 - LAM_INIT, None, ALU.mult, None, ["dag"], ["dag"])
        for i in range(2):
            P.tt("dve", lt[:], lqb[:, (2 * i) * 64:(2 * i + 1) * 64], lqb[:, (2 * i + 1) * 64:(2 * i + 2) * 64], ALU.mult, ["lqb", "lt"], ["lt"])
            P.op("dve", (lambda o, i_: (lambda e: e.reduce_sum(out=o, in_=i_, axis=AX.X)))(lam[:, 4 + i:5 + i], lt[:]), ["lt"], ["lam"])
        P.act(lam[:, 0:2], lam[:, 4:6], AF.Exp, ["lam"], ["lam"])
        P.tt("dve", lam[:, 2:3], lam[:, 0:1], lam[:, 1:2], ALU.subtract, ["lam"], ["lam"])
        P.ts("dve", lam[:, 3:4], lam[:, 2:3], LAM_INIT, -1.0, ALU.add, ALU.mult, ["lam"], ["lam"])
        rS, rP, r2 = Rot(3), Rot(4), Rot(2)
        cvj = ConvD(P, T)
        cvj.k = 8 * (CONV_PER_GROUP[1] + CONV_PER_GROUP[2])
        its = [(h, g, c, j) for h in range(4) for g in range(8) for c in range(2) for j in range(4 * g + 4)]

        def emit_S(it):
            h, g, c, j = it
            prow = slice(c * 64, (c + 1) * 64)
            i_lo = max(j, 4 * g) - 4 * g
            sB = rS.next()
            pb = rP.next()
            kS, kP = "pS%d" % sB, "PT%d" % pb
            P.mm(pS[sB][:, i_lo * 128:512], kT[prow, h, j * 128:(j + 1) * 128], qT[prow, h, g * 512 + i_lo * 128:(g + 1) * 512],
                 ["kT", "qT"], [kS])
            far_lo = None
            for i in range(i_lo, 4):
                dist = 4 * g + i - j
                if dist >= 2:
                    far_lo = i
                    break
                n2 = r2.next()
                P.stt("dve", tmpn[n2][:], pS[sB][:, i * 128:(i + 1) * 128], DA_SCALE, biasT[:, dist, h, :], ALU.mult, ALU.add,
                      [kS, "biasT"], ["tmpn%d" % n2])
                P.act(PT[pb][:, i * 128:(i + 1) * 128], tmpn[n2][:], AF.Exp, ["tmpn%d" % n2], [kP])
            if far_lo is not None:
                P.act(PT[pb][:, far_lo * 128:512], pS[sB][:, far_lo * 128:512], AF.Exp, [kS, "rbb"], [kP],
                      bias=rbb[:, 31 * 4 + h:31 * 4 + h + 1], scale=DA_SCALE)
            return pb, i_lo

        def emit_AV(it, pb, i_lo):
            h, g, c, j = it
            kP = "PT%d" % pb
            if c == 0 and j == 0:
                cvj.emit(CONV_PER_GROUP[3])
                for a_ in range(4):
                    P.memset("dve", acc[a_][:, :], 0.0, ["acc%d" % a_])
            for i in range(i_lo, 4):
                a_ = c * 2 + i // 2
                off = (i % 2) * 256
                P.mm(acc[a_][:, off:off + 129], PT[pb][:, i * 128:(i + 1) * 128], vaug[:, j, h, :], [kP, "vaug"], ["acc%d" % a_],
                     start=False, stop=False, skip=True)
            if c == 1 and j == 4 * g + 3:
                finalize(h, g)

        def finalize(h, g):
            yb = (h * 8 + g) % 2
            for i in range(4):
                n2 = r2.next()
                ks = "sm3%d" % n2
                smt = sm[n2]
                a0, a1 = acc[i // 2], acc[2 + i // 2]
                k0, k1 = "acc%d" % (i // 2), "acc%d" % (2 + i // 2)
                off = (i % 2) * 256
                P.op("dve", (lambda o, i_: (lambda e: e.reciprocal(out=o, in_=i_)))(smt[:, 0:1], a0[:, off + 128:off + 129]), [k0], [ks])
                P.op("dve", (lambda o, i_: (lambda e: e.reciprocal(out=o, in_=i_)))(smt[:, 1:2], a1[:, off + 128:off + 129]), [k1], [ks])
                P.tt("dve", smt[:, 2:3], smt[:, 1:2], lam[:, 3:4], ALU.mult, [ks, "lam"], [ks])
                P.op("act", (lambda o, i_, sc: (lambda e: e.activation(out=o, in_=i_, func=AF.Copy, scale=sc)))(t0s[n2][:], a0[:, off:off + 128], smt[:, 0:1]),
                     [k0, ks], ["t0s%d" % n2])
                P.stt("dve", av[n2][:], a1[:, off:off + 128], smt[:, 2:3], t0s[n2][:], ALU.mult, ALU.add, [k1, ks, "t0s%d" % n2], ["av%d" % n2])
                P.act(junk[:], av[n2][:], AF.Square, ["av%d" % n2], ["junk3", ks], accum_out=smt[:, 3:4])
                P.ts("dve", smt[:, 4:5], smt[:, 3:4], 1.0 / 128, SUBLN_EPS, ALU.mult, ALU.add, [ks], [ks])
                P.act(smt[:, 5:6], smt[:, 4:5], AF.Ln, [ks], [ks])
                P.act(smt[:, 6:7], smt[:, 5:6], AF.Exp, [ks], [ks], scale=-0.5)
                P.ts("dve", an[n2][:], av[n2][:], smt[:, 6:7], None, ALU.mult, None, ["av%d" % n2, ks], ["an%d" % n2])
                P.tr(ptb[:, n2 * 512:n2 * 512 + 128], an[n2][:], identb[:], ["an%d" % n2, "identb"], ["ptb"])
                P.ts("dve", ydg[yb][:, i * 128:(i + 1) * 128], ptb[:, n2 * 512:n2 * 512 + 128], dag[:, 0:1], None, ALU.mult, None,
                     ["ptb", "dag"], ["ydg%d" % yb])
            P.load("act", T["ydT"][h * 128:(h + 1) * 128, g * 512:(g + 1) * 512], ydg[yb][:], ["ydg%d" % yb], ["ydT"])

        pend = []
        for it in its:
            pend.append((it,) + emit_S(it))
            if len(pend) > 2:
                emit_AV(*pend.pop(0))
        while pend:
            emit_AV(*pend.pop(0))
        return P.emit()


def stage4(nc, sems, T):
    with contextlib.ExitStack() as st:
        sb, ps = tens(nc, st)
        P = Prog(nc, sems)
        ident = sb("ident", [128, 128])
        wo = sb("wo", [128, 8, D], BF16)
        yT = sb("yT", [128, 8, S], BF16)
        g2b = sb("g2b", [128, D])
        wr = sb("wr", [128, 8, 36])
        brb = sb("brb", [128, 36])
        xt = [sb("xt%d" % i, [128, D]) for i in range(2)]
        x2 = [sb("x2%d" % i, [128, D]) for i in range(2)]
        junk = sb("junk4", [128, D], BF16)
        stat = [sb("stat4%d" % i, [128, 4]) for i in range(2)]
        h2 = [sb("h2%d" % i, [128, D]) for i in range(2)]
        h2T = [sb("h2T%d" % i, [128, 8, 128]) for i in range(2)]
        h2bf = [sb("h2bf%d" % i, [128, D], BF16) for i in range(2)]
        lstr = sb("lstr", [128, 128])
        ones = sb("ones", [128, 128])
        thr = sb("thr", [128, 16 + NSUP])
        E1 = sb("E1", [128, NT, 4, 8])
        E2 = sb("E2", [128, NT, 4, 8])
        Es = sb("Es", [128, NT * 32])
        within = sb("within", [128, NT, 32])
        csb = sb("csb", [128, NT, 32])
        incl = sb("incl", [128, NT, 32])
        zer = sb("zer", [128, NT])
        cmpb = sb("cmpb", [128, NSUP * 32])
        ntl = sb("ntl", [128, 32])
        inct = sb("inct", [128, 32])
        offb = sb("offb", [128, 32])
        offe = sb("offe", [128, 32])
        Rr = sb("Rr", [128, NT, 32])
        posf = sb("posf", [128, NT, 2])
        tef = sb("tef", [128, 64])
        pidx = sb("pidx", [128, 1])
        h2Tb = [sb("h2Tb%d" % i, [128, 8, 128], BF16) for i in range(2)]
        lgt = sb("lgt", [128, NT, 36])
        mxg = sb("mxg", [128, NT])
        ohg = sb("ohg", [128, NT, 4])
        eg = sb("eg", [128, NT, 4])
        sg = sb("sg", [128, NT])
        tmp4 = sb("tmp4", [128, NT, 4, 8])
        les = sb("les", [128, NT, 8])
        le2 = sb("le2", [128, NT, 8])
        m1 = sb("m1", [128, NT])
        m2 = sb("m2", [128, NT])
        oh1 = sb("oh1", [128, NT, 8])
        oh2 = sb("oh2", [128, NT, 8])
        w1 = sb("w1", [128, NT])
        w2 = sb("w2", [128, NT])
        gf = sb("gf", [128, NT, 8])
        gts = sb("gts", [128, NT, 4, 8])
        pO = [ps("pO%d" % i, [128, 512]) for i in range(4)]
        pT = [ps("pT%d" % i, [128, 512]) for i in range(2)]
        pR = ps("pR", [128, 512])

        P.load("sp", ident[:], T["ident"], [], ["ident"])
        zt = sb("zt", [128, 3, D], BF16)
        P.memset("pool", zt[:], 0.0, ["zt"])
        xs_v = T["xs"].rearrange("(n p) d -> p n d", p=128)
        for i in range(42):
            P.load("pool", xs_v[:, i * 3:(i + 1) * 3, :], zt[:], ["zt"], ["xs_zero%d" % i])
        P.load("pool", wo[:], T["w_out"].rearrange("(c p) n -> p c n", p=128), [], ["wo"])
        P.load("sp", yT[:, 0:4, :], T["ymT"].rearrange("(c p) t -> p c t", p=128), [], ["yT"])
        P.load("act", yT[:, 4:8, :], T["ydT"].rearrange("(c p) t -> p c t", p=128), [], ["yT"])
        P.load("sp", g2b[:], T["norm2_g"].partition_broadcast(128), [], ["g2b"])
        P.load("sp", wr[:], T["w_r"].rearrange("(c p) n -> p c n", p=128), [], ["wr"])
        P.load("sp", brb[:], T["b_r"].partition_broadcast(128), [], ["brb"])
        rO = Rot(2)
        for t in range(NT):
            b = t % 2
            ts_ = slice(t * 128, (t + 1) * 128)
            P.load("sp", xt[b][:], T["x"][ts_, :], [], ["xt%d" % b])
            for half in range(2):
                pb = rO.next() * 2 + half
                for kc in range(8):
                    P.mm(pO[pb][:, :], yT[:, kc, ts_], wo[:, kc, half * 512:(half + 1) * 512], ["yT", "wo"], ["pO%d" % pb], start=(kc == 0), stop=(kc == 7))
                P.tt("dve", x2[b][:, half * 512:(half + 1) * 512], pO[pb][:, :], xt[b][:, half * 512:(half + 1) * 512], ALU.add,
                     ["pO%d" % pb, "xt%d" % b], ["x2%d" % b])
            P.load("act", T["x2"][ts_, :], x2[b][:], ["x2%d" % b], ["x2d"])
            sk = "stat4%d" % b
            P.act(junk[:], x2[b][:], AF.Square, ["x2%d" % b], ["junk4", sk], accum_out=stat[b][:, 0:1])
            P.ts("dve", stat[b][:, 1:2], stat[b][:, 0:1], 1.0 / D, EPS, ALU.mult, ALU.add, [sk], [sk])
            P.act(stat[b][:, 2:3], stat[b][:, 1:2], AF.Ln, [sk], [sk])
            P.act(stat[b][:, 3:4], stat[b][:, 2:3], AF.Exp, [sk], [sk], scale=-0.5)
            P.stt("dve", h2[b][:], x2[b][:], stat[b][:, 3:4], g2b[:], ALU.mult, ALU.mult, ["x2%d" % b, sk, "g2b"], ["h2%d" % b])
            P.copy("pool", h2bf[b][:], h2[b][:], ["h2%d" % b], ["h2bf%d" % b])
            P.load("act", T["h2b"][ts_, :], h2bf[b][:], ["h2bf%d" % b], ["h2bd"])
            for kc in range(8):
                pz = pT[kc // 4]
                P.tr(pz[:, (kc % 4) * 128:(kc % 4 + 1) * 128], h2[b][:, kc * 128:(kc + 1) * 128], ident[:], ["h2%d" % b, "ident"], ["pT%d" % (kc // 4)])
            for hf in range(2):
                P.copy("act", h2T[b][:, hf * 4:(hf + 1) * 4, :].rearrange("p k t -> p (k t)"), pT[hf][:, :], ["pT%d" % hf], ["h2T%d" % b])
                P.copy("dve", h2Tb[b][:, hf * 4:(hf + 1) * 4, :].rearrange("p k t -> p (k t)"), pT[hf][:, :], ["pT%d" % hf], ["h2Tb%d" % b])
            P.load("sp", T["h2T"].rearrange("(c p) t -> p c t", p=128)[:, :, ts_], h2Tb[b][:], ["h2Tb%d" % b], ["h2Td"])
            for kc in range(8):
                P.mm(pR[:, 0:36], h2T[b][:, kc, :], wr[:, kc, :], ["h2T%d" % b, "wr"], ["pR"], start=(kc == 0), stop=(kc == 7))
            P.tt("dve", lgt[:, t, :], pR[:, 0:36], brb[:], ALU.add, ["pR", "brb"], ["lgt"])
        lg = lgt[:, :, 0:4]
        le = lgt[:, :, 4:36].rearrange("p t (g e) -> p t g e", e=8)
        red = lambda o, i_, op: (lambda e: e.tensor_reduce(out=o, in_=i_, axis=AX.X, op=op))
        P.op("dve", red(mxg[:], lg, ALU.max), ["lgt"], ["mxg"])
        P.tt("dve", ohg[:], lg, mxg[:].unsqueeze(2).to_broadcast([128, NT, 4]), ALU.is_ge, ["lgt", "mxg"], ["ohg"])
        P.tt("dve", eg[:], lg, mxg[:].unsqueeze(2).to_broadcast([128, NT, 4]), ALU.subtract, ["lgt", "mxg"], ["eg"])
        P.act(eg[:], eg[:], AF.Exp, ["eg"], ["eg"])
        P.op("dve", red(sg[:], eg[:], ALU.add), ["eg"], ["sg"])
        P.op("dve", (lambda o, i_: (lambda e: e.reciprocal(out=o, in_=i_)))(sg[:], sg[:]), ["sg"], ["sg"])
        P.tt("dve", tmp4[:], le, ohg[:].unsqueeze(3).to_broadcast([128, NT, 4, 8]), ALU.mult, ["lgt", "ohg"], ["tmp4"])
        P.op("dve", red(les[:], tmp4[:].rearrange("p t g e -> p t e g"), ALU.add), ["tmp4"], ["les"])
        P.op("dve", red(m1[:], les[:], ALU.max), ["les"], ["m1"])
        P.tt("dve", oh1[:], les[:], m1[:].unsqueeze(2).to_broadcast([128, NT, 8]), ALU.is_ge, ["les", "m1"], ["oh1"])
        P.stt("dve", le2[:], oh1[:], -1e30, les[:], ALU.mult, ALU.add, ["oh1", "les"], ["le2"])
        P.op("dve", red(m2[:], le2[:], ALU.max), ["le2"], ["m2"])
        P.tt("dve", oh2[:], le2[:], m2[:].unsqueeze(2).to_broadcast([128, NT, 8]), ALU.is_ge, ["le2", "m2"], ["oh2"])
        P.tt("dve", w2[:], m2[:], m1[:], ALU.subtract, ["m1", "m2"], ["w2"])
        P.act(w2[:], w2[:], AF.Exp, ["w2"], ["w2"])
        P.ts("dve", w1[:], w2[:], 1.0, None, ALU.add, None, ["w2"], ["w1"])
        P.op("dve", (lambda o, i_: (lambda e: e.reciprocal(out=o, in_=i_)))(w1[:], w1[:]), ["w1"], ["w1"])
        P.tt("dve", w2[:], w2[:], w1[:], ALU.mult, ["w1", "w2"], ["w2"])
        P.tt("dve", w1[:], w1[:], sg[:], ALU.mult, ["w1", "sg"], ["w1"])
        P.tt("dve", w2[:], w2[:], sg[:], ALU.mult, ["w2", "sg"], ["w2"])
        wk_g, pos_i, te_i = T["wk_g"], T["pos_i"], T["te_i"]
        P.copy("dve", wk_g[:, :, 0], w1[:], ["w1"], ["wk_g"])
        P.copy("dve", wk_g[:, :, 1], w2[:], ["w2", "wk_g"], ["wk_g"])
        P.load("sp", lstr[:], T["lstrict"], [], ["lstr"])
        P.load("sp", thr[:], T["thr"].partition_broadcast(128), [], ["thr"])
        P.memset("pool", ones[:], 1.0, ["ones"])
        P.memset("pool", zer[:], 0.0, ["zer"])
        bc3 = lambda a: a.unsqueeze(3).to_broadcast([128, NT, 4, 8])
        bc2 = lambda a: a.unsqueeze(2).to_broadcast([128, NT, 4, 8])
        P.tt("dve", E1[:], bc3(ohg[:]), bc2(oh1[:]), ALU.mult, ["ohg", "oh1"], ["E1"])
        P.tt("dve", E2[:], bc3(ohg[:]), bc2(oh2[:]), ALU.mult, ["ohg", "oh2"], ["E2"])
        P.tt("dve", Es[:], E1[:].rearrange("p t g e -> p (t g e)"), E2[:].rearrange("p t g e -> p (t g e)"), ALU.add, ["E1", "E2"], ["Es"])
        for hf in range(2):
            P.mm(pO[hf][:, :], lstr[:], Es[:, hf * 512:(hf + 1) * 512], ["lstr", "Es"], ["pO%d" % hf])
            P.copy("act", within[:].rearrange("p t e -> p (t e)")[:, hf * 512:(hf + 1) * 512], pO[hf][:, :], ["pO%d" % hf], ["within"])
            P.mm(pO[2 + hf][:, :], ones[:], Es[:, hf * 512:(hf + 1) * 512], ["ones", "Es"], ["pO%d" % (2 + hf)])
            P.copy("dve", csb[:].rearrange("p t e -> p (t e)")[:, hf * 512:(hf + 1) * 512], pO[2 + hf][:, :], ["pO%d" % (2 + hf)], ["csb"])
        for e_ in range(32):
            P.op("dve", (lambda o, d0, d1: (lambda e: e.tensor_tensor_scan(out=o, data0=d0, data1=d1, initial=0.0, op0=ALU.add, op1=ALU.add)))(
                incl[:, :, e_], csb[:, :, e_], zer[:]), ["csb", "zer", "incl"], ["incl"])
        P.tt("dve", cmpb[:, 0:512].rearrange("p (e j) -> p e j", j=16), incl[:, NT - 1, :].unsqueeze(2).to_broadcast([128, 32, 16]),
             thr[:, 0:16].unsqueeze(1).to_broadcast([128, 32, 16]), ALU.is_gt, ["incl", "thr"], ["cmpb"])
        P.op("dve", red(ntl[:], cmpb[:, 0:512].rearrange("p (e j) -> p e j", j=16), ALU.add), ["cmpb"], ["ntl"])
        P.op("dve", (lambda o, d0, d1: (lambda e: e.tensor_tensor_scan(out=o, data0=d0, data1=d1, initial=0.0, op0=ALU.add, op1=ALU.add)))(
            inct[:], ntl[:], zer[:, 0:32]), ["ntl", "zer"], ["inct"])
        P.ts("dve", offe[:], inct[:], float(SUP), None, ALU.mult, None, ["inct"], ["offe"])
        P.tt("dve", offb[:], inct[:], ntl[:], ALU.subtract, ["inct", "ntl"], ["offb"])
        P.ts("dve", offb[:], offb[:], float(SUP), None, ALU.mult, None, ["offb"], ["offb"])
        P.tt("dve", Rr[:], incl[:], csb[:], ALU.subtract, ["incl", "csb"], ["Rr"])
        P.tt("dve", Rr[:], Rr[:], within[:], ALU.add, ["Rr", "within"], ["Rr"])
        P.tt("dve", Rr[:], Rr[:], offb[:].unsqueeze(1).to_broadcast([128, NT, 32]), ALU.add, ["Rr", "offb"], ["Rr"])
        for k_, Ek in enumerate((E1, E2)):
            kn = "E%d" % (k_ + 1)
            P.tt("dve", Ek[:].rearrange("p t g e -> p t (g e)"), Ek[:].rearrange("p t g e -> p t (g e)"), Rr[:], ALU.mult, [kn, "Rr"], [kn])
            P.op("dve", red(posf[:, :, k_], Ek[:].rearrange("p t g e -> p t (g e)"), ALU.add), [kn, "posf"], ["posf"])
        P.copy("dve", pos_i[:], posf[:], ["posf"], ["pos_i"])
        P.tt("dve", cmpb[:, 0:NSUP * 32].rearrange("p (j e) -> p j e", e=32), offe[:].unsqueeze(1).to_broadcast([128, NSUP, 32]),
             thr[:, 16:16 + NSUP].unsqueeze(2).to_broadcast([128, NSUP, 32]), ALU.is_le, ["offe", "thr", "cmpb"], ["cmpb"])
        P.memset("pool", tef[:], 0.0, ["tef"])
        P.op("dve", red(tef[:, 0:NSUP], cmpb[:, 0:NSUP * 32].rearrange("p (j e) -> p j e", e=32), ALU.add), ["cmpb", "tef"], ["tef"])
        P.ts("dve", tef[:], tef[:], 31.0, None, ALU.min, None, ["tef"], ["tef"])
        P.load("sp", pidx[:], T["pidx"], [], ["pidx"])
        P.ts("dve", tef[:], tef[:], 128.0, pidx[:, 0:1], ALU.mult, ALU.add, ["tef", "pidx"], ["tef"])
        P.copy("dve", te_i[:], tef[:], ["tef"], ["te_i"])
        P.tt("dve", oh1[:], oh1[:], w1[:].unsqueeze(2).to_broadcast([128, NT, 8]), ALU.mult, ["oh1", "w1"], ["oh1"])
        P.tt("dve", oh2[:], oh2[:], w2[:].unsqueeze(2).to_broadcast([128, NT, 8]), ALU.mult, ["oh2", "w2"], ["oh2"])
        P.tt("dve", gf[:], oh1[:], oh2[:], ALU.add, ["oh1", "oh2"], ["gf"])
        P.tt("dve", gts[:], ohg[:].unsqueeze(3).to_broadcast([128, NT, 4, 8]), gf[:].unsqueeze(2).to_broadcast([128, NT, 4, 8]), ALU.mult,
             ["ohg", "gf"], ["gts"])
        P.load("sp", T["gates"], gts[:].rearrange("p t g e -> p (t g e)"), ["gts"], ["gatesd"])
        return P.emit()


def stage5(nc, sems, T):
    IOA = bass.IndirectOffsetOnAxis
    with contextlib.ExitStack() as st:
        sb, ps = tens(nc, st)
        P = Prog(nc, sems)
        pos_i, te_i, wk_g = T["pos_i"], T["te_i"], T["wk_g"]
        identb = sb("identb", [128, 128], BF16)
        identf = sb("identf", [128, 128])
        gfb = sb("gfb", [128, D])
        hrow = [sb("hrow%d" % i, [128, D], BF16) for i in range(3)]
        wall = [sb("wall%d" % i, [128, 3 * 4096], BF16) for i in range(2)]
        xs = [sb("xs%d" % i, [128, D], BF16) for i in range(3)]
        XT = [sb("XT%d" % i, [128, 8, 128], BF16) for i in range(2)]
        sgl = [sb("sgl%d" % i, [128, DFF], BF16) for i in range(2)]
        hid = [sb("hid%d" % i, [128, DFF], BF16) for i in range(2)]
        hidT = [sb("hidT%d" % i, [128, 4, 128], BF16) for i in range(2)]
        ysb = [sb("ysb%d" % i, [128, D], BF16) for i in range(3)]
        yg = [sb("yg%d" % i, [128, 2, D], BF16) for i in range(2)]
        x2 = [sb("x2%d" % i, [128, D]) for i in range(2)]
        junk = sb("junk5", [128, D], BF16)
        stat = [sb("stat5%d" % i, [128, 4]) for i in range(2)]
        ot = [sb("ot%d" % i, [128, D]) for i in range(2)]
        ptx = [ps("ptx%d" % i, [128, D], BF16) for i in range(2)]
        pg = [ps("pg%d" % i, [128, 512]) for i in range(2)]
        pu = [ps("pu%d" % i, [128, 512]) for i in range(2)]
        pth = [ps("pth%d" % i, [128, D], BF16) for i in range(2)]

        P.load("sp", identf[:], T["ident"], [], ["identf"])
        P.copy("dve", identb[:], identf[:], ["identf"], ["identb"])
        P.load("sp", gfb[:], T["normf_g"].partition_broadcast(128), [], ["gfb"])
        zkeys = []
        sckeys = []
        for t in range(NT):
            hb = t % 3
            P.load("sp", hrow[hb][:], T["h2b"][t * 128:(t + 1) * 128, :], [], ["hrow%d" % hb])
            for k_ in range(2):
                key = "xs_sc%d_%d" % (t, k_)
                sckeys.append(key)
                P.dma("pool", (lambda o, off, i_: (lambda e: e.indirect_dma_start(out=o, out_offset=off, in_=i_, in_offset=None)))(
                    T["xs"], IOA(ap=pos_i[:, t, k_:k_ + 1], axis=0), hrow[hb][:]), ["hrow%d" % hb, "pos_i"] + zkeys, [key])
        ykeys = []
        rx, r2 = Rot(3), Rot(2)
        for j in range(NSUP):
            wb = j % 2
            P.dma("pool", (lambda o, i_, off: (lambda e: e.indirect_dma_start(out=o, out_offset=None, in_=i_, in_offset=off)))(
                wall[wb][:, :], T["wall"], IOA(ap=te_i[:, j:j + 1], axis=0)), ["te_i"], ["wall%d" % wb])
            wg_v = wall[wb][:, 0:4096].rearrange("p (c f) -> p c f", f=512)
            wu_v = wall[wb][:, 4096:8192].rearrange("p (c f) -> p c f", f=512)
            wd_v = wall[wb][:, 8192:12288].rearrange("p (c d) -> p c d", d=1024)
            for s_ in range(SUP // 128):
                row0 = j * SUP + s_ * 128
                xb = rx.next()
                b2 = r2.next()
                P.load("act", xs[xb][:], T["xs"][row0:row0 + 128, :], sckeys + zkeys, ["xs%d" % xb])
                for kc in range(8):
                    P.tr(ptx[b2][:, kc * 128:(kc + 1) * 128], xs[xb][:, kc * 128:(kc + 1) * 128], identb[:], ["xs%d" % xb, "identb"], ["ptx%d" % b2])
                P.copy("dve" if b2 == 0 else "act", XT[b2][:].rearrange("p k t -> p (k t)"), ptx[b2][:, :], ["ptx%d" % b2], ["XT%d" % b2])
                for kc in range(8):
                    P.mm(pg[b2][:, :], XT[b2][:, kc, :], wg_v[:, kc, :], ["XT%d" % b2, "wall%d" % wb], ["pg%d" % b2], start=(kc == 0), stop=(kc == 7))
                for kc in range(8):
                    P.mm(pu[b2][:, :], XT[b2][:, kc, :], wu_v[:, kc, :], ["XT%d" % b2, "wall%d" % wb], ["pu%d" % b2], start=(kc == 0), stop=(kc == 7))
                P.act(sgl[b2][:], pg[b2][:, :], AF.Silu, ["pg%d" % b2], ["sgl%d" % b2])
                P.tt("dve", hid[b2][:], sgl[b2][:], pu[b2][:, :], ALU.mult, ["sgl%d" % b2, "pu%d" % b2], ["hid%d" % b2])
                for fc in range(4):
                    P.tr(pth[b2][:, fc * 128:(fc + 1) * 128], hid[b2][:, fc * 128:(fc + 1) * 128], identb[:], ["hid%d" % b2, "identb"], ["pth%d" % b2])
                P.copy("act" if b2 == 0 else "dve", hidT[b2][:].rearrange("p k t -> p (k t)"), pth[b2][:, 0:512], ["pth%d" % b2], ["hidT%d" % b2])
                yb = rx.i % 3
                for half, (pz, kz) in enumerate(((pg[b2], "pg%d" % b2), (pu[b2], "pu%d" % b2))):
                    for fc in range(4):
                        P.mm(pz[:, :], hidT[b2][:, fc, :], wd_v[:, fc, half * 512:(half + 1) * 512], ["hidT%d" % b2, "wall%d" % wb], [kz],
                             start=(fc == 0), stop=(fc == 3))
                    P.copy("act" if half == 0 else "dve", ysb[xb][:, half * 512:(half + 1) * 512], pz[:, :], [kz], ["ysb%d" % xb])
                yk = "ys%d" % (j * 2 + s_)
                ykeys.append(yk)
                P.load("sp", T["ys"][row0:row0 + 128, :], ysb[xb][:], ["ysb%d" % xb], [yk])
        for t in range(NT):
            b = t % 2
            ts_ = slice(t * 128, (t + 1) * 128)
            sk = "stat5%d" % b
            for k_ in range(2):
                P.dma("pool", (lambda o, i_, off: (lambda e: e.indirect_dma_start(out=o, out_offset=None, in_=i_, in_offset=off)))(
                    yg[b][:, k_, :], T["ys"], IOA(ap=pos_i[:, t, k_:k_ + 1], axis=0)), ykeys + ["pos_i"], ["yg%d_%d" % (b, k_)])
            P.load("sp", x2[b][:], T["x2"][ts_, :], [], ["x2%d" % b])
            P.stt("dve", x2[b][:], yg[b][:, 0, :], wk_g[:, t, 0:1], x2[b][:], ALU.mult, ALU.add, ["yg%d_0" % b, "wk_g", "x2%d" % b], ["x2%d" % b])
            P.stt("dve", x2[b][:], yg[b][:, 1, :], wk_g[:, t, 1:2], x2[b][:], ALU.mult, ALU.add, ["yg%d_1" % b, "wk_g", "x2%d" % b], ["x2%d" % b])
            P.act(junk[:], x2[b][:], AF.Square, ["x2%d" % b], ["junk5", sk], accum_out=stat[b][:, 0:1])
            P.ts("dve", stat[b][:, 1:2], stat[b][:, 0:1], 1.0 / D, EPS, ALU.mult, ALU.add, [sk], [sk])
            P.act(stat[b][:, 2:3], stat[b][:, 1:2], AF.Ln, [sk], [sk])
            P.act(stat[b][:, 3:4], stat[b][:, 2:3], AF.Exp, [sk], [sk], scale=-0.5)
            P.stt("dve", ot[b][:], x2[b][:], stat[b][:, 3:4], gfb[:], ALU.mult, ALU.mult, ["x2%d" % b, sk, "gfb"], ["ot%d" % b])
            P.load("act", T["out"][ts_, :], ot[b][:], ["ot%d" % b], ["outd"])
        return P.emit()


def _rel_bucket_np(n):
    n = np.maximum(n, 0)
    max_exact = 16
    nf = np.maximum(n, 1).astype(np.float32)
    large = max_exact + (np.log(nf / np.float32(max_exact)) / np.float32(math.log(128 / max_exact)) * np.float32(16)).astype(np.int32)
    large = np.minimum(large, 31)
    return np.where(n < max_exact, n, large)


def _constants():
    ident = np.eye(128, dtype=np.float32)
    s_ = np.arange(128)[:, None]
    t_ = np.arange(128)[None, :]
    tri = (s_ <= t_).astype(np.float32)
    sel = np.zeros((4, 4, 128), np.float32)
    for h in range(4):
        sel[h, h, :] = 1.0
    oh = np.zeros((128, 2, 33, 128), np.float32)
    for kind in range(2):
        n = (t_ - s_) + 128 * kind
        bk = _rel_bucket_np(n)
        valid = n >= 0
        for b in range(32):
            oh[:, kind, b, :] = ((bk == b) & valid).astype(np.float32)
        oh[:, kind, 32, :] = (~valid).astype(np.float32)
    lstrict = (s_ < t_).astype(np.float32)
    thr = np.concatenate([np.arange(16) * SUP, np.arange(NSUP) * SUP]).astype(np.float32)
    pidx = np.arange(128, dtype=np.float32).reshape(128, 1)
    return dict(ident=ident, tri=tri, sel=sel.reshape(4, 512), oh=oh.reshape(128, -1), lstrict=lstrict, thr=thr, pidx=pidx)


_CACHE = {}


def kernel(x, w_in, conv_w, conv_b, w_mq, w_mk, w_mgate, b_mgate, m_norm_g, m_skip, lambda_qk, da_norm_g, rel_bias, w_out,
           norm1_g, norm2_g, w_rg, b_rg, w_re, b_re, w_eg, w_eu, w_ed, normf_g):
    f = lambda a: np.ascontiguousarray(np.asarray(a, dtype=np.float32))
    if "nc" not in _CACHE:
        _CACHE["nc"], _CACHE["stats"] = build_program()
    nc = _CACHE["nc"]
    shared = dict(
        w_in=f(w_in)[0], conv_w=f(conv_w)[0], conv_b=f(conv_b)[0], w_mq=f(w_mq)[0], w_mk=f(w_mk)[0], w_mgate=f(w_mgate)[0],
        b_mgate=f(b_mgate)[0], m_norm_g=f(m_norm_g)[0], m_skip=f(m_skip)[0], lambda_qk=f(lambda_qk)[0].reshape(256),
        da_norm_g=f(da_norm_g)[0], rel_bias=f(rel_bias).reshape(128), w_out=f(w_out)[0], norm1_g=f(norm1_g)[0], norm2_g=f(norm2_g)[0],
        w_r=np.ascontiguousarray(np.concatenate([f(w_rg)[0], f(w_re)[0].reshape(D, 32)], axis=1)),
        b_r=np.ascontiguousarray(np.concatenate([f(b_rg)[0], f(b_re)[0].reshape(32)])),
        w_eg=f(w_eg)[0], w_eu=f(w_eu)[0], w_ed=f(w_ed)[0], normf_g=f(normf_g),
    )
    shared.update(_constants())
    xs = f(x)
    in_maps = []
    for b in range(8):
        m = dict(shared)
        m["x"] = xs[b]
        in_maps.append(m)
    res = run_bass_kernel_spmd(nc, in_maps, core_ids=list(range(8)))
    _CACHE["res"] = res
    return np.stack([np.asarray(r["out"], dtype=np.float32) for r in res.results], axis=0)
```

```python
import math
import contextlib
import numpy as np
import concourse.bass as bass
import concourse.mybir as mybir
from concourse.bass_utils import run_bass_kernel_spmd

F32 = mybir.dt.float32
BF16 = mybir.dt.bfloat16
AF = mybir.ActivationFunctionType
ALU = mybir.AluOpType
AX = mybir.AxisListType

S = 4096
D = 1024
NT = 32
EPS = 1e-6
SUBLN_EPS = 1e-5
N_EXP = 32
DFF = 512
LAM_INIT = 0.8 - 0.6 * math.exp(-0.3 * 0)
ML_SCALE = 128.0 ** -0.5
DA_SCALE = 64.0 ** -0.5
NEG = -30000.0
SUP = 256
NSUP = 63
NSLOT = NSUP * SUP
I32 = mybir.dt.int32

COMPUTE = ("pe", "act", "dve", "pool")
QUEUES = ("sp", "act", "pool")
N_DMA_SEMS = 8
DEBUG = False
CONV_PER_GROUP = {1: 3, 2: 4, 3: 2}
STAGES = (1, 2, 3, 4, 5)


class Sems:
    def __init__(self, nc, st):
        self.esem = {e: st.enter_context(nc.semaphore("s_" + e)) for e in COMPUTE}
        self.dsem = {(q, s): st.enter_context(nc.semaphore("d_%s_%d" % (q, s))) for q in QUEUES for s in range(N_DMA_SEMS)}
        self.cnt = {e: 0 for e in COMPUTE}
        self.dcnt = {k: 0 for k in self.dsem}
        self.rr = {q: 0 for q in QUEUES}


class Op:
    __slots__ = ("eng", "fn", "deps", "is_dma", "signal", "val", "sem", "slot", "prev")

    def __init__(self, eng, fn, is_dma):
        self.eng, self.fn, self.is_dma = eng, fn, is_dma
        self.deps = []
        self.signal = False
        self.val = None
        self.sem = None
        self.slot = None
        self.prev = None


class Prog:
    def __init__(self, nc, sems):
        self.nc = nc
        self.sems = sems
        self.ops = []
        self.last_writer = {}
        self.readers = {}
        self.slot_last = {}

    def _add(self, op, reads, writes):
        pr = [r for r in reads if r in PSUM_KEYS]
        if pr:
            reads = [r for r in reads if r not in PSUM_KEYS]
            writes = list(writes) + [r for r in pr if r not in writes]
        deps = []
        for r in reads:
            w = self.last_writer.get(r)
            if w is not None:
                deps.append(w)
        for w in writes:
            lw = self.last_writer.get(w)
            if lw is not None:
                deps.append(lw)
            deps.extend(self.readers.get(w, ()))
        seen = set()
        for d in deps:
            if id(d) not in seen and d is not op:
                seen.add(id(d))
                op.deps.append(d)
        for r in reads:
            self.readers.setdefault(r, []).append(op)
        for w in writes:
            self.last_writer[w] = op
            self.readers[w] = []
        self.ops.append(op)
        return op

    def op(self, eng, fn, reads=(), writes=()):
        return self._add(Op(eng, fn, False), reads, writes)

    def dma(self, queue, fn, reads=(), writes=()):
        op = Op(queue, fn, True)
        s = self.sems
        op.slot = (queue, s.rr[queue] % N_DMA_SEMS)
        s.rr[queue] += 1
        op.prev = self.slot_last.get(op.slot)
        self.slot_last[op.slot] = op
        return self._add(op, reads, writes)

    def mm(self, out, lhsT, rhs, r, w, start=True, stop=True, skip=False):
        if skip:
            return self.op("pe", lambda e: e.matmul(out, lhsT=lhsT, rhs=rhs, start=start, stop=stop, skip_group_check=True), r, w)
        return self.op("pe", lambda e: e.matmul(out, lhsT=lhsT, rhs=rhs, start=start, stop=stop), r, w)

    def tr(self, out, in_, ident, r, w):
        return self.op("pe", lambda e: e.transpose(out=out, in_=in_, identity=ident), r, w)

    def act(self, out, in_, func, r, w, bias=None, scale=None, accum_out=None):
        kw = {}
        if bias is not None:
            kw["bias"] = bias
        if scale is not None:
            kw["scale"] = scale
        if accum_out is not None:
            kw["accum_out"] = accum_out
        return self.op("act", lambda e: e.activation(out=out, in_=in_, func=func, **kw), r, w)

    def copy(self, eng, out, in_, r, w):
        if eng == "act":
            return self.op("act", lambda e: e.copy(out=out, in_=in_), r, w)
        return self.op(eng, lambda e: e.tensor_copy(out=out, in_=in_), r, w)

    def tt(self, eng, out, in0, in1, op, r, w):
        return self.op(eng, lambda e: e.tensor_tensor(out=out, in0=in0, in1=in1, op=op), r, w)

    def ts(self, eng, out, in0, s1, s2, op0, op1, r, w):
        if s2 is None:
            return self.op(eng, lambda e: e.tensor_scalar(out=out, in0=in0, scalar1=s1, scalar2=None, op0=op0), r, w)
        return self.op(eng, lambda e: e.tensor_scalar(out=out, in0=in0, scalar1=s1, scalar2=s2, op0=op0, op1=op1), r, w)

    def stt(self, eng, out, in0, scalar, in1, op0, op1, r, w):
        eng = "dve"
        return self.op(eng, lambda e: e.scalar_tensor_tensor(out=out, in0=in0, scalar=scalar, in1=in1, op0=op0, op1=op1), r, w)

    def memset(self, eng, ap, val, w):
        return self.op(eng, lambda e: e.memset(ap, val), (), w)

    def load(self, q, out, in_, r, w):
        return self.dma(q, lambda e: e.dma_start(out=out, in_=in_), r, w)

    def emit(self):
        nc, s, ops = self.nc, self.sems, self.ops

        def same_skip(d, o):
            return (not d.is_dma) and (not o.is_dma) and d.eng == o.eng and d.eng == "pe"

        for o in ops:
            for d in o.deps:
                if d.is_dma or same_skip(d, o):
                    continue
                d.signal = True
        for o in ops:
            if o.is_dma:
                s.dcnt[o.slot] += 16
                o.val = s.dcnt[o.slot]
                o.sem = s.dsem[o.slot]
            else:
                o.sem = s.esem[o.eng]
                if o.signal:
                    s.cnt[o.eng] += 1
                    o.val = s.cnt[o.eng]
        by_eng = {e: [] for e in ("pe", "act", "dve", "pool", "sp")}
        for o in ops:
            by_eng[o.eng].append(o)
        final = dict(s.dcnt)

        def run(engname, e):
            waited = {}

            def wait(sem, val):
                if waited.get(id(sem), 0) >= val:
                    return
                waited[id(sem)] = val
                e.wait_ge(sem, val)

            for o in by_eng[engname]:
                for d in o.deps:
                    if same_skip(d, o):
                        continue
                    wait(d.sem, d.val)
                if o.is_dma and o.prev is not None:
                    wait(o.prev.sem, o.prev.val)
                ins = o.fn(e)
                if o.is_dma:
                    ins.then_inc(o.sem, 16)
                elif o.signal:
                    ins.then_inc(o.sem, 1)
            if engname == "sp":
                for k, v in final.items():
                    if v > 0:
                        wait(s.dsem[k], v)

        with nc.Block() as block:
            block.sync(lambda e: run("sp", e))
            if by_eng["pe"]:
                block.tensor(lambda e: run("pe", e))
            if by_eng["act"]:
                block.scalar(lambda e: run("act", e))
            if by_eng["dve"]:
                block.vector(lambda e: run("dve", e))
            if by_eng["pool"]:
                block.gpsimd(lambda e: run("pool", e))
        return {k: len(v) for k, v in by_eng.items()}


class Rot:
    def __init__(self, n):
        self.n, self.i = n, 0

    def next(self):
        v = self.i % self.n
        self.i += 1
        return v


def build_program():
    nc = bass.Bass("TRN2", target_bir_lowering=False)
    I = lambda name, shape, dt=F32: nc.dram_tensor(name, list(shape), dt, kind="ExternalInput").ap()
    skind = "ExternalOutput" if DEBUG else "Internal"
    SC = lambda name, shape, dt: nc.dram_tensor(name, list(shape), dt, kind=skind).ap()
    T = {}
    T["x"] = I("x", [S, D])
    T["w_in"] = I("w_in", [D, 3072])
    T["conv_w"] = I("conv_w", [4, 512])
    T["conv_b"] = I("conv_b", [512])
    T["w_mq"] = I("w_mq", [4, 128, 128])
    T["w_mk"] = I("w_mk", [4, 128, 128])
    T["w_mgate"] = I("w_mgate", [1536, 8])
    T["b_mgate"] = I("b_mgate", [8])
    T["m_norm_g"] = I("m_norm_g", [512])
    T["m_skip"] = I("m_skip", [512])
    T["lambda_qk"] = I("lambda_qk", [256])
    T["da_norm_g"] = I("da_norm_g", [128])
    T["rel_bias"] = I("rel_bias", [128])
    T["w_out"] = I("w_out", [D, D])
    T["norm1_g"] = I("norm1_g", [D])
    T["norm2_g"] = I("norm2_g", [D])
    T["w_r"] = I("w_r", [D, 36])
    T["b_r"] = I("b_r", [36])
    T["w_eg"] = I("w_eg", [N_EXP, D, DFF])
    T["w_eu"] = I("w_eu", [N_EXP, D, DFF])
    T["w_ed"] = I("w_ed", [N_EXP, DFF, D])
    T["normf_g"] = I("normf_g", [D])
    T["ident"] = I("ident", [128, 128])
    T["tri"] = I("tri", [128, 128])
    T["sel"] = I("sel", [4, 512])
    T["oh"] = I("oh", [128, 2 * 33 * 128])
    T["lstrict"] = I("lstrict", [128, 128])
    T["thr"] = I("thr", [16 + NSUP])
    T["pidx"] = I("pidx", [128, 1])
    T["out"] = nc.dram_tensor("out", [S, D], F32, kind="ExternalOutput").ap()
    T["featT"] = SC("featT", [5, 512, S], BF16)
    T["vm_tok"] = SC("vm_tok", [S, 512], BF16)
    T["vd_tok"] = SC("vd_tok", [S, 512], BF16)
    T["ymT"] = SC("ymT", [512, S], BF16)
    T["ydT"] = SC("ydT", [512, S], BF16)
    T["x2"] = SC("x2", [S, D], F32)
    T["h2T"] = SC("h2T", [D, S], BF16)
    T["gates"] = SC("gates", [128, NT * 32], F32)
    T["h2b"] = SC("h2b", [S, D], BF16)
    T["xs"] = nc.dram_tensor("xs", [NSLOT, D], BF16, kind="Internal").ap()
    T["ys"] = nc.dram_tensor("ys", [NSLOT, D], BF16, kind="Internal").ap()
    T["wall"] = nc.dram_tensor("wall", [N_EXP * 128, 3 * 4096], BF16, kind="Internal").ap()

    stats = {}
    with contextlib.ExitStack() as gst:
        gst.enter_context(nc.allow_non_contiguous_dma(reason="small strided parameter loads"))
        sems = Sems(nc, gst)
        T["biasT_sb"] = gst.enter_context(nc.sbuf_tensor("g_biasT", [128, 2, 4, 128], F32))
        T["rbb_sb"] = gst.enter_context(nc.sbuf_tensor("g_rbb", [128, 128], F32))
        T["pos_i"] = gst.enter_context(nc.sbuf_tensor("g_pos_i", [128, NT, 2], I32))
        T["te_i"] = gst.enter_context(nc.sbuf_tensor("g_te_i", [128, 64], I32))
        T["wk_g"] = gst.enter_context(nc.sbuf_tensor("g_wk", [128, NT, 2], F32))
        if 0 in STAGES:
            stats["s0"] = stage0(nc, sems, T)
        if 1 in STAGES:
            stats["s1"] = stage1(nc, sems, T)
        if 2 in STAGES:
            stats["s2"] = stage2(nc, sems, T)
        if 3 in STAGES:
            stats["s3"] = stage3(nc, sems, T)
        if 4 in STAGES:
            stats["s4"] = stage4(nc, sems, T)
        if 5 in STAGES:
            stats["s5"] = stage5(nc, sems, T)
        if 6 in STAGES:
            stats["s6"] = stage6(nc, sems, T)
    return nc, stats


_TN = [0]
PSUM_KEYS = set()


def tens(nc, st):
    _TN[0] += 1
    pre = "t%d_" % _TN[0]
    sb = lambda n, s, d=F32: st.enter_context(nc.sbuf_tensor(pre + n, list(s), d))
    def ps(n, s, d=F32):
        PSUM_KEYS.add(n)
        return st.enter_context(nc.psum_tensor(pre + n, list(s), d))
    return sb, ps


def conv_jobs():
    return [(name, m, e) for m, name in enumerate(("w_eg", "w_eu", "w_ed")) for e in range(N_EXP)]


class Conv:
    def __init__(self, P, sb, T, engs=("pool",), queues=("sp", "sp"), nb=3):
        self.P, self.T = P, T
        self.stg = [sb("w0s%d" % i, [128, 8, 512], F32) for i in range(nb)]
        self.cvt = [sb("w0c%d" % i, [128, 8, 512], BF16) for i in range(nb)]
        self.rot = Rot(nb)
        self.engs, self.queues = engs, queues
        self.jobs = conv_jobs()
        self.k = 0

    def emit(self, n):
        P, T = self.P, self.T
        for _ in range(n):
            if self.k >= len(self.jobs):
                return
            name, m, e = self.jobs[self.k]
            b = self.rot.next()
            src = T[name][e].rearrange("(c p) f -> p c f", p=128)
            dstap = T["wall"][e * 128:(e + 1) * 128, m * 4096:(m + 1) * 4096].rearrange("p (c f) -> p c f", f=512)
            sv = self.stg[b][:].rearrange("p (c h) f -> p c (h f)", c=4) if name == "w_ed" else self.stg[b][:]
            P.load(self.queues[0], sv, src, [], ["stg%d" % b])
            P.copy(self.engs[self.k % len(self.engs)], self.cvt[b][:], self.stg[b][:], ["stg%d" % b], ["cvt%d" % b])
            P.load(self.queues[1], dstap, self.cvt[b][:], ["cvt%d" % b], ["wall"])
            self.k += 1


class ConvD:
    def __init__(self, P, T):
        self.P, self.T = P, T
        self.jobs = conv_jobs()
        self.k = 0

    def emit(self, n):
        P, T = self.P, self.T
        for _ in range(n):
            if self.k >= len(self.jobs):
                return
            name, m, e = self.jobs[self.k]
            cols = T["wall"][e * 128:(e + 1) * 128, m * 4096:(m + 1) * 4096]
            if name == "w_ed":
                src = T[name][e].rearrange("(c p) d -> p c d", p=128)
                dst = cols.rearrange("p (c d) -> p c d", d=1024)
            else:
                src = T[name][e].rearrange("(c p) f -> p c f", p=128)
                dst = cols.rearrange("p (c f) -> p c f", f=512)
            P.load("pool", dst, src, [], ["wall%d" % self.k])
            self.k += 1


def stage0(nc, sems, T):
    with contextlib.ExitStack() as st:
        sb, ps = tens(nc, st)
        P = Prog(nc, sems)
        cv = Conv(P, sb, T, engs=("dve", "pool", "act"), queues=("sp", "act"))
        cv.emit(96)
        return P.emit()


def stage1(nc, sems, T):
    with contextlib.ExitStack() as st:
        sb, ps = tens(nc, st)
        P = Prog(nc, sems)
        ident = sb("ident", [128, 128])
        identb = sb("identb", [128, 128], BF16)
        g1b = sb("g1b", [128, D])
        w_bf = sb("w_in_bf", [128, 8, 3072], BF16)
        xt = [sb("xt%d" % i, [128, D]) for i in range(2)]
        junk = sb("junk", [128, D], BF16)
        stat = sb("stat", [128, 4])
        xn = [sb("xn%d" % i, [128, D], BF16) for i in range(2)]
        hT = [sb("hT%d" % i, [128, 8, 512], BF16) for i in range(2)]
        fstg = [sb("fstg%d" % i, [128, 4, 512], BF16) for i in range(2)]
        tstg = [sb("tstg%d" % i, [128, 4, 512], BF16) for i in range(2)]
        pt = [ps("pt%d" % i, [128, D], BF16) for i in range(2)]
        pp = [ps("pp%d" % i, [128, 512]) for i in range(4)]

        P.load("sp", ident[:], T["ident"], [], ["ident"])
        P.copy("dve", identb[:], ident[:], ["ident"], ["identb"])
        P.load("act", g1b[:], T["norm1_g"].partition_broadcast(128), [], ["g1b"])
        for kc in range(8):
            P.load("pool", w_bf[:, kc, :], T["w_in"][kc * 128:(kc + 1) * 128, :], [], ["w_bf"])
        oh = sb("oh", [128, 2, 33, 128])
        rbb, biasT = T["rbb_sb"], T["biasT_sb"]
        P.load("act", oh[:].rearrange("p a b c -> p (a b c)"), T["oh"], [], ["oh"])
        P.load("act", rbb[:], T["rel_bias"].partition_broadcast(128), [], ["rbb"])

        def emit_bias(idx):
            kind, h = idx // 4, idx % 4
            dst_ = biasT[:, kind, h, :]
            kb = "biasT%d%d" % (kind, h)
            P.ts("pool", dst_, oh[:, kind, 32, :], NEG, None, ALU.mult, None, ["oh"], [kb])
            for b_ in range(32):
                P.stt("dve", dst_, oh[:, kind, b_, :], rbb[:, b_ * 4 + h:b_ * 4 + h + 1], dst_, ALU.mult, ALU.add, ["oh", "rbb", kb], [kb])

        rpp = Rot(4)
        cvj = ConvD(P, T)
        for g in range(8):
            hb = g % 2
            emit_bias(g)
            cvj.emit(CONV_PER_GROUP[1])
            for ti in range(4):
                t = g * 4 + ti
                b = t % 2
                P.load("sp", xt[b][:], T["x"][t * 128:(t + 1) * 128, :], [], ["xt%d" % b])
                P.act(junk[:], xt[b][:], AF.Square, ["xt%d" % b], ["junk", "stat"], accum_out=stat[:, 0:1])
                P.ts("dve", stat[:, 1:2], stat[:, 0:1], 1.0 / D, EPS, ALU.mult, ALU.add, ["stat"], ["stat"])
                P.act(stat[:, 2:3], stat[:, 1:2], AF.Ln, ["stat"], ["stat"])
                P.act(stat[:, 3:4], stat[:, 2:3], AF.Exp, ["stat"], ["stat"], scale=-0.5)
                P.stt("dve", xn[b][:], xt[b][:], stat[:, 3:4], g1b[:], ALU.mult, ALU.mult, ["xt%d" % b, "stat", "g1b"], ["xn%d" % b])
                for kc in range(8):
                    P.tr(pt[b][:, kc * 128:(kc + 1) * 128], xn[b][:, kc * 128:(kc + 1) * 128], identb[:], ["xn%d" % b, "identb"], ["pt%d" % b])
                P.copy("act" if ti % 2 == 0 else "dve", hT[hb][:, :, ti * 128:(ti + 1) * 128], pt[b][:, :].rearrange("p (k t) -> p k t", k=8),
                       ["pt%d" % b], ["hT%d" % hb])
            for blk in range(5):
                fb = (g * 5 + blk) % 2
                for ch in range(4):
                    col0 = blk * 512 + ch * 128
                    pb = rpp.next()
                    for kc in range(8):
                        P.mm(pp[pb][:, :], w_bf[:, kc, col0:col0 + 128], hT[hb][:, kc, :], ["w_bf", "hT%d" % hb], ["pp%d" % pb],
                             start=(kc == 0), stop=(kc == 7))
                    P.copy("act" if ch % 2 == 0 else "dve", fstg[fb][:, ch, :], pp[pb][:, :], ["pp%d" % pb], ["fstg%d" % fb])
                P.load("act", T["featT"][blk].rearrange("(c p) t -> p c t", p=128)[:, :, g * 512:(g + 1) * 512], fstg[fb][:],
                       ["fstg%d" % fb], ["featT"])
            for bi, (blk, dst) in enumerate(((1, "vm_tok"), (5, "vd_tok"))):
                tb = (g * 2 + bi) % 2
                for ti in range(4):
                    pb = rpp.next()
                    for kc in range(8):
                        P.mm(pp[pb][:, :], hT[hb][:, kc, ti * 128:(ti + 1) * 128], w_bf[:, kc, blk * 512:(blk + 1) * 512],
                             ["w_bf", "hT%d" % hb], ["pp%d" % pb], start=(kc == 0), stop=(kc == 7))
                    P.copy("dve" if ti % 2 == 0 else "act", tstg[tb][:, ti, :], pp[pb][:, :], ["pp%d" % pb], ["tstg%d" % tb])
                P.load("sp", T[dst][g * 512:(g + 1) * 512, :].rearrange("(t p) f -> p t f", p=128), tstg[tb][:], ["tstg%d" % tb], [dst])
        return P.emit()


def stage2(nc, sems, T):
    with contextlib.ExitStack() as st:
        sb, ps = tens(nc, st)
        P = Prog(nc, sems)
        ident = sb("ident", [128, 128])
        identb = sb("identb", [128, 128], BF16)
        tri = sb("tri", [128, 128])
        bigtri = sb("bigtri", [128, 128])
        sel = sb("sel", [4, 512])
        cw = sb("cw", [128, 4, 4])
        cb = sb("cb", [128, 4])
        mg = sb("mg", [128, 4])
        msk = sb("msk", [128, 4])
        wq = sb("wq", [128, 4, 128], BF16)
        wk = sb("wk", [128, 4, 128], BF16)
        wgt = sb("wgt", [128, 12, 8], BF16)
        bi = sb("bi", [4, 1])
        bfn = sb("bfn", [4, 1])
        zeros = sb("zeros", [4, 512])
        carryB = sb("carryB", [4, 1])
        carryM = sb("carryM", [4, 1])
        Cf = sb("Cf", [128, 4, 129])
        Cb = sb("Cb", [128, 4, 129], BF16)
        c_sb = [sb("c_sb%d" % i, [128, 4, 515], BF16) for i in range(2)]
        z_sb = [sb("z_sb%d" % i, [128, 4, 512], BF16) for i in range(2)]
        vmT = [sb("vmT%d" % i, [128, 4, 512], BF16) for i in range(2)]
        vaug = [sb("vaug%d" % i, [128, 4, 4, 129], BF16) for i in range(2)]
        cacc = [sb("cacc%d" % i, [128, 512]) for i in range(2)]
        cact = sb("cact", [128, 4, 512], BF16)
        sigz = sb("sigz", [128, 4, 512], BF16)
        scs = sb("scs", [128, 4, 512], BF16)
        qT = sb("qT", [128, 4, 512], BF16)
        kT = sb("kT", [128, 4, 512], BF16)
        ktok = sb("ktok", [128, 4, 4, 128], BF16)
        i_row = sb("i_row", [4, 512])
        e_row = sb("e_row", [4, 512])
        sp_row = sb("sp_row", [4, 512])
        Bn = sb("Bn", [4, 513])
        A_row = sb("A_row", [4, 512])
        Mx = sb("Mx", [4, 513])
        N_row = sb("N_row", [4, 512])
        cols = sb("cols", [128, 4, 3, 4])
        eN = sb("eN", [128, 4, 4])
        Mb = sb("Mb", [128, 4, 5])
        nMb = sb("nMb", [128, 4, 5])
        dec = sb("dec", [128, 4, 4])
        spa = sb("spa", [128, 4, 4])
        Mrow = sb("Mrow", [128, 4, 512])
        tmpD = [sb("tmpD%d" % i, [128, 128]) for i in range(4)]
        Dt = [sb("Dt%d" % i, [128, 128]) for i in range(4)]
        Dm = [sb("Dm%d" % i, [128, 128]) for i in range(2)]
        wT = [sb("wT%d" % i, [128, 128], BF16) for i in range(4)]
        intra = [sb("intra%d" % i, [128, 129]) for i in range(4)]
        comb = [sb("comb%d" % i, [128, 129]) for i in range(4)]
        sm = [sb("sm%d" % i, [128, 16]) for i in range(4)]
        hh = [sb("hh%d" % i, [128, 128]) for i in range(4)]
        hn = [sb("hn%d" % i, [128, 128], BF16) for i in range(4)]
        y1 = [sb("y1%d" % i, [128, 128], BF16) for i in range(4)]
        ymg = [sb("ymg%d" % i, [128, 4, 512], BF16) for i in range(2)]
        wkc = [sb("wkc%d" % i, [128, 1]) for i in range(4)]
        vw = [sb("vw%d" % i, [128, 129], BF16) for i in range(4)]
        pA = ps("pA", [128, 512])
        pB = ps("pB", [128, 512])
        pG = ps("pG", [128, 512])
        ptb = ps("ptb", [128, 1024], BF16)
        pS = [ps("pS%d" % i, [128, 512]) for i in range(2)]
        pO = [ps("pO%d" % i, [128, 512]) for i in range(2)]
        P.load("sp", ident[:], T["ident"], [], ["ident"])
        P.copy("dve", identb[:], ident[:], ["ident"], ["identb"])
        P.load("sp", tri[:], T["tri"], [], ["tri"])
        P.ts("dve", bigtri[:], tri[:], -1.0, -1.0e4, ALU.add, ALU.mult, ["tri"], ["bigtri"])
        P.load("sp", sel[:], T["sel"], [], ["sel"])
        P.load("sp", cw[:], T["conv_w"].rearrange("j (c p) -> p j c", p=128), [], ["cw"])
        P.load("sp", cb[:], T["conv_b"].rearrange("(c p) -> p c", p=128), [], ["cb"])
        P.load("sp", mg[:], T["m_norm_g"].rearrange("(c p) -> p c", p=128), [], ["mg"])
        P.load("sp", msk[:], T["m_skip"].rearrange("(c p) -> p c", p=128), [], ["msk"])
        P.load("pool", wq[:], T["w_mq"].rearrange("h d e -> d h e"), [], ["wq"])
        P.load("pool", wk[:], T["w_mk"].rearrange("h d e -> d h e"), [], ["wk"])
        P.load("pool", wgt[:], T["w_mgate"].rearrange("(c p) g -> p c g", p=128), [], ["wgt"])
        P.load("sp", bi[:], T["b_mgate"][0:4].rearrange("(p o) -> p o", o=1), [], ["bi"])
        P.load("sp", bfn[:], T["b_mgate"][4:8].rearrange("(p o) -> p o", o=1), [], ["bfn"])
        P.ts("dve", bfn[:], bfn[:], -1.0, None, ALU.mult, None, ["bfn"], ["bfn"])
        P.memset("pool", zeros[:], 0.0, ["zeros"])
        P.memset("pool", carryB[:], 0.0, ["carryB"])
        P.memset("pool", carryM[:], 0.0, ["carryM"])
        P.memset("pool", Cf[:], 0.0, ["Cf%d" % h_ for h_ in range(4)])
        P.memset("pool", Cb[:], 0.0, ["Cb%d" % h_ for h_ in range(4)])
        for i in range(2):
            P.memset("pool", vaug[i][:], 1.0, ["vaug%d" % i])
            P.memset("pool", c_sb[i][:], 0.0, ["c_sb%d" % i])

        featT = T["featT"]
        rS, rO, r2 = Rot(2), Rot(2), Rot(2)
        cvj = ConvD(P, T)
        cvj.k = 8 * CONV_PER_GROUP[1]
        for g in range(8):
            b = g % 2
            t0 = g * 512
            cvj.emit(CONV_PER_GROUP[2])
            kc_, kz, kv, kva = "c_sb%d" % b, "z_sb%d" % b, "vmT%d" % b, "vaug%d" % b
            cview = featT[0].rearrange("(c p) t -> p c t", p=128)
            if g == 0:
                P.load("sp", c_sb[b][:, :, 3:515], cview[:, :, 0:512], [], [kc_])
            else:
                P.load("sp", c_sb[b][:, :, 0:515], cview[:, :, t0 - 3:t0 + 512], [], [kc_])
            P.load("act", z_sb[b][:], featT[2].rearrange("(c p) t -> p c t", p=128)[:, :, t0:t0 + 512], [], [kz])
            P.load("act", vmT[b][:], featT[1].rearrange("(c p) t -> p c t", p=128)[:, :, t0:t0 + 512], [], [kv])
            for ti in range(4):
                P.load("sp" if ti % 2 == 0 else "act", vaug[b][:, ti, :, 0:128],
                       T["vm_tok"][t0 + ti * 128:t0 + (ti + 1) * 128, :].rearrange("p (h e) -> p h e", e=128), [kva], [kva])
            for ch in range(4):
                ab = ch % 2
                ka = "cacc%d" % ab
                e1 = "dve" if ch % 2 == 0 else "pool"
                P.ts("dve", cacc[ab][:], c_sb[b][:, ch, 0:512], cw[:, 0, ch:ch + 1], cb[:, ch:ch + 1], ALU.mult, ALU.add, [kc_, "cw", "cb"], [ka])
                for j in range(1, 4):
                    P.stt("dve" if j % 2 == 0 else "pool", cacc[ab][:], c_sb[b][:, ch, j:j + 512], cw[:, j, ch:ch + 1], cacc[ab][:], ALU.mult, ALU.add,
                          [kc_, "cw", ka], [ka])
                P.act(cact[:, ch, :], cacc[ab][:], AF.Silu, [ka], ["cact"])
                P.ts("pool", scs[:, ch, :], cact[:, ch, :], msk[:, ch:ch + 1], None, ALU.mult, None, ["cact", "msk"], ["scs"])
            P.act(sigz[:].rearrange("p c t -> p (c t)"), z_sb[b][:].rearrange("p c t -> p (c t)"), AF.Sigmoid, [kz], ["sigz"])
            for h in range(4):
                P.mm(pA[:, :], wq[:, h, :], cact[:, h, :], ["wq", "cact"], ["pA"])
                P.copy("act", qT[:, h, :], pA[:, :], ["pA"], ["qT"])
                P.mm(pB[:, :], wk[:, h, :], cact[:, h, :], ["wk", "cact"], ["pB"])
                P.copy("dve", kT[:, h, :], pB[:, :], ["pB"], ["kT"])
            for ti in range(4):
                pz = pA if ti % 2 == 0 else pB
                kz_ = "pA" if ti % 2 == 0 else "pB"
                for h in range(4):
                    P.mm(pz[:, h * 128:(h + 1) * 128], cact[:, h, ti * 128:(ti + 1) * 128], wk[:, h, :], ["cact", "wk"], [kz_])
                P.copy("act" if ti % 2 == 0 else "dve", ktok[:, ti, :, :].rearrange("p h e -> p (h e)"), pz[:, :], [kz_], ["ktok"])
            srcs = [(qT, "qT")] * 4 + [(kT, "kT")] * 4 + [(vmT[b], kv)] * 4
            for c in range(12):
                sap, skey = srcs[c]
                P.mm(pG[0:4, :], wgt[:, c, 0:4], sap[:, c % 4, :], ["wgt", skey], ["pG"], start=(c == 0), stop=(c == 11))
            for c in range(12):
                sap, skey = srcs[c]
                P.mm(pB[0:4, :], wgt[:, c, 4:8], sap[:, c % 4, :], ["wgt", skey], ["pB"], start=(c == 0), stop=(c == 11))
            P.ts("dve", i_row[:], pG[0:4, :], bi[:, 0:1], None, ALU.add, None, ["pG", "bi"], ["i_row"])
            P.act(e_row[:], pB[0:4, :], AF.Exp, ["pB", "bfn"], ["e_row"], bias=bfn[:, 0:1], scale=-1.0)
            P.act(sp_row[:], e_row[:], AF.Ln, ["e_row"], ["sp_row"], bias=1.0)
            P.op("dve", (lambda o, d0, d1, ini: (lambda e: e.tensor_tensor_scan(out=o, data0=d0, data1=d1, initial=ini, op0=ALU.add, op1=ALU.add)))(
                Bn[:, 1:513], sp_row[:], zeros[:], carryB[:, 0:1]), ["sp_row", "zeros", "carryB"], ["Bn"])
            P.tt("dve", A_row[:], i_row[:], Bn[:, 1:513], ALU.add, ["i_row", "Bn"], ["A_row"])
            P.copy("dve", Mx[:, 0:1], carryM[:, 0:1], ["carryM"], ["Mx"])
            P.op("dve", (lambda o, d0, d1, ini: (lambda e: e.tensor_tensor_scan(out=o, data0=d0, data1=d1, initial=ini, op0=ALU.max, op1=ALU.max)))(
                Mx[:, 1:513], A_row[:], A_row[:], carryM[:, 0:1]), ["A_row", "carryM", "Mx"], ["Mx"])
            P.copy("dve", carryB[:, 0:1], Bn[:, 512:513], ["Bn"], ["carryB"])
            P.copy("dve", carryM[:, 0:1], Mx[:, 512:513], ["Mx"], ["carryM"])
            P.tt("dve", N_row[:], Bn[:, 1:513], Mx[:, 1:513], ALU.subtract, ["Bn", "Mx"], ["N_row"])
            for c in range(4):
                for k3, (rap, rkey, off) in enumerate(((A_row, "A_row", 0), (Mx, "Mx", 1), (N_row, "N_row", 0))):
                    o0 = c * 12 + k3 * 4
                    P.tr(pA[:, o0:o0 + 4], rap[:, off + c * 128: off + (c + 1) * 128], ident[0:4, 0:4], [rkey, "ident"], ["pA"])
            P.copy("dve", cols[:].rearrange("p c k h -> p (c k h)"), pA[:, 0:48], ["pA"], ["cols"])
            P.act(eN[:], cols[:, :, 2, :], AF.Exp, ["cols"], ["eN"])
            for h in range(4):
                P.mm(pB[:, h * 5:(h + 1) * 5], sel[:, h * 128:(h + 1) * 128], Mx[:, 0:513:128], ["sel", "Mx"], ["pB"])
            P.copy("dve", Mb[:].rearrange("p h c -> p (h c)"), pB[:, 0:20], ["pB"], ["Mb"])
            P.ts("dve", nMb[:], Mb[:], -1.0, None, ALU.mult, None, ["Mb"], ["nMb"])
            P.tt("dve", dec[:], Mb[:, :, 0:4], Mb[:, :, 1:5], ALU.subtract, ["Mb"], ["dec"])
            P.act(dec[:], dec[:], AF.Exp, ["dec"], ["dec"])
            P.tt("dve", spa[:], Mb[:, :, 0:4].rearrange("p h c -> p c h"), cols[:, :, 1, :], ALU.subtract, ["Mb", "cols"], ["spa"])
            P.act(spa[:], spa[:], AF.Exp, ["spa"], ["spa"])
            for h in range(4):
                pz, kz_ = (pA, "pA") if h % 2 == 0 else (pB, "pB")
                P.mm(pz[:, :], sel[:, h * 128:(h + 1) * 128], Mx[:, 1:513], ["sel", "Mx"], [kz_])
                P.tt("dve", Mrow[:, h, :].rearrange("p (c t) -> p c t", t=128), pz[:, :].rearrange("p (c t) -> p c t", t=128),
                     bigtri[:].unsqueeze(1).to_broadcast([128, 4, 128]), ALU.add, [kz_, "bigtri"], ["Mrow"])
            HB = [(pS[0], "pS0"), (pS[1], "pS1"), (pO[0], "pO0"), (pO[1], "pO1")]
            UB = [(pA, "pA"), (pB, "pB")]
            for c in range(4):
                cs = slice(c * 128, (c + 1) * 128)

                def phases(h, c=c, cs=cs):
                    hb_, kH = HB[h]
                    ub_, kU = UB[h // 2]
                    uo = (h % 2) * 256
                    Acol = cols[:, c, 0, h:h + 1]
                    ks = "sm%d" % h
                    smt = sm[h]
                    kCf, kCb = "Cf%d" % h, "Cb%d" % h

                    def p0():
                        P.mm(hb_[:, 0:128], kT[:, h, cs], qT[:, h, cs], ["kT", "qT"], [kH])
                        P.ts("dve", tmpD[h][:], Mrow[:, h, cs], Acol, 0.0, ALU.subtract, ALU.max, ["Mrow", "cols"], ["tmpD%d" % h])
                        P.act(wkc[h][:], Acol, AF.Exp, ["cols", "nMb"], ["wkc%d" % h], bias=nMb[:, h, c + 1:c + 2])

                    def p1():
                        P.act(Dt[h][:], tmpD[h][:], AF.Exp, ["tmpD%d" % h], ["Dt%d" % h], scale=-1.0)
                        P.op("act", (lambda o, i_, sc: (lambda e: e.activation(out=o, in_=i_, func=AF.Copy, scale=sc)))(
                            vw[h][:], vaug[b][:, c, h, :], wkc[h][:, 0:1]), [kva, "wkc%d" % h], ["vw%d" % h])

                    def p2():
                        P.tt("dve", wT[h][:], hb_[:, 0:128], Dt[h][:], ALU.mult, [kH, "Dt%d" % h], ["wT%d" % h])

                    def p3():
                        P.mm(hb_[:, 128:257], wT[h][:], vaug[b][:, c, h, :], ["wT%d" % h, kva], [kH])
                        P.mm(hb_[:, 257:386], qT[:, h, cs], Cb[:, h, :], ["qT", kCb], [kH])
                        P.mm(ub_[:, uo:uo + 129], ktok[:, c, h, :], vw[h][:], ["ktok", "vw%d" % h], [kU])

                    def p4():
                        P.copy("act", intra[h][:], hb_[:, 128:257], [kH], ["intra%d" % h])
                        P.stt("dve", Cf[:, h, :], Cf[:, h, :], dec[:, h, c:c + 1], ub_[:, uo:uo + 129], ALU.mult, ALU.add, [kCf, "dec", kU], [kCf])

                    def p5():
                        P.stt("dve", comb[h][:], hb_[:, 257:386], spa[:, c, h:h + 1], intra[h][:], ALU.mult, ALU.add,
                              [kH, "spa", "intra%d" % h], ["comb%d" % h])
                        P.copy("act", Cb[:, h, :], Cf[:, h, :], [kCf], [kCb])

                    def p6():
                        P.stt("dve", smt[:, 0:1], comb[h][:, 128:129], -1.0, comb[h][:, 128:129], ALU.mult, ALU.max, ["comb%d" % h], [ks])
                        P.stt("dve", smt[:, 1:2], smt[:, 0:1], ML_SCALE, eN[:, c, h:h + 1], ALU.mult, ALU.max, [ks, "eN"], [ks])
                        P.op("dve", (lambda o, i_: (lambda e: e.reciprocal(out=o, in_=i_)))(smt[:, 2:3], smt[:, 1:2]), [ks], [ks])
                        P.ts("dve", hh[h][:], comb[h][:, 0:128], smt[:, 2:3], ML_SCALE, ALU.mult, ALU.mult, ["comb%d" % h, ks], ["hh%d" % h])

                    def p7():
                        P.op("dve", (lambda o, i_: (lambda e: e.bn_stats(out=o, in_=i_)))(smt[:, 4:10], hh[h][:]), ["hh%d" % h], [ks])
                        P.op("dve", (lambda o, i_: (lambda e: e.bn_aggr(out=o, in_=i_)))(smt[:, 10:12], smt[:, 4:10]), [ks], [ks])
                        P.ts("dve", smt[:, 12:13], smt[:, 11:12], EPS, None, ALU.add, None, [ks], [ks])

                    def p8():
                        P.act(smt[:, 13:14], smt[:, 12:13], AF.Ln, [ks], [ks])
                        P.act(smt[:, 14:15], smt[:, 13:14], AF.Exp, [ks], [ks], scale=-0.5)

                    def p9():
                        P.ts("dve", hn[h][:], hh[h][:], smt[:, 10:11], smt[:, 14:15], ALU.subtract, ALU.mult, ["hh%d" % h, ks], ["hn%d" % h])

                    def p10():
                        P.tr(ptb[:, h * 128:(h + 1) * 128], hn[h][:], identb[:], ["hn%d" % h, "identb"], ["ptb"])

                    def p11():
                        P.stt("dve", y1[h][:], ptb[:, h * 128:(h + 1) * 128], mg[:, h:h + 1], scs[:, h, cs], ALU.mult, ALU.add,
                              ["ptb", "mg", "scs"], ["y1%d" % h])
                        P.tt("dve", ymg[b][:, h, cs], y1[h][:], sigz[:, h, cs], ALU.mult, ["y1%d" % h, "sigz"], ["ymg%d_%d" % (b, h)])

                    return [p0, p1, p2, p3, p4, p5, p6, p7, p8, p9, p10, p11]

                plist = [phases(h) for h in range(4)]
                for k_ in range(12):
                    for h in range(4):
                        plist[h][k_]()
            P.load("sp", T["ymT"].rearrange("(c p) t -> p c t", p=128)[:, :, t0:t0 + 512], ymg[b][:], ["ymg%d_%d" % (b, h_) for h_ in range(4)], ["ymT"])
        return P.emit()


def stage3(nc, sems, T):
    with contextlib.ExitStack() as st:
        sb, ps = tens(nc, st)
        P = Prog(nc, sems)
        ident = sb("ident", [128, 128])
        identb = sb("identb", [128, 128], BF16)
        qT = sb("qT", [128, 4, S], BF16)
        kT = sb("kT", [128, 4, S], BF16)
        vaug = sb("vaug", [128, NT, 4, 129], BF16)
        rbb = T["rbb_sb"]
        biasT = T["biasT_sb"]
        lqb = sb("lqb", [128, 256])
        lt = sb("lt", [128, 64])
        lam = sb("lam", [128, 8])
        dag = sb("dag", [128, 1])
        PT = [sb("PT%d" % i, [128, 512], BF16) for i in range(4)]
        tmpn = [sb("tmpn%d" % i, [128, 128]) for i in range(2)]
        t0s = [sb("t0s%d" % i, [128, 128]) for i in range(2)]
        av = [sb("av%d" % i, [128, 128]) for i in range(2)]
        junk = sb("junk3", [128, 128])
        sm = [sb("sm3%d" % i, [128, 8]) for i in range(4)]
        an = [sb("an%d" % i, [128, 128], BF16) for i in range(2)]
        ydg = [sb("ydg%d" % i, [128, 512], BF16) for i in range(2)]
        pS = [ps("pS%d" % i, [128, 512]) for i in range(3)]
        acc = [ps("acc%d" % i, [128, 512]) for i in range(4)]
        ptb = ps("ptb", [128, 1024], BF16)

        P.load("sp", ident[:], T["ident"], [], ["ident"])
        P.copy("dve", identb[:], ident[:], ["ident"], ["identb"])
        P.memset("pool", vaug[:], 1.0, ["vaug"])
        P.load("sp", qT[:], T["featT"][3].rearrange("(h p) t -> p h t", p=128), [], ["qT"])
        P.load("act", kT[:], T["featT"][4].rearrange("(h p) t -> p h t", p=128), [], ["kT"])
        for t in range(NT):
            P.load("sp" if t % 2 == 0 else "act", vaug[:, t, :, 0:128],
                   T["vd_tok"][t * 128:(t + 1) * 128, :].rearrange("p (h e) -> p h e", e=128), ["vaug"], ["vaug"])
        P.load("sp", lqb[:], T["lambda_qk"].partition_broadcast(128), [], ["lqb"])
        P.load("sp", dag[:], T["da_norm_g"].rearrange("(p o) -> p o", o=1), [], ["dag"])
        P.ts("dve", dag[:], dag[:], 1.0 - LAM_INIT, None, ALU.mult, None, ["dag"], ["dag"])
        for i in range(2):
            P.tt("dve", lt[:], lqb[:, (2 * i) * 64:(2 * i + 1) * 64], lqb[:, (2 * i + 1) * 64:(2 * i + 2) * 64], ALU.mult, ["lqb", "lt"], ["lt"])
            P.op("dve", (lambda o, i_: (lambda e: e.reduce_sum(out=o, in_=i_, axis=AX.X)))(lam[:, 4 + i:5 + i], lt[:]), ["lt"], ["lam"])
        P.act(lam[:, 0:2], lam[:, 4:6], AF.Exp, ["lam"], ["lam"])
        P.tt("dve", lam[:, 2:3], lam[:, 0:1], lam[:, 1:2], ALU.subtract, ["lam"], ["lam"])
        P.ts("dve", lam[:, 3:4], lam[:, 2:3], LAM_INIT, -1.0, ALU.add, ALU.mult, ["lam"], ["lam"])
        rS, rP, r2 = Rot(3), Rot(4), Rot(2)
        cvj = ConvD(P, T)
        cvj.k = 8 * (CONV_PER_GROUP[1] + CONV_PER_GROUP[2])
        its = [(h, g, c, j) for h in range(4) for g in range(8) for c in range(2) for j in range(4 * g + 4)]

        def emit_S(it):
            h, g, c, j = it
            prow = slice(c * 64, (c + 1) * 64)
            i_lo = max(j, 4 * g) - 4 * g
            sB = rS.next()
            pb = rP.next()
            kS, kP = "pS%d" % sB, "PT%d" % pb
            P.mm(pS[sB][:, i_lo * 128:512], kT[prow, h, j * 128:(j + 1) * 128], qT[prow, h, g * 512 + i_lo * 128:(g + 1) * 512],
                 ["kT", "qT"], [kS])
            far_lo = None
            for i in range(i_lo, 4):
                dist = 4 * g + i - j
                if dist >= 2:
                    far_lo = i
                    break
                n2 = r2.next()
                P.stt("dve", tmpn[n2][:], pS[sB][:, i * 128:(i + 1) * 128], DA_SCALE, biasT[:, dist, h, :], ALU.mult, ALU.add,
                      [kS, "biasT"], ["tmpn%d" % n2])
                P.act(PT[pb][:, i * 128:(i + 1) * 128], tmpn[n2][:], AF.Exp, ["tmpn%d" % n2], [kP])
            if far_lo is not None:
                P.act(PT[pb][:, far_lo * 128:512], pS[sB][:, far_lo * 128:512], AF.Exp, [kS, "rbb"], [kP],
                      bias=rbb[:, 31 * 4 + h:31 * 4 + h + 1], scale=DA_SCALE)
            return pb, i_lo

        def emit_AV(it, pb, i_lo):
            h, g, c, j = it
            kP = "PT%d" % pb
            if c == 0 and j == 0:
                cvj.emit(CONV_PER_GROUP[3])
                for a_ in range(4):
                    P.memset("dve", acc[a_][:, :], 0.0, ["acc%d" % a_])
            for i in range(i_lo, 4):
                a_ = c * 2 + i // 2
                off = (i % 2) * 256
                P.mm(acc[a_][:, off:off + 129], PT[pb][:, i * 128:(i + 1) * 128], vaug[:, j, h, :], [kP, "vaug"], ["acc%d" % a_],
                     start=False, stop=False, skip=True)
            if c == 1 and j == 4 * g + 3:
                finalize(h, g)

        def finalize(h, g):
            yb = (h * 8 + g) % 2
            for i in range(4):
                n2 = r2.next()
                ks = "sm3%d" % n2
                smt = sm[n2]
                a0, a1 = acc[i // 2], acc[2 + i // 2]
                k0, k1 = "acc%d" % (i // 2), "acc%d" % (2 + i // 2)
                off = (i % 2) * 256
                P.op("dve", (lambda o, i_: (lambda e: e.reciprocal(out=o, in_=i_)))(smt[:, 0:1], a0[:, off + 128:off + 129]), [k0], [ks])
                P.op("dve", (lambda o, i_: (lambda e: e.reciprocal(out=o, in_=i_)))(smt[:, 1:2], a1[:, off + 128:off + 129]), [k1], [ks])
                P.tt("dve", smt[:, 2:3], smt[:, 1:2], lam[:, 3:4], ALU.mult, [ks, "lam"], [ks])
                P.op("act", (lambda o, i_, sc: (lambda e: e.activation(out=o, in_=i_, func=AF.Copy, scale=sc)))(t0s[n2][:], a0[:, off:off + 128], smt[:, 0:1]),
                     [k0, ks], ["t0s%d" % n2])
                P.stt("dve", av[n2][:], a1[:, off:off + 128], smt[:, 2:3], t0s[n2][:], ALU.mult, ALU.add, [k1, ks, "t0s%d" % n2], ["av%d" % n2])
                P.act(junk[:], av[n2][:], AF.Square, ["av%d" % n2], ["junk3", ks], accum_out=smt[:, 3:4])
                P.ts("dve", smt[:, 4:5], smt[:, 3:4], 1.0 / 128, SUBLN_EPS, ALU.mult, ALU.add, [ks], [ks])
                P.act(smt[:, 5:6], smt[:, 4:5], AF.Ln, [ks], [ks])
                P.act(smt[:, 6:7], smt[:, 5:6], AF.Exp, [ks], [ks], scale=-0.5)
                P.ts("dve", an[n2][:], av[n2][:], smt[:, 6:7], None, ALU.mult, None, ["av%d" % n2, ks], ["an%d" % n2])
                P.tr(ptb[:, n2 * 512:n2 * 512 + 128], an[n2][:], identb[:], ["an%d" % n2, "identb"], ["ptb"])
                P.ts("dve", ydg[yb][:, i * 128:(i + 1) * 128], ptb[:, n2 * 512:n2 * 512 + 128], dag[:, 0:1], None, ALU.mult, None,
                     ["ptb", "dag"], ["ydg%d" % yb])
            P.load("act", T["ydT"][h * 128:(h + 1) * 128, g * 512:(g + 1) * 512], ydg[yb][:], ["ydg%d" % yb], ["ydT"])

        pend = []
        for it in its:
            pend.append((it,) + emit_S(it))
            if len(pend) > 2:
                emit_AV(*pend.pop(0))
        while pend:
            emit_AV(*pend.pop(0))
        return P.emit()


def stage4(nc, sems, T):
    with contextlib.ExitStack() as st:
        sb, ps = tens(nc, st)
        P = Prog(nc, sems)
        ident = sb("ident", [128, 128])
        wo = sb("wo", [128, 8, D], BF16)
        yT = sb("yT", [128, 8, S], BF16)
        g2b = sb("g2b", [128, D])
        wr = sb("wr", [128, 8, 36])
        brb = sb("brb", [128, 36])
        xt = [sb("xt%d" % i, [128, D]) for i in range(2)]
        x2 = [sb("x2%d" % i, [128, D]) for i in range(2)]
        junk = sb("junk4", [128, D], BF16)
        stat = [sb("stat4%d" % i, [128, 4]) for i in range(2)]
        h2 = [sb("h2%d" % i, [128, D]) for i in range(2)]
        h2T = [sb("h2T%d" % i, [128, 8, 128]) for i in range(2)]
        h2bf = [sb("h2bf%d" % i, [128, D], BF16) for i in range(2)]
        lstr = sb("lstr", [128, 128])
        ones = sb("ones", [128, 128])
        thr = sb("thr", [128, 16 + NSUP])
        E1 = sb("E1", [128, NT, 4, 8])
        E2 = sb("E2", [128, NT, 4, 8])
        Es = sb("Es", [128, NT * 32])
        within = sb("within", [128, NT, 32])
        csb = sb("csb", [128, NT, 32])
        incl = sb("incl", [128, NT, 32])
        zer = sb("zer", [128, NT])
        cmpb = sb("cmpb", [128, NSUP * 32])
        ntl = sb("ntl", [128, 32])
        inct = sb("inct", [128, 32])
        offb = sb("offb", [128, 32])
        offe = sb("offe", [128, 32])
        Rr = sb("Rr", [128, NT, 32])
        posf = sb("posf", [128, NT, 2])
        tef = sb("tef", [128, 64])
        pidx = sb("pidx", [128, 1])
        h2Tb = [sb("h2Tb%d" % i, [128, 8, 128], BF16) for i in range(2)]
        lgt = sb("lgt", [128, NT, 36])
        mxg = sb("mxg", [128, NT])
        ohg = sb("ohg", [128, NT, 4])
        eg = sb("eg", [128, NT, 4])
        sg = sb("sg", [128, NT])
        tmp4 = sb("tmp4", [128, NT, 4, 8])
        les = sb("les", [128, NT, 8])
        le2 = sb("le2", [128, NT, 8])
        m1 = sb("m1", [128, NT])
        m2 = sb("m2", [128, NT])
        oh1 = sb("oh1", [128, NT, 8])
        oh2 = sb("oh2", [128, NT, 8])
        w1 = sb("w1", [128, NT])
        w2 = sb("w2", [128, NT])
        gf = sb("gf", [128, NT, 8])
        gts = sb("gts", [128, NT, 4, 8])
        pO = [ps("pO%d" % i, [128, 512]) for i in range(4)]
        pT = [ps("pT%d" % i, [128, 512]) for i in range(2)]
        pR = ps("pR", [128, 512])

        P.load("sp", ident[:], T["ident"], [], ["ident"])
        zt = sb("zt", [128, 3, D], BF16)
        P.memset("pool", zt[:], 0.0, ["zt"])
        xs_v = T["xs"].rearrange("(n p) d -> p n d", p=128)
        for i in range(42):
            P.load("pool", xs_v[:, i * 3:(i + 1) * 3, :], zt[:], ["zt"], ["xs_zero%d" % i])
        P.load("pool", wo[:], T["w_out"].rearrange("(c p) n -> p c n", p=128), [], ["wo"])
        P.load("sp", yT[:, 0:4, :], T["ymT"].rearrange("(c p) t -> p c t", p=128), [], ["yT"])
        P.load("act", yT[:, 4:8, :], T["ydT"].rearrange("(c p) t -> p c t", p=128), [], ["yT"])
        P.load("sp", g2b[:], T["norm2_g"].partition_broadcast(128), [], ["g2b"])
        P.load("sp", wr[:], T["w_r"].rearrange("(c p) n -> p c n", p=128), [], ["wr"])
        P.load("sp", brb[:], T["b_r"].partition_broadcast(128), [], ["brb"])
        rO = Rot(2)

        def s4_a(t):
            b = t % 2
            ts_ = slice(t * 128, (t + 1) * 128)
            P.load("sp", xt[b][:], T["x"][ts_, :], [], ["xt%d" % b])
            for half in range(2):
                pb = rO.next() * 2 + half
                for kc in range(8):
                    P.mm(pO[pb][:, :], yT[:, kc, ts_], wo[:, kc, half * 512:(half + 1) * 512], ["yT", "wo"], ["pO%d" % pb], start=(kc == 0), stop=(kc == 7))
                P.tt("dve", x2[b][:, half * 512:(half + 1) * 512], pO[pb][:, :], xt[b][:, half * 512:(half + 1) * 512], ALU.add,
                     ["pO%d" % pb, "xt%d" % b], ["x2%d" % b])
            P.load("act", T["x2"][ts_, :], x2[b][:], ["x2%d" % b], ["x2d"])
            sk = "stat4%d" % b
            P.act(junk[:], x2[b][:], AF.Square, ["x2%d" % b], ["junk4", sk], accum_out=stat[b][:, 0:1])
            P.ts("dve", stat[b][:, 1:2], stat[b][:, 0:1], 1.0 / D, EPS, ALU.mult, ALU.add, [sk], [sk])
            P.act(stat[b][:, 2:3], stat[b][:, 1:2], AF.Ln, [sk], [sk])
            P.act(stat[b][:, 3:4], stat[b][:, 2:3], AF.Exp, [sk], [sk], scale=-0.5)
            P.stt("dve", h2[b][:], x2[b][:], stat[b][:, 3:4], g2b[:], ALU.mult, ALU.mult, ["x2%d" % b, sk, "g2b"], ["h2%d" % b])
            P.copy("pool", h2bf[b][:], h2[b][:], ["h2%d" % b], ["h2bf%d" % b])
            P.load("act", T["h2b"][ts_, :], h2bf[b][:], ["h2bf%d" % b], ["h2bd"])

        def s4_b(t):
            b = t % 2
            ts_ = slice(t * 128, (t + 1) * 128)
            for kc in range(8):
                pz = pT[kc // 4]
                P.tr(pz[:, (kc % 4) * 128:(kc % 4 + 1) * 128], h2[b][:, kc * 128:(kc + 1) * 128], ident[:], ["h2%d" % b, "ident"], ["pT%d" % (kc // 4)])
            for hf in range(2):
                P.copy("act", h2T[b][:, hf * 4:(hf + 1) * 4, :].rearrange("p k t -> p (k t)"), pT[hf][:, :], ["pT%d" % hf], ["h2T%d" % b])
                P.copy("dve", h2Tb[b][:, hf * 4:(hf + 1) * 4, :].rearrange("p k t -> p (k t)"), pT[hf][:, :], ["pT%d" % hf], ["h2Tb%d" % b])
            P.load("sp", T["h2T"].rearrange("(c p) t -> p c t", p=128)[:, :, ts_], h2Tb[b][:], ["h2Tb%d" % b], ["h2Td"])
            for kc in range(8):
                P.mm(pR[:, 0:36], h2T[b][:, kc, :], wr[:, kc, :], ["h2T%d" % b, "wr"], ["pR"], start=(kc == 0), stop=(kc == 7))
            P.tt("dve", lgt[:, t, :], pR[:, 0:36], brb[:], ALU.add, ["pR", "brb"], ["lgt"])

        for t in range(NT + 1):
            if t < NT:
                s4_a(t)
            if t >= 1:
                s4_b(t - 1)
        lg = lgt[:, :, 0:4]
        le = lgt[:, :, 4:36].rearrange("p t (g e) -> p t g e", e=8)
        red = lambda o, i_, op: (lambda e: e.tensor_reduce(out=o, in_=i_, axis=AX.X, op=op))
        P.op("dve", red(mxg[:], lg, ALU.max), ["lgt"], ["mxg"])
        P.tt("dve", ohg[:], lg, mxg[:].unsqueeze(2).to_broadcast([128, NT, 4]), ALU.is_ge, ["lgt", "mxg"], ["ohg"])
        P.tt("dve", eg[:], lg, mxg[:].unsqueeze(2).to_broadcast([128, NT, 4]), ALU.subtract, ["lgt", "mxg"], ["eg"])
        P.act(eg[:], eg[:], AF.Exp, ["eg"], ["eg"])
        P.op("dve", red(sg[:], eg[:], ALU.add), ["eg"], ["sg"])
        P.op("dve", (lambda o, i_: (lambda e: e.reciprocal(out=o, in_=i_)))(sg[:], sg[:]), ["sg"], ["sg"])
        P.tt("dve", tmp4[:], le, ohg[:].unsqueeze(3).to_broadcast([128, NT, 4, 8]), ALU.mult, ["lgt", "ohg"], ["tmp4"])
        P.op("dve", red(les[:], tmp4[:].rearrange("p t g e -> p t e g"), ALU.add), ["tmp4"], ["les"])
        P.op("dve", red(m1[:], les[:], ALU.max), ["les"], ["m1"])
        P.tt("dve", oh1[:], les[:], m1[:].unsqueeze(2).to_broadcast([128, NT, 8]), ALU.is_ge, ["les", "m1"], ["oh1"])
        P.stt("dve", le2[:], oh1[:], -1e30, les[:], ALU.mult, ALU.add, ["oh1", "les"], ["le2"])
        P.op("dve", red(m2[:], le2[:], ALU.max), ["le2"], ["m2"])
        P.tt("dve", oh2[:], le2[:], m2[:].unsqueeze(2).to_broadcast([128, NT, 8]), ALU.is_ge, ["le2", "m2"], ["oh2"])
        P.tt("dve", w2[:], m2[:], m1[:], ALU.subtract, ["m1", "m2"], ["w2"])
        P.act(w2[:], w2[:], AF.Exp, ["w2"], ["w2"])
        P.ts("dve", w1[:], w2[:], 1.0, None, ALU.add, None, ["w2"], ["w1"])
        P.op("dve", (lambda o, i_: (lambda e: e.reciprocal(out=o, in_=i_)))(w1[:], w1[:]), ["w1"], ["w1"])
        P.tt("dve", w2[:], w2[:], w1[:], ALU.mult, ["w1", "w2"], ["w2"])
        P.tt("dve", w1[:], w1[:], sg[:], ALU.mult, ["w1", "sg"], ["w1"])
        P.tt("dve", w2[:], w2[:], sg[:], ALU.mult, ["w2", "sg"], ["w2"])
        wk_g, pos_i, te_i = T["wk_g"], T["pos_i"], T["te_i"]
        P.copy("dve", wk_g[:, :, 0], w1[:], ["w1"], ["wk_g"])
        P.copy("dve", wk_g[:, :, 1], w2[:], ["w2", "wk_g"], ["wk_g"])
        P.load("sp", lstr[:], T["lstrict"], [], ["lstr"])
        P.load("sp", thr[:], T["thr"].partition_broadcast(128), [], ["thr"])
        P.memset("pool", ones[:], 1.0, ["ones"])
        P.memset("pool", zer[:], 0.0, ["zer"])
        bc3 = lambda a: a.unsqueeze(3).to_broadcast([128, NT, 4, 8])
        bc2 = lambda a: a.unsqueeze(2).to_broadcast([128, NT, 4, 8])
        P.tt("dve", E1[:], bc3(ohg[:]), bc2(oh1[:]), ALU.mult, ["ohg", "oh1"], ["E1"])
        P.tt("dve", E2[:], bc3(ohg[:]), bc2(oh2[:]), ALU.mult, ["ohg", "oh2"], ["E2"])
        P.tt("dve", Es[:], E1[:].rearrange("p t g e -> p (t g e)"), E2[:].rearrange("p t g e -> p (t g e)"), ALU.add, ["E1", "E2"], ["Es"])
        for hf in range(2):
            P.mm(pO[hf][:, :], lstr[:], Es[:, hf * 512:(hf + 1) * 512], ["lstr", "Es"], ["pO%d" % hf])
            P.copy("act", within[:].rearrange("p t e -> p (t e)")[:, hf * 512:(hf + 1) * 512], pO[hf][:, :], ["pO%d" % hf], ["within"])
            P.mm(pO[2 + hf][:, :], ones[:], Es[:, hf * 512:(hf + 1) * 512], ["ones", "Es"], ["pO%d" % (2 + hf)])
            P.copy("dve", csb[:].rearrange("p t e -> p (t e)")[:, hf * 512:(hf + 1) * 512], pO[2 + hf][:, :], ["pO%d" % (2 + hf)], ["csb"])
        for e_ in range(32):
            P.op("dve", (lambda o, d0, d1: (lambda e: e.tensor_tensor_scan(out=o, data0=d0, data1=d1, initial=0.0, op0=ALU.add, op1=ALU.add)))(
                incl[:, :, e_], csb[:, :, e_], zer[:]), ["csb", "zer", "incl"], ["incl"])
        P.tt("dve", cmpb[:, 0:512].rearrange("p (e j) -> p e j", j=16), incl[:, NT - 1, :].unsqueeze(2).to_broadcast([128, 32, 16]),
             thr[:, 0:16].unsqueeze(1).to_broadcast([128, 32, 16]), ALU.is_gt, ["incl", "thr"], ["cmpb"])
        P.op("dve", red(ntl[:], cmpb[:, 0:512].rearrange("p (e j) -> p e j", j=16), ALU.add), ["cmpb"], ["ntl"])
        P.op("dve", (lambda o, d0, d1: (lambda e: e.tensor_tensor_scan(out=o, data0=d0, data1=d1, initial=0.0, op0=ALU.add, op1=ALU.add)))(
            inct[:], ntl[:], zer[:, 0:32]), ["ntl", "zer"], ["inct"])
        P.ts("dve", offe[:], inct[:], float(SUP), None, ALU.mult, None, ["inct"], ["offe"])
        P.tt("dve", offb[:], inct[:], ntl[:], ALU.subtract, ["inct", "ntl"], ["offb"])
        P.ts("dve", offb[:], offb[:], float(SUP), None, ALU.mult, None, ["offb"], ["offb"])
        P.tt("dve", Rr[:], incl[:], csb[:], ALU.subtract, ["incl", "csb"], ["Rr"])
        P.tt("dve", Rr[:], Rr[:], within[:], ALU.add, ["Rr", "within"], ["Rr"])
        P.tt("dve", Rr[:], Rr[:], offb[:].unsqueeze(1).to_broadcast([128, NT, 32]), ALU.add, ["Rr", "offb"], ["Rr"])
        for k_, Ek in enumerate((E1, E2)):
            kn = "E%d" % (k_ + 1)
            P.tt("dve", Ek[:].rearrange("p t g e -> p t (g e)"), Ek[:].rearrange("p t g e -> p t (g e)"), Rr[:], ALU.mult, [kn, "Rr"], [kn])
            P.op("dve", red(posf[:, :, k_], Ek[:].rearrange("p t g e -> p t (g e)"), ALU.add), [kn, "posf"], ["posf"])
        P.copy("dve", pos_i[:], posf[:], ["posf"], ["pos_i"])
        P.tt("dve", cmpb[:, 0:NSUP * 32].rearrange("p (j e) -> p j e", e=32), offe[:].unsqueeze(1).to_broadcast([128, NSUP, 32]),
             thr[:, 16:16 + NSUP].unsqueeze(2).to_broadcast([128, NSUP, 32]), ALU.is_le, ["offe", "thr", "cmpb"], ["cmpb"])
        P.memset("pool", tef[:], 0.0, ["tef"])
        P.op("dve", red(tef[:, 0:NSUP], cmpb[:, 0:NSUP * 32].rearrange("p (j e) -> p j e", e=32), ALU.add), ["cmpb", "tef"], ["tef"])
        P.ts("dve", tef[:], tef[:], 31.0, None, ALU.min, None, ["tef"], ["tef"])
        P.load("sp", pidx[:], T["pidx"], [], ["pidx"])
        P.ts("dve", tef[:], tef[:], 128.0, pidx[:, 0:1], ALU.mult, ALU.add, ["tef", "pidx"], ["tef"])
        P.copy("dve", te_i[:], tef[:], ["tef"], ["te_i"])
        P.tt("dve", oh1[:], oh1[:], w1[:].unsqueeze(2).to_broadcast([128, NT, 8]), ALU.mult, ["oh1", "w1"], ["oh1"])
        P.tt("dve", oh2[:], oh2[:], w2[:].unsqueeze(2).to_broadcast([128, NT, 8]), ALU.mult, ["oh2", "w2"], ["oh2"])
        P.tt("dve", gf[:], oh1[:], oh2[:], ALU.add, ["oh1", "oh2"], ["gf"])
        P.tt("dve", gts[:], ohg[:].unsqueeze(3).to_broadcast([128, NT, 4, 8]), gf[:].unsqueeze(2).to_broadcast([128, NT, 4, 8]), ALU.mult,
             ["ohg", "gf"], ["gts"])
        P.load("sp", T["gates"], gts[:].rearrange("p t g e -> p (t g e)"), ["gts"], ["gatesd"])
        return P.emit()


def stage5(nc, sems, T):
    IOA = bass.IndirectOffsetOnAxis
    with contextlib.ExitStack() as st:
        sb, ps = tens(nc, st)
        P = Prog(nc, sems)
        pos_i, te_i, wk_g = T["pos_i"], T["te_i"], T["wk_g"]
        identb = sb("identb", [128, 128], BF16)
        identf = sb("identf", [128, 128])
        gfb = sb("gfb", [128, D])
        hrow = [sb("hrow%d" % i, [128, D], BF16) for i in range(3)]
        wall = [sb("wall%d" % i, [128, 3 * 4096], BF16) for i in range(2)]
        xs = [sb("xs%d" % i, [128, D], BF16) for i in range(3)]
        XT = [sb("XT%d" % i, [128, 8, 128], BF16) for i in range(2)]
        sgl = [sb("sgl%d" % i, [128, DFF], BF16) for i in range(2)]
        hid = [sb("hid%d" % i, [128, DFF], BF16) for i in range(2)]
        hidT = [sb("hidT%d" % i, [128, 4, 128], BF16) for i in range(2)]
        ysb = [sb("ysb%d" % i, [128, D], BF16) for i in range(3)]
        yg = [sb("yg%d" % i, [128, 2, D], BF16) for i in range(2)]
        x2 = [sb("x2%d" % i, [128, D]) for i in range(2)]
        junk = sb("junk5", [128, D], BF16)
        stat = [sb("stat5%d" % i, [128, 4]) for i in range(2)]
        ot = [sb("ot%d" % i, [128, D]) for i in range(2)]
        ptx = [ps("ptx%d" % i, [128, D], BF16) for i in range(2)]
        pg = [ps("pg%d" % i, [128, 512]) for i in range(2)]
        pu = [ps("pu%d" % i, [128, 512]) for i in range(2)]
        pth = [ps("pth%d" % i, [128, D], BF16) for i in range(2)]

        P.load("sp", identf[:], T["ident"], [], ["identf"])
        P.copy("dve", identb[:], identf[:], ["identf"], ["identb"])
        P.load("sp", gfb[:], T["normf_g"].partition_broadcast(128), [], ["gfb"])
        zkeys = []
        sckeys = []
        for t in range(NT):
            hb = t % 3
            P.load("sp", hrow[hb][:], T["h2b"][t * 128:(t + 1) * 128, :], [], ["hrow%d" % hb])
            for k_ in range(2):
                key = "xs_sc%d_%d" % (t, k_)
                sckeys.append(key)
                P.dma("pool", (lambda o, off, i_: (lambda e: e.indirect_dma_start(out=o, out_offset=off, in_=i_, in_offset=None)))(
                    T["xs"], IOA(ap=pos_i[:, t, k_:k_ + 1], axis=0), hrow[hb][:]), ["hrow%d" % hb, "pos_i"] + zkeys, [key])
        ykeys = []
        rx, r2 = Rot(3), Rot(2)
        NSUB = NSUP * (SUP // 128)

        def gather_w(j):
            wb = j % 2
            P.dma("pool", (lambda o, i_, off: (lambda e: e.indirect_dma_start(out=o, out_offset=None, in_=i_, in_offset=off)))(
                wall[wb][:, :], T["wall"], IOA(ap=te_i[:, j:j + 1], axis=0)), ["te_i"], ["wall%d" % wb])

        def wviews(j):
            wb = j % 2
            return (wall[wb][:, 0:4096].rearrange("p (c f) -> p c f", f=512), wall[wb][:, 4096:8192].rearrange("p (c f) -> p c f", f=512),
                    wall[wb][:, 8192:12288].rearrange("p (c d) -> p c d", d=1024), "wall%d" % wb)

        def phase_a(n):
            j = n // 2
            wg_v, wu_v, wd_v, kw = wviews(j)
            row0 = n * 128
            xb, b2 = n % 3, n % 2
            P.load("act", xs[xb][:], T["xs"][row0:row0 + 128, :], sckeys + zkeys, ["xs%d" % xb])
            for kc in range(8):
                P.tr(ptx[b2][:, kc * 128:(kc + 1) * 128], xs[xb][:, kc * 128:(kc + 1) * 128], identb[:], ["xs%d" % xb, "identb"], ["ptx%d" % b2])
            P.copy("dve" if b2 == 0 else "act", XT[b2][:].rearrange("p k t -> p (k t)"), ptx[b2][:, :], ["ptx%d" % b2], ["XT%d" % b2])
            for kc in range(8):
                P.mm(pg[b2][:, :], XT[b2][:, kc, :], wg_v[:, kc, :], ["XT%d" % b2, kw], ["pg%d" % b2], start=(kc == 0), stop=(kc == 7))
            for kc in range(8):
                P.mm(pu[b2][:, :], XT[b2][:, kc, :], wu_v[:, kc, :], ["XT%d" % b2, kw], ["pu%d" % b2], start=(kc == 0), stop=(kc == 7))
            P.act(sgl[b2][:], pg[b2][:, :], AF.Silu, ["pg%d" % b2], ["sgl%d" % b2])
            P.tt("dve", hid[b2][:], sgl[b2][:], pu[b2][:, :], ALU.mult, ["sgl%d" % b2, "pu%d" % b2], ["hid%d" % b2])

        def phase_b(n):
            j = n // 2
            wg_v, wu_v, wd_v, kw = wviews(j)
            row0 = n * 128
            xb, b2 = n % 3, n % 2
            for fc in range(4):
                P.tr(pth[b2][:, fc * 128:(fc + 1) * 128], hid[b2][:, fc * 128:(fc + 1) * 128], identb[:], ["hid%d" % b2, "identb"], ["pth%d" % b2])
            P.copy("act" if b2 == 0 else "dve", hidT[b2][:].rearrange("p k t -> p (k t)"), pth[b2][:, 0:512], ["pth%d" % b2], ["hidT%d" % b2])
            for half, (pz, kz) in enumerate(((pg[b2], "pg%d" % b2), (pu[b2], "pu%d" % b2))):
                for fc in range(4):
                    P.mm(pz[:, :], hidT[b2][:, fc, :], wd_v[:, fc, half * 512:(half + 1) * 512], ["hidT%d" % b2, kw], [kz],
                         start=(fc == 0), stop=(fc == 3))
                P.copy("act" if half == 0 else "dve", ysb[xb][:, half * 512:(half + 1) * 512], pz[:, :], [kz], ["ysb%d" % xb])
            yk = "ys%d" % n
            ykeys.append(yk)
            P.load("sp", T["ys"][row0:row0 + 128, :], ysb[xb][:], ["ysb%d" % xb], [yk])

        gather_w(0)
        gather_w(1)
        for n in range(NSUB + 1):
            if n < NSUB:
                phase_a(n)
            if n >= 1:
                m = n - 1
                phase_b(m)
                if m % 2 == 1 and m // 2 + 2 < NSUP:
                    gather_w(m // 2 + 2)
        for t in range(NT):
            b = t % 2
            ts_ = slice(t * 128, (t + 1) * 128)
            sk = "stat5%d" % b
            for k_ in range(2):
                P.dma("pool", (lambda o, i_, off: (lambda e: e.indirect_dma_start(out=o, out_offset=None, in_=i_, in_offset=off)))(
                    yg[b][:, k_, :], T["ys"], IOA(ap=pos_i[:, t, k_:k_ + 1], axis=0)), ykeys + ["pos_i"], ["yg%d_%d" % (b, k_)])
            P.load("sp", x2[b][:], T["x2"][ts_, :], [], ["x2%d" % b])
            P.stt("dve", x2[b][:], yg[b][:, 0, :], wk_g[:, t, 0:1], x2[b][:], ALU.mult, ALU.add, ["yg%d_0" % b, "wk_g", "x2%d" % b], ["x2%d" % b])
            P.stt("dve", x2[b][:], yg[b][:, 1, :], wk_g[:, t, 1:2], x2[b][:], ALU.mult, ALU.add, ["yg%d_1" % b, "wk_g", "x2%d" % b], ["x2%d" % b])
            P.act(junk[:], x2[b][:], AF.Square, ["x2%d" % b], ["junk5", sk], accum_out=stat[b][:, 0:1])
            P.ts("dve", stat[b][:, 1:2], stat[b][:, 0:1], 1.0 / D, EPS, ALU.mult, ALU.add, [sk], [sk])
            P.act(stat[b][:, 2:3], stat[b][:, 1:2], AF.Ln, [sk], [sk])
            P.act(stat[b][:, 3:4], stat[b][:, 2:3], AF.Exp, [sk], [sk], scale=-0.5)
            P.stt("dve", ot[b][:], x2[b][:], stat[b][:, 3:4], gfb[:], ALU.mult, ALU.mult, ["x2%d" % b, sk, "gfb"], ["ot%d" % b])
            P.load("act", T["out"][ts_, :], ot[b][:], ["ot%d" % b], ["outd"])
        return P.emit()


def _rel_bucket_np(n):
    n = np.maximum(n, 0)
    max_exact = 16
    nf = np.maximum(n, 1).astype(np.float32)
    large = max_exact + (np.log(nf / np.float32(max_exact)) / np.float32(math.log(128 / max_exact)) * np.float32(16)).astype(np.int32)
    large = np.minimum(large, 31)
    return np.where(n < max_exact, n, large)


def _constants():
    ident = np.eye(128, dtype=np.float32)
    s_ = np.arange(128)[:, None]
    t_ = np.arange(128)[None, :]
    tri = (s_ <= t_).astype(np.float32)
    sel = np.zeros((4, 4, 128), np.float32)
    for h in range(4):
        sel[h, h, :] = 1.0
    oh = np.zeros((128, 2, 33, 128), np.float32)
    for kind in range(2):
        n = (t_ - s_) + 128 * kind
        bk = _rel_bucket_np(n)
        valid = n >= 0
        for b in range(32):
            oh[:, kind, b, :] = ((bk == b) & valid).astype(np.float32)
        oh[:, kind, 32, :] = (~valid).astype(np.float32)
    lstrict = (s_ < t_).astype(np.float32)
    thr = np.concatenate([np.arange(16) * SUP, np.arange(NSUP) * SUP]).astype(np.float32)
    pidx = np.arange(128, dtype=np.float32).reshape(128, 1)
    return dict(ident=ident, tri=tri, sel=sel.reshape(4, 512), oh=oh.reshape(128, -1), lstrict=lstrict, thr=thr, pidx=pidx)


_CACHE = {}


def kernel(x, w_in, conv_w, conv_b, w_mq, w_mk, w_mgate, b_mgate, m_norm_g, m_skip, lambda_qk, da_norm_g, rel_bias, w_out,
           norm1_g, norm2_g, w_rg, b_rg, w_re, b_re, w_eg, w_eu, w_ed, normf_g):
    f = lambda a: np.ascontiguousarray(np.asarray(a, dtype=np.float32))
    if "nc" not in _CACHE:
        _CACHE["nc"], _CACHE["stats"] = build_program()
    nc = _CACHE["nc"]
    shared = dict(
        w_in=f(w_in)[0], conv_w=f(conv_w)[0], conv_b=f(conv_b)[0], w_mq=f(w_mq)[0], w_mk=f(w_mk)[0], w_mgate=f(w_mgate)[0],
        b_mgate=f(b_mgate)[0], m_norm_g=f(m_norm_g)[0], m_skip=f(m_skip)[0], lambda_qk=f(lambda_qk)[0].reshape(256),
        da_norm_g=f(da_norm_g)[0], rel_bias=f(rel_bias).reshape(128), w_out=f(w_out)[0], norm1_g=f(norm1_g)[0], norm2_g=f(norm2_g)[0],
        w_r=np.ascontiguousarray(np.concatenate([f(w_rg)[0], f(w_re)[0].reshape(D, 32)], axis=1)),
        b_r=np.ascontiguousarray(np.concatenate([f(b_rg)[0], f(b_re)[0].reshape(32)])),
        w_eg=f(w_eg)[0], w_eu=f(w_eu)[0], w_ed=f(w_ed)[0], normf_g=f(normf_g),
    )
    shared.update(_constants())
    xs = f(x)
    in_maps = []
    for b in range(8):
        m = dict(shared)
        m["x"] = xs[b]
        in_maps.append(m)
    res = run_bass_kernel_spmd(nc, in_maps, core_ids=list(range(8)))
    _CACHE["res"] = res
    return np.stack([np.asarray(r["out"], dtype=np.float32) for r in res.results], axis=0)
```

```python
import math
import contextlib
import numpy as np
import concourse.bass as bass
import concourse.mybir as mybir
from concourse.bass_utils import run_bass_kernel_spmd

F32 = mybir.dt.float32
BF16 = mybir.dt.bfloat16
AF = mybir.ActivationFunctionType
ALU = mybir.AluOpType
AX = mybir.AxisListType

S = 4096
D = 1024
NT = 32
EPS = 1e-6
SUBLN_EPS = 1e-5
N_EXP = 32
DFF = 512
LAM_INIT = 0.8 - 0.6 * math.exp(-0.3 * 0)
ML_SCALE = 128.0 ** -0.5
DA_SCALE = 64.0 ** -0.5
NEG = -30000.0
SUP = 256
NSUP = 63
NSLOT = NSUP * SUP
I32 = mybir.dt.int32

COMPUTE = ("pe", "act", "dve", "pool")
QUEUES = ("sp", "act", "pool")
N_DMA_SEMS = 8
DEBUG = False
CONV_PER_GROUP = {1: 3, 2: 4, 3: 2}
STAGES = (1, 2, 3, 4, 5)


class Sems:
    def __init__(self, nc, st):
        self.esem = {e: st.enter_context(nc.semaphore("s_" + e)) for e in COMPUTE}
        self.dsem = {(q, s): st.enter_context(nc.semaphore("d_%s_%d" % (q, s))) for q in QUEUES for s in range(N_DMA_SEMS)}
        self.cnt = {e: 0 for e in COMPUTE}
        self.dcnt = {k: 0 for k in self.dsem}
        self.rr = {q: 0 for q in QUEUES}


class Op:
    __slots__ = ("eng", "fn", "deps", "is_dma", "signal", "val", "sem", "slot", "prev")

    def __init__(self, eng, fn, is_dma):
        self.eng, self.fn, self.is_dma = eng, fn, is_dma
        self.deps = []
        self.signal = False
        self.val = None
        self.sem = None
        self.slot = None
        self.prev = None


class Prog:
    def __init__(self, nc, sems):
        self.nc = nc
        self.sems = sems
        self.ops = []
        self.last_writer = {}
        self.readers = {}
        self.slot_last = {}

    def _add(self, op, reads, writes):
        pr = [r for r in reads if r in PSUM_KEYS]
        if pr:
            reads = [r for r in reads if r not in PSUM_KEYS]
            writes = list(writes) + [r for r in pr if r not in writes]
        deps = []
        for r in reads:
            w = self.last_writer.get(r)
            if w is not None:
                deps.append(w)
        for w in writes:
            lw = self.last_writer.get(w)
            if lw is not None:
                deps.append(lw)
            deps.extend(self.readers.get(w, ()))
        seen = set()
        for d in deps:
            if id(d) not in seen and d is not op:
                seen.add(id(d))
                op.deps.append(d)
        for r in reads:
            self.readers.setdefault(r, []).append(op)
        for w in writes:
            self.last_writer[w] = op
            self.readers[w] = []
        self.ops.append(op)
        return op

    def op(self, eng, fn, reads=(), writes=()):
        return self._add(Op(eng, fn, False), reads, writes)

    def dma(self, queue, fn, reads=(), writes=()):
        op = Op(queue, fn, True)
        s = self.sems
        op.slot = (queue, s.rr[queue] % N_DMA_SEMS)
        s.rr[queue] += 1
        op.prev = self.slot_last.get(op.slot)
        self.slot_last[op.slot] = op
        return self._add(op, reads, writes)

    def mm(self, out, lhsT, rhs, r, w, start=True, stop=True, skip=False):
        if skip:
            return self.op("pe", lambda e: e.matmul(out, lhsT=lhsT, rhs=rhs, start=start, stop=stop, skip_group_check=True), r, w)
        return self.op("pe", lambda e: e.matmul(out, lhsT=lhsT, rhs=rhs, start=start, stop=stop), r, w)

    def tr(self, out, in_, ident, r, w):
        return self.op("pe", lambda e: e.transpose(out=out, in_=in_, identity=ident), r, w)

    def act(self, out, in_, func, r, w, bias=None, scale=None, accum_out=None):
        kw = {}
        if bias is not None:
            kw["bias"] = bias
        if scale is not None:
            kw["scale"] = scale
        if accum_out is not None:
            kw["accum_out"] = accum_out
        return self.op("act", lambda e: e.activation(out=out, in_=in_, func=func, **kw), r, w)

    def copy(self, eng, out, in_, r, w):
        if eng == "act":
            return self.op("act", lambda e: e.copy(out=out, in_=in_), r, w)
        return self.op(eng, lambda e: e.tensor_copy(out=out, in_=in_), r, w)

    def tt(self, eng, out, in0, in1, op, r, w):
        return self.op(eng, lambda e: e.tensor_tensor(out=out, in0=in0, in1=in1, op=op), r, w)

    def ts(self, eng, out, in0, s1, s2, op0, op1, r, w):
        if s2 is None:
            return self.op(eng, lambda e: e.tensor_scalar(out=out, in0=in0, scalar1=s1, scalar2=None, op0=op0), r, w)
        return self.op(eng, lambda e: e.tensor_scalar(out=out, in0=in0, scalar1=s1, scalar2=s2, op0=op0, op1=op1), r, w)

    def stt(self, eng, out, in0, scalar, in1, op0, op1, r, w):
        eng = "dve"
        return self.op(eng, lambda e: e.scalar_tensor_tensor(out=out, in0=in0, scalar=scalar, in1=in1, op0=op0, op1=op1), r, w)

    def memset(self, eng, ap, val, w):
        return self.op(eng, lambda e: e.memset(ap, val), (), w)

    def load(self, q, out, in_, r, w):
        return self.dma(q, lambda e: e.dma_start(out=out, in_=in_), r, w)

    def emit(self):
        nc, s, ops = self.nc, self.sems, self.ops

        def same_skip(d, o):
            return (not d.is_dma) and (not o.is_dma) and d.eng == o.eng and d.eng == "pe"

        for o in ops:
            for d in o.deps:
                if d.is_dma or same_skip(d, o):
                    continue
                d.signal = True
        for o in ops:
            if o.is_dma:
                s.dcnt[o.slot] += 16
                o.val = s.dcnt[o.slot]
                o.sem = s.dsem[o.slot]
            else:
                o.sem = s.esem[o.eng]
                if o.signal:
                    s.cnt[o.eng] += 1
                    o.val = s.cnt[o.eng]
        by_eng = {e: [] for e in ("pe", "act", "dve", "pool", "sp")}
        for o in ops:
            by_eng[o.eng].append(o)
        final = dict(s.dcnt)

        def run(engname, e):
            waited = {}

            def wait(sem, val):
                if waited.get(id(sem), 0) >= val:
                    return
                waited[id(sem)] = val
                e.wait_ge(sem, val)

            for o in by_eng[engname]:
                for d in o.deps:
                    if same_skip(d, o):
                        continue
                    wait(d.sem, d.val)
                if o.is_dma and o.prev is not None:
                    wait(o.prev.sem, o.prev.val)
                ins = o.fn(e)
                if o.is_dma:
                    ins.then_inc(o.sem, 16)
                elif o.signal:
                    ins.then_inc(o.sem, 1)
            if engname == "sp":
                for k, v in final.items():
                    if v > 0:
                        wait(s.dsem[k], v)

        with nc.Block() as block:
            block.sync(lambda e: run("sp", e))
            if by_eng["pe"]:
                block.tensor(lambda e: run("pe", e))
            if by_eng["act"]:
                block.scalar(lambda e: run("act", e))
            if by_eng["dve"]:
                block.vector(lambda e: run("dve", e))
            if by_eng["pool"]:
                block.gpsimd(lambda e: run("pool", e))
        return {k: len(v) for k, v in by_eng.items()}


class Rot:
    def __init__(self, n):
        self.n, self.i = n, 0

    def next(self):
        v = self.i % self.n
        self.i += 1
        return v


def build_program():
    nc = bass.Bass("TRN2", target_bir_lowering=False)
    I = lambda name, shape, dt=F32: nc.dram_tensor(name, list(shape), dt, kind="ExternalInput").ap()
    skind = "ExternalOutput" if DEBUG else "Internal"
    SC = lambda name, shape, dt: nc.dram_tensor(name, list(shape), dt, kind=skind).ap()
    T = {}
    T["x"] = I("x", [S, D])
    T["w_in"] = I("w_in", [D, 3072])
    T["conv_w"] = I("conv_w", [4, 512])
    T["conv_b"] = I("conv_b", [512])
    T["w_mq"] = I("w_mq", [4, 128, 128])
    T["w_mk"] = I("w_mk", [4, 128, 128])
    T["w_mgate"] = I("w_mgate", [1536, 8])
    T["b_mgate"] = I("b_mgate", [8])
    T["m_norm_g"] = I("m_norm_g", [512])
    T["m_skip"] = I("m_skip", [512])
    T["lambda_qk"] = I("lambda_qk", [256])
    T["da_norm_g"] = I("da_norm_g", [128])
    T["rel_bias"] = I("rel_bias", [128])
    T["w_out"] = I("w_out", [D, D])
    T["norm1_g"] = I("norm1_g", [D])
    T["norm2_g"] = I("norm2_g", [D])
    T["w_r"] = I("w_r", [D, 36])
    T["b_r"] = I("b_r", [36])
    T["w_eg"] = I("w_eg", [N_EXP, D, DFF])
    T["w_eu"] = I("w_eu", [N_EXP, D, DFF])
    T["w_ed"] = I("w_ed", [N_EXP, DFF, D])
    T["normf_g"] = I("normf_g", [D])
    T["ident"] = I("ident", [128, 128])
    T["tri"] = I("tri", [128, 128])
    T["sel"] = I("sel", [4, 512])
    T["oh"] = I("oh", [128, 2 * 33 * 128])
    T["lstrict"] = I("lstrict", [128, 128])
    T["thr"] = I("thr", [16 + NSUP])
    T["pidx"] = I("pidx", [128, 1])
    T["out"] = nc.dram_tensor("out", [S, D], F32, kind="ExternalOutput").ap()
    T["featT"] = SC("featT", [5, 512, S], BF16)
    T["vm_tok"] = SC("vm_tok", [S, 512], BF16)
    T["vd_tok"] = SC("vd_tok", [S, 512], BF16)
    T["ymT"] = SC("ymT", [512, S], BF16)
    T["ydT"] = SC("ydT", [512, S], BF16)
    T["x2"] = SC("x2", [S, D], F32)
    T["h2T"] = SC("h2T", [D, S], BF16)
    T["gates"] = SC("gates", [128, NT * 32], F32)
    T["h2b"] = SC("h2b", [S, D], BF16)
    T["xs"] = nc.dram_tensor("xs", [NSLOT, D], BF16, kind="Internal").ap()
    T["ys"] = nc.dram_tensor("ys", [NSLOT, D], BF16, kind="Internal").ap()
    T["wall"] = nc.dram_tensor("wall", [N_EXP * 128, 3 * 4096], BF16, kind="Internal").ap()

    stats = {}
    with contextlib.ExitStack() as gst:
        gst.enter_context(nc.allow_non_contiguous_dma(reason="small strided parameter loads"))
        sems = Sems(nc, gst)
        T["biasT_sb"] = gst.enter_context(nc.sbuf_tensor("g_biasT", [128, 2, 4, 128], F32))
        T["rbb_sb"] = gst.enter_context(nc.sbuf_tensor("g_rbb", [128, 128], F32))
        T["pos_i"] = gst.enter_context(nc.sbuf_tensor("g_pos_i", [128, NT, 2], I32))
        T["te_i"] = gst.enter_context(nc.sbuf_tensor("g_te_i", [128, 64], I32))
        T["wk_g"] = gst.enter_context(nc.sbuf_tensor("g_wk", [128, NT, 2], F32))
        if 0 in STAGES:
            stats["s0"] = stage0(nc, sems, T)
        if 1 in STAGES:
            stats["s1"] = stage1(nc, sems, T)
        if 2 in STAGES:
            stats["s2"] = stage2(nc, sems, T)
        if 3 in STAGES:
            stats["s3"] = stage3(nc, sems, T)
        if 4 in STAGES:
            stats["s4"] = stage4(nc, sems, T)
        if 5 in STAGES:
            stats["s5"] = stage5(nc, sems, T)
        if 6 in STAGES:
            stats["s6"] = stage6(nc, sems, T)
    return nc, stats


_TN = [0]
PSUM_KEYS = set()


def tens(nc, st):
    _TN[0] += 1
    pre = "t%d_" % _TN[0]
    sb = lambda n, s, d=F32: st.enter_context(nc.sbuf_tensor(pre + n, list(s), d))
    def ps(n, s, d=F32):
        PSUM_KEYS.add(n)
        return st.enter_context(nc.psum_tensor(pre + n, list(s), d))
    return sb, ps


def conv_jobs():
    return [(name, m, e) for m, name in enumerate(("w_eg", "w_eu", "w_ed")) for e in range(N_EXP)]


class Conv:
    def __init__(self, P, sb, T, engs=("pool",), queues=("sp", "sp"), nb=3):
        self.P, self.T = P, T
        self.stg = [sb("w0s%d" % i, [128, 8, 512], F32) for i in range(nb)]
        self.cvt = [sb("w0c%d" % i, [128, 8, 512], BF16) for i in range(nb)]
        self.rot = Rot(nb)
        self.engs, self.queues = engs, queues
        self.jobs = conv_jobs()
        self.k = 0

    def emit(self, n):
        P, T = self.P, self.T
        for _ in range(n):
            if self.k >= len(self.jobs):
                return
            name, m, e = self.jobs[self.k]
            b = self.rot.next()
            src = T[name][e].rearrange("(c p) f -> p c f", p=128)
            dstap = T["wall"][e * 128:(e + 1) * 128, m * 4096:(m + 1) * 4096].rearrange("p (c f) -> p c f", f=512)
            sv = self.stg[b][:].rearrange("p (c h) f -> p c (h f)", c=4) if name == "w_ed" else self.stg[b][:]
            P.load(self.queues[0], sv, src, [], ["stg%d" % b])
            P.copy(self.engs[self.k % len(self.engs)], self.cvt[b][:], self.stg[b][:], ["stg%d" % b], ["cvt%d" % b])
            P.load(self.queues[1], dstap, self.cvt[b][:], ["cvt%d" % b], ["wall"])
            self.k += 1


class ConvD:
    def __init__(self, P, T):
        self.P, self.T = P, T
        self.jobs = conv_jobs()
        self.k = 0

    def emit(self, n):
        P, T = self.P, self.T
        for _ in range(n):
            if self.k >= len(self.jobs):
                return
            name, m, e = self.jobs[self.k]
            cols = T["wall"][e * 128:(e + 1) * 128, m * 4096:(m + 1) * 4096]
            if name == "w_ed":
                src = T[name][e].rearrange("(c p) d -> p c d", p=128)
                dst = cols.rearrange("p (c d) -> p c d", d=1024)
            else:
                src = T[name][e].rearrange("(c p) f -> p c f", p=128)
                dst = cols.rearrange("p (c f) -> p c f", f=512)
            P.load("pool", dst, src, [], ["wall%d" % self.k])
            self.k += 1


def stage0(nc, sems, T):
    with contextlib.ExitStack() as st:
        sb, ps = tens(nc, st)
        P = Prog(nc, sems)
        cv = Conv(P, sb, T, engs=("dve", "pool", "act"), queues=("sp", "act"))
        cv.emit(96)
        return P.emit()


def stage1(nc, sems, T):
    with contextlib.ExitStack() as st:
        sb, ps = tens(nc, st)
        P = Prog(nc, sems)
        ident = sb("ident", [128, 128])
        identb = sb("identb", [128, 128], BF16)
        g1b = sb("g1b", [128, D])
        w_bf = sb("w_in_bf", [128, 8, 3072], BF16)
        xt = [sb("xt%d" % i, [128, D]) for i in range(2)]
        junk = sb("junk", [128, D], BF16)
        stat = sb("stat", [128, 4])
        xn = [sb("xn%d" % i, [128, D], BF16) for i in range(2)]
        hT = [sb("hT%d" % i, [128, 8, 512], BF16) for i in range(2)]
        fstg = [sb("fstg%d" % i, [128, 4, 512], BF16) for i in range(2)]
        tstg = [sb("tstg%d" % i, [128, 4, 512], BF16) for i in range(2)]
        pt = [ps("pt%d" % i, [128, D], BF16) for i in range(2)]
        pp = [ps("pp%d" % i, [128, 512]) for i in range(4)]

        P.load("sp", ident[:], T["ident"], [], ["ident"])
        P.copy("dve", identb[:], ident[:], ["ident"], ["identb"])
        P.load("act", g1b[:], T["norm1_g"].partition_broadcast(128), [], ["g1b"])
        for kc in range(8):
            P.load("pool", w_bf[:, kc, :], T["w_in"][kc * 128:(kc + 1) * 128, :], [], ["w_bf"])
        oh = sb("oh", [128, 2, 33, 128])
        rbb, biasT = T["rbb_sb"], T["biasT_sb"]
        P.load("act", oh[:].rearrange("p a b c -> p (a b c)"), T["oh"], [], ["oh"])
        P.load("act", rbb[:], T["rel_bias"].partition_broadcast(128), [], ["rbb"])

        def emit_bias(idx):
            kind, h = idx // 4, idx % 4
            dst_ = biasT[:, kind, h, :]
            kb = "biasT%d%d" % (kind, h)
            P.ts("pool", dst_, oh[:, kind, 32, :], NEG, None, ALU.mult, None, ["oh"], [kb])
            for b_ in range(32):
                P.stt("dve", dst_, oh[:, kind, b_, :], rbb[:, b_ * 4 + h:b_ * 4 + h + 1], dst_, ALU.mult, ALU.add, ["oh", "rbb", kb], [kb])

        rpp = Rot(4)
        cvj = ConvD(P, T)

        def s1_a(g):
            hb = g % 2
            emit_bias(g)
            cvj.emit(CONV_PER_GROUP[1])
            for ti in range(4):
                t = g * 4 + ti
                b = t % 2
                P.load("sp", xt[b][:], T["x"][t * 128:(t + 1) * 128, :], [], ["xt%d" % b])
                P.act(junk[:], xt[b][:], AF.Square, ["xt%d" % b], ["junk", "stat"], accum_out=stat[:, 0:1])
                P.ts("dve", stat[:, 1:2], stat[:, 0:1], 1.0 / D, EPS, ALU.mult, ALU.add, ["stat"], ["stat"])
                P.act(stat[:, 2:3], stat[:, 1:2], AF.Ln, ["stat"], ["stat"])
                P.act(stat[:, 3:4], stat[:, 2:3], AF.Exp, ["stat"], ["stat"], scale=-0.5)
                P.stt("dve", xn[b][:], xt[b][:], stat[:, 3:4], g1b[:], ALU.mult, ALU.mult, ["xt%d" % b, "stat", "g1b"], ["xn%d" % b])
                for kc in range(8):
                    P.tr(pt[b][:, kc * 128:(kc + 1) * 128], xn[b][:, kc * 128:(kc + 1) * 128], identb[:], ["xn%d" % b, "identb"], ["pt%d" % b])
                P.copy("act" if ti % 2 == 0 else "dve", hT[hb][:, :, ti * 128:(ti + 1) * 128], pt[b][:, :].rearrange("p (k t) -> p k t", k=8),
                       ["pt%d" % b], ["hT%d" % hb])

        def s1_b(g):
            hb = g % 2
            for blk in range(5):
                fb = (g * 5 + blk) % 2
                for ch in range(4):
                    col0 = blk * 512 + ch * 128
                    pb = rpp.next()
                    for kc in range(8):
                        P.mm(pp[pb][:, :], w_bf[:, kc, col0:col0 + 128], hT[hb][:, kc, :], ["w_bf", "hT%d" % hb], ["pp%d" % pb],
                             start=(kc == 0), stop=(kc == 7))
                    P.copy("act" if ch % 2 == 0 else "dve", fstg[fb][:, ch, :], pp[pb][:, :], ["pp%d" % pb], ["fstg%d" % fb])
                P.load("sp", T["featT"][blk].rearrange("(c p) t -> p c t", p=128)[:, :, g * 512:(g + 1) * 512], fstg[fb][:],
                       ["fstg%d" % fb], ["featT"])
            for bi, (blk, dst) in enumerate(((1, "vm_tok"), (5, "vd_tok"))):
                tb = (g * 2 + bi) % 2
                for ti in range(4):
                    pb = rpp.next()
                    for kc in range(8):
                        P.mm(pp[pb][:, :], hT[hb][:, kc, ti * 128:(ti + 1) * 128], w_bf[:, kc, blk * 512:(blk + 1) * 512],
                             ["w_bf", "hT%d" % hb], ["pp%d" % pb], start=(kc == 0), stop=(kc == 7))
                    P.copy("dve" if ti % 2 == 0 else "act", tstg[tb][:, ti, :], pp[pb][:, :], ["pp%d" % pb], ["tstg%d" % tb])
                P.load("sp", T[dst][g * 512:(g + 1) * 512, :].rearrange("(t p) f -> p t f", p=128), tstg[tb][:], ["tstg%d" % tb], [dst])

        for g in range(9):
            if g < 8:
                s1_a(g)
            if g >= 1:
                s1_b(g - 1)
        return P.emit()


def stage2(nc, sems, T):
    with contextlib.ExitStack() as st:
        sb, ps = tens(nc, st)
        P = Prog(nc, sems)
        ident = sb("ident", [128, 128])
        identb = sb("identb", [128, 128], BF16)
        tri = sb("tri", [128, 128])
        bigtri = sb("bigtri", [128, 128])
        sel = sb("sel", [4, 512])
        cw = sb("cw", [128, 4, 4])
        cb = sb("cb", [128, 4])
        mg = sb("mg", [128, 4])
        msk = sb("msk", [128, 4])
        wq = sb("wq", [128, 4, 128], BF16)
        wk = sb("wk", [128, 4, 128], BF16)
        wgt = sb("wgt", [128, 12, 8], BF16)
        bi = sb("bi", [4, 1])
        bfn = sb("bfn", [4, 1])
        zeros = sb("zeros", [4, 512])
        carryB = sb("carryB", [4, 1])
        carryM = sb("carryM", [4, 1])
        Cf = sb("Cf", [128, 4, 129])
        Cb = sb("Cb", [128, 4, 129], BF16)
        c_sb = [sb("c_sb%d" % i, [128, 4, 515], BF16) for i in range(2)]
        z_sb = [sb("z_sb%d" % i, [128, 4, 512], BF16) for i in range(2)]
        vmT = [sb("vmT%d" % i, [128, 4, 512], BF16) for i in range(2)]
        vaug = [sb("vaug%d" % i, [128, 4, 4, 129], BF16) for i in range(2)]
        cacc = [sb("cacc%d" % i, [128, 512]) for i in range(2)]
        cact = sb("cact", [128, 4, 512], BF16)
        sigz = sb("sigz", [128, 4, 512], BF16)
        scs = sb("scs", [128, 4, 512], BF16)
        qT = sb("qT", [128, 4, 512], BF16)
        kT = sb("kT", [128, 4, 512], BF16)
        ktok = sb("ktok", [128, 4, 4, 128], BF16)
        i_row = sb("i_row", [4, 512])
        e_row = sb("e_row", [4, 512])
        sp_row = sb("sp_row", [4, 512])
        Bn = sb("Bn", [4, 513])
        A_row = sb("A_row", [4, 512])
        Mx = sb("Mx", [4, 513])
        N_row = sb("N_row", [4, 512])
        cols = sb("cols", [128, 4, 3, 4])
        eN = sb("eN", [128, 4, 4])
        Mb = sb("Mb", [128, 4, 5])
        nMb = sb("nMb", [128, 4, 5])
        dec = sb("dec", [128, 4, 4])
        spa = sb("spa", [128, 4, 4])
        Mrow = sb("Mrow", [128, 4, 512])
        tmpD = [sb("tmpD%d" % i, [128, 128]) for i in range(4)]
        Dt = [sb("Dt%d" % i, [128, 128]) for i in range(4)]
        Dm = [sb("Dm%d" % i, [128, 128]) for i in range(2)]
        wT = [sb("wT%d" % i, [128, 128], BF16) for i in range(4)]
        intra = [sb("intra%d" % i, [128, 129]) for i in range(4)]
        comb = [sb("comb%d" % i, [128, 129]) for i in range(4)]
        sm = [sb("sm%d" % i, [128, 16]) for i in range(4)]
        hh = [sb("hh%d" % i, [128, 128]) for i in range(4)]
        hn = [sb("hn%d" % i, [128, 128], BF16) for i in range(4)]
        y1 = [sb("y1%d" % i, [128, 128], BF16) for i in range(4)]
        ymg = [sb("ymg%d" % i, [128, 4, 512], BF16) for i in range(2)]
        wkc = [sb("wkc%d" % i, [128, 1]) for i in range(4)]
        vw = [sb("vw%d" % i, [128, 129], BF16) for i in range(4)]
        pA = ps("pA", [128, 512])
        pB = ps("pB", [128, 512])
        pG = ps("pG", [128, 512])
        ptb = ps("ptb", [128, 1024], BF16)
        pS = [ps("pS%d" % i, [128, 512]) for i in range(2)]
        pO = [ps("pO%d" % i, [128, 512]) for i in range(2)]
        P.load("sp", ident[:], T["ident"], [], ["ident"])
        P.copy("dve", identb[:], ident[:], ["ident"], ["identb"])
        P.load("sp", tri[:], T["tri"], [], ["tri"])
        P.ts("dve", bigtri[:], tri[:], -1.0, -1.0e4, ALU.add, ALU.mult, ["tri"], ["bigtri"])
        P.load("sp", sel[:], T["sel"], [], ["sel"])
        P.load("sp", cw[:], T["conv_w"].rearrange("j (c p) -> p j c", p=128), [], ["cw"])
        P.load("sp", cb[:], T["conv_b"].rearrange("(c p) -> p c", p=128), [], ["cb"])
        P.load("sp", mg[:], T["m_norm_g"].rearrange("(c p) -> p c", p=128), [], ["mg"])
        P.load("sp", msk[:], T["m_skip"].rearrange("(c p) -> p c", p=128), [], ["msk"])
        P.load("pool", wq[:], T["w_mq"].rearrange("h d e -> d h e"), [], ["wq"])
        P.load("pool", wk[:], T["w_mk"].rearrange("h d e -> d h e"), [], ["wk"])
        P.load("pool", wgt[:], T["w_mgate"].rearrange("(c p) g -> p c g", p=128), [], ["wgt"])
        P.load("sp", bi[:], T["b_mgate"][0:4].rearrange("(p o) -> p o", o=1), [], ["bi"])
        P.load("sp", bfn[:], T["b_mgate"][4:8].rearrange("(p o) -> p o", o=1), [], ["bfn"])
        P.ts("dve", bfn[:], bfn[:], -1.0, None, ALU.mult, None, ["bfn"], ["bfn"])
        P.memset("pool", zeros[:], 0.0, ["zeros"])
        P.memset("pool", carryB[:], 0.0, ["carryB"])
        P.memset("pool", carryM[:], 0.0, ["carryM"])
        P.memset("pool", Cf[:], 0.0, ["Cf%d" % h_ for h_ in range(4)])
        P.memset("pool", Cb[:], 0.0, ["Cb%d" % h_ for h_ in range(4)])
        for i in range(2):
            P.memset("pool", vaug[i][:], 1.0, ["vaug%d" % i])
            P.memset("pool", c_sb[i][:], 0.0, ["c_sb%d" % i])

        featT = T["featT"]
        rS, rO, r2 = Rot(2), Rot(2), Rot(2)
        cvj = ConvD(P, T)
        cvj.k = 8 * CONV_PER_GROUP[1]
        for g in range(8):
            b = g % 2
            t0 = g * 512
            cvj.emit(CONV_PER_GROUP[2])
            kc_, kz, kv, kva = "c_sb%d" % b, "z_sb%d" % b, "vmT%d" % b, "vaug%d" % b
            cview = featT[0].rearrange("(c p) t -> p c t", p=128)
            if g == 0:
                P.load("sp", c_sb[b][:, :, 3:515], cview[:, :, 0:512], [], [kc_])
            else:
                P.load("sp", c_sb[b][:, :, 0:515], cview[:, :, t0 - 3:t0 + 512], [], [kc_])
            P.load("act", z_sb[b][:], featT[2].rearrange("(c p) t -> p c t", p=128)[:, :, t0:t0 + 512], [], [kz])
            P.load("act", vmT[b][:], featT[1].rearrange("(c p) t -> p c t", p=128)[:, :, t0:t0 + 512], [], [kv])
            for ti in range(4):
                P.load("sp" if ti % 2 == 0 else "act", vaug[b][:, ti, :, 0:128],
                       T["vm_tok"][t0 + ti * 128:t0 + (ti + 1) * 128, :].rearrange("p (h e) -> p h e", e=128), [kva], [kva])
            for ch in range(4):
                ab = ch % 2
                ka = "cacc%d" % ab
                e1 = "dve" if ch % 2 == 0 else "pool"
                P.ts("dve", cacc[ab][:], c_sb[b][:, ch, 0:512], cw[:, 0, ch:ch + 1], cb[:, ch:ch + 1], ALU.mult, ALU.add, [kc_, "cw", "cb"], [ka])
                for j in range(1, 4):
                    P.stt("dve" if j % 2 == 0 else "pool", cacc[ab][:], c_sb[b][:, ch, j:j + 512], cw[:, j, ch:ch + 1], cacc[ab][:], ALU.mult, ALU.add,
                          [kc_, "cw", ka], [ka])
                P.act(cact[:, ch, :], cacc[ab][:], AF.Silu, [ka], ["cact"])
                P.ts("pool", scs[:, ch, :], cact[:, ch, :], msk[:, ch:ch + 1], None, ALU.mult, None, ["cact", "msk"], ["scs"])
            P.act(sigz[:].rearrange("p c t -> p (c t)"), z_sb[b][:].rearrange("p c t -> p (c t)"), AF.Sigmoid, [kz], ["sigz"])
            for h in range(4):
                P.mm(pA[:, :], wq[:, h, :], cact[:, h, :], ["wq", "cact"], ["pA"])
                P.copy("act", qT[:, h, :], pA[:, :], ["pA"], ["qT"])
                P.mm(pB[:, :], wk[:, h, :], cact[:, h, :], ["wk", "cact"], ["pB"])
                P.copy("dve", kT[:, h, :], pB[:, :], ["pB"], ["kT"])
            for ti in range(4):
                pz = pA if ti % 2 == 0 else pB
                kz_ = "pA" if ti % 2 == 0 else "pB"
                for h in range(4):
                    P.mm(pz[:, h * 128:(h + 1) * 128], cact[:, h, ti * 128:(ti + 1) * 128], wk[:, h, :], ["cact", "wk"], [kz_])
                P.copy("act" if ti % 2 == 0 else "dve", ktok[:, ti, :, :].rearrange("p h e -> p (h e)"), pz[:, :], [kz_], ["ktok"])
            srcs = [(qT, "qT")] * 4 + [(kT, "kT")] * 4 + [(vmT[b], kv)] * 4
            for c in range(12):
                sap, skey = srcs[c]
                P.mm(pG[0:4, :], wgt[:, c, 0:4], sap[:, c % 4, :], ["wgt", skey], ["pG"], start=(c == 0), stop=(c == 11))
            for c in range(12):
                sap, skey = srcs[c]
                P.mm(pB[0:4, :], wgt[:, c, 4:8], sap[:, c % 4, :], ["wgt", skey], ["pB"], start=(c == 0), stop=(c == 11))
            P.ts("dve", i_row[:], pG[0:4, :], bi[:, 0:1], None, ALU.add, None, ["pG", "bi"], ["i_row"])
            P.act(e_row[:], pB[0:4, :], AF.Exp, ["pB", "bfn"], ["e_row"], bias=bfn[:, 0:1], scale=-1.0)
            P.act(sp_row[:], e_row[:], AF.Ln, ["e_row"], ["sp_row"], bias=1.0)
            P.op("dve", (lambda o, d0, d1, ini: (lambda e: e.tensor_tensor_scan(out=o, data0=d0, data1=d1, initial=ini, op0=ALU.add, op1=ALU.add)))(
                Bn[:, 1:513], sp_row[:], zeros[:], carryB[:, 0:1]), ["sp_row", "zeros", "carryB"], ["Bn"])
            P.tt("dve", A_row[:], i_row[:], Bn[:, 1:513], ALU.add, ["i_row", "Bn"], ["A_row"])
            P.copy("dve", Mx[:, 0:1], carryM[:, 0:1], ["carryM"], ["Mx"])
            P.op("dve", (lambda o, d0, d1, ini: (lambda e: e.tensor_tensor_scan(out=o, data0=d0, data1=d1, initial=ini, op0=ALU.max, op1=ALU.max)))(
                Mx[:, 1:513], A_row[:], A_row[:], carryM[:, 0:1]), ["A_row", "carryM", "Mx"], ["Mx"])
            P.copy("dve", carryB[:, 0:1], Bn[:, 512:513], ["Bn"], ["carryB"])
            P.copy("dve", carryM[:, 0:1], Mx[:, 512:513], ["Mx"], ["carryM"])
            P.tt("dve", N_row[:], Bn[:, 1:513], Mx[:, 1:513], ALU.subtract, ["Bn", "Mx"], ["N_row"])
            for c in range(4):
                for k3, (rap, rkey, off) in enumerate(((A_row, "A_row", 0), (Mx, "Mx", 1), (N_row, "N_row", 0))):
                    o0 = c * 12 + k3 * 4
                    P.tr(pA[:, o0:o0 + 4], rap[:, off + c * 128: off + (c + 1) * 128], ident[0:4, 0:4], [rkey, "ident"], ["pA"])
            P.copy("dve", cols[:].rearrange("p c k h -> p (c k h)"), pA[:, 0:48], ["pA"], ["cols"])
            P.act(eN[:], cols[:, :, 2, :], AF.Exp, ["cols"], ["eN"])
            for h in range(4):
                P.mm(pB[:, h * 5:(h + 1) * 5], sel[:, h * 128:(h + 1) * 128], Mx[:, 0:513:128], ["sel", "Mx"], ["pB"])
            P.copy("dve", Mb[:].rearrange("p h c -> p (h c)"), pB[:, 0:20], ["pB"], ["Mb"])
            P.ts("dve", nMb[:], Mb[:], -1.0, None, ALU.mult, None, ["Mb"], ["nMb"])
            P.tt("dve", dec[:], Mb[:, :, 0:4], Mb[:, :, 1:5], ALU.subtract, ["Mb"], ["dec"])
            P.act(dec[:], dec[:], AF.Exp, ["dec"], ["dec"])
            P.tt("dve", spa[:], Mb[:, :, 0:4].rearrange("p h c -> p c h"), cols[:, :, 1, :], ALU.subtract, ["Mb", "cols"], ["spa"])
            P.act(spa[:], spa[:], AF.Exp, ["spa"], ["spa"])
            for h in range(4):
                pz, kz_ = (pA, "pA") if h % 2 == 0 else (pB, "pB")
                P.mm(pz[:, :], sel[:, h * 128:(h + 1) * 128], Mx[:, 1:513], ["sel", "Mx"], [kz_])
                P.tt("dve", Mrow[:, h, :].rearrange("p (c t) -> p c t", t=128), pz[:, :].rearrange("p (c t) -> p c t", t=128),
                     bigtri[:].unsqueeze(1).to_broadcast([128, 4, 128]), ALU.add, [kz_, "bigtri"], ["Mrow"])
            HB = [(pS[0], "pS0"), (pS[1], "pS1"), (pO[0], "pO0"), (pO[1], "pO1")]
            UB = [(pA, "pA"), (pB, "pB")]
            for c in range(4):
                cs = slice(c * 128, (c + 1) * 128)

                def phases(h, c=c, cs=cs):
                    hb_, kH = HB[h]
                    ub_, kU = UB[h // 2]
                    uo = (h % 2) * 256
                    Acol = cols[:, c, 0, h:h + 1]
                    ks = "sm%d" % h
                    smt = sm[h]
                    kCf, kCb = "Cf%d" % h, "Cb%d" % h

                    def p0():
                        P.mm(hb_[:, 0:128], kT[:, h, cs], qT[:, h, cs], ["kT", "qT"], [kH])
                        P.ts("dve", tmpD[h][:], Mrow[:, h, cs], Acol, 0.0, ALU.subtract, ALU.max, ["Mrow", "cols"], ["tmpD%d" % h])
                        P.act(wkc[h][:], Acol, AF.Exp, ["cols", "nMb"], ["wkc%d" % h], bias=nMb[:, h, c + 1:c + 2])

                    def p1():
                        P.act(Dt[h][:], tmpD[h][:], AF.Exp, ["tmpD%d" % h], ["Dt%d" % h], scale=-1.0)
                        P.op("act", (lambda o, i_, sc: (lambda e: e.activation(out=o, in_=i_, func=AF.Copy, scale=sc)))(
                            vw[h][:], vaug[b][:, c, h, :], wkc[h][:, 0:1]), [kva, "wkc%d" % h], ["vw%d" % h])

                    def p2():
                        P.tt("dve", wT[h][:], hb_[:, 0:128], Dt[h][:], ALU.mult, [kH, "Dt%d" % h], ["wT%d" % h])

                    def p3():
                        P.mm(hb_[:, 128:257], wT[h][:], vaug[b][:, c, h, :], ["wT%d" % h, kva], [kH])
                        P.mm(hb_[:, 257:386], qT[:, h, cs], Cb[:, h, :], ["qT", kCb], [kH])
                        P.mm(ub_[:, uo:uo + 129], ktok[:, c, h, :], vw[h][:], ["ktok", "vw%d" % h], [kU])

                    def p4():
                        P.copy("act", intra[h][:], hb_[:, 128:257], [kH], ["intra%d" % h])
                        P.stt("dve", Cf[:, h, :], Cf[:, h, :], dec[:, h, c:c + 1], ub_[:, uo:uo + 129], ALU.mult, ALU.add, [kCf, "dec", kU], [kCf])

                    def p5():
                        P.stt("dve", comb[h][:], hb_[:, 257:386], spa[:, c, h:h + 1], intra[h][:], ALU.mult, ALU.add,
                              [kH, "spa", "intra%d" % h], ["comb%d" % h])
                        P.copy("act", Cb[:, h, :], Cf[:, h, :], [kCf], [kCb])

                    def p6():
                        P.stt("dve", smt[:, 0:1], comb[h][:, 128:129], -1.0, comb[h][:, 128:129], ALU.mult, ALU.max, ["comb%d" % h], [ks])
                        P.stt("dve", smt[:, 1:2], smt[:, 0:1], ML_SCALE, eN[:, c, h:h + 1], ALU.mult, ALU.max, [ks, "eN"], [ks])
                        P.op("dve", (lambda o, i_: (lambda e: e.reciprocal(out=o, in_=i_)))(smt[:, 2:3], smt[:, 1:2]), [ks], [ks])
                        P.ts("dve", hh[h][:], comb[h][:, 0:128], smt[:, 2:3], ML_SCALE, ALU.mult, ALU.mult, ["comb%d" % h, ks], ["hh%d" % h])

                    def p7():
                        P.op("dve", (lambda o, i_: (lambda e: e.bn_stats(out=o, in_=i_)))(smt[:, 4:10], hh[h][:]), ["hh%d" % h], [ks])
                        P.op("dve", (lambda o, i_: (lambda e: e.bn_aggr(out=o, in_=i_)))(smt[:, 10:12], smt[:, 4:10]), [ks], [ks])
                        P.ts("dve", smt[:, 12:13], smt[:, 11:12], EPS, None, ALU.add, None, [ks], [ks])

                    def p8():
                        P.act(smt[:, 13:14], smt[:, 12:13], AF.Ln, [ks], [ks])
                        P.act(smt[:, 14:15], smt[:, 13:14], AF.Exp, [ks], [ks], scale=-0.5)

                    def p9():
                        P.ts("dve", hn[h][:], hh[h][:], smt[:, 10:11], smt[:, 14:15], ALU.subtract, ALU.mult, ["hh%d" % h, ks], ["hn%d" % h])

                    def p10():
                        P.tr(ptb[:, h * 128:(h + 1) * 128], hn[h][:], identb[:], ["hn%d" % h, "identb"], ["ptb"])

                    def p11():
                        P.stt("dve", y1[h][:], ptb[:, h * 128:(h + 1) * 128], mg[:, h:h + 1], scs[:, h, cs], ALU.mult, ALU.add,
                              ["ptb", "mg", "scs"], ["y1%d" % h])
                        P.tt("dve", ymg[b][:, h, cs], y1[h][:], sigz[:, h, cs], ALU.mult, ["y1%d" % h, "sigz"], ["ymg%d_%d" % (b, h)])

                    return [p0, p1, p2, p3, p4, p5, p6, p7, p8, p9, p10, p11]

                plist = [phases(h) for h in range(4)]
                for k_ in range(12):
                    for h in range(4):
                        plist[h][k_]()
            P.load("sp", T["ymT"].rearrange("(c p) t -> p c t", p=128)[:, :, t0:t0 + 512], ymg[b][:], ["ymg%d_%d" % (b, h_) for h_ in range(4)], ["ymT"])
        return P.emit()


def stage3(nc, sems, T):
    with contextlib.ExitStack() as st:
        sb, ps = tens(nc, st)
        P = Prog(nc, sems)
        ident = sb("ident", [128, 128])
        identb = sb("identb", [128, 128], BF16)
        qT = sb("qT", [128, 4, S], BF16)
        kT = sb("kT", [128, 4, S], BF16)
        vaug = sb("vaug", [128, NT, 4, 129], BF16)
        rbb = T["rbb_sb"]
        biasT = T["biasT_sb"]
        lqb = sb("lqb", [128, 256])
        lt = sb("lt", [128, 64])
        lam = sb("lam", [128, 8])
        dag = sb("dag", [128, 1])
        PT = [sb("PT%d" % i, [128, 512], BF16) for i in range(4)]
        tmpn = [sb("tmpn%d" % i, [128, 128]) for i in range(2)]
        t0s = [sb("t0s%d" % i, [128, 128]) for i in range(2)]
        av = [sb("av%d" % i, [128, 128]) for i in range(2)]
        junk = sb("junk3", [128, 128])
        sm = [sb("sm3%d" % i, [128, 8]) for i in range(4)]
        an = [sb("an%d" % i, [128, 128], BF16) for i in range(2)]
        ydg = [sb("ydg%d" % i, [128, 512], BF16) for i in range(2)]
        pS = [ps("pS%d" % i, [128, 512]) for i in range(3)]
        acc = [ps("acc%d" % i, [128, 512]) for i in range(4)]
        ptb = ps("ptb", [128, 1024], BF16)

        P.load("sp", ident[:], T["ident"], [], ["ident"])
        P.copy("dve", identb[:], ident[:], ["ident"], ["identb"])
        P.memset("pool", vaug[:], 1.0, ["vaug"])
        P.load("sp", qT[:], T["featT"][3].rearrange("(h p) t -> p h t", p=128), [], ["qT"])
        P.load("act", kT[:], T["featT"][4].rearrange("(h p) t -> p h t", p=128), [], ["kT"])
        for t in range(NT):
            P.load("sp" if t % 2 == 0 else "act", vaug[:, t, :, 0:128],
                   T["vd_tok"][t * 128:(t + 1) * 128, :].rearrange("p (h e) -> p h e", e=128), ["vaug"], ["vaug"])
        P.load("sp", lqb[:], T["lambda_qk"].partition_broadcast(128), [], ["lqb"])
        P.load("sp", dag[:], T["da_norm_g"].rearrange("(p o) -> p o", o=1), [], ["dag"])
        P.ts("dve", dag[:], dag[:], 1.0 - LAM_INIT, None, ALU.mult, None, ["dag"], ["dag"])
        for i in range(2):
            P.tt("dve", lt[:], lqb[:, (2 * i) * 64:(2 * i + 1) * 64], lqb[:, (2 * i + 1) * 64:(2 * i + 2) * 64], ALU.mult, ["lqb", "lt"], ["lt"])
            P.op("dve", (lambda o, i_: (lambda e: e.reduce_sum(out=o, in_=i_, axis=AX.X)))(lam[:, 4 + i:5 + i], lt[:]), ["lt"], ["lam"])
        P.act(lam[:, 0:2], lam[:, 4:6], AF.Exp, ["lam"], ["lam"])
        P.tt("dve", lam[:, 2:3], lam[:, 0:1], lam[:, 1:2], ALU.subtract, ["lam"], ["lam"])
        P.ts("dve", lam[:, 3:4], lam[:, 2:3], LAM_INIT, -1.0, ALU.add, ALU.mult, ["lam"], ["lam"])
        rS, rP, r2 = Rot(3), Rot(4), Rot(2)
        cvj = ConvD(P, T)
        cvj.k = 8 * (CONV_PER_GROUP[1] + CONV_PER_GROUP[2])
        its = [(h, g, c, j) for h in range(4) for g in range(8) for c in range(2) for j in range(4 * g + 4)]

        def emit_S(it):
            h, g, c, j = it
            prow = slice(c * 64, (c + 1) * 64)
            i_lo = max(j, 4 * g) - 4 * g
            sB = rS.next()
            pb = rP.next()
            kS, kP = "pS%d" % sB, "PT%d" % pb
            P.mm(pS[sB][:, i_lo * 128:512], kT[prow, h, j * 128:(j + 1) * 128], qT[prow, h, g * 512 + i_lo * 128:(g + 1) * 512],
                 ["kT", "qT"], [kS])
            far_lo = None
            for i in range(i_lo, 4):
                dist = 4 * g + i - j
                if dist >= 2:
                    far_lo = i
                    break
                n2 = r2.next()
                P.stt("dve", tmpn[n2][:], pS[sB][:, i * 128:(i + 1) * 128], DA_SCALE, biasT[:, dist, h, :], ALU.mult, ALU.add,
                      [kS, "biasT"], ["tmpn%d" % n2])
                P.act(PT[pb][:, i * 128:(i + 1) * 128], tmpn[n2][:], AF.Exp, ["tmpn%d" % n2], [kP])
            if far_lo is not None:
                P.act(PT[pb][:, far_lo * 128:512], pS[sB][:, far_lo * 128:512], AF.Exp, [kS, "rbb"], [kP],
                      bias=rbb[:, 31 * 4 + h:31 * 4 + h + 1], scale=DA_SCALE)
            return pb, i_lo

        def emit_AV(it, pb, i_lo):
            h, g, c, j = it
            kP = "PT%d" % pb
            if c == 0 and j == 0:
                cvj.emit(CONV_PER_GROUP[3])
                for a_ in range(4):
                    P.memset("dve", acc[a_][:, :], 0.0, ["acc%d" % a_])
            for i in range(i_lo, 4):
                a_ = c * 2 + i // 2
                off = (i % 2) * 256
                P.mm(acc[a_][:, off:off + 129], PT[pb][:, i * 128:(i + 1) * 128], vaug[:, j, h, :], [kP, "vaug"], ["acc%d" % a_],
                     start=False, stop=False, skip=True)
            if c == 1 and j == 4 * g + 3:
                finalize(h, g)

        def finalize(h, g):
            yb = (h * 8 + g) % 2
            for i in range(4):
                n2 = r2.next()
                ks = "sm3%d" % n2
                smt = sm[n2]
                a0, a1 = acc[i // 2], acc[2 + i // 2]
                k0, k1 = "acc%d" % (i // 2), "acc%d" % (2 + i // 2)
                off = (i % 2) * 256
                P.op("dve", (lambda o, i_: (lambda e: e.reciprocal(out=o, in_=i_)))(smt[:, 0:1], a0[:, off + 128:off + 129]), [k0], [ks])
                P.op("dve", (lambda o, i_: (lambda e: e.reciprocal(out=o, in_=i_)))(smt[:, 1:2], a1[:, off + 128:off + 129]), [k1], [ks])
                P.tt("dve", smt[:, 2:3], smt[:, 1:2], lam[:, 3:4], ALU.mult, [ks, "lam"], [ks])
                P.op("act", (lambda o, i_, sc: (lambda e: e.activation(out=o, in_=i_, func=AF.Copy, scale=sc)))(t0s[n2][:], a0[:, off:off + 128], smt[:, 0:1]),
                     [k0, ks], ["t0s%d" % n2])
                P.stt("dve", av[n2][:], a1[:, off:off + 128], smt[:, 2:3], t0s[n2][:], ALU.mult, ALU.add, [k1, ks, "t0s%d" % n2], ["av%d" % n2])
                P.act(junk[:], av[n2][:], AF.Square, ["av%d" % n2], ["junk3", ks], accum_out=smt[:, 3:4])
                P.ts("dve", smt[:, 4:5], smt[:, 3:4], 1.0 / 128, SUBLN_EPS, ALU.mult, ALU.add, [ks], [ks])
                P.act(smt[:, 5:6], smt[:, 4:5], AF.Ln, [ks], [ks])
                P.act(smt[:, 6:7], smt[:, 5:6], AF.Exp, [ks], [ks], scale=-0.5)
                P.ts("dve", an[n2][:], av[n2][:], smt[:, 6:7], None, ALU.mult, None, ["av%d" % n2, ks], ["an%d" % n2])
                P.tr(ptb[:, n2 * 512:n2 * 512 + 128], an[n2][:], identb[:], ["an%d" % n2, "identb"], ["ptb"])
                P.ts("dve", ydg[yb][:, i * 128:(i + 1) * 128], ptb[:, n2 * 512:n2 * 512 + 128], dag[:, 0:1], None, ALU.mult, None,
                     ["ptb", "dag"], ["ydg%d" % yb])
            P.load("act", T["ydT"][h * 128:(h + 1) * 128, g * 512:(g + 1) * 512], ydg[yb][:], ["ydg%d" % yb], ["ydT"])

        pend = []
        for it in its:
            pend.append((it,) + emit_S(it))
            if len(pend) > 2:
                emit_AV(*pend.pop(0))
        while pend:
            emit_AV(*pend.pop(0))
        return P.emit()


def stage4(nc, sems, T):
    with contextlib.ExitStack() as st:
        sb, ps = tens(nc, st)
        P = Prog(nc, sems)
        ident = sb("ident", [128, 128])
        wo = sb("wo", [128, 8, D], BF16)
        yT = sb("yT", [128, 8, S], BF16)
        g2b = sb("g2b", [128, D])
        wr = sb("wr", [128, 8, 36])
        brb = sb("brb", [128, 36])
        xt = [sb("xt%d" % i, [128, D]) for i in range(2)]
        x2 = [sb("x2%d" % i, [128, D]) for i in range(2)]
        junk = sb("junk4", [128, D], BF16)
        stat = [sb("stat4%d" % i, [128, 4]) for i in range(2)]
        h2 = [sb("h2%d" % i, [128, D]) for i in range(2)]
        h2T = [sb("h2T%d" % i, [128, 8, 128]) for i in range(2)]
        h2bf = [sb("h2bf%d" % i, [128, D], BF16) for i in range(2)]
        lstr = sb("lstr", [128, 128])
        ones = sb("ones", [128, 128])
        thr = sb("thr", [128, 16 + NSUP])
        E1 = sb("E1", [128, NT, 4, 8])
        E2 = sb("E2", [128, NT, 4, 8])
        Es = sb("Es", [128, NT * 32])
        within = sb("within", [128, NT, 32])
        csb = sb("csb", [128, NT, 32])
        incl = sb("incl", [128, NT, 32])
        zer = sb("zer", [128, NT])
        cmpb = sb("cmpb", [128, NSUP * 32])
        ntl = sb("ntl", [128, 32])
        inct = sb("inct", [128, 32])
        offb = sb("offb", [128, 32])
        offe = sb("offe", [128, 32])
        Rr = sb("Rr", [128, NT, 32])
        posf = sb("posf", [128, NT, 2])
        tef = sb("tef", [128, 64])
        pidx = sb("pidx", [128, 1])
        h2Tb = [sb("h2Tb%d" % i, [128, 8, 128], BF16) for i in range(2)]
        lgt = sb("lgt", [128, NT, 36])
        mxg = sb("mxg", [128, NT])
        ohg = sb("ohg", [128, NT, 4])
        eg = sb("eg", [128, NT, 4])
        sg = sb("sg", [128, NT])
        tmp4 = sb("tmp4", [128, NT, 4, 8])
        les = sb("les", [128, NT, 8])
        le2 = sb("le2", [128, NT, 8])
        m1 = sb("m1", [128, NT])
        m2 = sb("m2", [128, NT])
        oh1 = sb("oh1", [128, NT, 8])
        oh2 = sb("oh2", [128, NT, 8])
        w1 = sb("w1", [128, NT])
        w2 = sb("w2", [128, NT])
        gf = sb("gf", [128, NT, 8])
        gts = sb("gts", [128, NT, 4, 8])
        pO = [ps("pO%d" % i, [128, 512]) for i in range(4)]
        pT = [ps("pT%d" % i, [128, 512]) for i in range(2)]
        pR = ps("pR", [128, 512])

        P.load("sp", ident[:], T["ident"], [], ["ident"])
        zt = sb("zt", [128, 3, D], BF16)
        P.memset("pool", zt[:], 0.0, ["zt"])
        xs_v = T["xs"].rearrange("(n p) d -> p n d", p=128)
        for i in range(42):
            P.load("pool", xs_v[:, i * 3:(i + 1) * 3, :], zt[:], ["zt"], ["xs_zero%d" % i])
        P.load("pool", wo[:], T["w_out"].rearrange("(c p) n -> p c n", p=128), [], ["wo"])
        P.load("sp", yT[:, 0:4, :], T["ymT"].rearrange("(c p) t -> p c t", p=128), [], ["yT"])
        P.load("act", yT[:, 4:8, :], T["ydT"].rearrange("(c p) t -> p c t", p=128), [], ["yT"])
        P.load("sp", g2b[:], T["norm2_g"].partition_broadcast(128), [], ["g2b"])
        P.load("sp", wr[:], T["w_r"].rearrange("(c p) n -> p c n", p=128), [], ["wr"])
        P.load("sp", brb[:], T["b_r"].partition_broadcast(128), [], ["brb"])
        rO = Rot(2)

        def s4_a(t):
            b = t % 2
            ts_ = slice(t * 128, (t + 1) * 128)
            P.load("sp", xt[b][:], T["x"][ts_, :], [], ["xt%d" % b])
            for half in range(2):
                pb = rO.next() * 2 + half
                for kc in range(8):
                    P.mm(pO[pb][:, :], yT[:, kc, ts_], wo[:, kc, half * 512:(half + 1) * 512], ["yT", "wo"], ["pO%d" % pb], start=(kc == 0), stop=(kc == 7))
                P.tt("dve", x2[b][:, half * 512:(half + 1) * 512], pO[pb][:, :], xt[b][:, half * 512:(half + 1) * 512], ALU.add,
                     ["pO%d" % pb, "xt%d" % b], ["x2%d" % b])
            P.load("sp", T["x2"][ts_, :], x2[b][:], ["x2%d" % b], ["x2d"])
            sk = "stat4%d" % b
            P.act(junk[:], x2[b][:], AF.Square, ["x2%d" % b], ["junk4", sk], accum_out=stat[b][:, 0:1])
            P.ts("dve", stat[b][:, 1:2], stat[b][:, 0:1], 1.0 / D, EPS, ALU.mult, ALU.add, [sk], [sk])
            P.act(stat[b][:, 2:3], stat[b][:, 1:2], AF.Ln, [sk], [sk])
            P.act(stat[b][:, 3:4], stat[b][:, 2:3], AF.Exp, [sk], [sk], scale=-0.5)
            P.stt("dve", h2[b][:], x2[b][:], stat[b][:, 3:4], g2b[:], ALU.mult, ALU.mult, ["x2%d" % b, sk, "g2b"], ["h2%d" % b])
            P.copy("dve", h2bf[b][:], h2[b][:], ["h2%d" % b], ["h2bf%d" % b])
            P.load("sp", T["h2b"][ts_, :], h2bf[b][:], ["h2bf%d" % b], ["h2bd"])

        def s4_b(t):
            b = t % 2
            ts_ = slice(t * 128, (t + 1) * 128)
            for kc in range(8):
                pz = pT[kc // 4]
                P.tr(pz[:, (kc % 4) * 128:(kc % 4 + 1) * 128], h2[b][:, kc * 128:(kc + 1) * 128], ident[:], ["h2%d" % b, "ident"], ["pT%d" % (kc // 4)])
            for hf in range(2):
                P.copy("act", h2T[b][:, hf * 4:(hf + 1) * 4, :].rearrange("p k t -> p (k t)"), pT[hf][:, :], ["pT%d" % hf], ["h2T%d" % b])
                P.copy("dve", h2Tb[b][:, hf * 4:(hf + 1) * 4, :].rearrange("p k t -> p (k t)"), pT[hf][:, :], ["pT%d" % hf], ["h2Tb%d" % b])
            P.load("sp", T["h2T"].rearrange("(c p) t -> p c t", p=128)[:, :, ts_], h2Tb[b][:], ["h2Tb%d" % b], ["h2Td"])
            for kc in range(8):
                P.mm(pR[:, 0:36], h2T[b][:, kc, :], wr[:, kc, :], ["h2T%d" % b, "wr"], ["pR"], start=(kc == 0), stop=(kc == 7))
            P.tt("dve", lgt[:, t, :], pR[:, 0:36], brb[:], ALU.add, ["pR", "brb"], ["lgt"])

        for t in range(NT + 1):
            if t < NT:
                s4_a(t)
            if t >= 1:
                s4_b(t - 1)
        lg = lgt[:, :, 0:4]
        le = lgt[:, :, 4:36].rearrange("p t (g e) -> p t g e", e=8)
        red = lambda o, i_, op: (lambda e: e.tensor_reduce(out=o, in_=i_, axis=AX.X, op=op))
        P.op("dve", red(mxg[:], lg, ALU.max), ["lgt"], ["mxg"])
        P.tt("dve", ohg[:], lg, mxg[:].unsqueeze(2).to_broadcast([128, NT, 4]), ALU.is_ge, ["lgt", "mxg"], ["ohg"])
        P.tt("dve", eg[:], lg, mxg[:].unsqueeze(2).to_broadcast([128, NT, 4]), ALU.subtract, ["lgt", "mxg"], ["eg"])
        P.act(eg[:], eg[:], AF.Exp, ["eg"], ["eg"])
        P.op("dve", red(sg[:], eg[:], ALU.add), ["eg"], ["sg"])
        P.op("dve", (lambda o, i_: (lambda e: e.reciprocal(out=o, in_=i_)))(sg[:], sg[:]), ["sg"], ["sg"])
        P.tt("dve", tmp4[:], le, ohg[:].unsqueeze(3).to_broadcast([128, NT, 4, 8]), ALU.mult, ["lgt", "ohg"], ["tmp4"])
        P.op("dve", red(les[:], tmp4[:].rearrange("p t g e -> p t e g"), ALU.add), ["tmp4"], ["les"])
        P.op("dve", red(m1[:], les[:], ALU.max), ["les"], ["m1"])
        P.tt("dve", oh1[:], les[:], m1[:].unsqueeze(2).to_broadcast([128, NT, 8]), ALU.is_ge, ["les", "m1"], ["oh1"])
        P.stt("dve", le2[:], oh1[:], -1e30, les[:], ALU.mult, ALU.add, ["oh1", "les"], ["le2"])
        P.op("dve", red(m2[:], le2[:], ALU.max), ["le2"], ["m2"])
        P.tt("dve", oh2[:], le2[:], m2[:].unsqueeze(2).to_broadcast([128, NT, 8]), ALU.is_ge, ["le2", "m2"], ["oh2"])
        P.tt("dve", w2[:], m2[:], m1[:], ALU.subtract, ["m1", "m2"], ["w2"])
        P.act(w2[:], w2[:], AF.Exp, ["w2"], ["w2"])
        P.ts("dve", w1[:], w2[:], 1.0, None, ALU.add, None, ["w2"], ["w1"])
        P.op("dve", (lambda o, i_: (lambda e: e.reciprocal(out=o, in_=i_)))(w1[:], w1[:]), ["w1"], ["w1"])
        P.tt("dve", w2[:], w2[:], w1[:], ALU.mult, ["w1", "w2"], ["w2"])
        P.tt("dve", w1[:], w1[:], sg[:], ALU.mult, ["w1", "sg"], ["w1"])
        P.tt("dve", w2[:], w2[:], sg[:], ALU.mult, ["w2", "sg"], ["w2"])
        wk_g, pos_i, te_i = T["wk_g"], T["pos_i"], T["te_i"]
        P.copy("dve", wk_g[:, :, 0], w1[:], ["w1"], ["wk_g"])
        P.copy("dve", wk_g[:, :, 1], w2[:], ["w2", "wk_g"], ["wk_g"])
        P.load("sp", lstr[:], T["lstrict"], [], ["lstr"])
        P.load("sp", thr[:], T["thr"].partition_broadcast(128), [], ["thr"])
        P.memset("pool", ones[:], 1.0, ["ones"])
        P.memset("pool", zer[:], 0.0, ["zer"])
        bc3 = lambda a: a.unsqueeze(3).to_broadcast([128, NT, 4, 8])
        bc2 = lambda a: a.unsqueeze(2).to_broadcast([128, NT, 4, 8])
        P.tt("dve", E1[:], bc3(ohg[:]), bc2(oh1[:]), ALU.mult, ["ohg", "oh1"], ["E1"])
        P.tt("dve", E2[:], bc3(ohg[:]), bc2(oh2[:]), ALU.mult, ["ohg", "oh2"], ["E2"])
        P.tt("dve", Es[:], E1[:].rearrange("p t g e -> p (t g e)"), E2[:].rearrange("p t g e -> p (t g e)"), ALU.add, ["E1", "E2"], ["Es"])
        for hf in range(2):
            P.mm(pO[hf][:, :], lstr[:], Es[:, hf * 512:(hf + 1) * 512], ["lstr", "Es"], ["pO%d" % hf])
            P.copy("act", within[:].rearrange("p t e -> p (t e)")[:, hf * 512:(hf + 1) * 512], pO[hf][:, :], ["pO%d" % hf], ["within"])
            P.mm(pO[2 + hf][:, :], ones[:], Es[:, hf * 512:(hf + 1) * 512], ["ones", "Es"], ["pO%d" % (2 + hf)])
            P.copy("dve", csb[:].rearrange("p t e -> p (t e)")[:, hf * 512:(hf + 1) * 512], pO[2 + hf][:, :], ["pO%d" % (2 + hf)], ["csb"])
        for e_ in range(32):
            P.op("dve", (lambda o, d0, d1: (lambda e: e.tensor_tensor_scan(out=o, data0=d0, data1=d1, initial=0.0, op0=ALU.add, op1=ALU.add)))(
                incl[:, :, e_], csb[:, :, e_], zer[:]), ["csb", "zer", "incl"], ["incl"])
        P.tt("dve", cmpb[:, 0:512].rearrange("p (e j) -> p e j", j=16), incl[:, NT - 1, :].unsqueeze(2).to_broadcast([128, 32, 16]),
             thr[:, 0:16].unsqueeze(1).to_broadcast([128, 32, 16]), ALU.is_gt, ["incl", "thr"], ["cmpb"])
        P.op("dve", red(ntl[:], cmpb[:, 0:512].rearrange("p (e j) -> p e j", j=16), ALU.add), ["cmpb"], ["ntl"])
        P.op("dve", (lambda o, d0, d1: (lambda e: e.tensor_tensor_scan(out=o, data0=d0, data1=d1, initial=0.0, op0=ALU.add, op1=ALU.add)))(
            inct[:], ntl[:], zer[:, 0:32]), ["ntl", "zer"], ["inct"])
        P.ts("dve", offe[:], inct[:], float(SUP), None, ALU.mult, None, ["inct"], ["offe"])
        P.tt("dve", offb[:], inct[:], ntl[:], ALU.subtract, ["inct", "ntl"], ["offb"])
        P.ts("dve", offb[:], offb[:], float(SUP), None, ALU.mult, None, ["offb"], ["offb"])
        P.tt("dve", Rr[:], incl[:], csb[:], ALU.subtract, ["incl", "csb"], ["Rr"])
        P.tt("dve", Rr[:], Rr[:], within[:], ALU.add, ["Rr", "within"], ["Rr"])
        P.tt("dve", Rr[:], Rr[:], offb[:].unsqueeze(1).to_broadcast([128, NT, 32]), ALU.add, ["Rr", "offb"], ["Rr"])
        for k_, Ek in enumerate((E1, E2)):
            kn = "E%d" % (k_ + 1)
            P.tt("dve", Ek[:].rearrange("p t g e -> p t (g e)"), Ek[:].rearrange("p t g e -> p t (g e)"), Rr[:], ALU.mult, [kn, "Rr"], [kn])
            P.op("dve", red(posf[:, :, k_], Ek[:].rearrange("p t g e -> p t (g e)"), ALU.add), [kn, "posf"], ["posf"])
        P.copy("dve", pos_i[:], posf[:], ["posf"], ["pos_i"])
        P.tt("dve", cmpb[:, 0:NSUP * 32].rearrange("p (j e) -> p j e", e=32), offe[:].unsqueeze(1).to_broadcast([128, NSUP, 32]),
             thr[:, 16:16 + NSUP].unsqueeze(2).to_broadcast([128, NSUP, 32]), ALU.is_le, ["offe", "thr", "cmpb"], ["cmpb"])
        P.memset("pool", tef[:], 0.0, ["tef"])
        P.op("dve", red(tef[:, 0:NSUP], cmpb[:, 0:NSUP * 32].rearrange("p (j e) -> p j e", e=32), ALU.add), ["cmpb", "tef"], ["tef"])
        P.ts("dve", tef[:], tef[:], 31.0, None, ALU.min, None, ["tef"], ["tef"])
        P.load("sp", pidx[:], T["pidx"], [], ["pidx"])
        P.ts("dve", tef[:], tef[:], 128.0, pidx[:, 0:1], ALU.mult, ALU.add, ["tef", "pidx"], ["tef"])
        P.copy("dve", te_i[:], tef[:], ["tef"], ["te_i"])
        P.tt("dve", oh1[:], oh1[:], w1[:].unsqueeze(2).to_broadcast([128, NT, 8]), ALU.mult, ["oh1", "w1"], ["oh1"])
        P.tt("dve", oh2[:], oh2[:], w2[:].unsqueeze(2).to_broadcast([128, NT, 8]), ALU.mult, ["oh2", "w2"], ["oh2"])
        P.tt("dve", gf[:], oh1[:], oh2[:], ALU.add, ["oh1", "oh2"], ["gf"])
        P.tt("dve", gts[:], ohg[:].unsqueeze(3).to_broadcast([128, NT, 4, 8]), gf[:].unsqueeze(2).to_broadcast([128, NT, 4, 8]), ALU.mult,
             ["ohg", "gf"], ["gts"])
        P.load("sp", T["gates"], gts[:].rearrange("p t g e -> p (t g e)"), ["gts"], ["gatesd"])
        return P.emit()


def stage5(nc, sems, T):
    IOA = bass.IndirectOffsetOnAxis
    with contextlib.ExitStack() as st:
        sb, ps = tens(nc, st)
        P = Prog(nc, sems)
        pos_i, te_i, wk_g = T["pos_i"], T["te_i"], T["wk_g"]
        identb = sb("identb", [128, 128], BF16)
        identf = sb("identf", [128, 128])
        gfb = sb("gfb", [128, D])
        hrow = [sb("hrow%d" % i, [128, D], BF16) for i in range(3)]
        wall = [sb("wall%d" % i, [128, 3 * 4096], BF16) for i in range(2)]
        xs = [sb("xs%d" % i, [128, D], BF16) for i in range(3)]
        XT = [sb("XT%d" % i, [128, 8, 128], BF16) for i in range(2)]
        sgl = [sb("sgl%d" % i, [128, DFF], BF16) for i in range(2)]
        hid = [sb("hid%d" % i, [128, DFF], BF16) for i in range(2)]
        hidT = [sb("hidT%d" % i, [128, 4, 128], BF16) for i in range(2)]
        ysb = [sb("ysb%d" % i, [128, D], BF16) for i in range(3)]
        yg = [sb("yg%d" % i, [128, 2, D], BF16) for i in range(2)]
        x2 = [sb("x2%d" % i, [128, D]) for i in range(2)]
        junk = sb("junk5", [128, D], BF16)
        stat = [sb("stat5%d" % i, [128, 4]) for i in range(2)]
        ot = [sb("ot%d" % i, [128, D]) for i in range(2)]
        ptx = [ps("ptx%d" % i, [128, D], BF16) for i in range(2)]
        pg = [ps("pg%d" % i, [128, 512]) for i in range(2)]
        pu = [ps("pu%d" % i, [128, 512]) for i in range(2)]
        pth = [ps("pth%d" % i, [128, D], BF16) for i in range(2)]

        P.load("sp", identf[:], T["ident"], [], ["identf"])
        P.copy("dve", identb[:], identf[:], ["identf"], ["identb"])
        P.load("sp", gfb[:], T["normf_g"].partition_broadcast(128), [], ["gfb"])
        zkeys = []
        sckeys = []
        for t in range(NT):
            hb = t % 3
            P.load("sp", hrow[hb][:], T["h2b"][t * 128:(t + 1) * 128, :], [], ["hrow%d" % hb])
            for k_ in range(2):
                key = "xs_sc%d_%d" % (t, k_)
                sckeys.append(key)
                P.dma("pool", (lambda o, off, i_: (lambda e: e.indirect_dma_start(out=o, out_offset=off, in_=i_, in_offset=None)))(
                    T["xs"], IOA(ap=pos_i[:, t, k_:k_ + 1], axis=0), hrow[hb][:]), ["hrow%d" % hb, "pos_i"] + zkeys, [key])
        ykeys = []
        rx, r2 = Rot(3), Rot(2)
        NSUB = NSUP * (SUP // 128)

        def gather_w(j):
            wb = j % 2
            P.dma("pool", (lambda o, i_, off: (lambda e: e.indirect_dma_start(out=o, out_offset=None, in_=i_, in_offset=off)))(
                wall[wb][:, :], T["wall"], IOA(ap=te_i[:, j:j + 1], axis=0)), ["te_i"], ["wall%d" % wb])

        def wviews(j):
            wb = j % 2
            return (wall[wb][:, 0:4096].rearrange("p (c f) -> p c f", f=512), wall[wb][:, 4096:8192].rearrange("p (c f) -> p c f", f=512),
                    wall[wb][:, 8192:12288].rearrange("p (c d) -> p c d", d=1024), "wall%d" % wb)

        def phase_a(n):
            j = n // 2
            wg_v, wu_v, wd_v, kw = wviews(j)
            row0 = n * 128
            xb, b2 = n % 3, n % 2
            P.load("act", xs[xb][:], T["xs"][row0:row0 + 128, :], sckeys + zkeys, ["xs%d" % xb])
            for kc in range(8):
                P.tr(ptx[b2][:, kc * 128:(kc + 1) * 128], xs[xb][:, kc * 128:(kc + 1) * 128], identb[:], ["xs%d" % xb, "identb"], ["ptx%d" % b2])
            P.copy("dve" if b2 == 0 else "act", XT[b2][:].rearrange("p k t -> p (k t)"), ptx[b2][:, :], ["ptx%d" % b2], ["XT%d" % b2])

        def phase_a2(n):
            j = n // 2
            wg_v, wu_v, wd_v, kw = wviews(j)
            xb, b2 = n % 3, n % 2
            for kc in range(8):
                P.mm(pg[b2][:, :], XT[b2][:, kc, :], wg_v[:, kc, :], ["XT%d" % b2, kw], ["pg%d" % b2], start=(kc == 0), stop=(kc == 7))
            for kc in range(8):
                P.mm(pu[b2][:, :], XT[b2][:, kc, :], wu_v[:, kc, :], ["XT%d" % b2, kw], ["pu%d" % b2], start=(kc == 0), stop=(kc == 7))
            P.act(sgl[b2][:], pg[b2][:, :], AF.Silu, ["pg%d" % b2], ["sgl%d" % b2])
            P.tt("dve", hid[b2][:], sgl[b2][:], pu[b2][:, :], ALU.mult, ["sgl%d" % b2, "pu%d" % b2], ["hid%d" % b2])

        def phase_b(n):
            j = n // 2
            wg_v, wu_v, wd_v, kw = wviews(j)
            row0 = n * 128
            xb, b2 = n % 3, n % 2
            for fc in range(4):
                P.tr(pth[b2][:, fc * 128:(fc + 1) * 128], hid[b2][:, fc * 128:(fc + 1) * 128], identb[:], ["hid%d" % b2, "identb"], ["pth%d" % b2])
            P.copy("act" if b2 == 0 else "dve", hidT[b2][:].rearrange("p k t -> p (k t)"), pth[b2][:, 0:512], ["pth%d" % b2], ["hidT%d" % b2])

        def phase_b2(n):
            j = n // 2
            wg_v, wu_v, wd_v, kw = wviews(j)
            row0 = n * 128
            xb, b2 = n % 3, n % 2
            for half, (pz, kz) in enumerate(((pg[b2], "pg%d" % b2), (pu[b2], "pu%d" % b2))):
                for fc in range(4):
                    P.mm(pz[:, :], hidT[b2][:, fc, :], wd_v[:, fc, half * 512:(half + 1) * 512], ["hidT%d" % b2, kw], [kz],
                         start=(fc == 0), stop=(fc == 3))
                P.copy("act" if half == 0 else "dve", ysb[xb][:, half * 512:(half + 1) * 512], pz[:, :], [kz], ["ysb%d" % xb])
            yk = "ys%d" % n
            ykeys.append(yk)
            P.load("sp", T["ys"][row0:row0 + 128, :], ysb[xb][:], ["ysb%d" % xb], [yk])

        gather_w(0)
        gather_w(1)
        for n in range(NSUB + 1):
            if n < NSUB:
                phase_a(n)
            if n >= 1:
                phase_b(n - 1)
            if n < NSUB:
                phase_a2(n)
            if n >= 1:
                m = n - 1
                phase_b2(m)
                if m % 2 == 1 and m // 2 + 2 < NSUP:
                    gather_w(m // 2 + 2)
        for t in range(NT):
            b = t % 2
            ts_ = slice(t * 128, (t + 1) * 128)
            sk = "stat5%d" % b
            for k_ in range(2):
                P.dma("pool", (lambda o, i_, off: (lambda e: e.indirect_dma_start(out=o, out_offset=None, in_=i_, in_offset=off)))(
                    yg[b][:, k_, :], T["ys"], IOA(ap=pos_i[:, t, k_:k_ + 1], axis=0)), ykeys + ["pos_i"], ["yg%d_%d" % (b, k_)])
            P.load("sp", x2[b][:], T["x2"][ts_, :], [], ["x2%d" % b])
            P.stt("dve", x2[b][:], yg[b][:, 0, :], wk_g[:, t, 0:1], x2[b][:], ALU.mult, ALU.add, ["yg%d_0" % b, "wk_g", "x2%d" % b], ["x2%d" % b])
            P.stt("dve", x2[b][:], yg[b][:, 1, :], wk_g[:, t, 1:2], x2[b][:], ALU.mult, ALU.add, ["yg%d_1" % b, "wk_g", "x2%d" % b], ["x2%d" % b])
            P.act(junk[:], x2[b][:], AF.Square, ["x2%d" % b], ["junk5", sk], accum_out=stat[b][:, 0:1])
            P.ts("dve", stat[b][:, 1:2], stat[b][:, 0:1], 1.0 / D, EPS, ALU.mult, ALU.add, [sk], [sk])
            P.act(stat[b][:, 2:3], stat[b][:, 1:2], AF.Ln, [sk], [sk])
            P.act(stat[b][:, 3:4], stat[b][:, 2:3], AF.Exp, [sk], [sk], scale=-0.5)
            P.stt("dve", ot[b][:], x2[b][:], stat[b][:, 3:4], gfb[:], ALU.mult, ALU.mult, ["x2%d" % b, sk, "gfb"], ["ot%d" % b])
            P.load("act", T["out"][ts_, :], ot[b][:], ["ot%d" % b], ["outd"])
        return P.emit()


def _rel_bucket_np(n):
    n = np.maximum(n, 0)
    max_exact = 16
    nf = np.maximum(n, 1).astype(np.float32)
    large = max_exact + (np.log(nf / np.float32(max_exact)) / np.float32(math.log(128 / max_exact)) * np.float32(16)).astype(np.int32)
    large = np.minimum(large, 31)
    return np.where(n < max_exact, n, large)


def _constants():
    ident = np.eye(128, dtype=np.float32)
    s_ = np.arange(128)[:, None]
    t_ = np.arange(128)[None, :]
    tri = (s_ <= t_).astype(np.float32)
    sel = np.zeros((4, 4, 128), np.float32)
    for h in range(4):
        sel[h, h, :] = 1.0
    oh = np.zeros((128, 2, 33, 128), np.float32)
    for kind in range(2):
        n = (t_ - s_) + 128 * kind
        bk = _rel_bucket_np(n)
        valid = n >= 0
        for b in range(32):
            oh[:, kind, b, :] = ((bk == b) & valid).astype(np.float32)
        oh[:, kind, 32, :] = (~valid).astype(np.float32)
    lstrict = (s_ < t_).astype(np.float32)
    thr = np.concatenate([np.arange(16) * SUP, np.arange(NSUP) * SUP]).astype(np.float32)
    pidx = np.arange(128, dtype=np.float32).reshape(128, 1)
    return dict(ident=ident, tri=tri, sel=sel.reshape(4, 512), oh=oh.reshape(128, -1), lstrict=lstrict, thr=thr, pidx=pidx)


_CACHE = {}


def kernel(x, w_in, conv_w, conv_b, w_mq, w_mk, w_mgate, b_mgate, m_norm_g, m_skip, lambda_qk, da_norm_g, rel_bias, w_out,
           norm1_g, norm2_g, w_rg, b_rg, w_re, b_re, w_eg, w_eu, w_ed, normf_g):
    f = lambda a: np.ascontiguousarray(np.asarray(a, dtype=np.float32))
    if "nc" not in _CACHE:
        _CACHE["nc"], _CACHE["stats"] = build_program()
    nc = _CACHE["nc"]
    shared = dict(
        w_in=f(w_in)[0], conv_w=f(conv_w)[0], conv_b=f(conv_b)[0], w_mq=f(w_mq)[0], w_mk=f(w_mk)[0], w_mgate=f(w_mgate)[0],
        b_mgate=f(b_mgate)[0], m_norm_g=f(m_norm_g)[0], m_skip=f(m_skip)[0], lambda_qk=f(lambda_qk)[0].reshape(256),
        da_norm_g=f(da_norm_g)[0], rel_bias=f(rel_bias).reshape(128), w_out=f(w_out)[0], norm1_g=f(norm1_g)[0], norm2_g=f(norm2_g)[0],
        w_r=np.ascontiguousarray(np.concatenate([f(w_rg)[0], f(w_re)[0].reshape(D, 32)], axis=1)),
        b_r=np.ascontiguousarray(np.concatenate([f(b_rg)[0], f(b_re)[0].reshape(32)])),
        w_eg=f(w_eg)[0], w_eu=f(w_eu)[0], w_ed=f(w_ed)[0], normf_g=f(normf_g),
    )
    shared.update(_constants())
    xs = f(x)
    in_maps = []
    for b in range(8):
        m = dict(shared)
        m["x"] = xs[b]
        in_maps.append(m)
    res = run_bass_kernel_spmd(nc, in_maps, core_ids=list(range(8)))
    _CACHE["res"] = res
    return np.stack([np.asarray(r["out"], dtype=np.float32) for r in res.results], axis=0)
```

```python
import math
import contextlib
import numpy as np
import concourse.bass as bass
import concourse.mybir as mybir
from concourse.bass_utils import run_bass_kernel_spmd

F32 = mybir.dt.float32
BF16 = mybir.dt.bfloat16
AF = mybir.ActivationFunctionType
ALU = mybir.AluOpType
AX = mybir.AxisListType

S = 4096
D = 1024
NT = 32
EPS = 1e-6
SUBLN_EPS = 1e-5
N_EXP = 32
DFF = 512
LAM_INIT = 0.8 - 0.6 * math.exp(-0.3 * 0)
ML_SCALE = 128.0 ** -0.5
DA_SCALE = 64.0 ** -0.5
NEG = -30000.0
SUP = 256
NSUP = 63
NSLOT = NSUP * SUP
I32 = mybir.dt.int32

COMPUTE = ("pe", "act", "dve", "pool")
QUEUES = ("sp", "act", "pool")
N_DMA_SEMS = 8
DEBUG = False
CONV_PER_GROUP = {1: 0, 2: 7, 3: 2}
STAGES = (1, 2, 3, 4, 5)


class Sems:
    def __init__(self, nc, st):
        self.esem = {e: st.enter_context(nc.semaphore("s_" + e)) for e in COMPUTE}
        self.dsem = {(q, s): st.enter_context(nc.semaphore("d_%s_%d" % (q, s))) for q in QUEUES for s in range(N_DMA_SEMS)}
        self.cnt = {e: 0 for e in COMPUTE}
        self.dcnt = {k: 0 for k in self.dsem}
        self.rr = {q: 0 for q in QUEUES}


class Op:
    __slots__ = ("eng", "fn", "deps", "is_dma", "signal", "val", "sem", "slot", "prev")

    def __init__(self, eng, fn, is_dma):
        self.eng, self.fn, self.is_dma = eng, fn, is_dma
        self.deps = []
        self.signal = False
        self.val = None
        self.sem = None
        self.slot = None
        self.prev = None


class Prog:
    def __init__(self, nc, sems):
        self.nc = nc
        self.sems = sems
        self.ops = []
        self.last_writer = {}
        self.readers = {}
        self.slot_last = {}

    def _add(self, op, reads, writes):
        pr = [r for r in reads if r in PSUM_KEYS]
        if pr:
            reads = [r for r in reads if r not in PSUM_KEYS]
            writes = list(writes) + [r for r in pr if r not in writes]
        deps = []
        for r in reads:
            w = self.last_writer.get(r)
            if w is not None:
                deps.append(w)
        for w in writes:
            lw = self.last_writer.get(w)
            if lw is not None:
                deps.append(lw)
            deps.extend(self.readers.get(w, ()))
        seen = set()
        for d in deps:
            if id(d) not in seen and d is not op:
                seen.add(id(d))
                op.deps.append(d)
        for r in reads:
            self.readers.setdefault(r, []).append(op)
        for w in writes:
            self.last_writer[w] = op
            self.readers[w] = []
        self.ops.append(op)
        return op

    def op(self, eng, fn, reads=(), writes=()):
        return self._add(Op(eng, fn, False), reads, writes)

    def dma(self, queue, fn, reads=(), writes=()):
        op = Op(queue, fn, True)
        s = self.sems
        op.slot = (queue, s.rr[queue] % N_DMA_SEMS)
        s.rr[queue] += 1
        op.prev = self.slot_last.get(op.slot)
        self.slot_last[op.slot] = op
        return self._add(op, reads, writes)

    def mm(self, out, lhsT, rhs, r, w, start=True, stop=True, skip=False):
        if skip:
            return self.op("pe", lambda e: e.matmul(out, lhsT=lhsT, rhs=rhs, start=start, stop=stop, skip_group_check=True), r, w)
        return self.op("pe", lambda e: e.matmul(out, lhsT=lhsT, rhs=rhs, start=start, stop=stop), r, w)

    def tr(self, out, in_, ident, r, w):
        return self.op("pe", lambda e: e.transpose(out=out, in_=in_, identity=ident), r, w)

    def act(self, out, in_, func, r, w, bias=None, scale=None, accum_out=None):
        kw = {}
        if bias is not None:
            kw["bias"] = bias
        if scale is not None:
            kw["scale"] = scale
        if accum_out is not None:
            kw["accum_out"] = accum_out
        return self.op("act", lambda e: e.activation(out=out, in_=in_, func=func, **kw), r, w)

    def copy(self, eng, out, in_, r, w):
        if eng == "act":
            return self.op("act", lambda e: e.copy(out=out, in_=in_), r, w)
        return self.op(eng, lambda e: e.tensor_copy(out=out, in_=in_), r, w)

    def tt(self, eng, out, in0, in1, op, r, w):
        return self.op(eng, lambda e: e.tensor_tensor(out=out, in0=in0, in1=in1, op=op), r, w)

    def ts(self, eng, out, in0, s1, s2, op0, op1, r, w):
        if s2 is None:
            return self.op(eng, lambda e: e.tensor_scalar(out=out, in0=in0, scalar1=s1, scalar2=None, op0=op0), r, w)
        return self.op(eng, lambda e: e.tensor_scalar(out=out, in0=in0, scalar1=s1, scalar2=s2, op0=op0, op1=op1), r, w)

    def stt(self, eng, out, in0, scalar, in1, op0, op1, r, w):
        eng = "dve"
        return self.op(eng, lambda e: e.scalar_tensor_tensor(out=out, in0=in0, scalar=scalar, in1=in1, op0=op0, op1=op1), r, w)

    def memset(self, eng, ap, val, w):
        return self.op(eng, lambda e: e.memset(ap, val), (), w)

    def load(self, q, out, in_, r, w):
        return self.dma(q, lambda e: e.dma_start(out=out, in_=in_), r, w)

    def emit(self):
        nc, s, ops = self.nc, self.sems, self.ops

        def same_skip(d, o):
            return (not d.is_dma) and (not o.is_dma) and d.eng == o.eng and d.eng == "pe"

        for o in ops:
            for d in o.deps:
                if d.is_dma or same_skip(d, o):
                    continue
                d.signal = True
        for o in ops:
            if o.is_dma:
                s.dcnt[o.slot] += 16
                o.val = s.dcnt[o.slot]
                o.sem = s.dsem[o.slot]
            else:
                o.sem = s.esem[o.eng]
                if o.signal:
                    s.cnt[o.eng] += 1
                    o.val = s.cnt[o.eng]
        by_eng = {e: [] for e in ("pe", "act", "dve", "pool", "sp")}
        for o in ops:
            by_eng[o.eng].append(o)
        final = dict(s.dcnt)

        def run(engname, e):
            waited = {}

            def wait(sem, val):
                if waited.get(id(sem), 0) >= val:
                    return
                waited[id(sem)] = val
                e.wait_ge(sem, val)

            for o in by_eng[engname]:
                for d in o.deps:
                    if same_skip(d, o):
                        continue
                    wait(d.sem, d.val)
                if o.is_dma and o.prev is not None:
                    wait(o.prev.sem, o.prev.val)
                ins = o.fn(e)
                if o.is_dma:
                    ins.then_inc(o.sem, 16)
                elif o.signal:
                    ins.then_inc(o.sem, 1)
            if engname == "sp":
                for k, v in final.items():
                    if v > 0:
                        wait(s.dsem[k], v)

        with nc.Block() as block:
            block.sync(lambda e: run("sp", e))
            if by_eng["pe"]:
                block.tensor(lambda e: run("pe", e))
            if by_eng["act"]:
                block.scalar(lambda e: run("act", e))
            if by_eng["dve"]:
                block.vector(lambda e: run("dve", e))
            if by_eng["pool"]:
                block.gpsimd(lambda e: run("pool", e))
        return {k: len(v) for k, v in by_eng.items()}


class Rot:
    def __init__(self, n):
        self.n, self.i = n, 0

    def next(self):
        v = self.i % self.n
        self.i += 1
        return v


def build_program():
    nc = bass.Bass("TRN2", target_bir_lowering=False)
    I = lambda name, shape, dt=F32: nc.dram_tensor(name, list(shape), dt, kind="ExternalInput").ap()
    skind = "ExternalOutput" if DEBUG else "Internal"
    SC = lambda name, shape, dt: nc.dram_tensor(name, list(shape), dt, kind=skind).ap()
    T = {}
    T["x"] = I("x", [S, D])
    T["w_in"] = I("w_in", [D, 3072])
    T["conv_w"] = I("conv_w", [4, 512])
    T["conv_b"] = I("conv_b", [512])
    T["w_mq"] = I("w_mq", [4, 128, 128])
    T["w_mk"] = I("w_mk", [4, 128, 128])
    T["w_mgate"] = I("w_mgate", [1536, 8])
    T["b_mgate"] = I("b_mgate", [8])
    T["m_norm_g"] = I("m_norm_g", [512])
    T["m_skip"] = I("m_skip", [512])
    T["lambda_qk"] = I("lambda_qk", [256])
    T["da_norm_g"] = I("da_norm_g", [128])
    T["rel_bias"] = I("rel_bias", [128])
    T["w_out"] = I("w_out", [D, D])
    T["norm1_g"] = I("norm1_g", [D])
    T["norm2_g"] = I("norm2_g", [D])
    T["w_r"] = I("w_r", [D, 36])
    T["b_r"] = I("b_r", [36])
    T["w_eg"] = I("w_eg", [N_EXP, D, DFF])
    T["w_eu"] = I("w_eu", [N_EXP, D, DFF])
    T["w_ed"] = I("w_ed", [N_EXP, DFF, D])
    T["normf_g"] = I("normf_g", [D])
    T["ident"] = I("ident", [128, 128])
    T["tri"] = I("tri", [128, 128])
    T["sel"] = I("sel", [4, 512])
    T["oh"] = I("oh", [128, 2 * 33 * 128])
    T["lstrict"] = I("lstrict", [128, 128])
    T["thr"] = I("thr", [16 + NSUP])
    T["pidx"] = I("pidx", [128, 1])
    T["out"] = nc.dram_tensor("out", [S, D], F32, kind="ExternalOutput").ap()
    T["featT"] = SC("featT", [5, 512, S], BF16)
    T["vm_tok"] = SC("vm_tok", [S, 512], BF16)
    T["vd_tok"] = SC("vd_tok", [S, 512], BF16)
    T["ymT"] = SC("ymT", [512, S], BF16)
    T["ydT"] = SC("ydT", [512, S], BF16)
    T["x2"] = SC("x2", [S, D], F32)
    T["h2T"] = SC("h2T", [D, S], BF16)
    T["gates"] = SC("gates", [128, NT * 32], F32)
    T["h2b"] = SC("h2b", [S, D], BF16)
    T["xs"] = nc.dram_tensor("xs", [NSLOT, D], BF16, kind="Internal").ap()
    T["ys"] = nc.dram_tensor("ys", [NSLOT, D], BF16, kind="Internal").ap()
    T["wall"] = nc.dram_tensor("wall", [N_EXP * 128, 3 * 4096], BF16, kind="Internal").ap()

    stats = {}
    with contextlib.ExitStack() as gst:
        gst.enter_context(nc.allow_non_contiguous_dma(reason="small strided parameter loads"))
        sems = Sems(nc, gst)
        T["biasT_sb"] = gst.enter_context(nc.sbuf_tensor("g_biasT", [128, 2, 4, 128], F32))
        T["rbb_sb"] = gst.enter_context(nc.sbuf_tensor("g_rbb", [128, 128], F32))
        T["pos_i"] = gst.enter_context(nc.sbuf_tensor("g_pos_i", [128, NT, 2], I32))
        T["te_i"] = gst.enter_context(nc.sbuf_tensor("g_te_i", [128, 64], I32))
        T["wk_g"] = gst.enter_context(nc.sbuf_tensor("g_wk", [128, NT, 2], F32))
        if 0 in STAGES:
            stats["s0"] = stage0(nc, sems, T)
        if 1 in STAGES:
            stats["s1"] = stage1(nc, sems, T)
        if 2 in STAGES:
            stats["s2"] = stage2(nc, sems, T)
        if 3 in STAGES:
            stats["s3"] = stage3(nc, sems, T)
        if 4 in STAGES:
            stats["s4"] = stage4(nc, sems, T)
        if 5 in STAGES:
            stats["s5"] = stage5(nc, sems, T)
        if 6 in STAGES:
            stats["s6"] = stage6(nc, sems, T)
    return nc, stats


_TN = [0]
PSUM_KEYS = set()


def tens(nc, st):
    _TN[0] += 1
    pre = "t%d_" % _TN[0]
    sb = lambda n, s, d=F32: st.enter_context(nc.sbuf_tensor(pre + n, list(s), d))
    def ps(n, s, d=F32):
        PSUM_KEYS.add(n)
        return st.enter_context(nc.psum_tensor(pre + n, list(s), d))
    return sb, ps


def conv_jobs():
    return [(name, m, e) for m, name in enumerate(("w_eg", "w_eu", "w_ed")) for e in range(N_EXP)]


class Conv:
    def __init__(self, P, sb, T, engs=("pool",), queues=("sp", "sp"), nb=3):
        self.P, self.T = P, T
        self.stg = [sb("w0s%d" % i, [128, 8, 512], F32) for i in range(nb)]
        self.cvt = [sb("w0c%d" % i, [128, 8, 512], BF16) for i in range(nb)]
        self.rot = Rot(nb)
        self.engs, self.queues = engs, queues
        self.jobs = conv_jobs()
        self.k = 0

    def emit(self, n):
        P, T = self.P, self.T
        for _ in range(n):
            if self.k >= len(self.jobs):
                return
            name, m, e = self.jobs[self.k]
            b = self.rot.next()
            src = T[name][e].rearrange("(c p) f -> p c f", p=128)
            dstap = T["wall"][e * 128:(e + 1) * 128, m * 4096:(m + 1) * 4096].rearrange("p (c f) -> p c f", f=512)
            sv = self.stg[b][:].rearrange("p (c h) f -> p c (h f)", c=4) if name == "w_ed" else self.stg[b][:]
            P.load(self.queues[0], sv, src, [], ["stg%d" % b])
            P.copy(self.engs[self.k % len(self.engs)], self.cvt[b][:], self.stg[b][:], ["stg%d" % b], ["cvt%d" % b])
            P.load(self.queues[1], dstap, self.cvt[b][:], ["cvt%d" % b], ["wall"])
            self.k += 1


class ConvD:
    def __init__(self, P, T):
        self.P, self.T = P, T
        self.jobs = conv_jobs()
        self.k = 0

    def emit(self, n):
        P, T = self.P, self.T
        for _ in range(n):
            if self.k >= len(self.jobs):
                return
            name, m, e = self.jobs[self.k]
            cols = T["wall"][e * 128:(e + 1) * 128, m * 4096:(m + 1) * 4096]
            if name == "w_ed":
                src = T[name][e].rearrange("(c p) d -> p c d", p=128)
                dst = cols.rearrange("p (c d) -> p c d", d=1024)
            else:
                src = T[name][e].rearrange("(c p) f -> p c f", p=128)
                dst = cols.rearrange("p (c f) -> p c f", f=512)
            P.load("pool", dst, src, [], ["wall%d" % self.k])
            self.k += 1


def stage0(nc, sems, T):
    with contextlib.ExitStack() as st:
        sb, ps = tens(nc, st)
        P = Prog(nc, sems)
        cv = Conv(P, sb, T, engs=("dve", "pool", "act"), queues=("sp", "act"))
        cv.emit(96)
        return P.emit()


def stage1(nc, sems, T):
    with contextlib.ExitStack() as st:
        sb, ps = tens(nc, st)
        P = Prog(nc, sems)
        ident = sb("ident", [128, 128])
        identb = sb("identb", [128, 128], BF16)
        g1b = sb("g1b", [128, D])
        w_bf = sb("w_in_bf", [128, 8, 3072], BF16)
        xt = [sb("xt%d" % i, [128, D]) for i in range(2)]
        junk = sb("junk", [128, D], BF16)
        stat = sb("stat", [128, 4])
        xn = [sb("xn%d" % i, [128, D], BF16) for i in range(2)]
        hT = [sb("hT%d" % i, [128, 8, 512], BF16) for i in range(2)]
        fstg = [sb("fstg%d" % i, [128, 4, 512], BF16) for i in range(2)]
        tstg = [sb("tstg%d" % i, [128, 4, 512], BF16) for i in range(2)]
        pt = [ps("pt%d" % i, [128, D], BF16) for i in range(2)]
        pp = [ps("pp%d" % i, [128, 512]) for i in range(4)]

        P.load("sp", ident[:], T["ident"], [], ["ident"])
        P.copy("dve", identb[:], ident[:], ["ident"], ["identb"])
        P.load("act", g1b[:], T["norm1_g"].partition_broadcast(128), [], ["g1b"])
        for blk in (0, 1, 2, 3, 4, 5):
            P.load("pool", w_bf[:, :, blk * 512:(blk + 1) * 512], T["w_in"][:, blk * 512:(blk + 1) * 512].rearrange("(c p) n -> p c n", p=128),
                   [], ["w_bf%d" % blk])
        oh = sb("oh", [128, 2, 33, 128])
        rbb, biasT = T["rbb_sb"], T["biasT_sb"]
        P.load("act", oh[:].rearrange("p a b c -> p (a b c)"), T["oh"], [], ["oh"])
        P.load("act", rbb[:], T["rel_bias"].partition_broadcast(128), [], ["rbb"])

        def emit_bias(idx):
            kind, h = idx // 4, idx % 4
            dst_ = biasT[:, kind, h, :]
            kb = "biasT%d%d" % (kind, h)
            P.ts("pool", dst_, oh[:, kind, 32, :], NEG, None, ALU.mult, None, ["oh"], [kb])
            for b_ in range(32):
                P.stt("dve", dst_, oh[:, kind, b_, :], rbb[:, b_ * 4 + h:b_ * 4 + h + 1], dst_, ALU.mult, ALU.add, ["oh", "rbb", kb], [kb])

        rpp = Rot(4)
        cvj = ConvD(P, T)

        def s1_a(g):
            hb = g % 2
            emit_bias(g)
            cvj.emit(CONV_PER_GROUP[1])
            for ti in range(4):
                t = g * 4 + ti
                b = t % 2
                P.load("sp", xt[b][:], T["x"][t * 128:(t + 1) * 128, :], [], ["xt%d" % b])
                P.act(junk[:], xt[b][:], AF.Square, ["xt%d" % b], ["junk", "stat"], accum_out=stat[:, 0:1])
                P.ts("dve", stat[:, 1:2], stat[:, 0:1], 1.0 / D, EPS, ALU.mult, ALU.add, ["stat"], ["stat"])
                P.act(stat[:, 2:3], stat[:, 1:2], AF.Ln, ["stat"], ["stat"])
                P.act(stat[:, 3:4], stat[:, 2:3], AF.Exp, ["stat"], ["stat"], scale=-0.5)
                P.stt("dve", xn[b][:], xt[b][:], stat[:, 3:4], g1b[:], ALU.mult, ALU.mult, ["xt%d" % b, "stat", "g1b"], ["xn%d" % b])
                for kc in range(8):
                    P.tr(pt[b][:, kc * 128:(kc + 1) * 128], xn[b][:, kc * 128:(kc + 1) * 128], identb[:], ["xn%d" % b, "identb"], ["pt%d" % b])
                P.copy("act" if ti % 2 == 0 else "dve", hT[hb][:, :, ti * 128:(ti + 1) * 128], pt[b][:, :].rearrange("p (k t) -> p k t", k=8),
                       ["pt%d" % b], ["hT%d" % hb])

        def s1_b(g):
            hb = g % 2
            for blk in range(5):
                fb = (g * 5 + blk) % 2
                for ch in range(4):
                    col0 = blk * 512 + ch * 128
                    pb = rpp.next()
                    for kc in range(8):
                        P.mm(pp[pb][:, :], w_bf[:, kc, col0:col0 + 128], hT[hb][:, kc, :], ["w_bf%d" % blk, "hT%d" % hb], ["pp%d" % pb],
                             start=(kc == 0), stop=(kc == 7))
                    P.copy("act" if ch % 2 == 0 else "dve", fstg[fb][:, ch, :], pp[pb][:, :], ["pp%d" % pb], ["fstg%d" % fb])
                P.load("sp", T["featT"][blk].rearrange("(c p) t -> p c t", p=128)[:, :, g * 512:(g + 1) * 512], fstg[fb][:],
                       ["fstg%d" % fb], ["featT"])
            for bi, (blk, dst) in enumerate(((1, "vm_tok"), (5, "vd_tok"))):
                tb = (g * 2 + bi) % 2
                for ti in range(4):
                    pb = rpp.next()
                    for kc in range(8):
                        P.mm(pp[pb][:, :], hT[hb][:, kc, ti * 128:(ti + 1) * 128], w_bf[:, kc, blk * 512:(blk + 1) * 512],
                             ["w_bf%d" % blk, "hT%d" % hb], ["pp%d" % pb], start=(kc == 0), stop=(kc == 7))
                    P.copy("dve" if ti % 2 == 0 else "act", tstg[tb][:, ti, :], pp[pb][:, :], ["pp%d" % pb], ["tstg%d" % tb])
                P.load("sp", T[dst][g * 512:(g + 1) * 512, :].rearrange("(t p) f -> p t f", p=128), tstg[tb][:], ["tstg%d" % tb], [dst])

        for g in range(9):
            if g < 8:
                s1_a(g)
            if g >= 1:
                s1_b(g - 1)
        return P.emit()


def stage2(nc, sems, T):
    with contextlib.ExitStack() as st:
        sb, ps = tens(nc, st)
        P = Prog(nc, sems)
        ident = sb("ident", [128, 128])
        identb = sb("identb", [128, 128], BF16)
        tri = sb("tri", [128, 128])
        bigtri = sb("bigtri", [128, 128])
        sel = sb("sel", [4, 512])
        cw = sb("cw", [128, 4, 4])
        cb = sb("cb", [128, 4])
        mg = sb("mg", [128, 4])
        msk = sb("msk", [128, 4])
        wq = sb("wq", [128, 4, 128], BF16)
        wk = sb("wk", [128, 4, 128], BF16)
        wgt = sb("wgt", [128, 12, 8], BF16)
        bi = sb("bi", [4, 1])
        bfn = sb("bfn", [4, 1])
        zeros = sb("zeros", [4, 512])
        carryB = sb("carryB", [4, 1])
        carryM = sb("carryM", [4, 1])
        Cf = sb("Cf", [128, 4, 129])
        Cb = sb("Cb", [128, 4, 129], BF16)
        c_sb = [sb("c_sb%d" % i, [128, 4, 515], BF16) for i in range(2)]
        z_sb = [sb("z_sb%d" % i, [128, 4, 512], BF16) for i in range(2)]
        vmT = [sb("vmT%d" % i, [128, 4, 512], BF16) for i in range(2)]
        vaug = [sb("vaug%d" % i, [128, 4, 4, 129], BF16) for i in range(2)]
        cacc = [sb("cacc%d" % i, [128, 512]) for i in range(2)]
        cact = sb("cact", [128, 4, 512], BF16)
        sigz = sb("sigz", [128, 4, 512], BF16)
        scs = sb("scs", [128, 4, 512], BF16)
        qT = sb("qT", [128, 4, 512], BF16)
        kT = sb("kT", [128, 4, 512], BF16)
        ktok = sb("ktok", [128, 4, 4, 128], BF16)
        i_row = sb("i_row", [4, 512])
        e_row = sb("e_row", [4, 512])
        sp_row = sb("sp_row", [4, 512])
        Bn = sb("Bn", [4, 513])
        A_row = sb("A_row", [4, 512])
        Mx = sb("Mx", [4, 513])
        N_row = sb("N_row", [4, 512])
        cols = sb("cols", [128, 4, 3, 4])
        eN = sb("eN", [128, 4, 4])
        Mb = sb("Mb", [128, 4, 5])
        nMb = sb("nMb", [128, 4, 5])
        dec = sb("dec", [128, 4, 4])
        spa = sb("spa", [128, 4, 4])
        Mrow = sb("Mrow", [128, 4, 512])
        tmpD = [sb("tmpD%d" % i, [128, 128]) for i in range(4)]
        Dt = [sb("Dt%d" % i, [128, 128]) for i in range(4)]
        Dm = [sb("Dm%d" % i, [128, 128]) for i in range(2)]
        wT = [sb("wT%d" % i, [128, 128], BF16) for i in range(4)]
        intra = [sb("intra%d" % i, [128, 129]) for i in range(4)]
        comb = [sb("comb%d" % i, [128, 129]) for i in range(4)]
        sm = [sb("sm%d" % i, [128, 16]) for i in range(4)]
        hh = [sb("hh%d" % i, [128, 128]) for i in range(4)]
        hn = [sb("hn%d" % i, [128, 128], BF16) for i in range(4)]
        y1 = [sb("y1%d" % i, [128, 128], BF16) for i in range(4)]
        ymg = [sb("ymg%d" % i, [128, 4, 512], BF16) for i in range(2)]
        wkc = [sb("wkc%d" % i, [128, 1]) for i in range(4)]
        vw = [sb("vw%d" % i, [128, 129], BF16) for i in range(4)]
        pA = ps("pA", [128, 512])
        pB = ps("pB", [128, 512])
        pG = ps("pG", [128, 512])
        ptb = ps("ptb", [128, 1024], BF16)
        pS = [ps("pS%d" % i, [128, 512]) for i in range(2)]
        pO = [ps("pO%d" % i, [128, 512]) for i in range(2)]
        P.load("sp", ident[:], T["ident"], [], ["ident"])
        P.copy("dve", identb[:], ident[:], ["ident"], ["identb"])
        P.load("sp", tri[:], T["tri"], [], ["tri"])
        P.ts("dve", bigtri[:], tri[:], -1.0, -1.0e4, ALU.add, ALU.mult, ["tri"], ["bigtri"])
        P.load("sp", sel[:], T["sel"], [], ["sel"])
        P.load("sp", cw[:], T["conv_w"].rearrange("j (c p) -> p j c", p=128), [], ["cw"])
        P.load("sp", cb[:], T["conv_b"].rearrange("(c p) -> p c", p=128), [], ["cb"])
        P.load("sp", mg[:], T["m_norm_g"].rearrange("(c p) -> p c", p=128), [], ["mg"])
        P.load("sp", msk[:], T["m_skip"].rearrange("(c p) -> p c", p=128), [], ["msk"])
        P.load("pool", wq[:], T["w_mq"].rearrange("h d e -> d h e"), [], ["wq"])
        P.load("pool", wk[:], T["w_mk"].rearrange("h d e -> d h e"), [], ["wk"])
        P.load("pool", wgt[:], T["w_mgate"].rearrange("(c p) g -> p c g", p=128), [], ["wgt"])
        P.load("sp", bi[:], T["b_mgate"][0:4].rearrange("(p o) -> p o", o=1), [], ["bi"])
        P.load("sp", bfn[:], T["b_mgate"][4:8].rearrange("(p o) -> p o", o=1), [], ["bfn"])
        P.ts("dve", bfn[:], bfn[:], -1.0, None, ALU.mult, None, ["bfn"], ["bfn"])
        P.memset("pool", zeros[:], 0.0, ["zeros"])
        P.memset("pool", carryB[:], 0.0, ["carryB"])
        P.memset("pool", carryM[:], 0.0, ["carryM"])
        P.memset("pool", Cf[:], 0.0, ["Cf%d" % h_ for h_ in range(4)])
        P.memset("pool", Cb[:], 0.0, ["Cb%d" % h_ for h_ in range(4)])
        for i in range(2):
            P.memset("pool", vaug[i][:], 1.0, ["vaug%d" % i])
            P.memset("pool", c_sb[i][:], 0.0, ["c_sb%d" % i])

        featT = T["featT"]
        rS, rO, r2 = Rot(2), Rot(2), Rot(2)
        cvj = ConvD(P, T)
        cvj.k = 8 * CONV_PER_GROUP[1]
        for g in range(8):
            b = g % 2
            t0 = g * 512
            cvj.emit(CONV_PER_GROUP[2])
            kc_, kz, kv, kva = "c_sb%d" % b, "z_sb%d" % b, "vmT%d" % b, "vaug%d" % b
            cview = featT[0].rearrange("(c p) t -> p c t", p=128)
            if g == 0:
                P.load("sp", c_sb[b][:, :, 3:515], cview[:, :, 0:512], [], [kc_])
            else:
                P.load("sp", c_sb[b][:, :, 0:515], cview[:, :, t0 - 3:t0 + 512], [], [kc_])
            P.load("act", z_sb[b][:], featT[2].rearrange("(c p) t -> p c t", p=128)[:, :, t0:t0 + 512], [], [kz])
            P.load("act", vmT[b][:], featT[1].rearrange("(c p) t -> p c t", p=128)[:, :, t0:t0 + 512], [], [kv])
            for ti in range(4):
                P.load("sp" if ti % 2 == 0 else "act", vaug[b][:, ti, :, 0:128],
                       T["vm_tok"][t0 + ti * 128:t0 + (ti + 1) * 128, :].rearrange("p (h e) -> p h e", e=128), [kva], [kva])
            for ch in range(4):
                ab = ch % 2
                ka = "cacc%d" % ab
                e1 = "dve" if ch % 2 == 0 else "pool"
                P.ts("dve", cacc[ab][:], c_sb[b][:, ch, 0:512], cw[:, 0, ch:ch + 1], cb[:, ch:ch + 1], ALU.mult, ALU.add, [kc_, "cw", "cb"], [ka])
                for j in range(1, 4):
                    P.stt("dve" if j % 2 == 0 else "pool", cacc[ab][:], c_sb[b][:, ch, j:j + 512], cw[:, j, ch:ch + 1], cacc[ab][:], ALU.mult, ALU.add,
                          [kc_, "cw", ka], [ka])
                P.act(cact[:, ch, :], cacc[ab][:], AF.Silu, [ka], ["cact"])
                P.ts("pool", scs[:, ch, :], cact[:, ch, :], msk[:, ch:ch + 1], None, ALU.mult, None, ["cact", "msk"], ["scs"])
            P.act(sigz[:].rearrange("p c t -> p (c t)"), z_sb[b][:].rearrange("p c t -> p (c t)"), AF.Sigmoid, [kz], ["sigz"])
            for h in range(4):
                P.mm(pA[:, :], wq[:, h, :], cact[:, h, :], ["wq", "cact"], ["pA"])
                P.copy("act", qT[:, h, :], pA[:, :], ["pA"], ["qT"])
                P.mm(pB[:, :], wk[:, h, :], cact[:, h, :], ["wk", "cact"], ["pB"])
                P.copy("dve", kT[:, h, :], pB[:, :], ["pB"], ["kT"])
            for ti in range(4):
                pz = pA if ti % 2 == 0 else pB
                kz_ = "pA" if ti % 2 == 0 else "pB"
                for h in range(4):
                    P.mm(pz[:, h * 128:(h + 1) * 128], cact[:, h, ti * 128:(ti + 1) * 128], wk[:, h, :], ["cact", "wk"], [kz_])
                P.copy("act" if ti % 2 == 0 else "dve", ktok[:, ti, :, :].rearrange("p h e -> p (h e)"), pz[:, :], [kz_], ["ktok"])
            srcs = [(qT, "qT")] * 4 + [(kT, "kT")] * 4 + [(vmT[b], kv)] * 4
            for c in range(12):
                sap, skey = srcs[c]
                P.mm(pG[0:4, :], wgt[:, c, 0:4], sap[:, c % 4, :], ["wgt", skey], ["pG"], start=(c == 0), stop=(c == 11))
            for c in range(12):
                sap, skey = srcs[c]
                P.mm(pB[0:4, :], wgt[:, c, 4:8], sap[:, c % 4, :], ["wgt", skey], ["pB"], start=(c == 0), stop=(c == 11))
            P.ts("dve", i_row[:], pG[0:4, :], bi[:, 0:1], None, ALU.add, None, ["pG", "bi"], ["i_row"])
            P.act(e_row[:], pB[0:4, :], AF.Exp, ["pB", "bfn"], ["e_row"], bias=bfn[:, 0:1], scale=-1.0)
            P.act(sp_row[:], e_row[:], AF.Ln, ["e_row"], ["sp_row"], bias=1.0)
            P.op("dve", (lambda o, d0, d1, ini: (lambda e: e.tensor_tensor_scan(out=o, data0=d0, data1=d1, initial=ini, op0=ALU.add, op1=ALU.add)))(
                Bn[:, 1:513], sp_row[:], zeros[:], carryB[:, 0:1]), ["sp_row", "zeros", "carryB"], ["Bn"])
            P.tt("dve", A_row[:], i_row[:], Bn[:, 1:513], ALU.add, ["i_row", "Bn"], ["A_row"])
            P.copy("dve", Mx[:, 0:1], carryM[:, 0:1], ["carryM"], ["Mx"])
            P.op("dve", (lambda o, d0, d1, ini: (lambda e: e.tensor_tensor_scan(out=o, data0=d0, data1=d1, initial=ini, op0=ALU.max, op1=ALU.max)))(
                Mx[:, 1:513], A_row[:], A_row[:], carryM[:, 0:1]), ["A_row", "carryM", "Mx"], ["Mx"])
            P.copy("dve", carryB[:, 0:1], Bn[:, 512:513], ["Bn"], ["carryB"])
            P.copy("dve", carryM[:, 0:1], Mx[:, 512:513], ["Mx"], ["carryM"])
            P.tt("dve", N_row[:], Bn[:, 1:513], Mx[:, 1:513], ALU.subtract, ["Bn", "Mx"], ["N_row"])
            for c in range(4):
                for k3, (rap, rkey, off) in enumerate(((A_row, "A_row", 0), (Mx, "Mx", 1), (N_row, "N_row", 0))):
                    o0 = c * 12 + k3 * 4
                    P.tr(pA[:, o0:o0 + 4], rap[:, off + c * 128: off + (c + 1) * 128], ident[0:4, 0:4], [rkey, "ident"], ["pA"])
            P.copy("dve", cols[:].rearrange("p c k h -> p (c k h)"), pA[:, 0:48], ["pA"], ["cols"])
            P.act(eN[:], cols[:, :, 2, :], AF.Exp, ["cols"], ["eN"])
            for h in range(4):
                P.mm(pB[:, h * 5:(h + 1) * 5], sel[:, h * 128:(h + 1) * 128], Mx[:, 0:513:128], ["sel", "Mx"], ["pB"])
            P.copy("dve", Mb[:].rearrange("p h c -> p (h c)"), pB[:, 0:20], ["pB"], ["Mb"])
            P.ts("dve", nMb[:], Mb[:], -1.0, None, ALU.mult, None, ["Mb"], ["nMb"])
            P.tt("dve", dec[:], Mb[:, :, 0:4], Mb[:, :, 1:5], ALU.subtract, ["Mb"], ["dec"])
            P.act(dec[:], dec[:], AF.Exp, ["dec"], ["dec"])
            P.tt("dve", spa[:], Mb[:, :, 0:4].rearrange("p h c -> p c h"), cols[:, :, 1, :], ALU.subtract, ["Mb", "cols"], ["spa"])
            P.act(spa[:], spa[:], AF.Exp, ["spa"], ["spa"])
            for h in range(4):
                pz, kz_ = (pA, "pA") if h % 2 == 0 else (pB, "pB")
                P.mm(pz[:, :], sel[:, h * 128:(h + 1) * 128], Mx[:, 1:513], ["sel", "Mx"], [kz_])
                P.tt("dve", Mrow[:, h, :].rearrange("p (c t) -> p c t", t=128), pz[:, :].rearrange("p (c t) -> p c t", t=128),
                     bigtri[:].unsqueeze(1).to_broadcast([128, 4, 128]), ALU.add, [kz_, "bigtri"], ["Mrow"])
            HB = [(pS[0], "pS0"), (pS[1], "pS1"), (pO[0], "pO0"), (pO[1], "pO1")]
            UB = [(pA, "pA"), (pB, "pB")]
            for c in range(4):
                cs = slice(c * 128, (c + 1) * 128)

                def phases(h, c=c, cs=cs):
                    hb_, kH = HB[h]
                    ub_, kU = UB[h // 2]
                    uo = (h % 2) * 256
                    Acol = cols[:, c, 0, h:h + 1]
                    ks = "sm%d" % h
                    smt = sm[h]
                    kCf, kCb = "Cf%d" % h, "Cb%d" % h

                    def p0():
                        P.mm(hb_[:, 0:128], kT[:, h, cs], qT[:, h, cs], ["kT", "qT"], [kH])
                        P.ts("dve", tmpD[h][:], Mrow[:, h, cs], Acol, 0.0, ALU.subtract, ALU.max, ["Mrow", "cols"], ["tmpD%d" % h])
                        P.act(wkc[h][:], Acol, AF.Exp, ["cols", "nMb"], ["wkc%d" % h], bias=nMb[:, h, c + 1:c + 2])

                    def p1():
                        P.act(Dt[h][:], tmpD[h][:], AF.Exp, ["tmpD%d" % h], ["Dt%d" % h], scale=-1.0)
                        P.op("act", (lambda o, i_, sc: (lambda e: e.activation(out=o, in_=i_, func=AF.Copy, scale=sc)))(
                            vw[h][:], vaug[b][:, c, h, :], wkc[h][:, 0:1]), [kva, "wkc%d" % h], ["vw%d" % h])

                    def p2():
                        P.tt("dve", wT[h][:], hb_[:, 0:128], Dt[h][:], ALU.mult, [kH, "Dt%d" % h], ["wT%d" % h])

                    def p3():
                        P.mm(hb_[:, 128:257], wT[h][:], vaug[b][:, c, h, :], ["wT%d" % h, kva], [kH])
                        P.mm(hb_[:, 257:386], qT[:, h, cs], Cb[:, h, :], ["qT", kCb], [kH])
                        P.mm(ub_[:, uo:uo + 129], ktok[:, c, h, :], vw[h][:], ["ktok", "vw%d" % h], [kU])

                    def p4():
                        P.copy("act", intra[h][:], hb_[:, 128:257], [kH], ["intra%d" % h])
                        P.stt("dve", Cf[:, h, :], Cf[:, h, :], dec[:, h, c:c + 1], ub_[:, uo:uo + 129], ALU.mult, ALU.add, [kCf, "dec", kU], [kCf])

                    def p5():
                        P.stt("dve", comb[h][:], hb_[:, 257:386], spa[:, c, h:h + 1], intra[h][:], ALU.mult, ALU.add,
                              [kH, "spa", "intra%d" % h], ["comb%d" % h])
                        P.copy("act", Cb[:, h, :], Cf[:, h, :], [kCf], [kCb])

                    def p6():
                        P.stt("dve", smt[:, 0:1], comb[h][:, 128:129], -1.0, comb[h][:, 128:129], ALU.mult, ALU.max, ["comb%d" % h], [ks])
                        P.stt("dve", smt[:, 1:2], smt[:, 0:1], ML_SCALE, eN[:, c, h:h + 1], ALU.mult, ALU.max, [ks, "eN"], [ks])
                        P.op("dve", (lambda o, i_: (lambda e: e.reciprocal(out=o, in_=i_)))(smt[:, 2:3], smt[:, 1:2]), [ks], [ks])
                        P.ts("dve", hh[h][:], comb[h][:, 0:128], smt[:, 2:3], ML_SCALE, ALU.mult, ALU.mult, ["comb%d" % h, ks], ["hh%d" % h])

                    def p7():
                        P.op("dve", (lambda o, i_: (lambda e: e.bn_stats(out=o, in_=i_)))(smt[:, 4:10], hh[h][:]), ["hh%d" % h], [ks])
                        P.op("dve", (lambda o, i_: (lambda e: e.bn_aggr(out=o, in_=i_)))(smt[:, 10:12], smt[:, 4:10]), [ks], [ks])
                        P.ts("dve", smt[:, 12:13], smt[:, 11:12], EPS, None, ALU.add, None, [ks], [ks])

                    def p8():
                        P.act(smt[:, 13:14], smt[:, 12:13], AF.Ln, [ks], [ks])
                        P.act(smt[:, 14:15], smt[:, 13:14], AF.Exp, [ks], [ks], scale=-0.5)

                    def p9():
                        P.ts("dve", hn[h][:], hh[h][:], smt[:, 10:11], smt[:, 14:15], ALU.subtract, ALU.mult, ["hh%d" % h, ks], ["hn%d" % h])

                    def p10():
                        P.tr(ptb[:, h * 128:(h + 1) * 128], hn[h][:], identb[:], ["hn%d" % h, "identb"], ["ptb"])

                    def p11():
                        P.stt("dve", y1[h][:], ptb[:, h * 128:(h + 1) * 128], mg[:, h:h + 1], scs[:, h, cs], ALU.mult, ALU.add,
                              ["ptb", "mg", "scs"], ["y1%d" % h])
                        P.tt("dve", ymg[b][:, h, cs], y1[h][:], sigz[:, h, cs], ALU.mult, ["y1%d" % h, "sigz"], ["ymg%d_%d" % (b, h)])

                    return [p0, p1, p2, p3, p4, p5, p6, p7, p8, p9, p10, p11]

                plist = [phases(h) for h in range(4)]
                for k_ in range(12):
                    for h in range(4):
                        plist[h][k_]()
            P.load("sp", T["ymT"].rearrange("(c p) t -> p c t", p=128)[:, :, t0:t0 + 512], ymg[b][:], ["ymg%d_%d" % (b, h_) for h_ in range(4)], ["ymT"])
        return P.emit()


def stage3(nc, sems, T):
    with contextlib.ExitStack() as st:
        sb, ps = tens(nc, st)
        P = Prog(nc, sems)
        ident = sb("ident", [128, 128])
        identb = sb("identb", [128, 128], BF16)
        qT = sb("qT", [128, 4, S], BF16)
        kT = sb("kT", [128, 4, S], BF16)
        vaug = sb("vaug", [128, NT, 4, 129], BF16)
        rbb = T["rbb_sb"]
        biasT = T["biasT_sb"]
        lqb = sb("lqb", [128, 256])
        lt = sb("lt", [128, 64])
        lam = sb("lam", [128, 8])
        dag = sb("dag", [128, 1])
        PT = [sb("PT%d" % i, [128, 512], BF16) for i in range(4)]
        tmpn = [sb("tmpn%d" % i, [128, 128]) for i in range(2)]
        t0s = [sb("t0s%d" % i, [128, 128]) for i in range(2)]
        av = [sb("av%d" % i, [128, 128]) for i in range(2)]
        junk = sb("junk3", [128, 128])
        sm = [sb("sm3%d" % i, [128, 8]) for i in range(4)]
        an = [sb("an%d" % i, [128, 128], BF16) for i in range(2)]
        ydg = [sb("ydg%d" % i, [128, 512], BF16) for i in range(2)]
        pS = [ps("pS%d" % i, [128, 512]) for i in range(3)]
        acc = [ps("acc%d" % i, [128, 512]) for i in range(4)]
        ptb = ps("ptb", [128, 1024], BF16)

        P.load("sp", ident[:], T["ident"], [], ["ident"])
        P.copy("dve", identb[:], ident[:], ["ident"], ["identb"])
        P.memset("pool", vaug[:], 1.0, ["vaug"])
        P.load("sp", qT[:], T["featT"][3].rearrange("(h p) t -> p h t", p=128), [], ["qT"])
        P.load("act", kT[:], T["featT"][4].rearrange("(h p) t -> p h t", p=128), [], ["kT"])
        for t in range(NT):
            P.load("sp" if t % 2 == 0 else "act", vaug[:, t, :, 0:128],
                   T["vd_tok"][t * 128:(t + 1) * 128, :].rearrange("p (h e) -> p h e", e=128), ["vaug"], ["vaug"])
        P.load("sp", lqb[:], T["lambda_qk"].partition_broadcast(128), [], ["lqb"])
        P.load("sp", dag[:], T["da_norm_g"].rearrange("(p o) -> p o", o=1), [], ["dag"])
        P.ts("dve", dag[:], dag[:], 1.0 - LAM_INIT, None, ALU.mult, None, ["dag"], ["dag"])
        for i in range(2):
            P.tt("dve", lt[:], lqb[:, (2 * i) * 64:(2 * i + 1) * 64], lqb[:, (2 * i + 1) * 64:(2 * i + 2) * 64], ALU.mult, ["lqb", "lt"], ["lt"])
            P.op("dve", (lambda o, i_: (lambda e: e.reduce_sum(out=o, in_=i_, axis=AX.X)))(lam[:, 4 + i:5 + i], lt[:]), ["lt"], ["lam"])
        P.act(lam[:, 0:2], lam[:, 4:6], AF.Exp, ["lam"], ["lam"])
        P.tt("dve", lam[:, 2:3], lam[:, 0:1], lam[:, 1:2], ALU.subtract, ["lam"], ["lam"])
        P.ts("dve", lam[:, 3:4], lam[:, 2:3], LAM_INIT, -1.0, ALU.add, ALU.mult, ["lam"], ["lam"])
        rS, rP, r2 = Rot(3), Rot(4), Rot(2)
        cvj = ConvD(P, T)
        cvj.k = 8 * (CONV_PER_GROUP[1] + CONV_PER_GROUP[2])
        its = [(h, g, c, j) for h in range(4) for g in range(8) for c in range(2) for j in range(4 * g + 4)]

        def emit_S(it):
            h, g, c, j = it
            prow = slice(c * 64, (c + 1) * 64)
            i_lo = max(j, 4 * g) - 4 * g
            sB = rS.next()
            pb = rP.next()
            kS, kP = "pS%d" % sB, "PT%d" % pb
            P.mm(pS[sB][:, i_lo * 128:512], kT[prow, h, j * 128:(j + 1) * 128], qT[prow, h, g * 512 + i_lo * 128:(g + 1) * 512],
                 ["kT", "qT"], [kS])
            far_lo = None
            for i in range(i_lo, 4):
                dist = 4 * g + i - j
                if dist >= 2:
                    far_lo = i
                    break
                n2 = r2.next()
                P.stt("dve", tmpn[n2][:], pS[sB][:, i * 128:(i + 1) * 128], DA_SCALE, biasT[:, dist, h, :], ALU.mult, ALU.add,
                      [kS, "biasT"], ["tmpn%d" % n2])
                P.act(PT[pb][:, i * 128:(i + 1) * 128], tmpn[n2][:], AF.Exp, ["tmpn%d" % n2], [kP])
            if far_lo is not None:
                P.act(PT[pb][:, far_lo * 128:512], pS[sB][:, far_lo * 128:512], AF.Exp, [kS, "rbb"], [kP],
                      bias=rbb[:, 31 * 4 + h:31 * 4 + h + 1], scale=DA_SCALE)
            return pb, i_lo

        def emit_AV(it, pb, i_lo):
            h, g, c, j = it
            kP = "PT%d" % pb
            if c == 0 and j == 0:
                cvj.emit(CONV_PER_GROUP[3])
                for a_ in range(4):
                    P.memset("dve", acc[a_][:, :], 0.0, ["acc%d" % a_])
            for i in range(i_lo, 4):
                a_ = c * 2 + i // 2
                off = (i % 2) * 256
                P.mm(acc[a_][:, off:off + 129], PT[pb][:, i * 128:(i + 1) * 128], vaug[:, j, h, :], [kP, "vaug"], ["acc%d" % a_],
                     start=False, stop=False, skip=True)
            if c == 1 and j == 4 * g + 3:
                finalize(h, g)

        def finalize(h, g):
            yb = (h * 8 + g) % 2
            for i in range(4):
                n2 = r2.next()
                ks = "sm3%d" % n2
                smt = sm[n2]
                a0, a1 = acc[i // 2], acc[2 + i // 2]
                k0, k1 = "acc%d" % (i // 2), "acc%d" % (2 + i // 2)
                off = (i % 2) * 256
                P.op("dve", (lambda o, i_: (lambda e: e.reciprocal(out=o, in_=i_)))(smt[:, 0:1], a0[:, off + 128:off + 129]), [k0], [ks])
                P.op("dve", (lambda o, i_: (lambda e: e.reciprocal(out=o, in_=i_)))(smt[:, 1:2], a1[:, off + 128:off + 129]), [k1], [ks])
                P.tt("dve", smt[:, 2:3], smt[:, 1:2], lam[:, 3:4], ALU.mult, [ks, "lam"], [ks])
                P.op("act", (lambda o, i_, sc: (lambda e: e.activation(out=o, in_=i_, func=AF.Copy, scale=sc)))(t0s[n2][:], a0[:, off:off + 128], smt[:, 0:1]),
                     [k0, ks], ["t0s%d" % n2])
                P.stt("dve", av[n2][:], a1[:, off:off + 128], smt[:, 2:3], t0s[n2][:], ALU.mult, ALU.add, [k1, ks, "t0s%d" % n2], ["av%d" % n2])
                P.act(junk[:], av[n2][:], AF.Square, ["av%d" % n2], ["junk3", ks], accum_out=smt[:, 3:4])
                P.ts("dve", smt[:, 4:5], smt[:, 3:4], 1.0 / 128, SUBLN_EPS, ALU.mult, ALU.add, [ks], [ks])
                P.act(smt[:, 5:6], smt[:, 4:5], AF.Ln, [ks], [ks])
                P.act(smt[:, 6:7], smt[:, 5:6], AF.Exp, [ks], [ks], scale=-0.5)
                P.ts("dve", an[n2][:], av[n2][:], smt[:, 6:7], None, ALU.mult, None, ["av%d" % n2, ks], ["an%d" % n2])
                P.tr(ptb[:, n2 * 512:n2 * 512 + 128], an[n2][:], identb[:], ["an%d" % n2, "identb"], ["ptb"])
                P.ts("dve", ydg[yb][:, i * 128:(i + 1) * 128], ptb[:, n2 * 512:n2 * 512 + 128], dag[:, 0:1], None, ALU.mult, None,
                     ["ptb", "dag"], ["ydg%d" % yb])
            P.load("act", T["ydT"][h * 128:(h + 1) * 128, g * 512:(g + 1) * 512], ydg[yb][:], ["ydg%d" % yb], ["ydT"])

        pend = []
        for it in its:
            pend.append((it,) + emit_S(it))
            if len(pend) > 2:
                emit_AV(*pend.pop(0))
        while pend:
            emit_AV(*pend.pop(0))
        return P.emit()


def stage4(nc, sems, T):
    with contextlib.ExitStack() as st:
        sb, ps = tens(nc, st)
        P = Prog(nc, sems)
        ident = sb("ident", [128, 128])
        wo = sb("wo", [128, 8, D], BF16)
        yT = sb("yT", [128, 8, S], BF16)
        g2b = sb("g2b", [128, D])
        wr = sb("wr", [128, 8, 36])
        brb = sb("brb", [128, 36])
        xt = [sb("xt%d" % i, [128, D]) for i in range(2)]
        x2 = [sb("x2%d" % i, [128, D]) for i in range(2)]
        junk = sb("junk4", [128, D], BF16)
        stat = [sb("stat4%d" % i, [128, 4]) for i in range(2)]
        h2 = [sb("h2%d" % i, [128, D]) for i in range(2)]
        h2T = [sb("h2T%d" % i, [128, 8, 128]) for i in range(2)]
        h2bf = [sb("h2bf%d" % i, [128, D], BF16) for i in range(2)]
        lstr = sb("lstr", [128, 128])
        ones = sb("ones", [128, 128])
        thr = sb("thr", [128, 16 + NSUP])
        E1 = sb("E1", [128, NT, 4, 8])
        E2 = sb("E2", [128, NT, 4, 8])
        Es = sb("Es", [128, NT * 32])
        within = sb("within", [128, NT, 32])
        csb = sb("csb", [128, NT, 32])
        incl = sb("incl", [128, NT, 32])
        zer = sb("zer", [128, NT])
        cmpb = sb("cmpb", [128, NSUP * 32])
        ntl = sb("ntl", [128, 32])
        inct = sb("inct", [128, 32])
        offb = sb("offb", [128, 32])
        offe = sb("offe", [128, 32])
        Rr = sb("Rr", [128, NT, 32])
        posf = sb("posf", [128, NT, 2])
        tef = sb("tef", [128, 64])
        pidx = sb("pidx", [128, 1])
        h2Tb = [sb("h2Tb%d" % i, [128, 8, 128], BF16) for i in range(2)]
        lgt = sb("lgt", [128, NT, 36])
        mxg = sb("mxg", [128, NT])
        ohg = sb("ohg", [128, NT, 4])
        eg = sb("eg", [128, NT, 4])
        sg = sb("sg", [128, NT])
        tmp4 = sb("tmp4", [128, NT, 4, 8])
        les = sb("les", [128, NT, 8])
        le2 = sb("le2", [128, NT, 8])
        m1 = sb("m1", [128, NT])
        m2 = sb("m2", [128, NT])
        oh1 = sb("oh1", [128, NT, 8])
        oh2 = sb("oh2", [128, NT, 8])
        w1 = sb("w1", [128, NT])
        w2 = sb("w2", [128, NT])
        gf = sb("gf", [128, NT, 8])
        gts = sb("gts", [128, NT, 4, 8])
        pO = [ps("pO%d" % i, [128, 512]) for i in range(4)]
        pT = [ps("pT%d" % i, [128, 512]) for i in range(2)]
        pR = ps("pR", [128, 512])

        P.load("sp", ident[:], T["ident"], [], ["ident"])
        zt = sb("zt", [128, 3, D], BF16)
        P.memset("pool", zt[:], 0.0, ["zt"])
        xs_v = T["xs"].rearrange("(n p) d -> p n d", p=128)
        for i in range(42):
            P.load("pool", xs_v[:, i * 3:(i + 1) * 3, :], zt[:], ["zt"], ["xs_zero%d" % i])
        P.load("pool", wo[:], T["w_out"].rearrange("(c p) n -> p c n", p=128), [], ["wo"])
        P.load("sp", yT[:, 0:4, :], T["ymT"].rearrange("(c p) t -> p c t", p=128), [], ["yT"])
        P.load("act", yT[:, 4:8, :], T["ydT"].rearrange("(c p) t -> p c t", p=128), [], ["yT"])
        P.load("sp", g2b[:], T["norm2_g"].partition_broadcast(128), [], ["g2b"])
        P.load("sp", wr[:], T["w_r"].rearrange("(c p) n -> p c n", p=128), [], ["wr"])
        P.load("sp", brb[:], T["b_r"].partition_broadcast(128), [], ["brb"])
        rO = Rot(2)

        def s4_a(t):
            b = t % 2
            ts_ = slice(t * 128, (t + 1) * 128)
            P.load("sp", xt[b][:], T["x"][ts_, :], [], ["xt%d" % b])
            for half in range(2):
                pb = rO.next() * 2 + half
                for kc in range(8):
                    P.mm(pO[pb][:, :], yT[:, kc, ts_], wo[:, kc, half * 512:(half + 1) * 512], ["yT", "wo"], ["pO%d" % pb], start=(kc == 0), stop=(kc == 7))
                P.tt("dve", x2[b][:, half * 512:(half + 1) * 512], pO[pb][:, :], xt[b][:, half * 512:(half + 1) * 512], ALU.add,
                     ["pO%d" % pb, "xt%d" % b], ["x2%d" % b])
            P.load("sp", T["x2"][ts_, :], x2[b][:], ["x2%d" % b], ["x2d"])
            sk = "stat4%d" % b
            P.act(junk[:], x2[b][:], AF.Square, ["x2%d" % b], ["junk4", sk], accum_out=stat[b][:, 0:1])
            P.ts("dve", stat[b][:, 1:2], stat[b][:, 0:1], 1.0 / D, EPS, ALU.mult, ALU.add, [sk], [sk])
            P.act(stat[b][:, 2:3], stat[b][:, 1:2], AF.Ln, [sk], [sk])
            P.act(stat[b][:, 3:4], stat[b][:, 2:3], AF.Exp, [sk], [sk], scale=-0.5)
            P.stt("dve", h2[b][:], x2[b][:], stat[b][:, 3:4], g2b[:], ALU.mult, ALU.mult, ["x2%d" % b, sk, "g2b"], ["h2%d" % b])
            P.copy("dve", h2bf[b][:], h2[b][:], ["h2%d" % b], ["h2bf%d" % b])
            P.load("sp", T["h2b"][ts_, :], h2bf[b][:], ["h2bf%d" % b], ["h2bd"])

        def s4_b(t):
            b = t % 2
            ts_ = slice(t * 128, (t + 1) * 128)
            for kc in range(8):
                pz = pT[kc // 4]
                P.tr(pz[:, (kc % 4) * 128:(kc % 4 + 1) * 128], h2[b][:, kc * 128:(kc + 1) * 128], ident[:], ["h2%d" % b, "ident"], ["pT%d" % (kc // 4)])
            for hf in range(2):
                P.copy("act", h2T[b][:, hf * 4:(hf + 1) * 4, :].rearrange("p k t -> p (k t)"), pT[hf][:, :], ["pT%d" % hf], ["h2T%d" % b])
                P.copy("dve", h2Tb[b][:, hf * 4:(hf + 1) * 4, :].rearrange("p k t -> p (k t)"), pT[hf][:, :], ["pT%d" % hf], ["h2Tb%d" % b])
            P.load("sp", T["h2T"].rearrange("(c p) t -> p c t", p=128)[:, :, ts_], h2Tb[b][:], ["h2Tb%d" % b], ["h2Td"])
            for kc in range(8):
                P.mm(pR[:, 0:36], h2T[b][:, kc, :], wr[:, kc, :], ["h2T%d" % b, "wr"], ["pR"], start=(kc == 0), stop=(kc == 7))
            P.tt("dve", lgt[:, t, :], pR[:, 0:36], brb[:], ALU.add, ["pR", "brb"], ["lgt"])

        for t in range(NT + 1):
            if t < NT:
                s4_a(t)
            if t >= 1:
                s4_b(t - 1)
        lg = lgt[:, :, 0:4]
        le = lgt[:, :, 4:36].rearrange("p t (g e) -> p t g e", e=8)
        red = lambda o, i_, op: (lambda e: e.tensor_reduce(out=o, in_=i_, axis=AX.X, op=op))
        P.op("dve", red(mxg[:], lg, ALU.max), ["lgt"], ["mxg"])
        P.tt("dve", ohg[:], lg, mxg[:].unsqueeze(2).to_broadcast([128, NT, 4]), ALU.is_ge, ["lgt", "mxg"], ["ohg"])
        P.tt("dve", eg[:], lg, mxg[:].unsqueeze(2).to_broadcast([128, NT, 4]), ALU.subtract, ["lgt", "mxg"], ["eg"])
        P.act(eg[:], eg[:], AF.Exp, ["eg"], ["eg"])
        P.op("dve", red(sg[:], eg[:], ALU.add), ["eg"], ["sg"])
        P.op("dve", (lambda o, i_: (lambda e: e.reciprocal(out=o, in_=i_)))(sg[:], sg[:]), ["sg"], ["sg"])
        P.tt("dve", tmp4[:], le, ohg[:].unsqueeze(3).to_broadcast([128, NT, 4, 8]), ALU.mult, ["lgt", "ohg"], ["tmp4"])
        P.op("dve", red(les[:], tmp4[:].rearrange("p t g e -> p t e g"), ALU.add), ["tmp4"], ["les"])
        P.op("dve", red(m1[:], les[:], ALU.max), ["les"], ["m1"])
        P.tt("dve", oh1[:], les[:], m1[:].unsqueeze(2).to_broadcast([128, NT, 8]), ALU.is_ge, ["les", "m1"], ["oh1"])
        P.stt("dve", le2[:], oh1[:], -1e30, les[:], ALU.mult, ALU.add, ["oh1", "les"], ["le2"])
        P.op("dve", red(m2[:], le2[:], ALU.max), ["le2"], ["m2"])
        P.tt("dve", oh2[:], le2[:], m2[:].unsqueeze(2).to_broadcast([128, NT, 8]), ALU.is_ge, ["le2", "m2"], ["oh2"])
        P.tt("dve", w2[:], m2[:], m1[:], ALU.subtract, ["m1", "m2"], ["w2"])
        P.act(w2[:], w2[:], AF.Exp, ["w2"], ["w2"])
        P.ts("dve", w1[:], w2[:], 1.0, None, ALU.add, None, ["w2"], ["w1"])
        P.op("dve", (lambda o, i_: (lambda e: e.reciprocal(out=o, in_=i_)))(w1[:], w1[:]), ["w1"], ["w1"])
        P.tt("dve", w2[:], w2[:], w1[:], ALU.mult, ["w1", "w2"], ["w2"])
        P.tt("dve", w1[:], w1[:], sg[:], ALU.mult, ["w1", "sg"], ["w1"])
        P.tt("dve", w2[:], w2[:], sg[:], ALU.mult, ["w2", "sg"], ["w2"])
        wk_g, pos_i, te_i = T["wk_g"], T["pos_i"], T["te_i"]
        P.copy("dve", wk_g[:, :, 0], w1[:], ["w1"], ["wk_g"])
        P.copy("dve", wk_g[:, :, 1], w2[:], ["w2", "wk_g"], ["wk_g"])
        P.load("sp", lstr[:], T["lstrict"], [], ["lstr"])
        P.load("sp", thr[:], T["thr"].partition_broadcast(128), [], ["thr"])
        P.memset("pool", ones[:], 1.0, ["ones"])
        P.memset("pool", zer[:], 0.0, ["zer"])
        bc3 = lambda a: a.unsqueeze(3).to_broadcast([128, NT, 4, 8])
        bc2 = lambda a: a.unsqueeze(2).to_broadcast([128, NT, 4, 8])
        P.tt("dve", E1[:], bc3(ohg[:]), bc2(oh1[:]), ALU.mult, ["ohg", "oh1"], ["E1"])
        P.tt("dve", E2[:], bc3(ohg[:]), bc2(oh2[:]), ALU.mult, ["ohg", "oh2"], ["E2"])
        P.tt("dve", Es[:], E1[:].rearrange("p t g e -> p (t g e)"), E2[:].rearrange("p t g e -> p (t g e)"), ALU.add, ["E1", "E2"], ["Es"])
        for hf in range(2):
            P.mm(pO[hf][:, :], lstr[:], Es[:, hf * 512:(hf + 1) * 512], ["lstr", "Es"], ["pO%d" % hf])
            P.copy("act", within[:].rearrange("p t e -> p (t e)")[:, hf * 512:(hf + 1) * 512], pO[hf][:, :], ["pO%d" % hf], ["within"])
            P.mm(pO[2 + hf][:, :], ones[:], Es[:, hf * 512:(hf + 1) * 512], ["ones", "Es"], ["pO%d" % (2 + hf)])
            P.copy("dve", csb[:].rearrange("p t e -> p (t e)")[:, hf * 512:(hf + 1) * 512], pO[2 + hf][:, :], ["pO%d" % (2 + hf)], ["csb"])
        for e_ in range(32):
            P.op("dve", (lambda o, d0, d1: (lambda e: e.tensor_tensor_scan(out=o, data0=d0, data1=d1, initial=0.0, op0=ALU.add, op1=ALU.add)))(
                incl[:, :, e_], csb[:, :, e_], zer[:]), ["csb", "zer", "incl"], ["incl"])
        P.tt("dve", cmpb[:, 0:512].rearrange("p (e j) -> p e j", j=16), incl[:, NT - 1, :].unsqueeze(2).to_broadcast([128, 32, 16]),
             thr[:, 0:16].unsqueeze(1).to_broadcast([128, 32, 16]), ALU.is_gt, ["incl", "thr"], ["cmpb"])
        P.op("dve", red(ntl[:], cmpb[:, 0:512].rearrange("p (e j) -> p e j", j=16), ALU.add), ["cmpb"], ["ntl"])
        P.op("dve", (lambda o, d0, d1: (lambda e: e.tensor_tensor_scan(out=o, data0=d0, data1=d1, initial=0.0, op0=ALU.add, op1=ALU.add)))(
            inct[:], ntl[:], zer[:, 0:32]), ["ntl", "zer"], ["inct"])
        P.ts("dve", offe[:], inct[:], float(SUP), None, ALU.mult, None, ["inct"], ["offe"])
        P.tt("dve", offb[:], inct[:], ntl[:], ALU.subtract, ["inct", "ntl"], ["offb"])
        P.ts("dve", offb[:], offb[:], float(SUP), None, ALU.mult, None, ["offb"], ["offb"])
        P.tt("dve", Rr[:], incl[:], csb[:], ALU.subtract, ["incl", "csb"], ["Rr"])
        P.tt("dve", Rr[:], Rr[:], within[:], ALU.add, ["Rr", "within"], ["Rr"])
        P.tt("dve", Rr[:], Rr[:], offb[:].unsqueeze(1).to_broadcast([128, NT, 32]), ALU.add, ["Rr", "offb"], ["Rr"])
        for k_, Ek in enumerate((E1, E2)):
            kn = "E%d" % (k_ + 1)
            P.tt("dve", Ek[:].rearrange("p t g e -> p t (g e)"), Ek[:].rearrange("p t g e -> p t (g e)"), Rr[:], ALU.mult, [kn, "Rr"], [kn])
            P.op("dve", red(posf[:, :, k_], Ek[:].rearrange("p t g e -> p t (g e)"), ALU.add), [kn, "posf"], ["posf"])
        P.copy("dve", pos_i[:], posf[:], ["posf"], ["pos_i"])
        P.tt("dve", cmpb[:, 0:NSUP * 32].rearrange("p (j e) -> p j e", e=32), offe[:].unsqueeze(1).to_broadcast([128, NSUP, 32]),
             thr[:, 16:16 + NSUP].unsqueeze(2).to_broadcast([128, NSUP, 32]), ALU.is_le, ["offe", "thr", "cmpb"], ["cmpb"])
        P.memset("pool", tef[:], 0.0, ["tef"])
        P.op("dve", red(tef[:, 0:NSUP], cmpb[:, 0:NSUP * 32].rearrange("p (j e) -> p j e", e=32), ALU.add), ["cmpb", "tef"], ["tef"])
        P.ts("dve", tef[:], tef[:], 31.0, None, ALU.min, None, ["tef"], ["tef"])
        P.load("sp", pidx[:], T["pidx"], [], ["pidx"])
        P.ts("dve", tef[:], tef[:], 128.0, pidx[:, 0:1], ALU.mult, ALU.add, ["tef", "pidx"], ["tef"])
        P.copy("dve", te_i[:], tef[:], ["tef"], ["te_i"])
        P.tt("dve", oh1[:], oh1[:], w1[:].unsqueeze(2).to_broadcast([128, NT, 8]), ALU.mult, ["oh1", "w1"], ["oh1"])
        P.tt("dve", oh2[:], oh2[:], w2[:].unsqueeze(2).to_broadcast([128, NT, 8]), ALU.mult, ["oh2", "w2"], ["oh2"])
        P.tt("dve", gf[:], oh1[:], oh2[:], ALU.add, ["oh1", "oh2"], ["gf"])
        P.tt("dve", gts[:], ohg[:].unsqueeze(3).to_broadcast([128, NT, 4, 8]), gf[:].unsqueeze(2).to_broadcast([128, NT, 4, 8]), ALU.mult,
             ["ohg", "gf"], ["gts"])
        P.load("sp", T["gates"], gts[:].rearrange("p t g e -> p (t g e)"), ["gts"], ["gatesd"])
        return P.emit()


def stage5(nc, sems, T):
    IOA = bass.IndirectOffsetOnAxis
    with contextlib.ExitStack() as st:
        sb, ps = tens(nc, st)
        P = Prog(nc, sems)
        pos_i, te_i, wk_g = T["pos_i"], T["te_i"], T["wk_g"]
        identb = sb("identb", [128, 128], BF16)
        identf = sb("identf", [128, 128])
        gfb = sb("gfb", [128, D])
        hrow = [sb("hrow%d" % i, [128, D], BF16) for i in range(3)]
        wall = [sb("wall%d" % i, [128, 3 * 4096], BF16) for i in range(2)]
        xs = [sb("xs%d" % i, [128, D], BF16) for i in range(3)]
        XT = [sb("XT%d" % i, [128, 8, 128], BF16) for i in range(2)]
        sgl = [sb("sgl%d" % i, [128, DFF], BF16) for i in range(2)]
        hid = [sb("hid%d" % i, [128, DFF], BF16) for i in range(2)]
        hidT = [sb("hidT%d" % i, [128, 4, 128], BF16) for i in range(2)]
        ysb = [sb("ysb%d" % i, [128, D], BF16) for i in range(3)]
        yg = [sb("yg%d" % i, [128, 2, D], BF16) for i in range(2)]
        x2 = [sb("x2%d" % i, [128, D]) for i in range(2)]
        junk = sb("junk5", [128, D], BF16)
        stat = [sb("stat5%d" % i, [128, 4]) for i in range(2)]
        ot = [sb("ot%d" % i, [128, D]) for i in range(2)]
        ptx = [ps("ptx%d" % i, [128, D], BF16) for i in range(2)]
        pg = [ps("pg%d" % i, [128, 512]) for i in range(2)]
        pu = [ps("pu%d" % i, [128, 512]) for i in range(2)]
        pth = [ps("pth%d" % i, [128, D], BF16) for i in range(2)]

        P.load("sp", identf[:], T["ident"], [], ["identf"])
        P.copy("dve", identb[:], identf[:], ["identf"], ["identb"])
        P.load("sp", gfb[:], T["normf_g"].partition_broadcast(128), [], ["gfb"])
        zkeys = []
        sckeys = []
        for t in range(NT):
            hb = t % 3
            P.load("sp", hrow[hb][:], T["h2b"][t * 128:(t + 1) * 128, :], [], ["hrow%d" % hb])
            for k_ in range(2):
                key = "xs_sc%d_%d" % (t, k_)
                sckeys.append(key)
                P.dma("pool", (lambda o, off, i_: (lambda e: e.indirect_dma_start(out=o, out_offset=off, in_=i_, in_offset=None)))(
                    T["xs"], IOA(ap=pos_i[:, t, k_:k_ + 1], axis=0), hrow[hb][:]), ["hrow%d" % hb, "pos_i"] + zkeys, [key])
        ykeys = []
        rx, r2 = Rot(3), Rot(2)
        NSUB = NSUP * (SUP // 128)

        def gather_w(j):
            wb = j % 2
            P.dma("pool", (lambda o, i_, off: (lambda e: e.indirect_dma_start(out=o, out_offset=None, in_=i_, in_offset=off)))(
                wall[wb][:, :], T["wall"], IOA(ap=te_i[:, j:j + 1], axis=0)), ["te_i"], ["wall%d" % wb])

        def wviews(j):
            wb = j % 2
            return (wall[wb][:, 0:4096].rearrange("p (c f) -> p c f", f=512), wall[wb][:, 4096:8192].rearrange("p (c f) -> p c f", f=512),
                    wall[wb][:, 8192:12288].rearrange("p (c d) -> p c d", d=1024), "wall%d" % wb)

        def phase_a(n):
            j = n // 2
            wg_v, wu_v, wd_v, kw = wviews(j)
            row0 = n * 128
            xb, b2 = n % 3, n % 2
            P.load("act", xs[xb][:], T["xs"][row0:row0 + 128, :], sckeys + zkeys, ["xs%d" % xb])
            for kc in range(8):
                P.tr(ptx[b2][:, kc * 128:(kc + 1) * 128], xs[xb][:, kc * 128:(kc + 1) * 128], identb[:], ["xs%d" % xb, "identb"], ["ptx%d" % b2])
            P.copy("dve" if b2 == 0 else "act", XT[b2][:].rearrange("p k t -> p (k t)"), ptx[b2][:, :], ["ptx%d" % b2], ["XT%d" % b2])

        def phase_a2(n):
            j = n // 2
            wg_v, wu_v, wd_v, kw = wviews(j)
            xb, b2 = n % 3, n % 2
            for kc in range(8):
                P.mm(pg[b2][:, :], XT[b2][:, kc, :], wg_v[:, kc, :], ["XT%d" % b2, kw], ["pg%d" % b2], start=(kc == 0), stop=(kc == 7))
            for kc in range(8):
                P.mm(pu[b2][:, :], XT[b2][:, kc, :], wu_v[:, kc, :], ["XT%d" % b2, kw], ["pu%d" % b2], start=(kc == 0), stop=(kc == 7))
            P.act(sgl[b2][:], pg[b2][:, :], AF.Silu, ["pg%d" % b2], ["sgl%d" % b2])
            P.tt("dve", hid[b2][:], sgl[b2][:], pu[b2][:, :], ALU.mult, ["sgl%d" % b2, "pu%d" % b2], ["hid%d" % b2])

        def phase_b(n):
            j = n // 2
            wg_v, wu_v, wd_v, kw = wviews(j)
            row0 = n * 128
            xb, b2 = n % 3, n % 2
            for fc in range(4):
                P.tr(pth[b2][:, fc * 128:(fc + 1) * 128], hid[b2][:, fc * 128:(fc + 1) * 128], identb[:], ["hid%d" % b2, "identb"], ["pth%d" % b2])
            P.copy("act" if b2 == 0 else "dve", hidT[b2][:].rearrange("p k t -> p (k t)"), pth[b2][:, 0:512], ["pth%d" % b2], ["hidT%d" % b2])

        def phase_b2(n):
            j = n // 2
            wg_v, wu_v, wd_v, kw = wviews(j)
            row0 = n * 128
            xb, b2 = n % 3, n % 2
            for half, (pz, kz) in enumerate(((pg[b2], "pg%d" % b2), (pu[b2], "pu%d" % b2))):
                for fc in range(4):
                    P.mm(pz[:, :], hidT[b2][:, fc, :], wd_v[:, fc, half * 512:(half + 1) * 512], ["hidT%d" % b2, kw], [kz],
                         start=(fc == 0), stop=(fc == 3))
                P.copy("act" if half == 0 else "dve", ysb[xb][:, half * 512:(half + 1) * 512], pz[:, :], [kz], ["ysb%d" % xb])
            yk = "ys%d" % n
            ykeys.append(yk)
            P.load("sp", T["ys"][row0:row0 + 128, :], ysb[xb][:], ["ysb%d" % xb], [yk])

        gather_w(0)
        gather_w(1)
        for n in range(NSUB + 1):
            if n < NSUB:
                phase_a(n)
            if n >= 1:
                phase_b(n - 1)
            if n < NSUB:
                phase_a2(n)
            if n >= 1:
                m = n - 1
                phase_b2(m)
                if m % 2 == 1 and m // 2 + 2 < NSUP:
                    gather_w(m // 2 + 2)
        for t in range(NT):
            b = t % 2
            ts_ = slice(t * 128, (t + 1) * 128)
            sk = "stat5%d" % b
            for k_ in range(2):
                P.dma("pool", (lambda o, i_, off: (lambda e: e.indirect_dma_start(out=o, out_offset=None, in_=i_, in_offset=off)))(
                    yg[b][:, k_, :], T["ys"], IOA(ap=pos_i[:, t, k_:k_ + 1], axis=0)), ykeys + ["pos_i"], ["yg%d_%d" % (b, k_)])
            P.load("sp", x2[b][:], T["x2"][ts_, :], [], ["x2%d" % b])
            P.stt("dve", x2[b][:], yg[b][:, 0, :], wk_g[:, t, 0:1], x2[b][:], ALU.mult, ALU.add, ["yg%d_0" % b, "wk_g", "x2%d" % b], ["x2%d" % b])
            P.stt("dve", x2[b][:], yg[b][:, 1, :], wk_g[:, t, 1:2], x2[b][:], ALU.mult, ALU.add, ["yg%d_1" % b, "wk_g", "x2%d" % b], ["x2%d" % b])
            P.act(junk[:], x2[b][:], AF.Square, ["x2%d" % b], ["junk5", sk], accum_out=stat[b][:, 0:1])
            P.ts("dve", stat[b][:, 1:2], stat[b][:, 0:1], 1.0 / D, EPS, ALU.mult, ALU.add, [sk], [sk])
            P.act(stat[b][:, 2:3], stat[b][:, 1:2], AF.Ln, [sk], [sk])
            P.act(stat[b][:, 3:4], stat[b][:, 2:3], AF.Exp, [sk], [sk], scale=-0.5)
            P.stt("dve", ot[b][:], x2[b][:], stat[b][:, 3:4], gfb[:], ALU.mult, ALU.mult, ["x2%d" % b, sk, "gfb"], ["ot%d" % b])
            P.load("act", T["out"][ts_, :], ot[b][:], ["ot%d" % b], ["outd"])
        return P.emit()


def _rel_bucket_np(n):
    n = np.maximum(n, 0)
    max_exact = 16
    nf = np.maximum(n, 1).astype(np.float32)
    large = max_exact + (np.log(nf / np.float32(max_exact)) / np.float32(math.log(128 / max_exact)) * np.float32(16)).astype(np.int32)
    large = np.minimum(large, 31)
    return np.where(n < max_exact, n, large)


def _constants():
    ident = np.eye(128, dtype=np.float32)
    s_ = np.arange(128)[:, None]
    t_ = np.arange(128)[None, :]
    tri = (s_ <= t_).astype(np.float32)
    sel = np.zeros((4, 4, 128), np.float32)
    for h in range(4):
        sel[h, h, :] = 1.0
    oh = np.zeros((128, 2, 33, 128), np.float32)
    for kind in range(2):
        n = (t_ - s_) + 128 * kind
        bk = _rel_bucket_np(n)
        valid = n >= 0
        for b in range(32):
            oh[:, kind, b, :] = ((bk == b) & valid).astype(np.float32)
        oh[:, kind, 32, :] = (~valid).astype(np.float32)
    lstrict = (s_ < t_).astype(np.float32)
    thr = np.concatenate([np.arange(16) * SUP, np.arange(NSUP) * SUP]).astype(np.float32)
    pidx = np.arange(128, dtype=np.float32).reshape(128, 1)
    return dict(ident=ident, tri=tri, sel=sel.reshape(4, 512), oh=oh.reshape(128, -1), lstrict=lstrict, thr=thr, pidx=pidx)


_CACHE = {}


def kernel(x, w_in, conv_w, conv_b, w_mq, w_mk, w_mgate, b_mgate, m_norm_g, m_skip, lambda_qk, da_norm_g, rel_bias, w_out,
           norm1_g, norm2_g, w_rg, b_rg, w_re, b_re, w_eg, w_eu, w_ed, normf_g):
    f = lambda a: np.ascontiguousarray(np.asarray(a, dtype=np.float32))
    if "nc" not in _CACHE:
        _CACHE["nc"], _CACHE["stats"] = build_program()
    nc = _CACHE["nc"]
    shared = dict(
        w_in=f(w_in)[0], conv_w=f(conv_w)[0], conv_b=f(conv_b)[0], w_mq=f(w_mq)[0], w_mk=f(w_mk)[0], w_mgate=f(w_mgate)[0],
        b_mgate=f(b_mgate)[0], m_norm_g=f(m_norm_g)[0], m_skip=f(m_skip)[0], lambda_qk=f(lambda_qk)[0].reshape(256),
        da_norm_g=f(da_norm_g)[0], rel_bias=f(rel_bias).reshape(128), w_out=f(w_out)[0], norm1_g=f(norm1_g)[0], norm2_g=f(norm2_g)[0],
        w_r=np.ascontiguousarray(np.concatenate([f(w_rg)[0], f(w_re)[0].reshape(D, 32)], axis=1)),
        b_r=np.ascontiguousarray(np.concatenate([f(b_rg)[0], f(b_re)[0].reshape(32)])),
        w_eg=f(w_eg)[0], w_eu=f(w_eu)[0], w_ed=f(w_ed)[0], normf_g=f(normf_g),
    )
    shared.update(_constants())
    xs = f(x)
    in_maps = []
    for b in range(8):
        m = dict(shared)
        m["x"] = xs[b]
        in_maps.append(m)
    res = run_bass_kernel_spmd(nc, in_maps, core_ids=list(range(8)))
    _CACHE["res"] = res
    return np.stack([np.asarray(r["out"], dtype=np.float32) for r in res.results], axis=0)
```

```python
import math
import contextlib
import numpy as np
import concourse.bass as bass
import concourse.mybir as mybir
from concourse.bass_utils import run_bass_kernel_spmd

F32 = mybir.dt.float32
BF16 = mybir.dt.bfloat16
AF = mybir.ActivationFunctionType
ALU = mybir.AluOpType
AX = mybir.AxisListType

S = 4096
D = 1024
NT = 32
EPS = 1e-6
SUBLN_EPS = 1e-5
N_EXP = 32
DFF = 512
LAM_INIT = 0.8 - 0.6 * math.exp(-0.3 * 0)
ML_SCALE = 128.0 ** -0.5
DA_SCALE = 64.0 ** -0.5
NEG = -30000.0
SUP = 256
NSUP = 63
NSLOT = NSUP * SUP
I32 = mybir.dt.int32

COMPUTE = ("pe", "act", "dve", "pool")
QUEUES = ("sp", "act", "pool")
N_DMA_SEMS = 8
DEBUG = False
CONV_PER_GROUP = {1: 0, 2: 7, 3: 2}
STAGES = (1, 2, 3, 4, 5)


class Sems:
    def __init__(self, nc, st):
        self.esem = {e: st.enter_context(nc.semaphore("s_" + e)) for e in COMPUTE}
        self.dsem = {(q, s): st.enter_context(nc.semaphore("d_%s_%d" % (q, s))) for q in QUEUES for s in range(N_DMA_SEMS)}
        self.cnt = {e: 0 for e in COMPUTE}
        self.dcnt = {k: 0 for k in self.dsem}
        self.rr = {q: 0 for q in QUEUES}


class Op:
    __slots__ = ("eng", "fn", "deps", "is_dma", "signal", "val", "sem", "slot", "prev")

    def __init__(self, eng, fn, is_dma):
        self.eng, self.fn, self.is_dma = eng, fn, is_dma
        self.deps = []
        self.signal = False
        self.val = None
        self.sem = None
        self.slot = None
        self.prev = None


class Prog:
    def __init__(self, nc, sems):
        self.nc = nc
        self.sems = sems
        self.ops = []
        self.last_writer = {}
        self.readers = {}
        self.slot_last = {}

    def _add(self, op, reads, writes):
        pr = [r for r in reads if r in PSUM_KEYS]
        if pr:
            reads = [r for r in reads if r not in PSUM_KEYS]
            writes = list(writes) + [r for r in pr if r not in writes]
        deps = []
        for r in reads:
            w = self.last_writer.get(r)
            if w is not None:
                deps.append(w)
        for w in writes:
            lw = self.last_writer.get(w)
            if lw is not None:
                deps.append(lw)
            deps.extend(self.readers.get(w, ()))
        seen = set()
        for d in deps:
            if id(d) not in seen and d is not op:
                seen.add(id(d))
                op.deps.append(d)
        for r in reads:
            self.readers.setdefault(r, []).append(op)
        for w in writes:
            self.last_writer[w] = op
            self.readers[w] = []
        self.ops.append(op)
        return op

    def op(self, eng, fn, reads=(), writes=()):
        return self._add(Op(eng, fn, False), reads, writes)

    def dma(self, queue, fn, reads=(), writes=()):
        op = Op(queue, fn, True)
        s = self.sems
        op.slot = (queue, s.rr[queue] % N_DMA_SEMS)
        s.rr[queue] += 1
        op.prev = self.slot_last.get(op.slot)
        self.slot_last[op.slot] = op
        return self._add(op, reads, writes)

    def mm(self, out, lhsT, rhs, r, w, start=True, stop=True, skip=False):
        if skip:
            return self.op("pe", lambda e: e.matmul(out, lhsT=lhsT, rhs=rhs, start=start, stop=stop, skip_group_check=True), r, w)
        return self.op("pe", lambda e: e.matmul(out, lhsT=lhsT, rhs=rhs, start=start, stop=stop), r, w)

    def tr(self, out, in_, ident, r, w):
        return self.op("pe", lambda e: e.transpose(out=out, in_=in_, identity=ident), r, w)

    def act(self, out, in_, func, r, w, bias=None, scale=None, accum_out=None):
        kw = {}
        if bias is not None:
            kw["bias"] = bias
        if scale is not None:
            kw["scale"] = scale
        if accum_out is not None:
            kw["accum_out"] = accum_out
        return self.op("act", lambda e: e.activation(out=out, in_=in_, func=func, **kw), r, w)

    def copy(self, eng, out, in_, r, w):
        if eng == "act":
            return self.op("act", lambda e: e.copy(out=out, in_=in_), r, w)
        return self.op(eng, lambda e: e.tensor_copy(out=out, in_=in_), r, w)

    def tt(self, eng, out, in0, in1, op, r, w):
        return self.op(eng, lambda e: e.tensor_tensor(out=out, in0=in0, in1=in1, op=op), r, w)

    def ts(self, eng, out, in0, s1, s2, op0, op1, r, w):
        if s2 is None:
            return self.op(eng, lambda e: e.tensor_scalar(out=out, in0=in0, scalar1=s1, scalar2=None, op0=op0), r, w)
        return self.op(eng, lambda e: e.tensor_scalar(out=out, in0=in0, scalar1=s1, scalar2=s2, op0=op0, op1=op1), r, w)

    def stt(self, eng, out, in0, scalar, in1, op0, op1, r, w):
        eng = "dve"
        return self.op(eng, lambda e: e.scalar_tensor_tensor(out=out, in0=in0, scalar=scalar, in1=in1, op0=op0, op1=op1), r, w)

    def memset(self, eng, ap, val, w):
        return self.op(eng, lambda e: e.memset(ap, val), (), w)

    def load(self, q, out, in_, r, w):
        return self.dma(q, lambda e: e.dma_start(out=out, in_=in_), r, w)

    def emit(self):
        nc, s, ops = self.nc, self.sems, self.ops

        def same_skip(d, o):
            return (not d.is_dma) and (not o.is_dma) and d.eng == o.eng and d.eng == "pe"

        for o in ops:
            for d in o.deps:
                if d.is_dma or same_skip(d, o):
                    continue
                d.signal = True
        for o in ops:
            if o.is_dma:
                s.dcnt[o.slot] += 16
                o.val = s.dcnt[o.slot]
                o.sem = s.dsem[o.slot]
            else:
                o.sem = s.esem[o.eng]
                if o.signal:
                    s.cnt[o.eng] += 1
                    o.val = s.cnt[o.eng]
        by_eng = {e: [] for e in ("pe", "act", "dve", "pool", "sp")}
        for o in ops:
            by_eng[o.eng].append(o)
        final = dict(s.dcnt)

        def run(engname, e):
            waited = {}

            def wait(sem, val):
                if waited.get(id(sem), 0) >= val:
                    return
                waited[id(sem)] = val
                e.wait_ge(sem, val)

            for o in by_eng[engname]:
                for d in o.deps:
                    if same_skip(d, o):
                        continue
                    wait(d.sem, d.val)
                if o.is_dma and o.prev is not None:
                    wait(o.prev.sem, o.prev.val)
                ins = o.fn(e)
                if o.is_dma:
                    ins.then_inc(o.sem, 16)
                elif o.signal:
                    ins.then_inc(o.sem, 1)
            if engname == "sp":
                for k, v in final.items():
                    if v > 0:
                        wait(s.dsem[k], v)

        with nc.Block() as block:
            block.sync(lambda e: run("sp", e))
            if by_eng["pe"]:
                block.tensor(lambda e: run("pe", e))
            if by_eng["act"]:
                block.scalar(lambda e: run("act", e))
            if by_eng["dve"]:
                block.vector(lambda e: run("dve", e))
            if by_eng["pool"]:
                block.gpsimd(lambda e: run("pool", e))
        return {k: len(v) for k, v in by_eng.items()}


class Rot:
    def __init__(self, n):
        self.n, self.i = n, 0

    def next(self):
        v = self.i % self.n
        self.i += 1
        return v


def build_program():
    nc = bass.Bass("TRN2", target_bir_lowering=False)
    I = lambda name, shape, dt=F32: nc.dram_tensor(name, list(shape), dt, kind="ExternalInput").ap()
    skind = "ExternalOutput" if DEBUG else "Internal"
    SC = lambda name, shape, dt: nc.dram_tensor(name, list(shape), dt, kind=skind).ap()
    T = {}
    T["x"] = I("x", [S, D])
    T["w_in"] = I("w_in", [D, 3072])
    T["conv_w"] = I("conv_w", [4, 512])
    T["conv_b"] = I("conv_b", [512])
    T["w_mq"] = I("w_mq", [4, 128, 128])
    T["w_mk"] = I("w_mk", [4, 128, 128])
    T["w_mgate"] = I("w_mgate", [1536, 8])
    T["b_mgate"] = I("b_mgate", [8])
    T["m_norm_g"] = I("m_norm_g", [512])
    T["m_skip"] = I("m_skip", [512])
    T["lambda_qk"] = I("lambda_qk", [256])
    T["da_norm_g"] = I("da_norm_g", [128])
    T["rel_bias"] = I("rel_bias", [128])
    T["w_out"] = I("w_out", [D, D])
    T["norm1_g"] = I("norm1_g", [D])
    T["norm2_g"] = I("norm2_g", [D])
    T["w_r"] = I("w_r", [D, 36])
    T["b_r"] = I("b_r", [36])
    T["w_eg"] = I("w_eg", [N_EXP, D, DFF])
    T["w_eu"] = I("w_eu", [N_EXP, D, DFF])
    T["w_ed"] = I("w_ed", [N_EXP, DFF, D])
    T["normf_g"] = I("normf_g", [D])
    T["ident"] = I("ident", [128, 128])
    T["tri"] = I("tri", [128, 128])
    T["sel"] = I("sel", [4, 512])
    T["oh"] = I("oh", [128, 2 * 33 * 128])
    T["lstrict"] = I("lstrict", [128, 128])
    T["thr"] = I("thr", [16 + NSUP])
    T["pidx"] = I("pidx", [128, 1])
    T["out"] = nc.dram_tensor("out", [S, D], F32, kind="ExternalOutput").ap()
    T["featT"] = SC("featT", [5, 512, S], BF16)
    T["vm_tok"] = SC("vm_tok", [S, 512], BF16)
    T["vd_tok"] = SC("vd_tok", [S, 512], BF16)
    T["ymT"] = SC("ymT", [512, S], BF16)
    T["ydT"] = SC("ydT", [512, S], BF16)
    T["x2"] = SC("x2", [S, D], F32)
    T["h2T"] = SC("h2T", [D, S], BF16)
    T["gates"] = SC("gates", [128, NT * 32], F32)
    T["h2b"] = SC("h2b", [S, D], BF16)
    T["xs"] = nc.dram_tensor("xs", [NSLOT, D], BF16, kind="Internal").ap()
    T["ys"] = nc.dram_tensor("ys", [NSLOT, D], BF16, kind="Internal").ap()
    T["wall"] = nc.dram_tensor("wall", [N_EXP * 128, 3 * 4096], BF16, kind="Internal").ap()

    stats = {}
    with contextlib.ExitStack() as gst:
        gst.enter_context(nc.allow_non_contiguous_dma(reason="small strided parameter loads"))
        sems = Sems(nc, gst)
        T["biasT_sb"] = gst.enter_context(nc.sbuf_tensor("g_biasT", [128, 2, 4, 128], F32))
        T["rbb_sb"] = gst.enter_context(nc.sbuf_tensor("g_rbb", [128, 128], F32))
        T["pos_i"] = gst.enter_context(nc.sbuf_tensor("g_pos_i", [128, NT, 2], I32))
        T["te_i"] = gst.enter_context(nc.sbuf_tensor("g_te_i", [128, 64], I32))
        T["wk_g"] = gst.enter_context(nc.sbuf_tensor("g_wk", [128, NT, 2], F32))
        if 0 in STAGES:
            stats["s0"] = stage0(nc, sems, T)
        if 1 in STAGES:
            stats["s1"] = stage1(nc, sems, T)
        if 2 in STAGES:
            stats["s2"] = stage2(nc, sems, T)
        if 3 in STAGES:
            stats["s3"] = stage3(nc, sems, T)
        if 4 in STAGES:
            stats["s4"] = stage4(nc, sems, T)
        if 5 in STAGES:
            stats["s5"] = stage5(nc, sems, T)
        if 6 in STAGES:
            stats["s6"] = stage6(nc, sems, T)
    return nc, stats


_TN = [0]
PSUM_KEYS = set()


def tens(nc, st):
    _TN[0] += 1
    pre = "t%d_" % _TN[0]
    sb = lambda n, s, d=F32: st.enter_context(nc.sbuf_tensor(pre + n, list(s), d))
    def ps(n, s, d=F32):
        PSUM_KEYS.add(n)
        return st.enter_context(nc.psum_tensor(pre + n, list(s), d))
    return sb, ps


def conv_jobs():
    return [(name, m, e) for m, name in enumerate(("w_eg", "w_eu", "w_ed")) for e in range(N_EXP)]


class Conv:
    def __init__(self, P, sb, T, engs=("pool",), queues=("sp", "sp"), nb=3):
        self.P, self.T = P, T
        self.stg = [sb("w0s%d" % i, [128, 8, 512], F32) for i in range(nb)]
        self.cvt = [sb("w0c%d" % i, [128, 8, 512], BF16) for i in range(nb)]
        self.rot = Rot(nb)
        self.engs, self.queues = engs, queues
        self.jobs = conv_jobs()
        self.k = 0

    def emit(self, n):
        P, T = self.P, self.T
        for _ in range(n):
            if self.k >= len(self.jobs):
                return
            name, m, e = self.jobs[self.k]
            b = self.rot.next()
            src = T[name][e].rearrange("(c p) f -> p c f", p=128)
            dstap = T["wall"][e * 128:(e + 1) * 128, m * 4096:(m + 1) * 4096].rearrange("p (c f) -> p c f", f=512)
            sv = self.stg[b][:].rearrange("p (c h) f -> p c (h f)", c=4) if name == "w_ed" else self.stg[b][:]
            P.load(self.queues[0], sv, src, [], ["stg%d" % b])
            P.copy(self.engs[self.k % len(self.engs)], self.cvt[b][:], self.stg[b][:], ["stg%d" % b], ["cvt%d" % b])
            P.load(self.queues[1], dstap, self.cvt[b][:], ["cvt%d" % b], ["wall"])
            self.k += 1


class ConvD:
    def __init__(self, P, T):
        self.P, self.T = P, T
        self.jobs = conv_jobs()
        self.k = 0

    def emit(self, n):
        P, T = self.P, self.T
        for _ in range(n):
            if self.k >= len(self.jobs):
                return
            name, m, e = self.jobs[self.k]
            cols = T["wall"][e * 128:(e + 1) * 128, m * 4096:(m + 1) * 4096]
            if name == "w_ed":
                src = T[name][e].rearrange("(c p) d -> p c d", p=128)
                dst = cols.rearrange("p (c d) -> p c d", d=1024)
            else:
                src = T[name][e].rearrange("(c p) f -> p c f", p=128)
                dst = cols.rearrange("p (c f) -> p c f", f=512)
            P.load("pool", dst, src, [], ["wall%d" % self.k])
            self.k += 1


def stage0(nc, sems, T):
    with contextlib.ExitStack() as st:
        sb, ps = tens(nc, st)
        P = Prog(nc, sems)
        cv = Conv(P, sb, T, engs=("dve", "pool", "act"), queues=("sp", "act"))
        cv.emit(96)
        return P.emit()


def stage1(nc, sems, T):
    with contextlib.ExitStack() as st:
        sb, ps = tens(nc, st)
        P = Prog(nc, sems)
        ident = sb("ident", [128, 128])
        identb = sb("identb", [128, 128], BF16)
        g1b = sb("g1b", [128, D])
        w_bf = sb("w_in_bf", [128, 8, 3072], BF16)
        xt = [sb("xt%d" % i, [128, D]) for i in range(2)]
        junk = sb("junk", [128, D], BF16)
        stat = sb("stat", [128, 4])
        xn = [sb("xn%d" % i, [128, D], BF16) for i in range(2)]
        hT = [sb("hT%d" % i, [128, 8, 512], BF16) for i in range(2)]
        fstg = [sb("fstg%d" % i, [128, 4, 512], BF16) for i in range(2)]
        tstg = [sb("tstg%d" % i, [128, 4, 512], BF16) for i in range(2)]
        pt = [ps("pt%d" % i, [128, D], BF16) for i in range(2)]
        pp = [ps("pp%d" % i, [128, 512]) for i in range(4)]

        P.load("sp", ident[:], T["ident"], [], ["ident"])
        P.copy("dve", identb[:], ident[:], ["ident"], ["identb"])
        P.load("act", g1b[:], T["norm1_g"].partition_broadcast(128), [], ["g1b"])
        for blk in (0, 1, 2, 3, 4, 5):
            P.load("pool", w_bf[:, :, blk * 512:(blk + 1) * 512], T["w_in"][:, blk * 512:(blk + 1) * 512].rearrange("(c p) n -> p c n", p=128),
                   [], ["w_bf%d" % blk])
        oh = sb("oh", [128, 2, 33, 128])
        rbb, biasT = T["rbb_sb"], T["biasT_sb"]
        P.load("act", oh[:].rearrange("p a b c -> p (a b c)"), T["oh"], [], ["oh"])
        P.load("act", rbb[:], T["rel_bias"].partition_broadcast(128), [], ["rbb"])

        def emit_bias(idx):
            kind, h = idx // 4, idx % 4
            dst_ = biasT[:, kind, h, :]
            kb = "biasT%d%d" % (kind, h)
            P.ts("pool", dst_, oh[:, kind, 32, :], NEG, None, ALU.mult, None, ["oh"], [kb])
            for b_ in range(32):
                P.stt("dve", dst_, oh[:, kind, b_, :], rbb[:, b_ * 4 + h:b_ * 4 + h + 1], dst_, ALU.mult, ALU.add, ["oh", "rbb", kb], [kb])

        rpp = Rot(4)
        cvj = ConvD(P, T)

        def s1_a(g):
            hb = g % 2
            emit_bias(g)
            cvj.emit(CONV_PER_GROUP[1])
            for ti in range(4):
                t = g * 4 + ti
                b = t % 2
                P.load("sp", xt[b][:], T["x"][t * 128:(t + 1) * 128, :], [], ["xt%d" % b])
                P.act(junk[:], xt[b][:], AF.Square, ["xt%d" % b], ["junk", "stat"], accum_out=stat[:, 0:1])
                P.ts("dve", stat[:, 1:2], stat[:, 0:1], 1.0 / D, EPS, ALU.mult, ALU.add, ["stat"], ["stat"])
                P.act(stat[:, 2:3], stat[:, 1:2], AF.Ln, ["stat"], ["stat"])
                P.act(stat[:, 3:4], stat[:, 2:3], AF.Exp, ["stat"], ["stat"], scale=-0.5)
                P.stt("dve", xn[b][:], xt[b][:], stat[:, 3:4], g1b[:], ALU.mult, ALU.mult, ["xt%d" % b, "stat", "g1b"], ["xn%d" % b])
                for kc in range(8):
                    P.tr(pt[b][:, kc * 128:(kc + 1) * 128], xn[b][:, kc * 128:(kc + 1) * 128], identb[:], ["xn%d" % b, "identb"], ["pt%d" % b])
                P.copy("act" if ti % 2 == 0 else "dve", hT[hb][:, :, ti * 128:(ti + 1) * 128], pt[b][:, :].rearrange("p (k t) -> p k t", k=8),
                       ["pt%d" % b], ["hT%d" % hb])

        def s1_b(g):
            hb = g % 2
            for blk in range(5):
                fb = (g * 5 + blk) % 2
                for ch in range(4):
                    col0 = blk * 512 + ch * 128
                    pb = rpp.next()
                    for kc in range(8):
                        P.mm(pp[pb][:, :], w_bf[:, kc, col0:col0 + 128], hT[hb][:, kc, :], ["w_bf%d" % blk, "hT%d" % hb], ["pp%d" % pb],
                             start=(kc == 0), stop=(kc == 7))
                    P.copy("act" if ch % 2 == 0 else "dve", fstg[fb][:, ch, :], pp[pb][:, :], ["pp%d" % pb], ["fstg%d" % fb])
                P.load("sp", T["featT"][blk].rearrange("(c p) t -> p c t", p=128)[:, :, g * 512:(g + 1) * 512], fstg[fb][:],
                       ["fstg%d" % fb], ["featT"])
            for bi, (blk, dst) in enumerate(((1, "vm_tok"), (5, "vd_tok"))):
                tb = (g * 2 + bi) % 2
                for ti in range(4):
                    pb = rpp.next()
                    for kc in range(8):
                        P.mm(pp[pb][:, :], hT[hb][:, kc, ti * 128:(ti + 1) * 128], w_bf[:, kc, blk * 512:(blk + 1) * 512],
                             ["w_bf%d" % blk, "hT%d" % hb], ["pp%d" % pb], start=(kc == 0), stop=(kc == 7))
                    P.copy("dve" if ti % 2 == 0 else "act", tstg[tb][:, ti, :], pp[pb][:, :], ["pp%d" % pb], ["tstg%d" % tb])
                P.load("sp", T[dst][g * 512:(g + 1) * 512, :].rearrange("(t p) f -> p t f", p=128), tstg[tb][:], ["tstg%d" % tb], [dst])

        for g in range(9):
            if g < 8:
                s1_a(g)
            if g >= 1:
                s1_b(g - 1)
        return P.emit()


def stage2(nc, sems, T):
    with contextlib.ExitStack() as st:
        sb, ps = tens(nc, st)
        P = Prog(nc, sems)
        ident = sb("ident", [128, 128])
        identb = sb("identb", [128, 128], BF16)
        tri = sb("tri", [128, 128])
        bigtri = sb("bigtri", [128, 128])
        sel = sb("sel", [4, 512])
        cw = sb("cw", [128, 4, 4])
        cb = sb("cb", [128, 4])
        mg = sb("mg", [128, 4])
        msk = sb("msk", [128, 4])
        wq = sb("wq", [128, 4, 128], BF16)
        wk = sb("wk", [128, 4, 128], BF16)
        wgt = sb("wgt", [128, 12, 8], BF16)
        bi = sb("bi", [4, 1])
        bfn = sb("bfn", [4, 1])
        zeros = sb("zeros", [4, 512])
        carryB = sb("carryB", [4, 1])
        carryM = sb("carryM", [4, 1])
        Cf = sb("Cf", [128, 4, 129])
        Cb = sb("Cb", [128, 4, 129], BF16)
        c_sb = [sb("c_sb%d" % i, [128, 4, 515], BF16) for i in range(2)]
        z_sb = [sb("z_sb%d" % i, [128, 4, 512], BF16) for i in range(2)]
        vmT = [sb("vmT%d" % i, [128, 4, 512], BF16) for i in range(2)]
        vaug = [sb("vaug%d" % i, [128, 4, 4, 129], BF16) for i in range(2)]
        cacc = [sb("cacc%d" % i, [128, 512]) for i in range(2)]
        cact = sb("cact", [128, 4, 512], BF16)
        sigz = sb("sigz", [128, 4, 512], BF16)
        scs = sb("scs", [128, 4, 512], BF16)
        qT = sb("qT", [128, 4, 512], BF16)
        kT = sb("kT", [128, 4, 512], BF16)
        ktok = sb("ktok", [128, 4, 4, 128], BF16)
        i_row = sb("i_row", [4, 512])
        e_row = sb("e_row", [4, 512])
        sp_row = sb("sp_row", [4, 512])
        Bn = sb("Bn", [4, 513])
        A_row = sb("A_row", [4, 512])
        Mx = sb("Mx", [4, 513])
        N_row = sb("N_row", [4, 512])
        cols = sb("cols", [128, 4, 3, 4])
        eN = sb("eN", [128, 4, 4])
        Mb = sb("Mb", [128, 4, 5])
        nMb = sb("nMb", [128, 4, 5])
        dec = sb("dec", [128, 4, 4])
        spa = sb("spa", [128, 4, 4])
        Mrow = sb("Mrow", [128, 4, 512])
        tmpD = [sb("tmpD%d" % i, [128, 128]) for i in range(4)]
        Dt = [sb("Dt%d" % i, [128, 128]) for i in range(4)]
        Dm = [sb("Dm%d" % i, [128, 128]) for i in range(2)]
        wT = [sb("wT%d" % i, [128, 128], BF16) for i in range(4)]
        intra = [sb("intra%d" % i, [128, 129]) for i in range(4)]
        comb = [sb("comb%d" % i, [128, 129]) for i in range(4)]
        sm = [sb("sm%d" % i, [128, 16]) for i in range(4)]
        hh = [sb("hh%d" % i, [128, 128]) for i in range(4)]
        hn = [sb("hn%d" % i, [128, 128], BF16) for i in range(4)]
        y1 = [sb("y1%d" % i, [128, 128], BF16) for i in range(4)]
        ymg = [sb("ymg%d" % i, [128, 4, 512], BF16) for i in range(2)]
        wkc = [sb("wkc%d" % i, [128, 1]) for i in range(4)]
        vw = [sb("vw%d" % i, [128, 129], BF16) for i in range(4)]
        pA = ps("pA", [128, 512])
        pB = ps("pB", [128, 512])
        pG = ps("pG", [128, 512])
        ptb = ps("ptb", [128, 1024], BF16)
        pS = [ps("pS%d" % i, [128, 512]) for i in range(2)]
        pO = [ps("pO%d" % i, [128, 512]) for i in range(2)]
        P.load("sp", ident[:], T["ident"], [], ["ident"])
        P.copy("dve", identb[:], ident[:], ["ident"], ["identb"])
        P.load("sp", tri[:], T["tri"], [], ["tri"])
        P.ts("dve", bigtri[:], tri[:], -1.0, -1.0e4, ALU.add, ALU.mult, ["tri"], ["bigtri"])
        P.load("sp", sel[:], T["sel"], [], ["sel"])
        P.load("sp", cw[:], T["conv_w"].rearrange("j (c p) -> p j c", p=128), [], ["cw"])
        P.load("sp", cb[:], T["conv_b"].rearrange("(c p) -> p c", p=128), [], ["cb"])
        P.load("sp", mg[:], T["m_norm_g"].rearrange("(c p) -> p c", p=128), [], ["mg"])
        P.load("sp", msk[:], T["m_skip"].rearrange("(c p) -> p c", p=128), [], ["msk"])
        P.load("pool", wq[:], T["w_mq"].rearrange("h d e -> d h e"), [], ["wq"])
        P.load("pool", wk[:], T["w_mk"].rearrange("h d e -> d h e"), [], ["wk"])
        P.load("pool", wgt[:], T["w_mgate"].rearrange("(c p) g -> p c g", p=128), [], ["wgt"])
        P.load("sp", bi[:], T["b_mgate"][0:4].rearrange("(p o) -> p o", o=1), [], ["bi"])
        P.load("sp", bfn[:], T["b_mgate"][4:8].rearrange("(p o) -> p o", o=1), [], ["bfn"])
        P.ts("dve", bfn[:], bfn[:], -1.0, None, ALU.mult, None, ["bfn"], ["bfn"])
        P.memset("pool", zeros[:], 0.0, ["zeros"])
        P.memset("pool", carryB[:], 0.0, ["carryB"])
        P.memset("pool", carryM[:], 0.0, ["carryM"])
        P.memset("pool", Cf[:], 0.0, ["Cf%d" % h_ for h_ in range(4)])
        P.memset("pool", Cb[:], 0.0, ["Cb%d" % h_ for h_ in range(4)])
        for i in range(2):
            P.memset("pool", vaug[i][:], 1.0, ["vaug%d" % i])
            P.memset("pool", c_sb[i][:], 0.0, ["c_sb%d" % i])

        featT = T["featT"]
        rS, rO, r2 = Rot(2), Rot(2), Rot(2)
        cvj = ConvD(P, T)
        cvj.k = 8 * CONV_PER_GROUP[1]
        for g in range(8):
            b = g % 2
            t0 = g * 512
            cvj.emit(CONV_PER_GROUP[2])
            kc_, kz, kv, kva = "c_sb%d" % b, "z_sb%d" % b, "vmT%d" % b, "vaug%d" % b
            cview = featT[0].rearrange("(c p) t -> p c t", p=128)
            if g == 0:
                P.load("sp", c_sb[b][:, :, 3:515], cview[:, :, 0:512], [], [kc_])
            else:
                P.load("sp", c_sb[b][:, :, 0:515], cview[:, :, t0 - 3:t0 + 512], [], [kc_])
            P.load("act", z_sb[b][:], featT[2].rearrange("(c p) t -> p c t", p=128)[:, :, t0:t0 + 512], [], [kz])
            P.load("act", vmT[b][:], featT[1].rearrange("(c p) t -> p c t", p=128)[:, :, t0:t0 + 512], [], [kv])
            for ti in range(4):
                P.load("sp" if ti % 2 == 0 else "act", vaug[b][:, ti, :, 0:128],
                       T["vm_tok"][t0 + ti * 128:t0 + (ti + 1) * 128, :].rearrange("p (h e) -> p h e", e=128), [kva], [kva])
            for ch in range(4):
                ab = ch % 2
                ka = "cacc%d" % ab
                e1 = "dve" if ch % 2 == 0 else "pool"
                P.ts("dve", cacc[ab][:], c_sb[b][:, ch, 0:512], cw[:, 0, ch:ch + 1], cb[:, ch:ch + 1], ALU.mult, ALU.add, [kc_, "cw", "cb"], [ka])
                for j in range(1, 4):
                    P.stt("dve" if j % 2 == 0 else "pool", cacc[ab][:], c_sb[b][:, ch, j:j + 512], cw[:, j, ch:ch + 1], cacc[ab][:], ALU.mult, ALU.add,
                          [kc_, "cw", ka], [ka])
                P.act(cact[:, ch, :], cacc[ab][:], AF.Silu, [ka], ["cact"])
                P.ts("pool", scs[:, ch, :], cact[:, ch, :], msk[:, ch:ch + 1], None, ALU.mult, None, ["cact", "msk"], ["scs"])
            P.act(sigz[:].rearrange("p c t -> p (c t)"), z_sb[b][:].rearrange("p c t -> p (c t)"), AF.Sigmoid, [kz], ["sigz"])
            for h in range(4):
                P.mm(pA[:, :], wq[:, h, :], cact[:, h, :], ["wq", "cact"], ["pA"])
                P.copy("act", qT[:, h, :], pA[:, :], ["pA"], ["qT"])
                P.mm(pB[:, :], wk[:, h, :], cact[:, h, :], ["wk", "cact"], ["pB"])
                P.copy("dve", kT[:, h, :], pB[:, :], ["pB"], ["kT"])
            for ti in range(4):
                pz = pA if ti % 2 == 0 else pB
                kz_ = "pA" if ti % 2 == 0 else "pB"
                for h in range(4):
                    P.mm(pz[:, h * 128:(h + 1) * 128], cact[:, h, ti * 128:(ti + 1) * 128], wk[:, h, :], ["cact", "wk"], [kz_])
                P.copy("act" if ti % 2 == 0 else "dve", ktok[:, ti, :, :].rearrange("p h e -> p (h e)"), pz[:, :], [kz_], ["ktok"])
            srcs = [(qT, "qT")] * 4 + [(kT, "kT")] * 4 + [(vmT[b], kv)] * 4
            for c in range(12):
                sap, skey = srcs[c]
                P.mm(pG[0:4, :], wgt[:, c, 0:4], sap[:, c % 4, :], ["wgt", skey], ["pG"], start=(c == 0), stop=(c == 11))
            for c in range(12):
                sap, skey = srcs[c]
                P.mm(pB[0:4, :], wgt[:, c, 4:8], sap[:, c % 4, :], ["wgt", skey], ["pB"], start=(c == 0), stop=(c == 11))
            P.ts("dve", i_row[:], pG[0:4, :], bi[:, 0:1], None, ALU.add, None, ["pG", "bi"], ["i_row"])
            P.act(e_row[:], pB[0:4, :], AF.Exp, ["pB", "bfn"], ["e_row"], bias=bfn[:, 0:1], scale=-1.0)
            P.act(sp_row[:], e_row[:], AF.Ln, ["e_row"], ["sp_row"], bias=1.0)
            P.op("dve", (lambda o, d0, d1, ini: (lambda e: e.tensor_tensor_scan(out=o, data0=d0, data1=d1, initial=ini, op0=ALU.add, op1=ALU.add)))(
                Bn[:, 1:513], sp_row[:], zeros[:], carryB[:, 0:1]), ["sp_row", "zeros", "carryB"], ["Bn"])
            P.tt("dve", A_row[:], i_row[:], Bn[:, 1:513], ALU.add, ["i_row", "Bn"], ["A_row"])
            P.copy("dve", Mx[:, 0:1], carryM[:, 0:1], ["carryM"], ["Mx"])
            P.op("dve", (lambda o, d0, d1, ini: (lambda e: e.tensor_tensor_scan(out=o, data0=d0, data1=d1, initial=ini, op0=ALU.max, op1=ALU.max)))(
                Mx[:, 1:513], A_row[:], A_row[:], carryM[:, 0:1]), ["A_row", "carryM", "Mx"], ["Mx"])
            P.copy("dve", carryB[:, 0:1], Bn[:, 512:513], ["Bn"], ["carryB"])
            P.copy("dve", carryM[:, 0:1], Mx[:, 512:513], ["Mx"], ["carryM"])
            P.tt("dve", N_row[:], Bn[:, 1:513], Mx[:, 1:513], ALU.subtract, ["Bn", "Mx"], ["N_row"])
            for c in range(4):
                for k3, (rap, rkey, off) in enumerate(((A_row, "A_row", 0), (Mx, "Mx", 1), (N_row, "N_row", 0))):
                    o0 = c * 12 + k3 * 4
                    P.tr(pA[:, o0:o0 + 4], rap[:, off + c * 128: off + (c + 1) * 128], ident[0:4, 0:4], [rkey, "ident"], ["pA"])
            P.copy("dve", cols[:].rearrange("p c k h -> p (c k h)"), pA[:, 0:48], ["pA"], ["cols"])
            P.act(eN[:], cols[:, :, 2, :], AF.Exp, ["cols"], ["eN"])
            for h in range(4):
                P.mm(pB[:, h * 5:(h + 1) * 5], sel[:, h * 128:(h + 1) * 128], Mx[:, 0:513:128], ["sel", "Mx"], ["pB"])
            P.copy("dve", Mb[:].rearrange("p h c -> p (h c)"), pB[:, 0:20], ["pB"], ["Mb"])
            P.ts("dve", nMb[:], Mb[:], -1.0, None, ALU.mult, None, ["Mb"], ["nMb"])
            P.tt("dve", dec[:], Mb[:, :, 0:4], Mb[:, :, 1:5], ALU.subtract, ["Mb"], ["dec"])
            P.act(dec[:], dec[:], AF.Exp, ["dec"], ["dec"])
            P.tt("dve", spa[:], Mb[:, :, 0:4].rearrange("p h c -> p c h"), cols[:, :, 1, :], ALU.subtract, ["Mb", "cols"], ["spa"])
            P.act(spa[:], spa[:], AF.Exp, ["spa"], ["spa"])
            for h in range(4):
                pz, kz_ = (pA, "pA") if h % 2 == 0 else (pB, "pB")
                P.mm(pz[:, :], sel[:, h * 128:(h + 1) * 128], Mx[:, 1:513], ["sel", "Mx"], [kz_])
                P.tt("dve", Mrow[:, h, :].rearrange("p (c t) -> p c t", t=128), pz[:, :].rearrange("p (c t) -> p c t", t=128),
                     bigtri[:].unsqueeze(1).to_broadcast([128, 4, 128]), ALU.add, [kz_, "bigtri"], ["Mrow"])
            HB = [(pS[0], "pS0"), (pS[1], "pS1"), (pO[0], "pO0"), (pO[1], "pO1")]
            UB = [(pA, "pA"), (pB, "pB")]
            for c in range(4):
                cs = slice(c * 128, (c + 1) * 128)

                def phases(h, c=c, cs=cs):
                    hb_, kH = HB[h]
                    ub_, kU = UB[h // 2]
                    uo = (h % 2) * 256
                    Acol = cols[:, c, 0, h:h + 1]
                    ks = "sm%d" % h
                    smt = sm[h]
                    kCf, kCb = "Cf%d" % h, "Cb%d" % h

                    def p0():
                        P.mm(hb_[:, 0:128], kT[:, h, cs], qT[:, h, cs], ["kT", "qT"], [kH])
                        P.ts("dve", tmpD[h][:], Mrow[:, h, cs], Acol, 0.0, ALU.subtract, ALU.max, ["Mrow", "cols"], ["tmpD%d" % h])
                        P.act(wkc[h][:], Acol, AF.Exp, ["cols", "nMb"], ["wkc%d" % h], bias=nMb[:, h, c + 1:c + 2])

                    def p1():
                        P.act(Dt[h][:], tmpD[h][:], AF.Exp, ["tmpD%d" % h], ["Dt%d" % h], scale=-1.0)
                        P.op("act", (lambda o, i_, sc: (lambda e: e.activation(out=o, in_=i_, func=AF.Copy, scale=sc)))(
                            vw[h][:], vaug[b][:, c, h, :], wkc[h][:, 0:1]), [kva, "wkc%d" % h], ["vw%d" % h])

                    def p2():
                        P.tt("dve", wT[h][:], hb_[:, 0:128], Dt[h][:], ALU.mult, [kH, "Dt%d" % h], ["wT%d" % h])

                    def p3():
                        P.mm(hb_[:, 128:257], wT[h][:], vaug[b][:, c, h, :], ["wT%d" % h, kva], [kH])
                        P.mm(hb_[:, 257:386], qT[:, h, cs], Cb[:, h, :], ["qT", kCb], [kH])
                        P.mm(ub_[:, uo:uo + 129], ktok[:, c, h, :], vw[h][:], ["ktok", "vw%d" % h], [kU])

                    def p4():
                        P.copy("act", intra[h][:], hb_[:, 128:257], [kH], ["intra%d" % h])
                        P.stt("dve", Cf[:, h, :], Cf[:, h, :], dec[:, h, c:c + 1], ub_[:, uo:uo + 129], ALU.mult, ALU.add, [kCf, "dec", kU], [kCf])

                    def p5():
                        P.stt("dve", comb[h][:], hb_[:, 257:386], spa[:, c, h:h + 1], intra[h][:], ALU.mult, ALU.add,
                              [kH, "spa", "intra%d" % h], ["comb%d" % h])
                        P.copy("act", Cb[:, h, :], Cf[:, h, :], [kCf], [kCb])

                    def p6():
                        P.stt("dve", smt[:, 0:1], comb[h][:, 128:129], -1.0, comb[h][:, 128:129], ALU.mult, ALU.max, ["comb%d" % h], [ks])
                        P.stt("dve", smt[:, 1:2], smt[:, 0:1], ML_SCALE, eN[:, c, h:h + 1], ALU.mult, ALU.max, [ks, "eN"], [ks])
                        P.op("dve", (lambda o, i_: (lambda e: e.reciprocal(out=o, in_=i_)))(smt[:, 2:3], smt[:, 1:2]), [ks], [ks])
                        P.ts("dve", hh[h][:], comb[h][:, 0:128], smt[:, 2:3], ML_SCALE, ALU.mult, ALU.mult, ["comb%d" % h, ks], ["hh%d" % h])

                    def p7():
                        P.op("dve", (lambda o, i_: (lambda e: e.bn_stats(out=o, in_=i_)))(smt[:, 4:10], hh[h][:]), ["hh%d" % h], [ks])
                        P.op("dve", (lambda o, i_: (lambda e: e.bn_aggr(out=o, in_=i_)))(smt[:, 10:12], smt[:, 4:10]), [ks], [ks])
                        P.ts("dve", smt[:, 12:13], smt[:, 11:12], EPS, None, ALU.add, None, [ks], [ks])

                    def p8():
                        P.act(smt[:, 13:14], smt[:, 12:13], AF.Ln, [ks], [ks])
                        P.act(smt[:, 14:15], smt[:, 13:14], AF.Exp, [ks], [ks], scale=-0.5)

                    def p9():
                        P.ts("dve", hn[h][:], hh[h][:], smt[:, 10:11], smt[:, 14:15], ALU.subtract, ALU.mult, ["hh%d" % h, ks], ["hn%d" % h])

                    def p10():
                        P.tr(ptb[:, h * 128:(h + 1) * 128], hn[h][:], identb[:], ["hn%d" % h, "identb"], ["ptb"])

                    def p11():
                        P.stt("dve", y1[h][:], ptb[:, h * 128:(h + 1) * 128], mg[:, h:h + 1], scs[:, h, cs], ALU.mult, ALU.add,
                              ["ptb", "mg", "scs"], ["y1%d" % h])
                        P.tt("dve", ymg[b][:, h, cs], y1[h][:], sigz[:, h, cs], ALU.mult, ["y1%d" % h, "sigz"], ["ymg%d_%d" % (b, h)])

                    return [p0, p1, p2, p3, p4, p5, p6, p7, p8, p9, p10, p11]

                plist = [phases(h) for h in range(4)]
                for k_ in range(12):
                    for h in range(4):
                        plist[h][k_]()
            P.load("sp", T["ymT"].rearrange("(c p) t -> p c t", p=128)[:, :, t0:t0 + 512], ymg[b][:], ["ymg%d_%d" % (b, h_) for h_ in range(4)], ["ymT"])
        return P.emit()


def stage3(nc, sems, T):
    with contextlib.ExitStack() as st:
        sb, ps = tens(nc, st)
        P = Prog(nc, sems)
        ident = sb("ident", [128, 128])
        identb = sb("identb", [128, 128], BF16)
        qT = sb("qT", [128, 4, S], BF16)
        kT = sb("kT", [128, 4, S], BF16)
        vaug = sb("vaug", [128, NT, 4, 129], BF16)
        rbb = T["rbb_sb"]
        biasT = T["biasT_sb"]
        lqb = sb("lqb", [128, 256])
        lt = sb("lt", [128, 64])
        lam = sb("lam", [128, 8])
        dag = sb("dag", [128, 1])
        PT = [sb("PT%d" % i, [128, 512], BF16) for i in range(4)]
        tmpn = [sb("tmpn%d" % i, [128, 128]) for i in range(2)]
        t0s = [sb("t0s%d" % i, [128, 128]) for i in range(2)]
        av = [sb("av%d" % i, [128, 128]) for i in range(2)]
        junk = sb("junk3", [128, 128])
        sm = [sb("sm3%d" % i, [128, 8]) for i in range(4)]
        an = [sb("an%d" % i, [128, 128], BF16) for i in range(2)]
        ydg = [sb("ydg%d" % i, [128, 512], BF16) for i in range(2)]
        pS = [ps("pS%d" % i, [128, 512]) for i in range(3)]
        acc = [ps("acc%d" % i, [128, 512]) for i in range(4)]
        ptb = ps("ptb", [128, 1024], BF16)

        P.load("sp", ident[:], T["ident"], [], ["ident"])
        P.copy("dve", identb[:], ident[:], ["ident"], ["identb"])
        P.memset("pool", vaug[:, 0:4], 1.0, ["vaug%d" % t for t in range(4)])
        P.memset("pool", vaug[:, 4:NT], 1.0, ["vaug%d" % t for t in range(4, NT)])
        P.load("sp", lqb[:], T["lambda_qk"].partition_broadcast(128), [], ["lqb"])
        P.load("sp", dag[:], T["da_norm_g"].rearrange("(p o) -> p o", o=1), [], ["dag"])
        for h in range(4):
            P.load("sp", qT[:, h, :], T["featT"][3][h * 128:(h + 1) * 128, :], [], ["qT%d" % h])
            P.load("act", kT[:, h, :], T["featT"][4][h * 128:(h + 1) * 128, :], [], ["kT%d" % h])
            if h == 0:
                for t in range(NT):
                    P.load("sp" if t % 2 == 0 else "act", vaug[:, t, :, 0:128],
                           T["vd_tok"][t * 128:(t + 1) * 128, :].rearrange("p (h e) -> p h e", e=128), ["vaug%d" % t], ["vaug%d" % t])
        zt = sb("zt", [128, 3, D], BF16)
        P.memset("dve", zt[:], 0.0, ["zt"])
        xs_v = T["xs"].rearrange("(n p) d -> p n d", p=128)
        for i in range(42):
            P.load("sp", xs_v[:, i * 3:(i + 1) * 3, :], zt[:], ["zt"], ["xs_zero%d" % i])
        P.ts("dve", dag[:], dag[:], 1.0 - LAM_INIT, None, ALU.mult, None, ["dag"], ["dag"])
        for i in range(2):
            P.tt("dve", lt[:], lqb[:, (2 * i) * 64:(2 * i + 1) * 64], lqb[:, (2 * i + 1) * 64:(2 * i + 2) * 64], ALU.mult, ["lqb", "lt"], ["lt"])
            P.op("dve", (lambda o, i_: (lambda e: e.reduce_sum(out=o, in_=i_, axis=AX.X)))(lam[:, 4 + i:5 + i], lt[:]), ["lt"], ["lam"])
        P.act(lam[:, 0:2], lam[:, 4:6], AF.Exp, ["lam"], ["lam"])
        P.tt("dve", lam[:, 2:3], lam[:, 0:1], lam[:, 1:2], ALU.subtract, ["lam"], ["lam"])
        P.ts("dve", lam[:, 3:4], lam[:, 2:3], LAM_INIT, -1.0, ALU.add, ALU.mult, ["lam"], ["lam"])
        rS, rP, r2 = Rot(3), Rot(4), Rot(2)
        cvj = ConvD(P, T)
        cvj.k = 8 * (CONV_PER_GROUP[1] + CONV_PER_GROUP[2])
        its = [(h, g, c, j) for h in range(4) for g in range(8) for c in range(2) for j in range(4 * g + 4)]

        def emit_S(it):
            h, g, c, j = it
            prow = slice(c * 64, (c + 1) * 64)
            i_lo = max(j, 4 * g) - 4 * g
            sB = rS.next()
            pb = rP.next()
            kS, kP = "pS%d" % sB, "PT%d" % pb
            P.mm(pS[sB][:, i_lo * 128:512], kT[prow, h, j * 128:(j + 1) * 128], qT[prow, h, g * 512 + i_lo * 128:(g + 1) * 512],
                 ["kT%d" % h, "qT%d" % h], [kS])
            far_lo = None
            for i in range(i_lo, 4):
                dist = 4 * g + i - j
                if dist >= 2:
                    far_lo = i
                    break
                n2 = r2.next()
                P.stt("dve", tmpn[n2][:], pS[sB][:, i * 128:(i + 1) * 128], DA_SCALE, biasT[:, dist, h, :], ALU.mult, ALU.add,
                      [kS, "biasT"], ["tmpn%d" % n2])
                P.act(PT[pb][:, i * 128:(i + 1) * 128], tmpn[n2][:], AF.Exp, ["tmpn%d" % n2], [kP])
            if far_lo is not None:
                P.act(PT[pb][:, far_lo * 128:512], pS[sB][:, far_lo * 128:512], AF.Exp, [kS, "rbb"], [kP],
                      bias=rbb[:, 31 * 4 + h:31 * 4 + h + 1], scale=DA_SCALE)
            return pb, i_lo

        def emit_AV(it, pb, i_lo):
            h, g, c, j = it
            kP = "PT%d" % pb
            if c == 0 and j == 0:
                cvj.emit(CONV_PER_GROUP[3])
                for a_ in range(4):
                    P.memset("dve", acc[a_][:, :], 0.0, ["acc%d" % a_])
            for i in range(i_lo, 4):
                a_ = c * 2 + i // 2
                off = (i % 2) * 256
                P.mm(acc[a_][:, off:off + 129], PT[pb][:, i * 128:(i + 1) * 128], vaug[:, j, h, :], [kP, "vaug%d" % j], ["acc%d" % a_],
                     start=False, stop=False, skip=True)
            if c == 1 and j == 4 * g + 3:
                finalize(h, g)

        def finalize(h, g):
            yb = (h * 8 + g) % 2
            for i in range(4):
                n2 = r2.next()
                ks = "sm3%d" % n2
                smt = sm[n2]
                a0, a1 = acc[i // 2], acc[2 + i // 2]
                k0, k1 = "acc%d" % (i // 2), "acc%d" % (2 + i // 2)
                off = (i % 2) * 256
                P.op("dve", (lambda o, i_: (lambda e: e.reciprocal(out=o, in_=i_)))(smt[:, 0:1], a0[:, off + 128:off + 129]), [k0], [ks])
                P.op("dve", (lambda o, i_: (lambda e: e.reciprocal(out=o, in_=i_)))(smt[:, 1:2], a1[:, off + 128:off + 129]), [k1], [ks])
                P.tt("dve", smt[:, 2:3], smt[:, 1:2], lam[:, 3:4], ALU.mult, [ks, "lam"], [ks])
                P.op("act", (lambda o, i_, sc: (lambda e: e.activation(out=o, in_=i_, func=AF.Copy, scale=sc)))(t0s[n2][:], a0[:, off:off + 128], smt[:, 0:1]),
                     [k0, ks], ["t0s%d" % n2])
                P.stt("dve", av[n2][:], a1[:, off:off + 128], smt[:, 2:3], t0s[n2][:], ALU.mult, ALU.add, [k1, ks, "t0s%d" % n2], ["av%d" % n2])
                P.act(junk[:], av[n2][:], AF.Square, ["av%d" % n2], ["junk3", ks], accum_out=smt[:, 3:4])
                P.ts("dve", smt[:, 4:5], smt[:, 3:4], 1.0 / 128, SUBLN_EPS, ALU.mult, ALU.add, [ks], [ks])
                P.act(smt[:, 5:6], smt[:, 4:5], AF.Ln, [ks], [ks])
                P.act(smt[:, 6:7], smt[:, 5:6], AF.Exp, [ks], [ks], scale=-0.5)
                P.ts("dve", an[n2][:], av[n2][:], smt[:, 6:7], None, ALU.mult, None, ["av%d" % n2, ks], ["an%d" % n2])
                P.tr(ptb[:, n2 * 512:n2 * 512 + 128], an[n2][:], identb[:], ["an%d" % n2, "identb"], ["ptb"])
                P.ts("dve", ydg[yb][:, i * 128:(i + 1) * 128], ptb[:, n2 * 512:n2 * 512 + 128], dag[:, 0:1], None, ALU.mult, None,
                     ["ptb", "dag"], ["ydg%d" % yb])
            P.load("act", T["ydT"][h * 128:(h + 1) * 128, g * 512:(g + 1) * 512], ydg[yb][:], ["ydg%d" % yb], ["ydT"])

        pend = []
        for it in its:
            pend.append((it,) + emit_S(it))
            if len(pend) > 2:
                emit_AV(*pend.pop(0))
        while pend:
            emit_AV(*pend.pop(0))
        return P.emit()


def stage4(nc, sems, T):
    with contextlib.ExitStack() as st:
        sb, ps = tens(nc, st)
        P = Prog(nc, sems)
        ident = sb("ident", [128, 128])
        wo = sb("wo", [128, 8, D], BF16)
        yT = sb("yT", [128, 8, S], BF16)
        g2b = sb("g2b", [128, D])
        wr = sb("wr", [128, 8, 36])
        brb = sb("brb", [128, 36])
        xt = [sb("xt%d" % i, [128, D]) for i in range(2)]
        x2 = [sb("x2%d" % i, [128, D]) for i in range(2)]
        junk = sb("junk4", [128, D], BF16)
        stat = [sb("stat4%d" % i, [128, 4]) for i in range(2)]
        h2 = [sb("h2%d" % i, [128, D]) for i in range(2)]
        h2T = [sb("h2T%d" % i, [128, 8, 128]) for i in range(2)]
        h2bf = [sb("h2bf%d" % i, [128, D], BF16) for i in range(2)]
        lstr = sb("lstr", [128, 128])
        ones = sb("ones", [128, 128])
        thr = sb("thr", [128, 16 + NSUP])
        E1 = sb("E1", [128, NT, 4, 8])
        E2 = sb("E2", [128, NT, 4, 8])
        Es = sb("Es", [128, NT * 32])
        within = sb("within", [128, NT, 32])
        csb = sb("csb", [128, NT, 32])
        incl = sb("incl", [128, NT, 32])
        zer = sb("zer", [128, NT])
        cmpb = sb("cmpb", [128, NSUP * 32])
        ntl = sb("ntl", [128, 32])
        inct = sb("inct", [128, 32])
        offb = sb("offb", [128, 32])
        offe = sb("offe", [128, 32])
        Rr = sb("Rr", [128, NT, 32])
        posf = sb("posf", [128, NT, 2])
        tef = sb("tef", [128, 64])
        pidx = sb("pidx", [128, 1])
        h2Tb = [sb("h2Tb%d" % i, [128, 8, 128], BF16) for i in range(2)]
        lgt = sb("lgt", [128, NT, 36])
        mxg = sb("mxg", [128, NT])
        ohg = sb("ohg", [128, NT, 4])
        eg = sb("eg", [128, NT, 4])
        sg = sb("sg", [128, NT])
        tmp4 = sb("tmp4", [128, NT, 4, 8])
        les = sb("les", [128, NT, 8])
        le2 = sb("le2", [128, NT, 8])
        m1 = sb("m1", [128, NT])
        m2 = sb("m2", [128, NT])
        oh1 = sb("oh1", [128, NT, 8])
        oh2 = sb("oh2", [128, NT, 8])
        w1 = sb("w1", [128, NT])
        w2 = sb("w2", [128, NT])
        gf = sb("gf", [128, NT, 8])
        gts = sb("gts", [128, NT, 4, 8])
        pO = [ps("pO%d" % i, [128, 512]) for i in range(4)]
        pT = [ps("pT%d" % i, [128, 512]) for i in range(2)]
        pR = ps("pR", [128, 512])

        P.load("sp", ident[:], T["ident"], [], ["ident"])
        P.load("pool", wo[:], T["w_out"].rearrange("(c p) n -> p c n", p=128), [], ["wo"])
        P.load("sp", g2b[:], T["norm2_g"].partition_broadcast(128), [], ["g2b"])
        P.load("sp", wr[:], T["w_r"].rearrange("(c p) n -> p c n", p=128), [], ["wr"])
        P.load("sp", brb[:], T["b_r"].partition_broadcast(128), [], ["brb"])
        ymT_v = T["ymT"].rearrange("(c p) t -> p c t", p=128)
        ydT_v = T["ydT"].rearrange("(c p) t -> p c t", p=128)
        for c in range(4):
            cs = slice(c * 1024, (c + 1) * 1024)
            P.load("sp", yT[:, 0:4, cs], ymT_v[:, :, cs], [], ["yTm%d" % c])
            P.load("act", yT[:, 4:8, cs], ydT_v[:, :, cs], [], ["yTd%d" % c])
        rO = Rot(2)

        def s4_a(t):
            b = t % 2
            ts_ = slice(t * 128, (t + 1) * 128)
            P.load("sp", xt[b][:], T["x"][ts_, :], [], ["xt%d" % b])
            for half in range(2):
                pb = rO.next() * 2 + half
                for kc in range(8):
                    P.mm(pO[pb][:, :], yT[:, kc, ts_], wo[:, kc, half * 512:(half + 1) * 512], [("yTm%d" if kc < 4 else "yTd%d") % (t // 8), "wo"], ["pO%d" % pb], start=(kc == 0), stop=(kc == 7))
                P.tt("dve", x2[b][:, half * 512:(half + 1) * 512], pO[pb][:, :], xt[b][:, half * 512:(half + 1) * 512], ALU.add,
                     ["pO%d" % pb, "xt%d" % b], ["x2%d" % b])
            P.load("sp", T["x2"][ts_, :], x2[b][:], ["x2%d" % b], ["x2d"])
            sk = "stat4%d" % b
            P.act(junk[:], x2[b][:], AF.Square, ["x2%d" % b], ["junk4", sk], accum_out=stat[b][:, 0:1])
            P.ts("dve", stat[b][:, 1:2], stat[b][:, 0:1], 1.0 / D, EPS, ALU.mult, ALU.add, [sk], [sk])
            P.act(stat[b][:, 2:3], stat[b][:, 1:2], AF.Ln, [sk], [sk])
            P.act(stat[b][:, 3:4], stat[b][:, 2:3], AF.Exp, [sk], [sk], scale=-0.5)
            P.stt("dve", h2[b][:], x2[b][:], stat[b][:, 3:4], g2b[:], ALU.mult, ALU.mult, ["x2%d" % b, sk, "g2b"], ["h2%d" % b])
            P.copy("dve", h2bf[b][:], h2[b][:], ["h2%d" % b], ["h2bf%d" % b])
            P.load("sp", T["h2b"][ts_, :], h2bf[b][:], ["h2bf%d" % b], ["h2bd"])

        def s4_b(t):
            b = t % 2
            ts_ = slice(t * 128, (t + 1) * 128)
            for kc in range(8):
                pz = pT[kc // 4]
                P.tr(pz[:, (kc % 4) * 128:(kc % 4 + 1) * 128], h2[b][:, kc * 128:(kc + 1) * 128], ident[:], ["h2%d" % b, "ident"], ["pT%d" % (kc // 4)])
            for hf in range(2):
                P.copy("act", h2T[b][:, hf * 4:(hf + 1) * 4, :].rearrange("p k t -> p (k t)"), pT[hf][:, :], ["pT%d" % hf], ["h2T%d" % b])
                P.copy("dve", h2Tb[b][:, hf * 4:(hf + 1) * 4, :].rearrange("p k t -> p (k t)"), pT[hf][:, :], ["pT%d" % hf], ["h2Tb%d" % b])
            P.load("sp", T["h2T"].rearrange("(c p) t -> p c t", p=128)[:, :, ts_], h2Tb[b][:], ["h2Tb%d" % b], ["h2Td"])
            for kc in range(8):
                P.mm(pR[:, 0:36], h2T[b][:, kc, :], wr[:, kc, :], ["h2T%d" % b, "wr"], ["pR"], start=(kc == 0), stop=(kc == 7))
            P.tt("dve", lgt[:, t, :], pR[:, 0:36], brb[:], ALU.add, ["pR", "brb"], ["lgt"])

        for t in range(NT + 1):
            if t < NT:
                s4_a(t)
            if t >= 1:
                s4_b(t - 1)
        lg = lgt[:, :, 0:4]
        le = lgt[:, :, 4:36].rearrange("p t (g e) -> p t g e", e=8)
        red = lambda o, i_, op: (lambda e: e.tensor_reduce(out=o, in_=i_, axis=AX.X, op=op))
        P.op("dve", red(mxg[:], lg, ALU.max), ["lgt"], ["mxg"])
        P.tt("dve", ohg[:], lg, mxg[:].unsqueeze(2).to_broadcast([128, NT, 4]), ALU.is_ge, ["lgt", "mxg"], ["ohg"])
        P.tt("dve", eg[:], lg, mxg[:].unsqueeze(2).to_broadcast([128, NT, 4]), ALU.subtract, ["lgt", "mxg"], ["eg"])
        P.act(eg[:], eg[:], AF.Exp, ["eg"], ["eg"])
        P.op("dve", red(sg[:], eg[:], ALU.add), ["eg"], ["sg"])
        P.op("dve", (lambda o, i_: (lambda e: e.reciprocal(out=o, in_=i_)))(sg[:], sg[:]), ["sg"], ["sg"])
        P.tt("dve", tmp4[:], le, ohg[:].unsqueeze(3).to_broadcast([128, NT, 4, 8]), ALU.mult, ["lgt", "ohg"], ["tmp4"])
        P.op("dve", red(les[:], tmp4[:].rearrange("p t g e -> p t e g"), ALU.add), ["tmp4"], ["les"])
        P.op("dve", red(m1[:], les[:], ALU.max), ["les"], ["m1"])
        P.tt("dve", oh1[:], les[:], m1[:].unsqueeze(2).to_broadcast([128, NT, 8]), ALU.is_ge, ["les", "m1"], ["oh1"])
        P.stt("dve", le2[:], oh1[:], -1e30, les[:], ALU.mult, ALU.add, ["oh1", "les"], ["le2"])
        P.op("dve", red(m2[:], le2[:], ALU.max), ["le2"], ["m2"])
        P.tt("dve", oh2[:], le2[:], m2[:].unsqueeze(2).to_broadcast([128, NT, 8]), ALU.is_ge, ["le2", "m2"], ["oh2"])
        P.tt("dve", w2[:], m2[:], m1[:], ALU.subtract, ["m1", "m2"], ["w2"])
        P.act(w2[:], w2[:], AF.Exp, ["w2"], ["w2"])
        P.ts("dve", w1[:], w2[:], 1.0, None, ALU.add, None, ["w2"], ["w1"])
        P.op("dve", (lambda o, i_: (lambda e: e.reciprocal(out=o, in_=i_)))(w1[:], w1[:]), ["w1"], ["w1"])
        P.tt("dve", w2[:], w2[:], w1[:], ALU.mult, ["w1", "w2"], ["w2"])
        P.tt("dve", w1[:], w1[:], sg[:], ALU.mult, ["w1", "sg"], ["w1"])
        P.tt("dve", w2[:], w2[:], sg[:], ALU.mult, ["w2", "sg"], ["w2"])
        wk_g, pos_i, te_i = T["wk_g"], T["pos_i"], T["te_i"]
        P.copy("dve", wk_g[:, :, 0], w1[:], ["w1"], ["wk_g"])
        P.copy("dve", wk_g[:, :, 1], w2[:], ["w2", "wk_g"], ["wk_g"])
        P.load("sp", lstr[:], T["lstrict"], [], ["lstr"])
        P.load("sp", thr[:], T["thr"].partition_broadcast(128), [], ["thr"])
        P.memset("pool", ones[:], 1.0, ["ones"])
        P.memset("pool", zer[:], 0.0, ["zer"])
        bc3 = lambda a: a.unsqueeze(3).to_broadcast([128, NT, 4, 8])
        bc2 = lambda a: a.unsqueeze(2).to_broadcast([128, NT, 4, 8])
        P.tt("dve", E1[:], bc3(ohg[:]), bc2(oh1[:]), ALU.mult, ["ohg", "oh1"], ["E1"])
        P.tt("dve", E2[:], bc3(ohg[:]), bc2(oh2[:]), ALU.mult, ["ohg", "oh2"], ["E2"])
        P.tt("dve", Es[:], E1[:].rearrange("p t g e -> p (t g e)"), E2[:].rearrange("p t g e -> p (t g e)"), ALU.add, ["E1", "E2"], ["Es"])
        for hf in range(2):
            P.mm(pO[hf][:, :], lstr[:], Es[:, hf * 512:(hf + 1) * 512], ["lstr", "Es"], ["pO%d" % hf])
            P.copy("act", within[:].rearrange("p t e -> p (t e)")[:, hf * 512:(hf + 1) * 512], pO[hf][:, :], ["pO%d" % hf], ["within"])
            P.mm(pO[2 + hf][:, :], ones[:], Es[:, hf * 512:(hf + 1) * 512], ["ones", "Es"], ["pO%d" % (2 + hf)])
            P.copy("dve", csb[:].rearrange("p t e -> p (t e)")[:, hf * 512:(hf + 1) * 512], pO[2 + hf][:, :], ["pO%d" % (2 + hf)], ["csb"])
        for e_ in range(32):
            P.op("dve", (lambda o, d0, d1: (lambda e: e.tensor_tensor_scan(out=o, data0=d0, data1=d1, initial=0.0, op0=ALU.add, op1=ALU.add)))(
                incl[:, :, e_], csb[:, :, e_], zer[:]), ["csb", "zer", "incl"], ["incl"])
        P.tt("dve", cmpb[:, 0:512].rearrange("p (e j) -> p e j", j=16), incl[:, NT - 1, :].unsqueeze(2).to_broadcast([128, 32, 16]),
             thr[:, 0:16].unsqueeze(1).to_broadcast([128, 32, 16]), ALU.is_gt, ["incl", "thr"], ["cmpb"])
        P.op("dve", red(ntl[:], cmpb[:, 0:512].rearrange("p (e j) -> p e j", j=16), ALU.add), ["cmpb"], ["ntl"])
        P.op("dve", (lambda o, d0, d1: (lambda e: e.tensor_tensor_scan(out=o, data0=d0, data1=d1, initial=0.0, op0=ALU.add, op1=ALU.add)))(
            inct[:], ntl[:], zer[:, 0:32]), ["ntl", "zer"], ["inct"])
        P.ts("dve", offe[:], inct[:], float(SUP), None, ALU.mult, None, ["inct"], ["offe"])
        P.tt("dve", offb[:], inct[:], ntl[:], ALU.subtract, ["inct", "ntl"], ["offb"])
        P.ts("dve", offb[:], offb[:], float(SUP), None, ALU.mult, None, ["offb"], ["offb"])
        P.tt("dve", Rr[:], incl[:], csb[:], ALU.subtract, ["incl", "csb"], ["Rr"])
        P.tt("dve", Rr[:], Rr[:], within[:], ALU.add, ["Rr", "within"], ["Rr"])
        P.tt("dve", Rr[:], Rr[:], offb[:].unsqueeze(1).to_broadcast([128, NT, 32]), ALU.add, ["Rr", "offb"], ["Rr"])
        for k_, Ek in enumerate((E1, E2)):
            kn = "E%d" % (k_ + 1)
            P.tt("dve", Ek[:].rearrange("p t g e -> p t (g e)"), Ek[:].rearrange("p t g e -> p t (g e)"), Rr[:], ALU.mult, [kn, "Rr"], [kn])
            P.op("dve", red(posf[:, :, k_], Ek[:].rearrange("p t g e -> p t (g e)"), ALU.add), [kn, "posf"], ["posf"])
        P.copy("dve", pos_i[:], posf[:], ["posf"], ["pos_i"])
        P.tt("dve", cmpb[:, 0:NSUP * 32].rearrange("p (j e) -> p j e", e=32), offe[:].unsqueeze(1).to_broadcast([128, NSUP, 32]),
             thr[:, 16:16 + NSUP].unsqueeze(2).to_broadcast([128, NSUP, 32]), ALU.is_le, ["offe", "thr", "cmpb"], ["cmpb"])
        P.memset("pool", tef[:], 0.0, ["tef"])
        P.op("dve", red(tef[:, 0:NSUP], cmpb[:, 0:NSUP * 32].rearrange("p (j e) -> p j e", e=32), ALU.add), ["cmpb", "tef"], ["tef"])
        P.ts("dve", tef[:], tef[:], 31.0, None, ALU.min, None, ["tef"], ["tef"])
        P.load("sp", pidx[:], T["pidx"], [], ["pidx"])
        P.ts("dve", tef[:], tef[:], 128.0, pidx[:, 0:1], ALU.mult, ALU.add, ["tef", "pidx"], ["tef"])
        P.copy("dve", te_i[:], tef[:], ["tef"], ["te_i"])
        P.tt("dve", oh1[:], oh1[:], w1[:].unsqueeze(2).to_broadcast([128, NT, 8]), ALU.mult, ["oh1", "w1"], ["oh1"])
        P.tt("dve", oh2[:], oh2[:], w2[:].unsqueeze(2).to_broadcast([128, NT, 8]), ALU.mult, ["oh2", "w2"], ["oh2"])
        P.tt("dve", gf[:], oh1[:], oh2[:], ALU.add, ["oh1", "oh2"], ["gf"])
        P.tt("dve", gts[:], ohg[:].unsqueeze(3).to_broadcast([128, NT, 4, 8]), gf[:].unsqueeze(2).to_broadcast([128, NT, 4, 8]), ALU.mult,
             ["ohg", "gf"], ["gts"])
        P.load("sp", T["gates"], gts[:].rearrange("p t g e -> p (t g e)"), ["gts"], ["gatesd"])
        return P.emit()


def stage5(nc, sems, T):
    IOA = bass.IndirectOffsetOnAxis
    with contextlib.ExitStack() as st:
        sb, ps = tens(nc, st)
        P = Prog(nc, sems)
        pos_i, te_i, wk_g = T["pos_i"], T["te_i"], T["wk_g"]
        identb = sb("identb", [128, 128], BF16)
        identf = sb("identf", [128, 128])
        gfb = sb("gfb", [128, D])
        hrow = [sb("hrow%d" % i, [128, D], BF16) for i in range(3)]
        wall = [sb("wall%d" % i, [128, 3 * 4096], BF16) for i in range(2)]
        xs = [sb("xs%d" % i, [128, D], BF16) for i in range(3)]
        XT = [sb("XT%d" % i, [128, 8, 128], BF16) for i in range(2)]
        sgl = [sb("sgl%d" % i, [128, DFF], BF16) for i in range(2)]
        hid = [sb("hid%d" % i, [128, DFF], BF16) for i in range(2)]
        hidT = [sb("hidT%d" % i, [128, 4, 128], BF16) for i in range(2)]
        ysb = [sb("ysb%d" % i, [128, D], BF16) for i in range(3)]
        yg = [sb("yg%d" % i, [128, 2, D], BF16) for i in range(3)]
        x2 = [sb("x2%d" % i, [128, D]) for i in range(8)]
        junk = sb("junk5", [128, D], BF16)
        stat = [sb("stat5%d" % i, [128, 4]) for i in range(3)]
        ot = [sb("ot%d" % i, [128, D]) for i in range(3)]
        ptx = [ps("ptx%d" % i, [128, D], BF16) for i in range(2)]
        pg = [ps("pg%d" % i, [128, 512]) for i in range(2)]
        pu = [ps("pu%d" % i, [128, 512]) for i in range(2)]
        pth = [ps("pth%d" % i, [128, D], BF16) for i in range(2)]

        P.load("sp", identf[:], T["ident"], [], ["identf"])
        P.copy("dve", identb[:], identf[:], ["identf"], ["identb"])
        P.load("sp", gfb[:], T["normf_g"].partition_broadcast(128), [], ["gfb"])
        def gather_w(j):
            wb = j % 2
            P.dma("pool", (lambda o, i_, off: (lambda e: e.indirect_dma_start(out=o, out_offset=None, in_=i_, in_offset=off)))(
                wall[wb][:, :], T["wall"], IOA(ap=te_i[:, j:j + 1], axis=0)), ["te_i"], ["wall%d" % wb])

        gather_w(0)
        gather_w(1)
        zkeys = []
        sckeys = []
        for t in range(NT):
            hb = t % 3
            P.load("sp", hrow[hb][:], T["h2b"][t * 128:(t + 1) * 128, :], [], ["hrow%d" % hb])
            for k_ in range(2):
                key = "xs_sc%d_%d" % (t, k_)
                sckeys.append(key)
                P.dma("pool", (lambda o, off, i_: (lambda e: e.indirect_dma_start(out=o, out_offset=off, in_=i_, in_offset=None)))(
                    T["xs"], IOA(ap=pos_i[:, t, k_:k_ + 1], axis=0), hrow[hb][:]), ["hrow%d" % hb, "pos_i"] + zkeys, [key])
        ykeys = []
        rx, r2 = Rot(3), Rot(2)
        NSUB = NSUP * (SUP // 128)

        def wviews(j):
            wb = j % 2
            return (wall[wb][:, 0:4096].rearrange("p (c f) -> p c f", f=512), wall[wb][:, 4096:8192].rearrange("p (c f) -> p c f", f=512),
                    wall[wb][:, 8192:12288].rearrange("p (c d) -> p c d", d=1024), "wall%d" % wb)

        def phase_a(n):
            j = n // 2
            wg_v, wu_v, wd_v, kw = wviews(j)
            row0 = n * 128
            xb, b2 = n % 3, n % 2
            P.load("act", xs[xb][:], T["xs"][row0:row0 + 128, :], sckeys + zkeys, ["xs%d" % xb])
            for kc in range(8):
                P.tr(ptx[b2][:, kc * 128:(kc + 1) * 128], xs[xb][:, kc * 128:(kc + 1) * 128], identb[:], ["xs%d" % xb, "identb"], ["ptx%d" % b2])
            P.copy("dve" if b2 == 0 else "act", XT[b2][:].rearrange("p k t -> p (k t)"), ptx[b2][:, :], ["ptx%d" % b2], ["XT%d" % b2])

        def phase_a2(n):
            j = n // 2
            wg_v, wu_v, wd_v, kw = wviews(j)
            xb, b2 = n % 3, n % 2
            for kc in range(8):
                P.mm(pg[b2][:, :], XT[b2][:, kc, :], wg_v[:, kc, :], ["XT%d" % b2, kw], ["pg%d" % b2], start=(kc == 0), stop=(kc == 7))
            for kc in range(8):
                P.mm(pu[b2][:, :], XT[b2][:, kc, :], wu_v[:, kc, :], ["XT%d" % b2, kw], ["pu%d" % b2], start=(kc == 0), stop=(kc == 7))
            P.act(sgl[b2][:], pg[b2][:, :], AF.Silu, ["pg%d" % b2], ["sgl%d" % b2])
            P.tt("dve", hid[b2][:], sgl[b2][:], pu[b2][:, :], ALU.mult, ["sgl%d" % b2, "pu%d" % b2], ["hid%d" % b2])

        def phase_b(n):
            j = n // 2
            wg_v, wu_v, wd_v, kw = wviews(j)
            row0 = n * 128
            xb, b2 = n % 3, n % 2
            for fc in range(4):
                P.tr(pth[b2][:, fc * 128:(fc + 1) * 128], hid[b2][:, fc * 128:(fc + 1) * 128], identb[:], ["hid%d" % b2, "identb"], ["pth%d" % b2])
            P.copy("act" if b2 == 0 else "dve", hidT[b2][:].rearrange("p k t -> p (k t)"), pth[b2][:, 0:512], ["pth%d" % b2], ["hidT%d" % b2])

        def phase_b2(n):
            j = n // 2
            wg_v, wu_v, wd_v, kw = wviews(j)
            row0 = n * 128
            xb, b2 = n % 3, n % 2
            for half, (pz, kz) in enumerate(((pg[b2], "pg%d" % b2), (pu[b2], "pu%d" % b2))):
                for fc in range(4):
                    P.mm(pz[:, :], hidT[b2][:, fc, :], wd_v[:, fc, half * 512:(half + 1) * 512], ["hidT%d" % b2, kw], [kz],
                         start=(fc == 0), stop=(fc == 3))
                P.copy("act" if half == 0 else "dve", ysb[xb][:, half * 512:(half + 1) * 512], pz[:, :], [kz], ["ysb%d" % xb])
            yk = "ys%d" % n
            ykeys.append(yk)
            P.load("sp", T["ys"][row0:row0 + 128, :], ysb[xb][:], ["ysb%d" % xb], [yk])

        x2_done = set()

        def load_x2(t):
            if t not in x2_done:
                x2_done.add(t)
                P.load("sp", x2[t % 8][:], T["x2"][t * 128:(t + 1) * 128, :], [], ["x2%d" % (t % 8)])

        for n in range(NSUB + 1):
            if NSUB - 24 <= n < NSUB - 16:
                load_x2(n - (NSUB - 24))
            if n < NSUB:
                phase_a(n)
            if n >= 1:
                phase_b(n - 1)
            if n < NSUB:
                phase_a2(n)
            if n >= 1:
                m = n - 1
                phase_b2(m)
                if m % 2 == 1 and m // 2 + 2 < NSUP:
                    gather_w(m // 2 + 2)
        for t in range(NT):
            b = t % 3
            ts_ = slice(t * 128, (t + 1) * 128)
            sk = "stat5%d" % b
            for k_ in range(2):
                P.dma("pool", (lambda o, i_, off: (lambda e: e.indirect_dma_start(out=o, out_offset=None, in_=i_, in_offset=off)))(
                    yg[b][:, k_, :], T["ys"], IOA(ap=pos_i[:, t, k_:k_ + 1], axis=0)), ykeys + ["pos_i"], ["yg%d_%d" % (b, k_)])
            bx = t % 8
            kx = "x2%d" % bx
            load_x2(t)
            P.stt("dve", x2[bx][:], yg[b][:, 0, :], wk_g[:, t, 0:1], x2[bx][:], ALU.mult, ALU.add, ["yg%d_0" % b, "wk_g", kx], [kx])
            P.stt("dve", x2[bx][:], yg[b][:, 1, :], wk_g[:, t, 1:2], x2[bx][:], ALU.mult, ALU.add, ["yg%d_1" % b, "wk_g", kx], [kx])
            P.act(junk[:], x2[bx][:], AF.Square, [kx], ["junk5", sk], accum_out=stat[b][:, 0:1])
            P.ts("dve", stat[b][:, 1:2], stat[b][:, 0:1], 1.0 / D, EPS, ALU.mult, ALU.add, [sk], [sk])
            P.act(stat[b][:, 2:3], stat[b][:, 1:2], AF.Ln, [sk], [sk])
            P.act(stat[b][:, 3:4], stat[b][:, 2:3], AF.Exp, [sk], [sk], scale=-0.5)
            P.stt("dve", ot[b][:], x2[bx][:], stat[b][:, 3:4], gfb[:], ALU.mult, ALU.mult, [kx, sk, "gfb"], ["ot%d" % b])
            P.load("act", T["out"][ts_, :], ot[b][:], ["ot%d" % b], ["outd"])
        return P.emit()


def _rel_bucket_np(n):
    n = np.maximum(n, 0)
    max_exact = 16
    nf = np.maximum(n, 1).astype(np.float32)
    large = max_exact + (np.log(nf / np.float32(max_exact)) / np.float32(math.log(128 / max_exact)) * np.float32(16)).astype(np.int32)
    large = np.minimum(large, 31)
    return np.where(n < max_exact, n, large)


def _constants():
    ident = np.eye(128, dtype=np.float32)
    s_ = np.arange(128)[:, None]
    t_ = np.arange(128)[None, :]
    tri = (s_ <= t_).astype(np.float32)
    sel = np.zeros((4, 4, 128), np.float32)
    for h in range(4):
        sel[h, h, :] = 1.0
    oh = np.zeros((128, 2, 33, 128), np.float32)
    for kind in range(2):
        n = (t_ - s_) + 128 * kind
        bk = _rel_bucket_np(n)
        valid = n >= 0
        for b in range(32):
            oh[:, kind, b, :] = ((bk == b) & valid).astype(np.float32)
        oh[:, kind, 32, :] = (~valid).astype(np.float32)
    lstrict = (s_ < t_).astype(np.float32)
    thr = np.concatenate([np.arange(16) * SUP, np.arange(NSUP) * SUP]).astype(np.float32)
    pidx = np.arange(128, dtype=np.float32).reshape(128, 1)
    return dict(ident=ident, tri=tri, sel=sel.reshape(4, 512), oh=oh.reshape(128, -1), lstrict=lstrict, thr=thr, pidx=pidx)


_CACHE = {}


def kernel(x, w_in, conv_w, conv_b, w_mq, w_mk, w_mgate, b_mgate, m_norm_g, m_skip, lambda_qk, da_norm_g, rel_bias, w_out,
           norm1_g, norm2_g, w_rg, b_rg, w_re, b_re, w_eg, w_eu, w_ed, normf_g):
    f = lambda a: np.ascontiguousarray(np.asarray(a, dtype=np.float32))
    if "nc" not in _CACHE:
        _CACHE["nc"], _CACHE["stats"] = build_program()
    nc = _CACHE["nc"]
    shared = dict(
        w_in=f(w_in)[0], conv_w=f(conv_w)[0], conv_b=f(conv_b)[0], w_mq=f(w_mq)[0], w_mk=f(w_mk)[0], w_mgate=f(w_mgate)[0],
        b_mgate=f(b_mgate)[0], m_norm_g=f(m_norm_g)[0], m_skip=f(m_skip)[0], lambda_qk=f(lambda_qk)[0].reshape(256),
        da_norm_g=f(da_norm_g)[0], rel_bias=f(rel_bias).reshape(128), w_out=f(w_out)[0], norm1_g=f(norm1_g)[0], norm2_g=f(norm2_g)[0],
        w_r=np.ascontiguousarray(np.concatenate([f(w_rg)[0], f(w_re)[0].reshape(D, 32)], axis=1)),
        b_r=np.ascontiguousarray(np.concatenate([f(b_rg)[0], f(b_re)[0].reshape(32)])),
        w_eg=f(w_eg)[0], w_eu=f(w_eu)[0], w_ed=f(w_ed)[0], normf_g=f(normf_g),
    )
    shared.update(_constants())
    xs = f(x)
    in_maps = []
    for b in range(8):
        m = dict(shared)
        m["x"] = xs[b]
        in_maps.append(m)
    res = run_bass_kernel_spmd(nc, in_maps, core_ids=list(range(8)))
    _CACHE["res"] = res
    return np.stack([np.asarray(r["out"], dtype=np.float32) for r in res.results], axis=0)
```

```python
import math
import contextlib
import numpy as np
import concourse.bass as bass
import concourse.mybir as mybir
from concourse.bass_utils import run_bass_kernel_spmd

F32 = mybir.dt.float32
BF16 = mybir.dt.bfloat16
AF = mybir.ActivationFunctionType
ALU = mybir.AluOpType
AX = mybir.AxisListType

S = 4096
D = 1024
NT = 32
EPS = 1e-6
SUBLN_EPS = 1e-5
N_EXP = 32
DFF = 512
LAM_INIT = 0.8 - 0.6 * math.exp(-0.3 * 0)
ML_SCALE = 128.0 ** -0.5
DA_SCALE = 64.0 ** -0.5
NEG = -30000.0
SUP = 256
NSUP = 63
NSLOT = NSUP * SUP
I32 = mybir.dt.int32

COMPUTE = ("pe", "act", "dve", "pool")
QUEUES = ("sp", "act", "pool")
N_DMA_SEMS = 8
DEBUG = False
CONV_PER_GROUP = {1: 0, 2: 7, 3: 2}
STAGES = (1, 2, 3, 4, 5)


class Sems:
    def __init__(self, nc, st):
        self.esem = {e: st.enter_context(nc.semaphore("s_" + e)) for e in COMPUTE}
        self.dsem = {(q, s): st.enter_context(nc.semaphore("d_%s_%d" % (q, s))) for q in QUEUES for s in range(N_DMA_SEMS)}
        self.cnt = {e: 0 for e in COMPUTE}
        self.dcnt = {k: 0 for k in self.dsem}
        self.rr = {q: 0 for q in QUEUES}


class Op:
    __slots__ = ("eng", "fn", "deps", "is_dma", "signal", "val", "sem", "slot", "prev")

    def __init__(self, eng, fn, is_dma):
        self.eng, self.fn, self.is_dma = eng, fn, is_dma
        self.deps = []
        self.signal = False
        self.val = None
        self.sem = None
        self.slot = None
        self.prev = None


class Prog:
    def __init__(self, nc, sems):
        self.nc = nc
        self.sems = sems
        self.ops = []
        self.last_writer = {}
        self.readers = {}
        self.slot_last = {}

    def _add(self, op, reads, writes):
        pr = [r for r in reads if r in PSUM_KEYS]
        if pr:
            reads = [r for r in reads if r not in PSUM_KEYS]
            writes = list(writes) + [r for r in pr if r not in writes]
        deps = []
        for r in reads:
            w = self.last_writer.get(r)
            if w is not None:
                deps.append(w)
        for w in writes:
            lw = self.last_writer.get(w)
            if lw is not None:
                deps.append(lw)
            deps.extend(self.readers.get(w, ()))
        seen = set()
        for d in deps:
            if id(d) not in seen and d is not op:
                seen.add(id(d))
                op.deps.append(d)
        for r in reads:
            self.readers.setdefault(r, []).append(op)
        for w in writes:
            self.last_writer[w] = op
            self.readers[w] = []
        self.ops.append(op)
        return op

    def op(self, eng, fn, reads=(), writes=()):
        return self._add(Op(eng, fn, False), reads, writes)

    def dma(self, queue, fn, reads=(), writes=()):
        op = Op(queue, fn, True)
        s = self.sems
        op.slot = (queue, s.rr[queue] % N_DMA_SEMS)
        s.rr[queue] += 1
        op.prev = self.slot_last.get(op.slot)
        self.slot_last[op.slot] = op
        return self._add(op, reads, writes)

    def mm(self, out, lhsT, rhs, r, w, start=True, stop=True, skip=False):
        if skip:
            return self.op("pe", lambda e: e.matmul(out, lhsT=lhsT, rhs=rhs, start=start, stop=stop, skip_group_check=True), r, w)
        return self.op("pe", lambda e: e.matmul(out, lhsT=lhsT, rhs=rhs, start=start, stop=stop), r, w)

    def tr(self, out, in_, ident, r, w):
        return self.op("pe", lambda e: e.transpose(out=out, in_=in_, identity=ident), r, w)

    def act(self, out, in_, func, r, w, bias=None, scale=None, accum_out=None):
        kw = {}
        if bias is not None:
            kw["bias"] = bias
        if scale is not None:
            kw["scale"] = scale
        if accum_out is not None:
            kw["accum_out"] = accum_out
        return self.op("act", lambda e: e.activation(out=out, in_=in_, func=func, **kw), r, w)

    def copy(self, eng, out, in_, r, w):
        if eng == "act":
            return self.op("act", lambda e: e.copy(out=out, in_=in_), r, w)
        return self.op(eng, lambda e: e.tensor_copy(out=out, in_=in_), r, w)

    def tt(self, eng, out, in0, in1, op, r, w):
        return self.op(eng, lambda e: e.tensor_tensor(out=out, in0=in0, in1=in1, op=op), r, w)

    def ts(self, eng, out, in0, s1, s2, op0, op1, r, w):
        if s2 is None:
            return self.op(eng, lambda e: e.tensor_scalar(out=out, in0=in0, scalar1=s1, scalar2=None, op0=op0), r, w)
        return self.op(eng, lambda e: e.tensor_scalar(out=out, in0=in0, scalar1=s1, scalar2=s2, op0=op0, op1=op1), r, w)

    def stt(self, eng, out, in0, scalar, in1, op0, op1, r, w):
        eng = "dve"
        return self.op(eng, lambda e: e.scalar_tensor_tensor(out=out, in0=in0, scalar=scalar, in1=in1, op0=op0, op1=op1), r, w)

    def memset(self, eng, ap, val, w):
        return self.op(eng, lambda e: e.memset(ap, val), (), w)

    def load(self, q, out, in_, r, w):
        return self.dma(q, lambda e: e.dma_start(out=out, in_=in_), r, w)

    def emit(self):
        nc, s, ops = self.nc, self.sems, self.ops

        def same_skip(d, o):
            return (not d.is_dma) and (not o.is_dma) and d.eng == o.eng and d.eng == "pe"

        for o in ops:
            for d in o.deps:
                if d.is_dma or same_skip(d, o):
                    continue
                d.signal = True
        for o in ops:
            if o.is_dma:
                s.dcnt[o.slot] += 16
                o.val = s.dcnt[o.slot]
                o.sem = s.dsem[o.slot]
            else:
                o.sem = s.esem[o.eng]
                if o.signal:
                    s.cnt[o.eng] += 1
                    o.val = s.cnt[o.eng]
        by_eng = {e: [] for e in ("pe", "act", "dve", "pool", "sp")}
        for o in ops:
            by_eng[o.eng].append(o)
        final = dict(s.dcnt)

        def run(engname, e):
            waited = {}

            def wait(sem, val):
                if waited.get(id(sem), 0) >= val:
                    return
                waited[id(sem)] = val
                e.wait_ge(sem, val)

            for o in by_eng[engname]:
                for d in o.deps:
                    if same_skip(d, o):
                        continue
                    wait(d.sem, d.val)
                if o.is_dma and o.prev is not None:
                    wait(o.prev.sem, o.prev.val)
                ins = o.fn(e)
                if o.is_dma:
                    ins.then_inc(o.sem, 16)
                elif o.signal:
                    ins.then_inc(o.sem, 1)
            if engname == "sp":
                for k, v in final.items():
                    if v > 0:
                        wait(s.dsem[k], v)

        with nc.Block() as block:
            block.sync(lambda e: run("sp", e))
            if by_eng["pe"]:
                block.tensor(lambda e: run("pe", e))
            if by_eng["act"]:
                block.scalar(lambda e: run("act", e))
            if by_eng["dve"]:
                block.vector(lambda e: run("dve", e))
            if by_eng["pool"]:
                block.gpsimd(lambda e: run("pool", e))
        return {k: len(v) for k, v in by_eng.items()}


class Rot:
    def __init__(self, n):
        self.n, self.i = n, 0

    def next(self):
        v = self.i % self.n
        self.i += 1
        return v


def build_program():
    nc = bass.Bass("TRN2", target_bir_lowering=False)
    I = lambda name, shape, dt=F32: nc.dram_tensor(name, list(shape), dt, kind="ExternalInput").ap()
    skind = "ExternalOutput" if DEBUG else "Internal"
    SC = lambda name, shape, dt: nc.dram_tensor(name, list(shape), dt, kind=skind).ap()
    T = {}
    T["x"] = I("x", [S, D])
    T["w_in"] = I("w_in", [D, 3072])
    T["conv_w"] = I("conv_w", [4, 512])
    T["conv_b"] = I("conv_b", [512])
    T["w_mq"] = I("w_mq", [4, 128, 128])
    T["w_mk"] = I("w_mk", [4, 128, 128])
    T["w_mgate"] = I("w_mgate", [1536, 8])
    T["b_mgate"] = I("b_mgate", [8])
    T["m_norm_g"] = I("m_norm_g", [512])
    T["m_skip"] = I("m_skip", [512])
    T["lambda_qk"] = I("lambda_qk", [256])
    T["da_norm_g"] = I("da_norm_g", [128])
    T["rel_bias"] = I("rel_bias", [128])
    T["w_out"] = I("w_out", [D, D])
    T["norm1_g"] = I("norm1_g", [D])
    T["norm2_g"] = I("norm2_g", [D])
    T["w_r"] = I("w_r", [D, 36])
    T["b_r"] = I("b_r", [36])
    T["w_eg"] = I("w_eg", [N_EXP, D, DFF])
    T["w_eu"] = I("w_eu", [N_EXP, D, DFF])
    T["w_ed"] = I("w_ed", [N_EXP, DFF, D])
    T["normf_g"] = I("normf_g", [D])
    T["ident"] = I("ident", [128, 128])
    T["tri"] = I("tri", [128, 128])
    T["sel"] = I("sel", [4, 512])
    T["oh"] = I("oh", [128, 2 * 33 * 128])
    T["lstrict"] = I("lstrict", [128, 128])
    T["thr"] = I("thr", [16 + NSUP])
    T["pidx"] = I("pidx", [128, 1])
    T["out"] = nc.dram_tensor("out", [S, D], F32, kind="ExternalOutput").ap()
    T["featT"] = SC("featT", [5, 512, S], BF16)
    T["vm_tok"] = SC("vm_tok", [S, 512], BF16)
    T["vd_tok"] = SC("vd_tok", [S, 512], BF16)
    T["ymT"] = SC("ymT", [512, S], BF16)
    T["ydT"] = SC("ydT", [512, S], BF16)
    T["x2"] = SC("x2", [S, D], F32)
    T["h2T"] = SC("h2T", [D, S], BF16)
    T["gates"] = SC("gates", [128, NT * 32], F32)
    T["h2b"] = SC("h2b", [S, D], BF16)
    T["xs"] = nc.dram_tensor("xs", [NSLOT, D], BF16, kind="Internal").ap()
    T["ys"] = nc.dram_tensor("ys", [NSLOT, D], BF16, kind="Internal").ap()
    T["wall"] = nc.dram_tensor("wall", [N_EXP * 128, 3 * 4096], BF16, kind="Internal").ap()

    stats = {}
    with contextlib.ExitStack() as gst:
        gst.enter_context(nc.allow_non_contiguous_dma(reason="small strided parameter loads"))
        sems = Sems(nc, gst)
        T["biasT_sb"] = gst.enter_context(nc.sbuf_tensor("g_biasT", [128, 2, 4, 128], F32))
        T["rbb_sb"] = gst.enter_context(nc.sbuf_tensor("g_rbb", [128, 128], F32))
        T["pos_i"] = gst.enter_context(nc.sbuf_tensor("g_pos_i", [128, NT, 2], I32))
        T["te_i"] = gst.enter_context(nc.sbuf_tensor("g_te_i", [128, 64], I32))
        T["wk_g"] = gst.enter_context(nc.sbuf_tensor("g_wk", [128, NT, 2], F32))
        if 0 in STAGES:
            stats["s0"] = stage0(nc, sems, T)
        if 1 in STAGES:
            stats["s1"] = stage1(nc, sems, T)
        if 2 in STAGES:
            stats["s2"] = stage2(nc, sems, T)
        if 3 in STAGES:
            stats["s3"] = stage3(nc, sems, T)
        if 4 in STAGES:
            stats["s4"] = stage4(nc, sems, T)
        if 5 in STAGES:
            stats["s5"] = stage5(nc, sems, T)
        if 6 in STAGES:
            stats["s6"] = stage6(nc, sems, T)
    return nc, stats


_TN = [0]
PSUM_KEYS = set()


def tens(nc, st):
    _TN[0] += 1
    pre = "t%d_" % _TN[0]
    sb = lambda n, s, d=F32: st.enter_context(nc.sbuf_tensor(pre + n, list(s), d))
    def ps(n, s, d=F32):
        PSUM_KEYS.add(n)
        return st.enter_context(nc.psum_tensor(pre + n, list(s), d))
    return sb, ps


def conv_jobs():
    return [(name, m, e) for m, name in enumerate(("w_eg", "w_eu", "w_ed")) for e in range(N_EXP)]


class Conv:
    def __init__(self, P, sb, T, engs=("pool",), queues=("sp", "sp"), nb=3):
        self.P, self.T = P, T
        self.stg = [sb("w0s%d" % i, [128, 8, 512], F32) for i in range(nb)]
        self.cvt = [sb("w0c%d" % i, [128, 8, 512], BF16) for i in range(nb)]
        self.rot = Rot(nb)
        self.engs, self.queues = engs, queues
        self.jobs = conv_jobs()
        self.k = 0

    def emit(self, n):
        P, T = self.P, self.T
        for _ in range(n):
            if self.k >= len(self.jobs):
                return
            name, m, e = self.jobs[self.k]
            b = self.rot.next()
            src = T[name][e].rearrange("(c p) f -> p c f", p=128)
            dstap = T["wall"][e * 128:(e + 1) * 128, m * 4096:(m + 1) * 4096].rearrange("p (c f) -> p c f", f=512)
            sv = self.stg[b][:].rearrange("p (c h) f -> p c (h f)", c=4) if name == "w_ed" else self.stg[b][:]
            P.load(self.queues[0], sv, src, [], ["stg%d" % b])
            P.copy(self.engs[self.k % len(self.engs)], self.cvt[b][:], self.stg[b][:], ["stg%d" % b], ["cvt%d" % b])
            P.load(self.queues[1], dstap, self.cvt[b][:], ["cvt%d" % b], ["wall"])
            self.k += 1


class ConvD:
    def __init__(self, P, T):
        self.P, self.T = P, T
        self.jobs = conv_jobs()
        self.k = 0

    def emit(self, n):
        P, T = self.P, self.T
        for _ in range(n):
            if self.k >= len(self.jobs):
                return
            name, m, e = self.jobs[self.k]
            cols = T["wall"][e * 128:(e + 1) * 128, m * 4096:(m + 1) * 4096]
            if name == "w_ed":
                src = T[name][e].rearrange("(c p) d -> p c d", p=128)
                dst = cols.rearrange("p (c d) -> p c d", d=1024)
            else:
                src = T[name][e].rearrange("(c p) f -> p c f", p=128)
                dst = cols.rearrange("p (c f) -> p c f", f=512)
            P.load("pool", dst, src, [], ["wall%d" % self.k])
            self.k += 1


def stage0(nc, sems, T):
    with contextlib.ExitStack() as st:
        sb, ps = tens(nc, st)
        P = Prog(nc, sems)
        cv = Conv(P, sb, T, engs=("dve", "pool", "act"), queues=("sp", "act"))
        cv.emit(96)
        return P.emit()


def stage1(nc, sems, T):
    with contextlib.ExitStack() as st:
        sb, ps = tens(nc, st)
        P = Prog(nc, sems)
        ident = sb("ident", [128, 128])
        identb = sb("identb", [128, 128], BF16)
        g1b = sb("g1b", [128, D])
        w_bf = sb("w_in_bf", [128, 8, 3072], BF16)
        xt = [sb("xt%d" % i, [128, D]) for i in range(2)]
        junk = sb("junk", [128, D], BF16)
        stat = sb("stat", [128, 4])
        xn = [sb("xn%d" % i, [128, D], BF16) for i in range(2)]
        hT = [sb("hT%d" % i, [128, 8, 512], BF16) for i in range(2)]
        fstg = [sb("fstg%d" % i, [128, 4, 512], BF16) for i in range(2)]
        tstg = [sb("tstg%d" % i, [128, 4, 512], BF16) for i in range(2)]
        pt = [ps("pt%d" % i, [128, D], BF16) for i in range(2)]
        pp = [ps("pp%d" % i, [128, 512]) for i in range(4)]

        P.load("sp", ident[:], T["ident"], [], ["ident"])
        P.copy("dve", identb[:], ident[:], ["ident"], ["identb"])
        P.load("act", g1b[:], T["norm1_g"].partition_broadcast(128), [], ["g1b"])
        for blk in (0, 1, 2, 3, 4, 5):
            P.load("pool", w_bf[:, :, blk * 512:(blk + 1) * 512], T["w_in"][:, blk * 512:(blk + 1) * 512].rearrange("(c p) n -> p c n", p=128),
                   [], ["w_bf%d" % blk])
        oh = sb("oh", [128, 2, 33, 128])
        rbb, biasT = T["rbb_sb"], T["biasT_sb"]
        P.load("act", oh[:].rearrange("p a b c -> p (a b c)"), T["oh"], [], ["oh"])
        P.load("act", rbb[:], T["rel_bias"].partition_broadcast(128), [], ["rbb"])

        def emit_bias(idx):
            kind, h = idx // 4, idx % 4
            dst_ = biasT[:, kind, h, :]
            kb = "biasT%d%d" % (kind, h)
            P.ts("pool", dst_, oh[:, kind, 32, :], NEG, None, ALU.mult, None, ["oh"], [kb])
            for b_ in range(32):
                P.stt("dve", dst_, oh[:, kind, b_, :], rbb[:, b_ * 4 + h:b_ * 4 + h + 1], dst_, ALU.mult, ALU.add, ["oh", "rbb", kb], [kb])

        rpp = Rot(4)
        cvj = ConvD(P, T)

        def s1_a(g):
            hb = g % 2
            emit_bias(g)
            cvj.emit(CONV_PER_GROUP[1])
            for ti in range(4):
                t = g * 4 + ti
                b = t % 2
                P.load("sp", xt[b][:], T["x"][t * 128:(t + 1) * 128, :], [], ["xt%d" % b])
                P.act(junk[:], xt[b][:], AF.Square, ["xt%d" % b], ["junk", "stat"], accum_out=stat[:, 0:1])
                P.ts("dve", stat[:, 1:2], stat[:, 0:1], 1.0 / D, EPS, ALU.mult, ALU.add, ["stat"], ["stat"])
                P.act(stat[:, 2:3], stat[:, 1:2], AF.Ln, ["stat"], ["stat"])
                P.act(stat[:, 3:4], stat[:, 2:3], AF.Exp, ["stat"], ["stat"], scale=-0.5)
                P.stt("dve", xn[b][:], xt[b][:], stat[:, 3:4], g1b[:], ALU.mult, ALU.mult, ["xt%d" % b, "stat", "g1b"], ["xn%d" % b])
                for kc in range(8):
                    P.tr(pt[b][:, kc * 128:(kc + 1) * 128], xn[b][:, kc * 128:(kc + 1) * 128], identb[:], ["xn%d" % b, "identb"], ["pt%d" % b])
                P.copy("act" if ti % 2 == 0 else "dve", hT[hb][:, :, ti * 128:(ti + 1) * 128], pt[b][:, :].rearrange("p (k t) -> p k t", k=8),
                       ["pt%d" % b], ["hT%d" % hb])

        def s1_b(g):
            hb = g % 2
            for blk in range(5):
                fb = (g * 5 + blk) % 2
                for ch in range(4):
                    col0 = blk * 512 + ch * 128
                    pb = rpp.next()
                    for kc in range(8):
                        P.mm(pp[pb][:, :], w_bf[:, kc, col0:col0 + 128], hT[hb][:, kc, :], ["w_bf%d" % blk, "hT%d" % hb], ["pp%d" % pb],
                             start=(kc == 0), stop=(kc == 7))
                    P.copy("act" if ch % 2 == 0 else "dve", fstg[fb][:, ch, :], pp[pb][:, :], ["pp%d" % pb], ["fstg%d" % fb])
                P.load("sp", T["featT"][blk].rearrange("(c p) t -> p c t", p=128)[:, :, g * 512:(g + 1) * 512], fstg[fb][:],
                       ["fstg%d" % fb], ["featT"])
            for bi, (blk, dst) in enumerate(((1, "vm_tok"), (5, "vd_tok"))):
                tb = (g * 2 + bi) % 2
                for ti in range(4):
                    pb = rpp.next()
                    for kc in range(8):
                        P.mm(pp[pb][:, :], hT[hb][:, kc, ti * 128:(ti + 1) * 128], w_bf[:, kc, blk * 512:(blk + 1) * 512],
                             ["w_bf%d" % blk, "hT%d" % hb], ["pp%d" % pb], start=(kc == 0), stop=(kc == 7))
                    P.copy("dve" if ti % 2 == 0 else "act", tstg[tb][:, ti, :], pp[pb][:, :], ["pp%d" % pb], ["tstg%d" % tb])
                P.load("sp", T[dst][g * 512:(g + 1) * 512, :].rearrange("(t p) f -> p t f", p=128), tstg[tb][:], ["tstg%d" % tb], [dst])

        for g in range(9):
            if g < 8:
                s1_a(g)
            if g >= 1:
                s1_b(g - 1)
        return P.emit()


def stage2(nc, sems, T):
    with contextlib.ExitStack() as st:
        sb, ps = tens(nc, st)
        P = Prog(nc, sems)
        ident = sb("ident", [128, 128])
        identb = sb("identb", [128, 128], BF16)
        tri = sb("tri", [128, 128])
        bigtri = sb("bigtri", [128, 128])
        sel = sb("sel", [4, 512])
        cw = sb("cw", [128, 4, 4])
        cb = sb("cb", [128, 4])
        mg = sb("mg", [128, 4])
        msk = sb("msk", [128, 4])
        wq = sb("wq", [128, 4, 128], BF16)
        wk = sb("wk", [128, 4, 128], BF16)
        wgt = sb("wgt", [128, 12, 8], BF16)
        bi = sb("bi", [4, 1])
        bfn = sb("bfn", [4, 1])
        zeros = sb("zeros", [4, 512])
        carryB = sb("carryB", [4, 1])
        carryM = sb("carryM", [4, 1])
        Cf = sb("Cf", [128, 4, 129])
        Cb = sb("Cb", [128, 4, 129], BF16)
        c_sb = [sb("c_sb%d" % i, [128, 4, 515], BF16) for i in range(2)]
        z_sb = [sb("z_sb%d" % i, [128, 4, 512], BF16) for i in range(2)]
        vmT = [sb("vmT%d" % i, [128, 4, 512], BF16) for i in range(2)]
        vaug = [sb("vaug%d" % i, [128, 4, 4, 129], BF16) for i in range(2)]
        cacc = [sb("cacc%d" % i, [128, 512]) for i in range(2)]
        cact = sb("cact", [128, 4, 512], BF16)
        sigz = sb("sigz", [128, 4, 512], BF16)
        scs = sb("scs", [128, 4, 512], BF16)
        qT = sb("qT", [128, 4, 512], BF16)
        kT = sb("kT", [128, 4, 512], BF16)
        ktok = sb("ktok", [128, 4, 4, 128], BF16)
        i_row = sb("i_row", [4, 512])
        e_row = sb("e_row", [4, 512])
        sp_row = sb("sp_row", [4, 512])
        Bn = sb("Bn", [4, 513])
        A_row = sb("A_row", [4, 512])
        Mx = sb("Mx", [4, 513])
        N_row = sb("N_row", [4, 512])
        cols = sb("cols", [128, 4, 3, 4])
        eN = sb("eN", [128, 4, 4])
        Mb = sb("Mb", [128, 4, 5])
        nMb = sb("nMb", [128, 4, 5])
        dec = sb("dec", [128, 4, 4])
        spa = sb("spa", [128, 4, 4])
        Mrow = sb("Mrow", [128, 4, 512])
        tmpD = [sb("tmpD%d" % i, [128, 128]) for i in range(4)]
        Dt = [sb("Dt%d" % i, [128, 128]) for i in range(4)]
        Dm = [sb("Dm%d" % i, [128, 128]) for i in range(2)]
        wT = [sb("wT%d" % i, [128, 128], BF16) for i in range(4)]
        intra = [sb("intra%d" % i, [128, 129]) for i in range(4)]
        comb = [sb("comb%d" % i, [128, 129]) for i in range(4)]
        sm = [sb("sm%d" % i, [128, 16]) for i in range(4)]
        hh = [sb("hh%d" % i, [128, 128]) for i in range(4)]
        hn = [sb("hn%d" % i, [128, 128], BF16) for i in range(4)]
        y1 = [sb("y1%d" % i, [128, 128], BF16) for i in range(4)]
        ymg = [sb("ymg%d" % i, [128, 4, 512], BF16) for i in range(2)]
        wkc = [sb("wkc%d" % i, [128, 1]) for i in range(4)]
        vw = [sb("vw%d" % i, [128, 129], BF16) for i in range(4)]
        pA = ps("pA", [128, 512])
        pB = ps("pB", [128, 512])
        pG = ps("pG", [128, 512])
        ptb = ps("ptb", [128, 1024], BF16)
        pS = [ps("pS%d" % i, [128, 512]) for i in range(2)]
        pO = [ps("pO%d" % i, [128, 512]) for i in range(2)]
        P.load("sp", ident[:], T["ident"], [], ["ident"])
        P.copy("dve", identb[:], ident[:], ["ident"], ["identb"])
        P.load("sp", tri[:], T["tri"], [], ["tri"])
        P.ts("dve", bigtri[:], tri[:], -1.0, -1.0e4, ALU.add, ALU.mult, ["tri"], ["bigtri"])
        P.load("sp", sel[:], T["sel"], [], ["sel"])
        P.load("sp", cw[:], T["conv_w"].rearrange("j (c p) -> p j c", p=128), [], ["cw"])
        P.load("sp", cb[:], T["conv_b"].rearrange("(c p) -> p c", p=128), [], ["cb"])
        P.load("sp", mg[:], T["m_norm_g"].rearrange("(c p) -> p c", p=128), [], ["mg"])
        P.load("sp", msk[:], T["m_skip"].rearrange("(c p) -> p c", p=128), [], ["msk"])
        P.load("pool", wq[:], T["w_mq"].rearrange("h d e -> d h e"), [], ["wq"])
        P.load("pool", wk[:], T["w_mk"].rearrange("h d e -> d h e"), [], ["wk"])
        P.load("pool", wgt[:], T["w_mgate"].rearrange("(c p) g -> p c g", p=128), [], ["wgt"])
        P.load("sp", bi[:], T["b_mgate"][0:4].rearrange("(p o) -> p o", o=1), [], ["bi"])
        P.load("sp", bfn[:], T["b_mgate"][4:8].rearrange("(p o) -> p o", o=1), [], ["bfn"])
        P.ts("dve", bfn[:], bfn[:], -1.0, None, ALU.mult, None, ["bfn"], ["bfn"])
        P.memset("pool", zeros[:], 0.0, ["zeros"])
        P.memset("pool", carryB[:], 0.0, ["carryB"])
        P.memset("pool", carryM[:], 0.0, ["carryM"])
        P.memset("pool", Cf[:], 0.0, ["Cf%d" % h_ for h_ in range(4)])
        P.memset("pool", Cb[:], 0.0, ["Cb%d" % h_ for h_ in range(4)])
        for i in range(2):
            P.memset("pool", vaug[i][:], 1.0, ["vaug%d" % i])
            P.memset("pool", c_sb[i][:], 0.0, ["c_sb%d" % i])

        featT = T["featT"]
        rS, rO, r2 = Rot(2), Rot(2), Rot(2)
        cvj = ConvD(P, T)
        cvj.k = 8 * CONV_PER_GROUP[1]
        for g in range(8):
            b = g % 2
            t0 = g * 512
            cvj.emit(CONV_PER_GROUP[2])
            kc_, kz, kv, kva = "c_sb%d" % b, "z_sb%d" % b, "vmT%d" % b, "vaug%d" % b
            cview = featT[0].rearrange("(c p) t -> p c t", p=128)
            if g == 0:
                P.load("sp", c_sb[b][:, :, 3:515], cview[:, :, 0:512], [], [kc_])
            else:
                P.load("sp", c_sb[b][:, :, 0:515], cview[:, :, t0 - 3:t0 + 512], [], [kc_])
            P.load("act", z_sb[b][:], featT[2].rearrange("(c p) t -> p c t", p=128)[:, :, t0:t0 + 512], [], [kz])
            P.load("act", vmT[b][:], featT[1].rearrange("(c p) t -> p c t", p=128)[:, :, t0:t0 + 512], [], [kv])
            for ti in range(4):
                P.load("sp" if ti % 2 == 0 else "act", vaug[b][:, ti, :, 0:128],
                       T["vm_tok"][t0 + ti * 128:t0 + (ti + 1) * 128, :].rearrange("p (h e) -> p h e", e=128), [kva], [kva])
            for ch in range(4):
                ab = ch % 2
                ka = "cacc%d" % ab
                e1 = "dve" if ch % 2 == 0 else "pool"
                P.ts("dve", cacc[ab][:], c_sb[b][:, ch, 0:512], cw[:, 0, ch:ch + 1], cb[:, ch:ch + 1], ALU.mult, ALU.add, [kc_, "cw", "cb"], [ka])
                for j in range(1, 4):
                    P.stt("dve" if j % 2 == 0 else "pool", cacc[ab][:], c_sb[b][:, ch, j:j + 512], cw[:, j, ch:ch + 1], cacc[ab][:], ALU.mult, ALU.add,
                          [kc_, "cw", ka], [ka])
                P.act(cact[:, ch, :], cacc[ab][:], AF.Silu, [ka], ["cact"])
                P.ts("pool", scs[:, ch, :], cact[:, ch, :], msk[:, ch:ch + 1], None, ALU.mult, None, ["cact", "msk"], ["scs"])
            P.act(sigz[:].rearrange("p c t -> p (c t)"), z_sb[b][:].rearrange("p c t -> p (c t)"), AF.Sigmoid, [kz], ["sigz"])
            for h in range(4):
                P.mm(pA[:, :], wq[:, h, :], cact[:, h, :], ["wq", "cact"], ["pA"])
                P.copy("act", qT[:, h, :], pA[:, :], ["pA"], ["qT"])
                P.mm(pB[:, :], wk[:, h, :], cact[:, h, :], ["wk", "cact"], ["pB"])
                P.copy("dve", kT[:, h, :], pB[:, :], ["pB"], ["kT"])
            for ti in range(4):
                pz = pA if ti % 2 == 0 else pB
                kz_ = "pA" if ti % 2 == 0 else "pB"
                for h in range(4):
                    P.mm(pz[:, h * 128:(h + 1) * 128], cact[:, h, ti * 128:(ti + 1) * 128], wk[:, h, :], ["cact", "wk"], [kz_])
                P.copy("act" if ti % 2 == 0 else "dve", ktok[:, ti, :, :].rearrange("p h e -> p (h e)"), pz[:, :], [kz_], ["ktok"])
            srcs = [(qT, "qT")] * 4 + [(kT, "kT")] * 4 + [(vmT[b], kv)] * 4
            for c in range(12):
                sap, skey = srcs[c]
                P.mm(pG[0:4, :], wgt[:, c, 0:4], sap[:, c % 4, :], ["wgt", skey], ["pG"], start=(c == 0), stop=(c == 11))
            for c in range(12):
                sap, skey = srcs[c]
                P.mm(pB[0:4, :], wgt[:, c, 4:8], sap[:, c % 4, :], ["wgt", skey], ["pB"], start=(c == 0), stop=(c == 11))
            P.ts("dve", i_row[:], pG[0:4, :], bi[:, 0:1], None, ALU.add, None, ["pG", "bi"], ["i_row"])
            P.act(e_row[:], pB[0:4, :], AF.Exp, ["pB", "bfn"], ["e_row"], bias=bfn[:, 0:1], scale=-1.0)
            P.act(sp_row[:], e_row[:], AF.Ln, ["e_row"], ["sp_row"], bias=1.0)
            P.op("dve", (lambda o, d0, d1, ini: (lambda e: e.tensor_tensor_scan(out=o, data0=d0, data1=d1, initial=ini, op0=ALU.add, op1=ALU.add)))(
                Bn[:, 1:513], sp_row[:], zeros[:], carryB[:, 0:1]), ["sp_row", "zeros", "carryB"], ["Bn"])
            P.tt("dve", A_row[:], i_row[:], Bn[:, 1:513], ALU.add, ["i_row", "Bn"], ["A_row"])
            P.copy("dve", Mx[:, 0:1], carryM[:, 0:1], ["carryM"], ["Mx"])
            P.op("dve", (lambda o, d0, d1, ini: (lambda e: e.tensor_tensor_scan(out=o, data0=d0, data1=d1, initial=ini, op0=ALU.max, op1=ALU.max)))(
                Mx[:, 1:513], A_row[:], A_row[:], carryM[:, 0:1]), ["A_row", "carryM", "Mx"], ["Mx"])
            P.copy("dve", carryB[:, 0:1], Bn[:, 512:513], ["Bn"], ["carryB"])
            P.copy("dve", carryM[:, 0:1], Mx[:, 512:513], ["Mx"], ["carryM"])
            P.tt("dve", N_row[:], Bn[:, 1:513], Mx[:, 1:513], ALU.subtract, ["Bn", "Mx"], ["N_row"])
            for c in range(4):
                for k3, (rap, rkey, off) in enumerate(((A_row, "A_row", 0), (Mx, "Mx", 1), (N_row, "N_row", 0))):
                    o0 = c * 12 + k3 * 4
                    P.tr(pA[:, o0:o0 + 4], rap[:, off + c * 128: off + (c + 1) * 128], ident[0:4, 0:4], [rkey, "ident"], ["pA"])
            P.copy("dve", cols[:].rearrange("p c k h -> p (c k h)"), pA[:, 0:48], ["pA"], ["cols"])
            P.act(eN[:], cols[:, :, 2, :], AF.Exp, ["cols"], ["eN"])
            for h in range(4):
                P.mm(pB[:, h * 5:(h + 1) * 5], sel[:, h * 128:(h + 1) * 128], Mx[:, 0:513:128], ["sel", "Mx"], ["pB"])
            P.copy("dve", Mb[:].rearrange("p h c -> p (h c)"), pB[:, 0:20], ["pB"], ["Mb"])
            P.ts("dve", nMb[:], Mb[:], -1.0, None, ALU.mult, None, ["Mb"], ["nMb"])
            P.tt("dve", dec[:], Mb[:, :, 0:4], Mb[:, :, 1:5], ALU.subtract, ["Mb"], ["dec"])
            P.act(dec[:], dec[:], AF.Exp, ["dec"], ["dec"])
            P.tt("dve", spa[:], Mb[:, :, 0:4].rearrange("p h c -> p c h"), cols[:, :, 1, :], ALU.subtract, ["Mb", "cols"], ["spa"])
            P.act(spa[:], spa[:], AF.Exp, ["spa"], ["spa"])
            for h in range(4):
                pz, kz_ = (pA, "pA") if h % 2 == 0 else (pB, "pB")
                P.mm(pz[:, :], sel[:, h * 128:(h + 1) * 128], Mx[:, 1:513], ["sel", "Mx"], [kz_])
                P.tt("dve", Mrow[:, h, :].rearrange("p (c t) -> p c t", t=128), pz[:, :].rearrange("p (c t) -> p c t", t=128),
                     bigtri[:].unsqueeze(1).to_broadcast([128, 4, 128]), ALU.add, [kz_, "bigtri"], ["Mrow"])
            HB = [(pS[0], "pS0"), (pS[1], "pS1"), (pO[0], "pO0"), (pO[1], "pO1")]
            UB = [(pA, "pA"), (pB, "pB")]
            for c in range(4):
                cs = slice(c * 128, (c + 1) * 128)

                def phases(h, c=c, cs=cs):
                    hb_, kH = HB[h]
                    ub_, kU = UB[h // 2]
                    uo = (h % 2) * 256
                    Acol = cols[:, c, 0, h:h + 1]
                    ks = "sm%d" % h
                    smt = sm[h]
                    kCf, kCb = "Cf%d" % h, "Cb%d" % h

                    def p0():
                        P.mm(hb_[:, 0:128], kT[:, h, cs], qT[:, h, cs], ["kT", "qT"], [kH])
                        P.ts("dve", tmpD[h][:], Mrow[:, h, cs], Acol, 0.0, ALU.subtract, ALU.max, ["Mrow", "cols"], ["tmpD%d" % h])
                        P.act(wkc[h][:], Acol, AF.Exp, ["cols", "nMb"], ["wkc%d" % h], bias=nMb[:, h, c + 1:c + 2])

                    def p1():
                        P.act(Dt[h][:], tmpD[h][:], AF.Exp, ["tmpD%d" % h], ["Dt%d" % h], scale=-1.0)
                        P.op("act", (lambda o, i_, sc: (lambda e: e.activation(out=o, in_=i_, func=AF.Copy, scale=sc)))(
                            vw[h][:], vaug[b][:, c, h, :], wkc[h][:, 0:1]), [kva, "wkc%d" % h], ["vw%d" % h])

                    def p2():
                        P.tt("dve", wT[h][:], hb_[:, 0:128], Dt[h][:], ALU.mult, [kH, "Dt%d" % h], ["wT%d" % h])

                    def p3():
                        P.mm(hb_[:, 128:257], wT[h][:], vaug[b][:, c, h, :], ["wT%d" % h, kva], [kH])
                        P.mm(hb_[:, 257:386], qT[:, h, cs], Cb[:, h, :], ["qT", kCb], [kH])
                        P.mm(ub_[:, uo:uo + 129], ktok[:, c, h, :], vw[h][:], ["ktok", "vw%d" % h], [kU])

                    def p4():
                        P.copy("act", intra[h][:], hb_[:, 128:257], [kH], ["intra%d" % h])
                        P.stt("dve", Cf[:, h, :], Cf[:, h, :], dec[:, h, c:c + 1], ub_[:, uo:uo + 129], ALU.mult, ALU.add, [kCf, "dec", kU], [kCf])

                    def p5():
                        P.stt("dve", comb[h][:], hb_[:, 257:386], spa[:, c, h:h + 1], intra[h][:], ALU.mult, ALU.add,
                              [kH, "spa", "intra%d" % h], ["comb%d" % h])
                        P.copy("act", Cb[:, h, :], Cf[:, h, :], [kCf], [kCb])

                    def p6():
                        P.stt("dve", smt[:, 0:1], comb[h][:, 128:129], -1.0, comb[h][:, 128:129], ALU.mult, ALU.max, ["comb%d" % h], [ks])
                        P.stt("dve", smt[:, 1:2], smt[:, 0:1], ML_SCALE, eN[:, c, h:h + 1], ALU.mult, ALU.max, [ks, "eN"], [ks])
                        P.op("dve", (lambda o, i_: (lambda e: e.reciprocal(out=o, in_=i_)))(smt[:, 2:3], smt[:, 1:2]), [ks], [ks])
                        P.ts("dve", hh[h][:], comb[h][:, 0:128], smt[:, 2:3], ML_SCALE, ALU.mult, ALU.mult, ["comb%d" % h, ks], ["hh%d" % h])

                    def p7():
                        P.op("dve", (lambda o, i_: (lambda e: e.bn_stats(out=o, in_=i_)))(smt[:, 4:10], hh[h][:]), ["hh%d" % h], [ks])
                        P.op("dve", (lambda o, i_: (lambda e: e.bn_aggr(out=o, in_=i_)))(smt[:, 10:12], smt[:, 4:10]), [ks], [ks])
                        P.ts("dve", smt[:, 12:13], smt[:, 11:12], EPS, None, ALU.add, None, [ks], [ks])

                    def p8():
                        P.act(smt[:, 13:14], smt[:, 12:13], AF.Ln, [ks], [ks])
                        P.act(smt[:, 14:15], smt[:, 13:14], AF.Exp, [ks], [ks], scale=-0.5)

                    def p9():
                        P.ts("dve", hn[h][:], hh[h][:], smt[:, 10:11], smt[:, 14:15], ALU.subtract, ALU.mult, ["hh%d" % h, ks], ["hn%d" % h])

                    def p10():
                        P.tr(ptb[:, h * 128:(h + 1) * 128], hn[h][:], identb[:], ["hn%d" % h, "identb"], ["ptb"])

                    def p11():
                        P.stt("dve", y1[h][:], ptb[:, h * 128:(h + 1) * 128], mg[:, h:h + 1], scs[:, h, cs], ALU.mult, ALU.add,
                              ["ptb", "mg", "scs"], ["y1%d" % h])
                        P.tt("dve", ymg[b][:, h, cs], y1[h][:], sigz[:, h, cs], ALU.mult, ["y1%d" % h, "sigz"], ["ymg%d_%d" % (b, h)])

                    return [p0, p1, p2, p3, p4, p5, p6, p7, p8, p9, p10, p11]

                plist = [phases(h) for h in range(4)]
                for k_ in range(12):
                    for h in range(4):
                        plist[h][k_]()
            P.load("sp", T["ymT"].rearrange("(c p) t -> p c t", p=128)[:, :, t0:t0 + 512], ymg[b][:], ["ymg%d_%d" % (b, h_) for h_ in range(4)], ["ymT"])
        return P.emit()


def stage3(nc, sems, T):
    with contextlib.ExitStack() as st:
        sb, ps = tens(nc, st)
        P = Prog(nc, sems)
        ident = sb("ident", [128, 128])
        identb = sb("identb", [128, 128], BF16)
        qT = sb("qT", [128, 4, S], BF16)
        kT = sb("kT", [128, 4, S], BF16)
        vaug = sb("vaug", [128, NT, 4, 129], BF16)
        rbb = T["rbb_sb"]
        biasT = T["biasT_sb"]
        lqb = sb("lqb", [128, 256])
        lt = sb("lt", [128, 64])
        lam = sb("lam", [128, 8])
        dag = sb("dag", [128, 1])
        PT = [sb("PT%d" % i, [128, 512], BF16) for i in range(5)]
        tmpn = [sb("tmpn%d" % i, [128, 128]) for i in range(2)]
        t0s = [sb("t0s%d" % i, [128, 128]) for i in range(2)]
        av = [sb("av%d" % i, [128, 128]) for i in range(2)]
        junk = sb("junk3", [128, 128])
        sm = [sb("sm3%d" % i, [128, 8]) for i in range(4)]
        an = [sb("an%d" % i, [128, 128], BF16) for i in range(2)]
        ydg = [sb("ydg%d" % i, [128, 512], BF16) for i in range(2)]
        pS = [ps("pS%d" % i, [128, 512]) for i in range(4)]
        acc = [ps("acc%d" % i, [128, 512]) for i in range(3)]

        def areg(c, i):
            r = c * 4 + i
            return r // 3, (r % 3) * 160
        ptb = ps("ptb", [128, 1024], BF16)

        P.load("sp", ident[:], T["ident"], [], ["ident"])
        P.copy("dve", identb[:], ident[:], ["ident"], ["identb"])
        P.memset("pool", vaug[:, 0:4], 1.0, ["vaug%d" % t for t in range(4)])
        P.memset("pool", vaug[:, 4:NT], 1.0, ["vaug%d" % t for t in range(4, NT)])
        P.load("sp", lqb[:], T["lambda_qk"].partition_broadcast(128), [], ["lqb"])
        P.load("sp", dag[:], T["da_norm_g"].rearrange("(p o) -> p o", o=1), [], ["dag"])
        for h in range(4):
            P.load("sp", qT[:, h, :], T["featT"][3][h * 128:(h + 1) * 128, :], [], ["qT%d" % h])
            P.load("act", kT[:, h, :], T["featT"][4][h * 128:(h + 1) * 128, :], [], ["kT%d" % h])
            if h == 0:
                for t in range(NT):
                    P.load("sp" if t % 2 == 0 else "act", vaug[:, t, :, 0:128],
                           T["vd_tok"][t * 128:(t + 1) * 128, :].rearrange("p (h e) -> p h e", e=128), ["vaug%d" % t], ["vaug%d" % t])
        zt = sb("zt", [128, 3, D], BF16)
        P.memset("dve", zt[:], 0.0, ["zt"])
        xs_v = T["xs"].rearrange("(n p) d -> p n d", p=128)
        for i in range(42):
            P.load("sp", xs_v[:, i * 3:(i + 1) * 3, :], zt[:], ["zt"], ["xs_zero%d" % i])
        P.ts("dve", dag[:], dag[:], 1.0 - LAM_INIT, None, ALU.mult, None, ["dag"], ["dag"])
        for i in range(2):
            P.tt("dve", lt[:], lqb[:, (2 * i) * 64:(2 * i + 1) * 64], lqb[:, (2 * i + 1) * 64:(2 * i + 2) * 64], ALU.mult, ["lqb", "lt"], ["lt"])
            P.op("dve", (lambda o, i_: (lambda e: e.reduce_sum(out=o, in_=i_, axis=AX.X)))(lam[:, 4 + i:5 + i], lt[:]), ["lt"], ["lam"])
        P.act(lam[:, 0:2], lam[:, 4:6], AF.Exp, ["lam"], ["lam"])
        P.tt("dve", lam[:, 2:3], lam[:, 0:1], lam[:, 1:2], ALU.subtract, ["lam"], ["lam"])
        P.ts("dve", lam[:, 3:4], lam[:, 2:3], LAM_INIT, -1.0, ALU.add, ALU.mult, ["lam"], ["lam"])
        rS, rP, r2 = Rot(4), Rot(5), Rot(2)
        cvj = ConvD(P, T)
        cvj.k = 8 * (CONV_PER_GROUP[1] + CONV_PER_GROUP[2])
        its = [(h, g, c, j) for h in range(4) for g in range(8) for c in range(2) for j in range(4 * g + 4)]

        def emit_S(it):
            h, g, c, j = it
            prow = slice(c * 64, (c + 1) * 64)
            i_lo = max(j, 4 * g) - 4 * g
            sB = rS.next()
            pb = rP.next()
            kS, kP = "pS%d" % sB, "PT%d" % pb
            P.mm(pS[sB][:, i_lo * 128:512], kT[prow, h, j * 128:(j + 1) * 128], qT[prow, h, g * 512 + i_lo * 128:(g + 1) * 512],
                 ["kT%d" % h, "qT%d" % h], [kS])
            far_lo = None
            for i in range(i_lo, 4):
                dist = 4 * g + i - j
                if dist >= 2:
                    far_lo = i
                    break
                n2 = r2.next()
                P.stt("dve", tmpn[n2][:], pS[sB][:, i * 128:(i + 1) * 128], DA_SCALE, biasT[:, dist, h, :], ALU.mult, ALU.add,
                      [kS, "biasT"], ["tmpn%d" % n2])
                P.act(PT[pb][:, i * 128:(i + 1) * 128], tmpn[n2][:], AF.Exp, ["tmpn%d" % n2], [kP])
            if far_lo is not None:
                P.act(PT[pb][:, far_lo * 128:512], pS[sB][:, far_lo * 128:512], AF.Exp, [kS, "rbb"], [kP],
                      bias=rbb[:, 31 * 4 + h:31 * 4 + h + 1], scale=DA_SCALE)
            return pb, i_lo

        def emit_AV(it, pb, i_lo):
            h, g, c, j = it
            kP = "PT%d" % pb
            if c == 0 and j == 0:
                cvj.emit(CONV_PER_GROUP[3])
                for a_ in range(3):
                    P.memset("dve", acc[a_][:, :], 0.0, ["acc%d" % a_])
            for i in range(i_lo, 4):
                a_, off = areg(c, i)
                P.mm(acc[a_][:, off:off + 129], PT[pb][:, i * 128:(i + 1) * 128], vaug[:, j, h, :], [kP, "vaug%d" % j], ["acc%d" % a_],
                     start=False, stop=False, skip=True)
            if c == 1 and j == 4 * g + 3:
                finalize(h, g)

        def finalize(h, g):
            yb = (h * 8 + g) % 2
            for i in range(4):
                n2 = r2.next()
                ks = "sm3%d" % n2
                smt = sm[n2]
                b0, o0 = areg(0, i)
                b1, o1 = areg(1, i)
                a0, a1 = acc[b0], acc[b1]
                k0, k1 = "acc%d" % b0, "acc%d" % b1
                P.op("dve", (lambda o, i_: (lambda e: e.reciprocal(out=o, in_=i_)))(smt[:, 0:1], a0[:, o0 + 128:o0 + 129]), [k0], [ks])
                P.op("dve", (lambda o, i_: (lambda e: e.reciprocal(out=o, in_=i_)))(smt[:, 1:2], a1[:, o1 + 128:o1 + 129]), [k1], [ks])
                P.tt("dve", smt[:, 2:3], smt[:, 1:2], lam[:, 3:4], ALU.mult, [ks, "lam"], [ks])
                P.op("act", (lambda o, i_, sc: (lambda e: e.activation(out=o, in_=i_, func=AF.Copy, scale=sc)))(t0s[n2][:], a0[:, o0:o0 + 128], smt[:, 0:1]),
                     [k0, ks], ["t0s%d" % n2])
                P.stt("dve", av[n2][:], a1[:, o1:o1 + 128], smt[:, 2:3], t0s[n2][:], ALU.mult, ALU.add, [k1, ks, "t0s%d" % n2], ["av%d" % n2])
                P.act(junk[:], av[n2][:], AF.Square, ["av%d" % n2], ["junk3", ks], accum_out=smt[:, 3:4])
                P.ts("dve", smt[:, 4:5], smt[:, 3:4], 1.0 / 128, SUBLN_EPS, ALU.mult, ALU.add, [ks], [ks])
                P.act(smt[:, 5:6], smt[:, 4:5], AF.Ln, [ks], [ks])
                P.act(smt[:, 6:7], smt[:, 5:6], AF.Exp, [ks], [ks], scale=-0.5)
                P.ts("dve", an[n2][:], av[n2][:], smt[:, 6:7], None, ALU.mult, None, ["av%d" % n2, ks], ["an%d" % n2])
                P.tr(ptb[:, n2 * 512:n2 * 512 + 128], an[n2][:], identb[:], ["an%d" % n2, "identb"], ["ptb"])
                P.ts("dve", ydg[yb][:, i * 128:(i + 1) * 128], ptb[:, n2 * 512:n2 * 512 + 128], dag[:, 0:1], None, ALU.mult, None,
                     ["ptb", "dag"], ["ydg%d" % yb])
            P.load("act", T["ydT"][h * 128:(h + 1) * 128, g * 512:(g + 1) * 512], ydg[yb][:], ["ydg%d" % yb], ["ydT"])

        pend = []
        for it in its:
            pend.append((it,) + emit_S(it))
            if len(pend) > 3:
                emit_AV(*pend.pop(0))
        while pend:
            emit_AV(*pend.pop(0))
        return P.emit()


def stage4(nc, sems, T):
    with contextlib.ExitStack() as st:
        sb, ps = tens(nc, st)
        P = Prog(nc, sems)
        ident = sb("ident", [128, 128])
        wo = sb("wo", [128, 8, D], BF16)
        yT = sb("yT", [128, 8, S], BF16)
        g2b = sb("g2b", [128, D])
        wr = sb("wr", [128, 8, 36])
        brb = sb("brb", [128, 36])
        xt = [sb("xt%d" % i, [128, D]) for i in range(2)]
        x2 = [sb("x2%d" % i, [128, D]) for i in range(2)]
        junk = sb("junk4", [128, D], BF16)
        stat = [sb("stat4%d" % i, [128, 4]) for i in range(2)]
        h2 = [sb("h2%d" % i, [128, D]) for i in range(2)]
        h2T = [sb("h2T%d" % i, [128, 8, 128]) for i in range(2)]
        h2bf = [sb("h2bf%d" % i, [128, D], BF16) for i in range(2)]
        lstr = sb("lstr", [128, 128])
        ones = sb("ones", [128, 128])
        thr = sb("thr", [128, 16 + NSUP])
        E1 = sb("E1", [128, NT, 4, 8])
        E2 = sb("E2", [128, NT, 4, 8])
        Es = sb("Es", [128, NT * 32])
        within = sb("within", [128, NT, 32])
        csb = sb("csb", [128, NT, 32])
        incl = sb("incl", [128, NT, 32])
        zer = sb("zer", [128, NT])
        cmpb = sb("cmpb", [128, NSUP * 32])
        ntl = sb("ntl", [128, 32])
        inct = sb("inct", [128, 32])
        offb = sb("offb", [128, 32])
        offe = sb("offe", [128, 32])
        Rr = sb("Rr", [128, NT, 32])
        posf = sb("posf", [128, NT, 2])
        tef = sb("tef", [128, 64])
        pidx = sb("pidx", [128, 1])
        h2Tb = [sb("h2Tb%d" % i, [128, 8, 128], BF16) for i in range(2)]
        lgt = sb("lgt", [128, NT, 36])
        mxg = sb("mxg", [128, NT])
        ohg = sb("ohg", [128, NT, 4])
        eg = sb("eg", [128, NT, 4])
        sg = sb("sg", [128, NT])
        tmp4 = sb("tmp4", [128, NT, 4, 8])
        les = sb("les", [128, NT, 8])
        le2 = sb("le2", [128, NT, 8])
        m1 = sb("m1", [128, NT])
        m2 = sb("m2", [128, NT])
        oh1 = sb("oh1", [128, NT, 8])
        oh2 = sb("oh2", [128, NT, 8])
        w1 = sb("w1", [128, NT])
        w2 = sb("w2", [128, NT])
        gf = sb("gf", [128, NT, 8])
        gts = sb("gts", [128, NT, 4, 8])
        pO = [ps("pO%d" % i, [128, 512]) for i in range(4)]
        pT = [ps("pT%d" % i, [128, 512]) for i in range(2)]
        pR = ps("pR", [128, 512])

        P.load("sp", ident[:], T["ident"], [], ["ident"])
        P.load("pool", wo[:], T["w_out"].rearrange("(c p) n -> p c n", p=128), [], ["wo"])
        P.load("sp", g2b[:], T["norm2_g"].partition_broadcast(128), [], ["g2b"])
        P.load("sp", wr[:], T["w_r"].rearrange("(c p) n -> p c n", p=128), [], ["wr"])
        P.load("sp", brb[:], T["b_r"].partition_broadcast(128), [], ["brb"])
        ymT_v = T["ymT"].rearrange("(c p) t -> p c t", p=128)
        ydT_v = T["ydT"].rearrange("(c p) t -> p c t", p=128)
        for c in range(4):
            cs = slice(c * 1024, (c + 1) * 1024)
            P.load("sp", yT[:, 0:4, cs], ymT_v[:, :, cs], [], ["yTm%d" % c])
            P.load("act", yT[:, 4:8, cs], ydT_v[:, :, cs], [], ["yTd%d" % c])
        rO = Rot(2)

        def s4_a(t):
            b = t % 2
            ts_ = slice(t * 128, (t + 1) * 128)
            P.load("sp", xt[b][:], T["x"][ts_, :], [], ["xt%d" % b])
            for half in range(2):
                pb = rO.next() * 2 + half
                for kc in range(8):
                    P.mm(pO[pb][:, :], yT[:, kc, ts_], wo[:, kc, half * 512:(half + 1) * 512], [("yTm%d" if kc < 4 else "yTd%d") % (t // 8), "wo"], ["pO%d" % pb], start=(kc == 0), stop=(kc == 7))
                P.tt("dve", x2[b][:, half * 512:(half + 1) * 512], pO[pb][:, :], xt[b][:, half * 512:(half + 1) * 512], ALU.add,
                     ["pO%d" % pb, "xt%d" % b], ["x2%d" % b])
            P.load("sp", T["x2"][ts_, :], x2[b][:], ["x2%d" % b], ["x2d"])
            sk = "stat4%d" % b
            P.act(junk[:], x2[b][:], AF.Square, ["x2%d" % b], ["junk4", sk], accum_out=stat[b][:, 0:1])
            P.ts("dve", stat[b][:, 1:2], stat[b][:, 0:1], 1.0 / D, EPS, ALU.mult, ALU.add, [sk], [sk])
            P.act(stat[b][:, 2:3], stat[b][:, 1:2], AF.Ln, [sk], [sk])
            P.act(stat[b][:, 3:4], stat[b][:, 2:3], AF.Exp, [sk], [sk], scale=-0.5)
            P.stt("dve", h2[b][:], x2[b][:], stat[b][:, 3:4], g2b[:], ALU.mult, ALU.mult, ["x2%d" % b, sk, "g2b"], ["h2%d" % b])
            P.copy("dve", h2bf[b][:], h2[b][:], ["h2%d" % b], ["h2bf%d" % b])
            P.load("sp", T["h2b"][ts_, :], h2bf[b][:], ["h2bf%d" % b], ["h2bd"])

        def s4_b(t):
            b = t % 2
            ts_ = slice(t * 128, (t + 1) * 128)
            for kc in range(8):
                pz = pT[kc // 4]
                P.tr(pz[:, (kc % 4) * 128:(kc % 4 + 1) * 128], h2[b][:, kc * 128:(kc + 1) * 128], ident[:], ["h2%d" % b, "ident"], ["pT%d" % (kc // 4)])
            for hf in range(2):
                P.copy("act", h2T[b][:, hf * 4:(hf + 1) * 4, :].rearrange("p k t -> p (k t)"), pT[hf][:, :], ["pT%d" % hf], ["h2T%d" % b])
                P.copy("dve", h2Tb[b][:, hf * 4:(hf + 1) * 4, :].rearrange("p k t -> p (k t)"), pT[hf][:, :], ["pT%d" % hf], ["h2Tb%d" % b])
            P.load("sp", T["h2T"].rearrange("(c p) t -> p c t", p=128)[:, :, ts_], h2Tb[b][:], ["h2Tb%d" % b], ["h2Td"])
            for kc in range(8):
                P.mm(pR[:, 0:36], h2T[b][:, kc, :], wr[:, kc, :], ["h2T%d" % b, "wr"], ["pR"], start=(kc == 0), stop=(kc == 7))
            P.tt("dve", lgt[:, t, :], pR[:, 0:36], brb[:], ALU.add, ["pR", "brb"], ["lgt"])

        for t in range(NT + 1):
            if t < NT:
                s4_a(t)
            if t >= 1:
                s4_b(t - 1)
        lg = lgt[:, :, 0:4]
        le = lgt[:, :, 4:36].rearrange("p t (g e) -> p t g e", e=8)
        red = lambda o, i_, op: (lambda e: e.tensor_reduce(out=o, in_=i_, axis=AX.X, op=op))
        P.op("dve", red(mxg[:], lg, ALU.max), ["lgt"], ["mxg"])
        P.tt("dve", ohg[:], lg, mxg[:].unsqueeze(2).to_broadcast([128, NT, 4]), ALU.is_ge, ["lgt", "mxg"], ["ohg"])
        P.tt("dve", eg[:], lg, mxg[:].unsqueeze(2).to_broadcast([128, NT, 4]), ALU.subtract, ["lgt", "mxg"], ["eg"])
        P.act(eg[:], eg[:], AF.Exp, ["eg"], ["eg"])
        P.op("dve", red(sg[:], eg[:], ALU.add), ["eg"], ["sg"])
        P.op("dve", (lambda o, i_: (lambda e: e.reciprocal(out=o, in_=i_)))(sg[:], sg[:]), ["sg"], ["sg"])
        P.tt("dve", tmp4[:], le, ohg[:].unsqueeze(3).to_broadcast([128, NT, 4, 8]), ALU.mult, ["lgt", "ohg"], ["tmp4"])
        P.op("dve", red(les[:], tmp4[:].rearrange("p t g e -> p t e g"), ALU.add), ["tmp4"], ["les"])
        P.op("dve", red(m1[:], les[:], ALU.max), ["les"], ["m1"])
        P.tt("dve", oh1[:], les[:], m1[:].unsqueeze(2).to_broadcast([128, NT, 8]), ALU.is_ge, ["les", "m1"], ["oh1"])
        P.stt("dve", le2[:], oh1[:], -1e30, les[:], ALU.mult, ALU.add, ["oh1", "les"], ["le2"])
        P.op("dve", red(m2[:], le2[:], ALU.max), ["le2"], ["m2"])
        P.tt("dve", oh2[:], le2[:], m2[:].unsqueeze(2).to_broadcast([128, NT, 8]), ALU.is_ge, ["le2", "m2"], ["oh2"])
        P.tt("dve", w2[:], m2[:], m1[:], ALU.subtract, ["m1", "m2"], ["w2"])
        P.act(w2[:], w2[:], AF.Exp, ["w2"], ["w2"])
        P.ts("dve", w1[:], w2[:], 1.0, None, ALU.add, None, ["w2"], ["w1"])
        P.op("dve", (lambda o, i_: (lambda e: e.reciprocal(out=o, in_=i_)))(w1[:], w1[:]), ["w1"], ["w1"])
        P.tt("dve", w2[:], w2[:], w1[:], ALU.mult, ["w1", "w2"], ["w2"])
        P.tt("dve", w1[:], w1[:], sg[:], ALU.mult, ["w1", "sg"], ["w1"])
        P.tt("dve", w2[:], w2[:], sg[:], ALU.mult, ["w2", "sg"], ["w2"])
        wk_g, pos_i, te_i = T["wk_g"], T["pos_i"], T["te_i"]
        P.copy("dve", wk_g[:, :, 0], w1[:], ["w1"], ["wk_g"])
        P.copy("dve", wk_g[:, :, 1], w2[:], ["w2", "wk_g"], ["wk_g"])
        P.load("sp", lstr[:], T["lstrict"], [], ["lstr"])
        P.load("sp", thr[:], T["thr"].partition_broadcast(128), [], ["thr"])
        P.memset("pool", ones[:], 1.0, ["ones"])
        P.memset("pool", zer[:], 0.0, ["zer"])
        bc3 = lambda a: a.unsqueeze(3).to_broadcast([128, NT, 4, 8])
        bc2 = lambda a: a.unsqueeze(2).to_broadcast([128, NT, 4, 8])
        P.tt("dve", E1[:], bc3(ohg[:]), bc2(oh1[:]), ALU.mult, ["ohg", "oh1"], ["E1"])
        P.tt("dve", E2[:], bc3(ohg[:]), bc2(oh2[:]), ALU.mult, ["ohg", "oh2"], ["E2"])
        P.tt("dve", Es[:], E1[:].rearrange("p t g e -> p (t g e)"), E2[:].rearrange("p t g e -> p (t g e)"), ALU.add, ["E1", "E2"], ["Es"])
        for hf in range(2):
            P.mm(pO[hf][:, :], lstr[:], Es[:, hf * 512:(hf + 1) * 512], ["lstr", "Es"], ["pO%d" % hf])
            P.copy("act", within[:].rearrange("p t e -> p (t e)")[:, hf * 512:(hf + 1) * 512], pO[hf][:, :], ["pO%d" % hf], ["within"])
            P.mm(pO[2 + hf][:, :], ones[:], Es[:, hf * 512:(hf + 1) * 512], ["ones", "Es"], ["pO%d" % (2 + hf)])
            P.copy("dve", csb[:].rearrange("p t e -> p (t e)")[:, hf * 512:(hf + 1) * 512], pO[2 + hf][:, :], ["pO%d" % (2 + hf)], ["csb"])
        for e_ in range(32):
            P.op("dve", (lambda o, d0, d1: (lambda e: e.tensor_tensor_scan(out=o, data0=d0, data1=d1, initial=0.0, op0=ALU.add, op1=ALU.add)))(
                incl[:, :, e_], csb[:, :, e_], zer[:]), ["csb", "zer", "incl"], ["incl"])
        P.tt("dve", cmpb[:, 0:512].rearrange("p (e j) -> p e j", j=16), incl[:, NT - 1, :].unsqueeze(2).to_broadcast([128, 32, 16]),
             thr[:, 0:16].unsqueeze(1).to_broadcast([128, 32, 16]), ALU.is_gt, ["incl", "thr"], ["cmpb"])
        P.op("dve", red(ntl[:], cmpb[:, 0:512].rearrange("p (e j) -> p e j", j=16), ALU.add), ["cmpb"], ["ntl"])
        P.op("dve", (lambda o, d0, d1: (lambda e: e.tensor_tensor_scan(out=o, data0=d0, data1=d1, initial=0.0, op0=ALU.add, op1=ALU.add)))(
            inct[:], ntl[:], zer[:, 0:32]), ["ntl", "zer"], ["inct"])
        P.ts("dve", offe[:], inct[:], float(SUP), None, ALU.mult, None, ["inct"], ["offe"])
        P.tt("dve", offb[:], inct[:], ntl[:], ALU.subtract, ["inct", "ntl"], ["offb"])
        P.ts("dve", offb[:], offb[:], float(SUP), None, ALU.mult, None, ["offb"], ["offb"])
        P.tt("dve", Rr[:], incl[:], csb[:], ALU.subtract, ["incl", "csb"], ["Rr"])
        P.tt("dve", Rr[:], Rr[:], within[:], ALU.add, ["Rr", "within"], ["Rr"])
        P.tt("dve", Rr[:], Rr[:], offb[:].unsqueeze(1).to_broadcast([128, NT, 32]), ALU.add, ["Rr", "offb"], ["Rr"])
        for k_, Ek in enumerate((E1, E2)):
            kn = "E%d" % (k_ + 1)
            P.tt("dve", Ek[:].rearrange("p t g e -> p t (g e)"), Ek[:].rearrange("p t g e -> p t (g e)"), Rr[:], ALU.mult, [kn, "Rr"], [kn])
            P.op("dve", red(posf[:, :, k_], Ek[:].rearrange("p t g e -> p t (g e)"), ALU.add), [kn, "posf"], ["posf"])
        P.copy("dve", pos_i[:], posf[:], ["posf"], ["pos_i"])
        P.tt("dve", cmpb[:, 0:NSUP * 32].rearrange("p (j e) -> p j e", e=32), offe[:].unsqueeze(1).to_broadcast([128, NSUP, 32]),
             thr[:, 16:16 + NSUP].unsqueeze(2).to_broadcast([128, NSUP, 32]), ALU.is_le, ["offe", "thr", "cmpb"], ["cmpb"])
        P.memset("pool", tef[:], 0.0, ["tef"])
        P.op("dve", red(tef[:, 0:NSUP], cmpb[:, 0:NSUP * 32].rearrange("p (j e) -> p j e", e=32), ALU.add), ["cmpb", "tef"], ["tef"])
        P.ts("dve", tef[:], tef[:], 31.0, None, ALU.min, None, ["tef"], ["tef"])
        P.load("sp", pidx[:], T["pidx"], [], ["pidx"])
        P.ts("dve", tef[:], tef[:], 128.0, pidx[:, 0:1], ALU.mult, ALU.add, ["tef", "pidx"], ["tef"])
        P.copy("dve", te_i[:], tef[:], ["tef"], ["te_i"])
        P.tt("dve", oh1[:], oh1[:], w1[:].unsqueeze(2).to_broadcast([128, NT, 8]), ALU.mult, ["oh1", "w1"], ["oh1"])
        P.tt("dve", oh2[:], oh2[:], w2[:].unsqueeze(2).to_broadcast([128, NT, 8]), ALU.mult, ["oh2", "w2"], ["oh2"])
        P.tt("dve", gf[:], oh1[:], oh2[:], ALU.add, ["oh1", "oh2"], ["gf"])
        P.tt("dve", gts[:], ohg[:].unsqueeze(3).to_broadcast([128, NT, 4, 8]), gf[:].unsqueeze(2).to_broadcast([128, NT, 4, 8]), ALU.mult,
             ["ohg", "gf"], ["gts"])
        P.load("sp", T["gates"], gts[:].rearrange("p t g e -> p (t g e)"), ["gts"], ["gatesd"])
        return P.emit()


def stage5(nc, sems, T):
    IOA = bass.IndirectOffsetOnAxis
    with contextlib.ExitStack() as st:
        sb, ps = tens(nc, st)
        P = Prog(nc, sems)
        pos_i, te_i, wk_g = T["pos_i"], T["te_i"], T["wk_g"]
        identb = sb("identb", [128, 128], BF16)
        identf = sb("identf", [128, 128])
        gfb = sb("gfb", [128, D])
        hrow = [sb("hrow%d" % i, [128, D], BF16) for i in range(3)]
        wall = [sb("wall%d" % i, [128, 3 * 4096], BF16) for i in range(2)]
        xs = [sb("xs%d" % i, [128, D], BF16) for i in range(3)]
        XT = [sb("XT%d" % i, [128, 8, 128], BF16) for i in range(2)]
        sgl = [sb("sgl%d" % i, [128, DFF], BF16) for i in range(2)]
        hid = [sb("hid%d" % i, [128, DFF], BF16) for i in range(2)]
        hidT = [sb("hidT%d" % i, [128, 4, 128], BF16) for i in range(2)]
        ysb = [sb("ysb%d" % i, [128, D], BF16) for i in range(3)]
        yg = [sb("yg%d" % i, [128, 2, D], BF16) for i in range(3)]
        x2 = [sb("x2%d" % i, [128, D]) for i in range(8)]
        junk = sb("junk5", [128, D], BF16)
        stat = [sb("stat5%d" % i, [128, 4]) for i in range(3)]
        ot = [sb("ot%d" % i, [128, D]) for i in range(3)]
        ptx = [ps("ptx%d" % i, [128, D], BF16) for i in range(2)]
        pg = [ps("pg%d" % i, [128, 512]) for i in range(2)]
        pu = [ps("pu%d" % i, [128, 512]) for i in range(2)]
        pth = [ps("pth%d" % i, [128, D], BF16) for i in range(2)]

        P.load("sp", identf[:], T["ident"], [], ["identf"])
        P.copy("dve", identb[:], identf[:], ["identf"], ["identb"])
        P.load("sp", gfb[:], T["normf_g"].partition_broadcast(128), [], ["gfb"])
        def gather_w(j):
            wb = j % 2
            P.dma("pool", (lambda o, i_, off: (lambda e: e.indirect_dma_start(out=o, out_offset=None, in_=i_, in_offset=off)))(
                wall[wb][:, :], T["wall"], IOA(ap=te_i[:, j:j + 1], axis=0)), ["te_i"], ["wall%d" % wb])

        gather_w(0)
        gather_w(1)
        zkeys = []
        sckeys = []
        for t in range(NT):
            hb = t % 3
            P.load("sp", hrow[hb][:], T["h2b"][t * 128:(t + 1) * 128, :], [], ["hrow%d" % hb])
            for k_ in range(2):
                key = "xs_sc%d_%d" % (t, k_)
                sckeys.append(key)
                P.dma("pool", (lambda o, off, i_: (lambda e: e.indirect_dma_start(out=o, out_offset=off, in_=i_, in_offset=None)))(
                    T["xs"], IOA(ap=pos_i[:, t, k_:k_ + 1], axis=0), hrow[hb][:]), ["hrow%d" % hb, "pos_i"] + zkeys, [key])
        ykeys = []
        rx, r2 = Rot(3), Rot(2)
        NSUB = NSUP * (SUP // 128)

        def wviews(j):
            wb = j % 2
            return (wall[wb][:, 0:4096].rearrange("p (c f) -> p c f", f=512), wall[wb][:, 4096:8192].rearrange("p (c f) -> p c f", f=512),
                    wall[wb][:, 8192:12288].rearrange("p (c d) -> p c d", d=1024), "wall%d" % wb)

        def phase_a(n):
            j = n // 2
            wg_v, wu_v, wd_v, kw = wviews(j)
            row0 = n * 128
            xb, b2 = n % 3, n % 2
            P.load("act", xs[xb][:], T["xs"][row0:row0 + 128, :], sckeys + zkeys, ["xs%d" % xb])
            for kc in range(8):
                P.tr(ptx[b2][:, kc * 128:(kc + 1) * 128], xs[xb][:, kc * 128:(kc + 1) * 128], identb[:], ["xs%d" % xb, "identb"], ["ptx%d" % b2])
            P.copy("dve" if b2 == 0 else "act", XT[b2][:].rearrange("p k t -> p (k t)"), ptx[b2][:, :], ["ptx%d" % b2], ["XT%d" % b2])

        def phase_a2(n):
            j = n // 2
            wg_v, wu_v, wd_v, kw = wviews(j)
            xb, b2 = n % 3, n % 2
            for kc in range(8):
                P.mm(pg[b2][:, :], XT[b2][:, kc, :], wg_v[:, kc, :], ["XT%d" % b2, kw], ["pg%d" % b2], start=(kc == 0), stop=(kc == 7))
            for kc in range(8):
                P.mm(pu[b2][:, :], XT[b2][:, kc, :], wu_v[:, kc, :], ["XT%d" % b2, kw], ["pu%d" % b2], start=(kc == 0), stop=(kc == 7))
            P.act(sgl[b2][:], pg[b2][:, :], AF.Silu, ["pg%d" % b2], ["sgl%d" % b2])
            P.tt("dve", hid[b2][:], sgl[b2][:], pu[b2][:, :], ALU.mult, ["sgl%d" % b2, "pu%d" % b2], ["hid%d" % b2])

        def phase_b(n):
            j = n // 2
            wg_v, wu_v, wd_v, kw = wviews(j)
            row0 = n * 128
            xb, b2 = n % 3, n % 2
            for fc in range(4):
                P.tr(pth[b2][:, fc * 128:(fc + 1) * 128], hid[b2][:, fc * 128:(fc + 1) * 128], identb[:], ["hid%d" % b2, "identb"], ["pth%d" % b2])
            P.copy("act" if b2 == 0 else "dve", hidT[b2][:].rearrange("p k t -> p (k t)"), pth[b2][:, 0:512], ["pth%d" % b2], ["hidT%d" % b2])

        def phase_b2(n):
            j = n // 2
            wg_v, wu_v, wd_v, kw = wviews(j)
            row0 = n * 128
            xb, b2 = n % 3, n % 2
            for half, (pz, kz) in enumerate(((pg[b2], "pg%d" % b2), (pu[b2], "pu%d" % b2))):
                for fc in range(4):
                    P.mm(pz[:, :], hidT[b2][:, fc, :], wd_v[:, fc, half * 512:(half + 1) * 512], ["hidT%d" % b2, kw], [kz],
                         start=(fc == 0), stop=(fc == 3))
                P.copy("act" if half == 0 else "dve", ysb[xb][:, half * 512:(half + 1) * 512], pz[:, :], [kz], ["ysb%d" % xb])
            yk = "ys%d" % n
            ykeys.append(yk)
            P.load("sp", T["ys"][row0:row0 + 128, :], ysb[xb][:], ["ysb%d" % xb], [yk])

        x2_done = set()

        def load_x2(t):
            if t not in x2_done:
                x2_done.add(t)
                P.load("sp", x2[t % 8][:], T["x2"][t * 128:(t + 1) * 128, :], [], ["x2%d" % (t % 8)])

        for n in range(NSUB + 1):
            if NSUB - 24 <= n < NSUB - 16:
                load_x2(n - (NSUB - 24))
            if n < NSUB:
                phase_a(n)
            if n >= 1:
                phase_b(n - 1)
            if n < NSUB:
                phase_a2(n)
            if n >= 1:
                m = n - 1
                phase_b2(m)
                if m % 2 == 1 and m // 2 + 2 < NSUP:
                    gather_w(m // 2 + 2)
        for t in range(NT):
            b = t % 3
            ts_ = slice(t * 128, (t + 1) * 128)
            sk = "stat5%d" % b
            for k_ in range(2):
                P.dma("pool", (lambda o, i_, off: (lambda e: e.indirect_dma_start(out=o, out_offset=None, in_=i_, in_offset=off)))(
                    yg[b][:, k_, :], T["ys"], IOA(ap=pos_i[:, t, k_:k_ + 1], axis=0)), ykeys + ["pos_i"], ["yg%d_%d" % (b, k_)])
            bx = t % 8
            kx = "x2%d" % bx
            load_x2(t)
            P.stt("dve", x2[bx][:], yg[b][:, 0, :], wk_g[:, t, 0:1], x2[bx][:], ALU.mult, ALU.add, ["yg%d_0" % b, "wk_g", kx], [kx])
            P.stt("dve", x2[bx][:], yg[b][:, 1, :], wk_g[:, t, 1:2], x2[bx][:], ALU.mult, ALU.add, ["yg%d_1" % b, "wk_g", kx], [kx])
            P.act(junk[:], x2[bx][:], AF.Square, [kx], ["junk5", sk], accum_out=stat[b][:, 0:1])
            P.ts("dve", stat[b][:, 1:2], stat[b][:, 0:1], 1.0 / D, EPS, ALU.mult, ALU.add, [sk], [sk])
            P.act(stat[b][:, 2:3], stat[b][:, 1:2], AF.Ln, [sk], [sk])
            P.act(stat[b][:, 3:4], stat[b][:, 2:3], AF.Exp, [sk], [sk], scale=-0.5)
            P.stt("dve", ot[b][:], x2[bx][:], stat[b][:, 3:4], gfb[:], ALU.mult, ALU.mult, [kx, sk, "gfb"], ["ot%d" % b])
            P.load("act", T["out"][ts_, :], ot[b][:], ["ot%d" % b], ["outd"])
        return P.emit()


def _rel_bucket_np(n):
    n = np.maximum(n, 0)
    max_exact = 16
    nf = np.maximum(n, 1).astype(np.float32)
    large = max_exact + (np.log(nf / np.float32(max_exact)) / np.float32(math.log(128 / max_exact)) * np.float32(16)).astype(np.int32)
    large = np.minimum(large, 31)
    return np.where(n < max_exact, n, large)


def _constants():
    ident = np.eye(128, dtype=np.float32)
    s_ = np.arange(128)[:, None]
    t_ = np.arange(128)[None, :]
    tri = (s_ <= t_).astype(np.float32)
    sel = np.zeros((4, 4, 128), np.float32)
    for h in range(4):
        sel[h, h, :] = 1.0
    oh = np.zeros((128, 2, 33, 128), np.float32)
    for kind in range(2):
        n = (t_ - s_) + 128 * kind
        bk = _rel_bucket_np(n)
        valid = n >= 0
        for b in range(32):
            oh[:, kind, b, :] = ((bk == b) & valid).astype(np.float32)
        oh[:, kind, 32, :] = (~valid).astype(np.float32)
    lstrict = (s_ < t_).astype(np.float32)
    thr = np.concatenate([np.arange(16) * SUP, np.arange(NSUP) * SUP]).astype(np.float32)
    pidx = np.arange(128, dtype=np.float32).reshape(128, 1)
    return dict(ident=ident, tri=tri, sel=sel.reshape(4, 512), oh=oh.reshape(128, -1), lstrict=lstrict, thr=thr, pidx=pidx)


_CACHE = {}


def kernel(x, w_in, conv_w, conv_b, w_mq, w_mk, w_mgate, b_mgate, m_norm_g, m_skip, lambda_qk, da_norm_g, rel_bias, w_out,
           norm1_g, norm2_g, w_rg, b_rg, w_re, b_re, w_eg, w_eu, w_ed, normf_g):
    f = lambda a: np.ascontiguousarray(np.asarray(a, dtype=np.float32))
    if "nc" not in _CACHE:
        _CACHE["nc"], _CACHE["stats"] = build_program()
    nc = _CACHE["nc"]
    shared = dict(
        w_in=f(w_in)[0], conv_w=f(conv_w)[0], conv_b=f(conv_b)[0], w_mq=f(w_mq)[0], w_mk=f(w_mk)[0], w_mgate=f(w_mgate)[0],
        b_mgate=f(b_mgate)[0], m_norm_g=f(m_norm_g)[0], m_skip=f(m_skip)[0], lambda_qk=f(lambda_qk)[0].reshape(256),
        da_norm_g=f(da_norm_g)[0], rel_bias=f(rel_bias).reshape(128), w_out=f(w_out)[0], norm1_g=f(norm1_g)[0], norm2_g=f(norm2_g)[0],
        w_r=np.ascontiguousarray(np.concatenate([f(w_rg)[0], f(w_re)[0].reshape(D, 32)], axis=1)),
        b_r=np.ascontiguousarray(np.concatenate([f(b_rg)[0], f(b_re)[0].reshape(32)])),
        w_eg=f(w_eg)[0], w_eu=f(w_eu)[0], w_ed=f(w_ed)[0], normf_g=f(normf_g),
    )
    shared.update(_constants())
    xs = f(x)
    in_maps = []
    for b in range(8):
        m = dict(shared)
        m["x"] = xs[b]
        in_maps.append(m)
    res = run_bass_kernel_spmd(nc, in_maps, core_ids=list(range(8)))
    _CACHE["res"] = res
    return np.stack([np.asarray(r["out"], dtype=np.float32) for r in res.results], axis=0)
```
